# Optimizing a Trainium2 kernel written in Bass

```python
import jax, jax.numpy as jnp
from jax import lax
import numpy as np

D_MODEL = 1024
BATCH = 8
SEQ = 4096
DEPTH = 2

POOL_GROUPS = 4
POOL_GROUP_DIM = D_MODEL // 16
POOL_WIDTH = POOL_GROUPS * POOL_GROUP_DIM
POOL_WINDOWS = (2, 4, 8, 16)
MLSTM_HEADS = 4
MLSTM_HEAD_DIM = D_MODEL // 16
MLSTM_WIDTH = MLSTM_HEADS * MLSTM_HEAD_DIM
MLSTM_CONV = 5
MLSTM_CHUNK = 64
MLSTM_N_GATES = 4 * MLSTM_HEADS
MLA_HEADS = 8
MLA_NOPE = D_MODEL // 16
MLA_ROPE = D_MODEL // 32
MLA_V = D_MODEL // 16
MLA_Q_LORA = D_MODEL // 4
MLA_KV_LORA = D_MODEL // 8
MLA_WIDTH = MLA_HEADS * MLA_V
ROPE_THETA = 10000.0
ATTN_BLOCK = 128
MIX_WIDTH = POOL_WIDTH + MLSTM_WIDTH + MLA_WIDTH
IN_SPLITS = (POOL_WIDTH, MLSTM_WIDTH, MLSTM_WIDTH, MLSTM_WIDTH, MLSTM_WIDTH,
             MLSTM_N_GATES, MLA_Q_LORA, MLA_KV_LORA, MLA_ROPE)
IN_WIDTH = sum(IN_SPLITS)
N_EXPERTS = 256
TOP_K = 8
N_GROUPS = 8
TOPK_GROUPS = 4
D_EXPERT = D_MODEL // 4
ROUTED_SCALE = 2.5
DISPATCH_BLOCK = 128
DEEPNORM_ALPHA = (2 * DEPTH) ** 0.25
DEEPNORM_BETA = (8 * DEPTH) ** -0.25
LN_EPS = 1e-5
RMS_EPS = 1e-6
NEG_BIG = -1e30

kernel_name = 'hybrid_pool_mlstm_mla_moe_encoder'


def layer_norm(x):
    x32 = x.astype(jnp.float32)
    mu = x32.mean(-1, keepdims=True)
    var = jnp.square(x32 - mu).mean(-1, keepdims=True)
    return ((x32 - mu) * lax.rsqrt(var + LN_EPS)).astype(x.dtype)


def rms_norm(x, g):
    x32 = x.astype(jnp.float32)
    y = x32 * lax.rsqrt(jnp.mean(jnp.square(x32), -1, keepdims=True) + RMS_EPS)
    return y.astype(x.dtype) * g


def rope(x, cos, sin):
    x1, x2 = jnp.split(x, 2, axis=-1)
    return jnp.concatenate([x1 * cos - x2 * sin, x2 * cos + x1 * sin], axis=-1)


def split_cols(u):
    return jnp.split(u, np.cumsum(IN_SPLITS)[:-1].tolist(), axis=-1)


def pool_mixer(u, w_pool, s_pool):
    B, S, _ = u.shape
    ug = u.reshape(B, S, POOL_GROUPS, POOL_GROUP_DIM).astype(jnp.float32)
    csum = jnp.concatenate([jnp.zeros((B, 1, POOL_GROUPS, POOL_GROUP_DIM), jnp.float32),
                            jnp.cumsum(ug, axis=1)], axis=1)
    t = jnp.arange(S)
    pooled = []
    for g, w in enumerate(POOL_WINDOWS):
        lo = jnp.clip(t - w // 2, 0, S)
        hi = jnp.clip(t + w // 2, 0, S)
        win_sum = csum[:, hi, g] - csum[:, lo, g]
        pooled.append(win_sum / (hi - lo).astype(jnp.float32)[:, None])
    pooled = jnp.stack(pooled, axis=2)
    d = (pooled - ug).astype(u.dtype)
    y = jnp.einsum('bsgc,gcd->bsgd', d, w_pool)
    return y.reshape(B, S, POOL_WIDTH) * s_pool


def depthwise_conv(x, w, b):
    y = lax.conv_general_dilated(x, w[:, None, :], window_strides=(1,),
                                 padding=[(MLSTM_CONV // 2, MLSTM_CONV // 2)],
                                 dimension_numbers=('NWC', 'WIO', 'NWC'),
                                 feature_group_count=x.shape[-1])
    return y + b


def mlstm_scan(q, k, v, i_pre, f_pre):
    B, H, S, dk = q.shape
    dv = v.shape[-1]
    nc = S // MLSTM_CHUNK

    def chunks(a):
        return jnp.moveaxis(a.reshape(B, H, nc, MLSTM_CHUNK, *a.shape[3:]), 2, 0)

    lower = jnp.tril(jnp.ones((MLSTM_CHUNK, MLSTM_CHUNK), bool))

    def step(carry, inp):
        C, n, m = carry
        qc, kc, vc, ic, fc = inp
        b = jnp.cumsum(jax.nn.log_sigmoid(fc), axis=-1)
        logw = jnp.where(lower, b[..., :, None] - b[..., None, :] + ic[..., None, :], -jnp.inf)
        m_inter = b + m[..., None]
        m_t = jnp.maximum(m_inter, logw.max(-1))
        w_intra = jnp.exp(logw - m_t[..., None])
        w_inter = jnp.exp(m_inter - m_t)
        qk = jnp.einsum('bhtd,bhsd->bhts', qc, kc) * w_intra
        num = jnp.einsum('bhts,bhse->bhte', qk, vc) + \
            w_inter[..., None] * jnp.einsum('bhtd,bhde->bhte', qc, C)
        den = qk.sum(-1) + w_inter * jnp.einsum('bhtd,bhd->bht', qc, n)
        h = num / jnp.maximum(jnp.abs(den), jnp.exp(-m_t))[..., None]
        b_last = b[..., -1]
        g = b_last[..., None] - b + ic
        m_new = jnp.maximum(b_last + m, g.max(-1))
        w_k = jnp.exp(g - m_new[..., None])
        decay = jnp.exp(b_last + m - m_new)
        C_new = decay[..., None, None] * C + jnp.einsum('bhs,bhsd,bhse->bhde', w_k, kc, vc)
        n_new = decay[..., None] * n + jnp.einsum('bhs,bhsd->bhd', w_k, kc)
        return (C_new, n_new, m_new), h

    init = (jnp.zeros((B, H, dk, dv), jnp.float32), jnp.zeros((B, H, dk), jnp.float32),
            jnp.full((B, H), NEG_BIG, jnp.float32))
    _, h = lax.scan(step, init, (chunks(q), chunks(k), chunks(v), chunks(i_pre), chunks(f_pre)))
    return jnp.moveaxis(h, 0, 2).reshape(B, H, S, dv)


def mlstm_mixer(u_q, u_k, u_v, u_o, u_gate, conv_w, conv_b, gate_b, gn_w):
    B, S, _ = u_q.shape
    qk = jax.nn.silu(depthwise_conv(jnp.concatenate([u_q, u_k], axis=-1), conv_w, conv_b))
    q, k = jnp.split(qk, 2, axis=-1)

    def heads(a):
        return a.reshape(B, S, MLSTM_HEADS, MLSTM_HEAD_DIM).transpose(0, 2, 1, 3).astype(jnp.float32)

    q, k, v = heads(q), heads(k) * MLSTM_HEAD_DIM ** -0.5, heads(u_v)
    gates = (u_gate + gate_b).astype(jnp.float32).reshape(B, S, 4, MLSTM_HEADS).transpose(2, 0, 3, 1)
    h_fwd = mlstm_scan(q, k, v, gates[0], gates[1])
    flip = lambda a: jnp.flip(a, axis=2)
    h_bwd = flip(mlstm_scan(flip(q), flip(k), flip(v), flip(gates[2]), flip(gates[3])))
    h = h_fwd + h_bwd
    mu = h.mean(-1, keepdims=True)
    var = jnp.square(h - mu).mean(-1, keepdims=True)
    h = ((h - mu) * lax.rsqrt(var + LN_EPS)).transpose(0, 2, 1, 3).reshape(B, S, MLSTM_WIDTH)
    return jax.nn.sigmoid(u_o) * (h.astype(u_q.dtype) * gn_w)


def mla_mixer(u_dq, u_dkv, u_kr, g_q, g_kv, w_uq, w_uk, w_uv, cos, sin):
    B, S, _ = u_dq.shape
    q = (rms_norm(u_dq, g_q) @ w_uq).reshape(B, S, MLA_HEADS, MLA_NOPE + MLA_ROPE)
    q_nope = q[..., :MLA_NOPE]
    q_rope = rope(q[..., MLA_NOPE:], cos[:, :, None], sin[:, :, None])
    c_kv = rms_norm(u_dkv, g_kv)
    k_nope = (c_kv @ w_uk).reshape(B, S, MLA_HEADS, MLA_NOPE)
    v = (c_kv @ w_uv).reshape(B, S, MLA_HEADS, MLA_V)
    k_rope = rope(u_kr, cos, sin)
    nb = S // ATTN_BLOCK
    scale = (MLA_NOPE + MLA_ROPE) ** -0.5

    def blocks(a):
        return jnp.moveaxis(a.reshape(B, nb, ATTN_BLOCK, *a.shape[2:]), 1, 0)

    def attend(blk):
        qn, qr = blk
        s = jnp.einsum('bqhd,bkhd->bhqk', qn, k_nope) + jnp.einsum('bqhr,bkr->bhqk', qr, k_rope)
        p = jax.nn.softmax(s.astype(jnp.float32) * scale, axis=-1).astype(v.dtype)
        return jnp.einsum('bhqk,bkhd->bqhd', p, v)

    o = lax.map(attend, (blocks(q_nope), blocks(q_rope)))
    return jnp.moveaxis(o, 0, 1).reshape(B, S, MLA_WIDTH)


def route(xf, w_router, e_bias):
    T = xf.shape[0]
    scores = jax.nn.sigmoid((xf @ w_router).astype(jnp.float32))
    sel = scores + e_bias.astype(jnp.float32)
    grp = sel.reshape(T, N_GROUPS, N_EXPERTS // N_GROUPS)
    gscore = lax.top_k(grp, 2)[0].sum(-1)
    _, gidx = lax.top_k(gscore, TOPK_GROUPS)
    gmask = jnp.any(gidx[:, :, None] == jnp.arange(N_GROUPS), axis=1)
    sel = jnp.where(jnp.repeat(gmask, N_EXPERTS // N_GROUPS, axis=1), sel, -jnp.inf)
    _, idx = lax.top_k(sel, TOP_K)
    w = jnp.take_along_axis(scores, idx, axis=-1)
    w = w / w.sum(-1, keepdims=True) * ROUTED_SCALE
    return idx, w


def routed_experts(xf, idx, wts, w1, w3, w2):
    T, D = xf.shape
    A = T * TOP_K
    e_flat = idx.reshape(-1)
    tok_flat = jnp.arange(A, dtype=jnp.int32) // TOP_K
    w_flat = wts.reshape(-1)
    order = jnp.argsort(e_flat)
    e_s, tok_s, w_s = e_flat[order], tok_flat[order], w_flat[order]
    counts = jnp.bincount(e_flat, length=N_EXPERTS)
    padded = (counts + DISPATCH_BLOCK - 1) // DISPATCH_BLOCK * DISPATCH_BLOCK
    start = jnp.cumsum(counts) - counts
    pend = jnp.cumsum(padded)
    pstart = pend - padded
    pos = pstart[e_s] + (jnp.arange(A, dtype=jnp.int32) - start[e_s])
    n_blocks = -(-A // DISPATCH_BLOCK) + N_EXPERTS
    P = n_blocks * DISPATCH_BLOCK
    tok_pad = jnp.full((P,), T, jnp.int32).at[pos].set(tok_s)
    w_pad = jnp.zeros((P,), w_s.dtype).at[pos].set(w_s)
    blk_start = jnp.arange(n_blocks, dtype=jnp.int32) * DISPATCH_BLOCK
    blk_e = jnp.minimum(jnp.searchsorted(pend, blk_start, side='right'), N_EXPERTS - 1)
    x_pad = jnp.concatenate([xf, jnp.zeros((1, D), xf.dtype)], axis=0)

    def step(acc, inp):
        e, tk, wv = inp
        xb = x_pad[tk]
        hb = jax.nn.silu(xb @ w1[e]) * (xb @ w3[e])
        yb = (hb @ w2[e]) * wv[:, None].astype(xf.dtype)
        return acc.at[tk].add(yb.astype(acc.dtype)), None

    acc, _ = lax.scan(step, jnp.zeros((T + 1, D), xf.dtype),
                      (blk_e, tok_pad.reshape(n_blocks, DISPATCH_BLOCK),
                       w_pad.reshape(n_blocks, DISPATCH_BLOCK)))
    return acc[:T]


def moe_ffn(h, w_router, e_bias, w1, w3, w2, ws1, ws3, ws2):
    B, S, D = h.shape
    xf = h.reshape(B * S, D)
    idx, wts = route(xf, w_router, e_bias)
    shared = (jax.nn.silu(xf @ ws1) * (xf @ ws3)) @ ws2
    return (shared + routed_experts(xf, idx, wts, w1, w3, w2)).reshape(B, S, D)


def setup_inputs(seed: int = 0) -> dict:
    key = jax.random.key(seed)
    ks = iter(jax.random.split(key, 40))
    f32 = jnp.float32
    L, D, H = DEPTH, D_MODEL, MLSTM_HEADS

    def nrm(shape, scale):
        return scale * jax.random.normal(next(ks), shape, f32)

    def gain(shape):
        return 1.0 + nrm(shape, 0.1)

    x = jax.random.normal(next(ks), (BATCH, SEQ, D), f32)
    c = jax.random.normal(next(ks), (BATCH, D), f32)
    positions = (jnp.arange(SEQ, dtype=jnp.int32)[None, :] +
                 jax.random.randint(next(ks), (BATCH, 1), 0, 1024, jnp.int32))
    forget_off = jnp.linspace(3.0, 6.0, H, dtype=f32)
    gate_off = jnp.concatenate([jnp.zeros((H,), f32), forget_off, jnp.zeros((H,), f32), forget_off])
    return {
        'x': x,
        'c': c,
        'positions': positions,
        'w_ada': nrm((L, D, 6 * D), 0.5 * D ** -0.5),
        'b_ada': nrm((L, 6 * D), 0.02),
        'w_in': nrm((L, D, IN_WIDTH), D ** -0.5),
        'w_pool': nrm((L, POOL_GROUPS, POOL_GROUP_DIM, POOL_GROUP_DIM), POOL_GROUP_DIM ** -0.5),
        's_pool': gain((L, POOL_WIDTH)),
        'conv_w': nrm((L, MLSTM_CONV, 2 * MLSTM_WIDTH), MLSTM_CONV ** -0.5),
        'conv_b': nrm((L, 2 * MLSTM_WIDTH), 0.02),
        'gate_b': gate_off[None, :] + nrm((L, MLSTM_N_GATES), 0.1),
        'gn_w': gain((L, MLSTM_WIDTH)),
        'g_q': gain((L, MLA_Q_LORA)),
        'g_kv': gain((L, MLA_KV_LORA)),
        'w_uq': nrm((L, MLA_Q_LORA, MLA_HEADS * (MLA_NOPE + MLA_ROPE)), MLA_Q_LORA ** -0.5),
        'w_uk': nrm((L, MLA_KV_LORA, MLA_HEADS * MLA_NOPE), MLA_KV_LORA ** -0.5),
        'w_uv': nrm((L, MLA_KV_LORA, MLA_HEADS * MLA_V), DEEPNORM_BETA * MLA_KV_LORA ** -0.5),
        'w_out': nrm((L, MIX_WIDTH, D), DEEPNORM_BETA * MIX_WIDTH ** -0.5),
        'ln1_g': gain((L, D)),
        'ln1_b': nrm((L, D), 0.02),
        'w_router': nrm((L, D, N_EXPERTS), D ** -0.5),
        'e_bias': nrm((L, N_EXPERTS), 0.01),
        'w1': nrm((L, N_EXPERTS, D, D_EXPERT), D ** -0.5),
        'w3': nrm((L, N_EXPERTS, D, D_EXPERT), D ** -0.5),
        'w2': nrm((L, N_EXPERTS, D_EXPERT, D), DEEPNORM_BETA * D_EXPERT ** -0.5),
        'ws1': nrm((L, D, D_EXPERT), D ** -0.5),
        'ws3': nrm((L, D, D_EXPERT), D ** -0.5),
        'ws2': nrm((L, D_EXPERT, D), DEEPNORM_BETA * D_EXPERT ** -0.5),
        'ln2_g': gain((L, D)),
        'ln2_b': nrm((L, D), 0.02),
    }


def reference(x, c, positions, w_ada, b_ada, w_in, w_pool, s_pool, conv_w, conv_b, gate_b,
              gn_w, g_q, g_kv, w_uq, w_uk, w_uv, w_out, ln1_g, ln1_b, w_router, e_bias,
              w1, w3, w2, ws1, ws3, ws2, ln2_g, ln2_b):
    inv_freq = ROPE_THETA ** (-jnp.arange(0, MLA_ROPE, 2, dtype=jnp.float32) / MLA_ROPE)
    ang = positions.astype(jnp.float32)[..., None] * inv_freq
    cos, sin = jnp.cos(ang).astype(x.dtype), jnp.sin(ang).astype(x.dtype)
    c_act = jax.nn.silu(c)
    for l in range(DEPTH):
        ada = c_act @ w_ada[l] + b_ada[l]
        sh1, sc1, g1, sh2, sc2, g2 = [a[:, None, :] for a in jnp.split(ada, 6, axis=-1)]
        h = layer_norm(x) * (1.0 + sc1) + sh1
        u = h @ w_in[l]
        u_pool, u_q, u_k, u_v, u_o, u_gate, u_dq, u_dkv, u_kr = split_cols(u)
        y_pool = pool_mixer(u_pool, w_pool[l], s_pool[l])
        y_mlstm = mlstm_mixer(u_q, u_k, u_v, u_o, u_gate, conv_w[l], conv_b[l], gate_b[l], gn_w[l])
        y_mla = mla_mixer(u_dq, u_dkv, u_kr, g_q[l], g_kv[l], w_uq[l], w_uk[l], w_uv[l], cos, sin)
        mix = jnp.concatenate([y_pool, y_mlstm, y_mla], axis=-1) @ w_out[l]
        x = layer_norm(DEEPNORM_ALPHA * x + g1 * mix) * ln1_g[l] + ln1_b[l]
        h = layer_norm(x) * (1.0 + sc2) + sh2
        ffn = moe_ffn(h, w_router[l], e_bias[l], w1[l], w3[l], w2[l], ws1[l], ws3[l], ws2[l])
        x = layer_norm(DEEPNORM_ALPHA * x + g2 * ffn) * ln2_g[l] + ln2_b[l]
    return x
```

```python
from contextlib import ExitStack
import numpy as np
import concourse.bass as bass
import concourse.mybir as mybir
from concourse.bass_utils import run_bass_kernel_spmd

F32 = mybir.dt.float32
F32R = mybir.dt.float32r
BF16 = mybir.dt.bfloat16
I32 = mybir.dt.int32
U32 = mybir.dt.uint32
AF = mybir.ActivationFunctionType
ALU = mybir.AluOpType
AX = mybir.AxisListType

S = 4096
D = 1024
NT = S // 128
L = 2
ALPHA = (2 * L) ** 0.25
LN_EPS = 1e-5
RMS_EPS = 1e-6

DEBUG = {}
STOP_AFTER = None
NBLK = 512


class R:
    __slots__ = ("w", "r", "name", "psum")

    def __init__(self, name="", psum=False):
        self.w = {}
        self.r = {}
        self.name = name
        self.psum = psum


class EngState:
    EPOCH = 16000

    def __init__(self, ctx, name, handle):
        self.ctx = ctx
        self.name = name
        self.h = handle
        self.sem = None
        self.count = 0
        self.own = set()
        self.seen = {}
        self.nsem = 0
        self.slots = []
        self.rr = 0

    def tick(self):
        if self.sem is None or self.count >= self.EPOCH:
            self.sem = self.ctx.new_sem(f"e_{self.name}_{self.nsem}")
            self.nsem += 1
            self.count = 0
            self.own.add(self.sem)
        self.count += 1
        return self.sem, self.count


class Ctx:
    def __init__(self, nc, es):
        self.nc = nc
        self.es = es
        self.nsems = 0
        self.E = {
            "pe": EngState(self, "pe", nc.tensor),
            "act": EngState(self, "act", nc.scalar),
            "dve": EngState(self, "dve", nc.vector),
            "pool": EngState(self, "pool", nc.gpsimd),
            "sp": EngState(self, "sp", nc.sync),
        }
        for q, n in (("sp", 40), ("pool", 40), ("act", 8)):
            self.E[q].slots = [[self.new_sem(f"d_{q}_{i}"), 0] for i in range(n)]

    def new_sem(self, name):
        self.nsems += 1
        return self.es.enter_context(self.nc.semaphore(name))

    def _waits(self, E, reads, writes, skip_own=False):
        need = {}
        for t in reads:
            for s, v in t.w.items():
                if need.get(s, 0) < v:
                    need[s] = v
            if t.psum:
                for s, v in t.r.items():
                    if s not in E.own and need.get(s, 0) < v:
                        need[s] = v
        for t in writes:
            for s, v in t.w.items():
                if need.get(s, 0) < v:
                    need[s] = v
            for s, v in t.r.items():
                if need.get(s, 0) < v:
                    need[s] = v
        for s, v in need.items():
            if skip_own and s in E.own:
                continue
            if E.seen.get(s, 0) >= v:
                continue
            E.h.wait_ge(s, v)
            E.seen[s] = v

    def op(self, eng, fn, reads=(), writes=()):
        E = self.E[eng]
        self._waits(E, reads, writes, skip_own=(eng == "pe"))
        ins = fn(E.h)
        s, v = E.tick()
        ins.then_inc(s, 1)
        for t in writes:
            t.w = {s: v}
            t.r = {}
        for t in reads:
            if t.r.get(s, 0) < v:
                t.r[s] = v
        return ins

    def dma(self, q, out, in_, reads=(), writes=(), fn=None):
        E = self.E[q]
        self._waits(E, reads, writes)
        slot = E.slots[E.rr % len(E.slots)]
        E.rr += 1
        s = slot[0]
        if slot[1] > 0 and E.seen.get(s, 0) < 16 * slot[1]:
            E.h.wait_ge(s, 16 * slot[1])
            E.seen[s] = 16 * slot[1]
        if fn is None:
            ins = E.h.dma_start(out=out, in_=in_)
        else:
            ins = fn(E.h)
        slot[1] += 1
        v = 16 * slot[1]
        ins.then_inc(s, 16)
        for t in writes:
            t.w = {s: v}
            t.r = {}
        for t in reads:
            if t.r.get(s, 0) < v:
                t.r[s] = v
        return ins

    def barrier(self):
        marks = []
        for e in self.E.values():
            if e.sem is not None and e.count > 0:
                marks.append((e.sem, e.count))
            for s, n in e.slots:
                if n > 0:
                    marks.append((s, 16 * n))
        for E in self.E.values():
            for s, v in marks:
                if s in E.own and E.name == "pe":
                    pass
                if E.seen.get(s, 0) < v:
                    E.h.wait_ge(s, v)
                    E.seen[s] = v

    def finish(self, extra=()):
        E = self.E["sp"]
        for e in self.E.values():
            if e.sem is not None and e.count > 0 and E.seen.get(e.sem, 0) < e.count:
                E.h.wait_ge(e.sem, e.count)
            for s, n in e.slots:
                if n > 0 and E.seen.get(s, 0) < 16 * n:
                    E.h.wait_ge(s, 16 * n)


def r32(ap):
    return ap.bitcast(F32R)


IN_OFF = dict(pool=0, q=256, k=512, v=768, o=1024, gate=1280, dq=1296, dkv=1552, kr=1680)


def build_program():
    nc = bass.Bass("TRN2", target_bir_lowering=False)
    es = ExitStack()
    cx = Ctx(nc, es)

    def din(name, shape, dt=F32):
        return nc.dram_tensor(name, list(shape), dt, kind="ExternalInput").ap()

    def dscr(name, shape, dt=F32):
        kind = "ExternalOutput" if DEBUG.get(name) else "Internal"
        return nc.dram_tensor(name, list(shape), dt, kind=kind).ap()

    def sb(name, shape, dt=F32):
        return es.enter_context(nc.sbuf_tensor(name, list(shape), dt))

    def ps(name, shape, dt=F32):
        return es.enter_context(nc.psum_tensor(name, list(shape), dt))

    x_in = din("x", [S, D])
    c_in = din("c_p", [128, 8])
    ident_in = din("ident", [128, 128])
    w_ada = din("w_ada", [L, D, 6 * D])
    b_ada_p = din("b_ada_p", [L, 128, 4, 8])
    b_ada = din("b_ada", [L, 6 * D])
    w_in_tok = din("w_in_tok", [L, D, 768])
    w_in_fm = din("w_in_fm", [L, D, 1024])
    band_in = din("band", [4, 5, 128, 128])
    w_pool_in = din("w_pool", [L, 4, 64, 64])
    s_pool_p = din("s_pool_p", [L, 64, 4])
    conv_p = din("conv_p", [L, 128, 4, 6])
    gate_b_in = din("gate_b4", [L, 4, 4])
    gn_w_in = din("gn_w", [L, 256])
    sel2_in = din("sel2", [4, 2, 128])
    mask_in = din("masks", [2, 128, 128])
    gbp_in = din("gbp", [L, 128, 4])
    pos_in = din("pos", [S], I32)
    ropec_in = din("ropec", [32, 2])
    g_q_p = din("g_q_p", [L, 128, 2])
    g_kv_p = din("g_kv_p", [L, 128, 1])
    w_uq_in = din("w_uq", [L, 256, 768])
    w_uq_sw_in = din("w_uq_sw", [L, 256, 768])
    w_uk_in = din("w_uk", [L, 128, 512])
    w_uv_in = din("w_uv", [L, 128, 512])
    sel64_in = din("sel64", [65, 64])
    w_out_in = din("w_out", [L, D, D])
    lnp_in = din("lnp", [L, 4, D])
    w_router_in = din("w_router", [L, D, 256])
    e_bias_in = din("e_bias", [L, 256])
    ws13_in = din("ws13", [L, D, 512])
    ws2_in = din("ws2", [L, 256, D])
    w1_in = din("w1", [L * 256 * 128, 2048])
    w3_in = din("w3", [L * 256 * 128, 2048])
    w2_in = din("w2", [L * 256 * 128, 2048])
    ustrict_in = din("ustrict", [128, 128])
    blkpos_in = din("blkpos", [128, 512])
    pidx_in = din("pidx", [128, 1])
    trih_in = din("trih", [2, 128, 128])
    cmask_in = din("cmask", [128, 64])
    psel_in = din("psel", [128, 128])
    y_out = nc.dram_tensor("y", [S, D], F32, kind="ExternalOutput").ap()

    U_tok = dscr("U_tok", [S, 768])
    U_fm = dscr("U_fm", [1024, S])
    mixT = dscr("mixT", [1024, S])
    ropeT = dscr("ropeT", [2, 32, S])
    X1 = dscr("X1", [S, D])
    XL = dscr("XL", [S, D])
    H2 = dscr("H2", [S, D], BF16)
    MS = dscr("MS", [S, 256], BF16)
    WN = dscr("WN", [S, 256])
    FFN = dscr("FFN", [S, D])
    NSLOT = 512 * 128
    Xs = dscr("Xs", [NSLOT, D], BF16)
    Ys = dscr("Ys", [NSLOT, D], BF16)

    ident = sb("ident_sb", [128, 128])
    r_ident = R("ident")
    cx.dma("sp", ident[:], ident_in[:, :], writes=[r_ident])
    identb = sb("identb_g", [128, 128], BF16)
    r_identb = R("identb")
    cx.op("dve", lambda e: e.tensor_copy(out=identb[:], in_=ident[:]), [r_ident], [r_identb])
    cact = sb("cact", [128, 8])
    cact_bc = sb("cact_bc", [128, 8, 128])
    r_cact = R("cact")
    craw = sb("craw", [128, 8])
    r_craw = R()
    cx.dma("sp", craw[:], c_in[:, :], writes=[r_craw])
    cx.op("act", lambda e: e.activation(out=cact[:], in_=craw[:], func=AF.Silu), reads=[r_craw], writes=[r_cact])
    r_cbc = R()
    for kc in range(8):
        cx.op("dve", lambda e, kc=kc: e.tensor_copy(out=cact_bc[:, kc, :], in_=cact[:, kc:kc + 1].to_broadcast([128, 128])),
              reads=[r_cact], writes=[r_cbc])

    def V(fn, r=(), w=()):
        return cx.op("dve", fn, r, w)

    def A(fn, r=(), w=()):
        return cx.op("act", fn, r, w)

    def P(fn, r=(), w=()):
        return cx.op("pe", fn, r, w)

    def G(fn, r=(), w=()):
        return cx.op("pool", fn, r, w)

    r_ropeT = R()
    with ExitStack() as ph:
        def sbp(name, shape, dt=F32):
            return ph.enter_context(nc.sbuf_tensor(f"rp_{name}", list(shape), dt))
        posi = sbp("posi", [32, S], I32)
        ang = sbp("ang", [32, S])
        kf = sbp("kf", [32, S])
        ki = sbp("ki", [32, S], I32)
        rr_ = sbp("rr", [32, S])
        mm = sbp("mm", [32, S])
        ropec = sbp("ropec", [32, 2])
        r_rp = R()
        cx.dma("sp", posi[:], pos_in.partition_broadcast(32), writes=[r_rp])
        cx.dma("sp", ropec[:], ropec_in[:, :], writes=[r_rp])
        V(lambda e: e.tensor_copy(out=ang[:], in_=posi[:]), [r_rp], [r_rp])
        V(lambda e: e.tensor_scalar(out=ang[:], in0=ang[:], scalar1=ropec[:, 0:1], scalar2=None, op0=ALU.mult), [r_rp], [r_rp])
        TWO_PI = 2.0 * np.pi
        C1 = 6.28125
        C2 = TWO_PI - C1
        PI_LO = 3.1415925
        for tb in range(2):
            src = ang
            if tb == 1:
                V(lambda e: e.tensor_scalar(out=mm[:], in0=ang[:], scalar1=float(np.pi / 2), scalar2=None, op0=ALU.add), [r_rp], [r_rp])
                src = mm
            V(lambda e, src=src: e.tensor_scalar(out=kf[:], in0=src[:], scalar1=float(1.0 / TWO_PI), scalar2=None, op0=ALU.mult), [r_rp], [r_rp])
            V(lambda e: e.tensor_copy(out=ki[:], in_=kf[:]), [r_rp], [r_rp])
            V(lambda e: e.tensor_copy(out=kf[:], in_=ki[:]), [r_rp], [r_rp])
            V(lambda e, src=src: e.scalar_tensor_tensor(out=rr_[:], in0=kf[:], scalar=-C1, in1=src[:], op0=ALU.mult, op1=ALU.add), [r_rp], [r_rp])
            V(lambda e: e.scalar_tensor_tensor(out=rr_[:], in0=kf[:], scalar=-C2, in1=rr_[:], op0=ALU.mult, op1=ALU.add), [r_rp], [r_rp])
            V(lambda e: e.tensor_scalar(out=kf[:], in0=rr_[:], scalar1=float(np.pi), scalar2=None, op0=ALU.is_gt), [r_rp], [r_rp])
            V(lambda e: e.scalar_tensor_tensor(out=rr_[:], in0=kf[:], scalar=-TWO_PI, in1=rr_[:], op0=ALU.mult, op1=ALU.add), [r_rp], [r_rp])
            V(lambda e: e.tensor_scalar(out=kf[:], in0=rr_[:], scalar1=float(-np.pi), scalar2=None, op0=ALU.is_lt), [r_rp], [r_rp])
            V(lambda e: e.scalar_tensor_tensor(out=rr_[:], in0=kf[:], scalar=TWO_PI, in1=rr_[:], op0=ALU.mult, op1=ALU.add), [r_rp], [r_rp])
            V(lambda e: e.tensor_scalar(out=rr_[:], in0=rr_[:], scalar1=PI_LO, scalar2=-PI_LO, op0=ALU.min, op1=ALU.max), [r_rp], [r_rp])
            A(lambda e: e.activation(out=rr_[:], in_=rr_[:], func=AF.Sin), [r_rp], [r_rp])
            if tb == 0:
                V(lambda e: e.tensor_scalar(out=rr_[:], in0=rr_[:], scalar1=ropec[:, 1:2], scalar2=None, op0=ALU.mult), [r_rp], [r_rp])
            cx.dma("sp", ropeT[tb], rr_[:], reads=[r_rp], writes=[r_ropeT])
        cx.barrier()

    adaP = sb("adaP", [128, 4, 8])
    gF = sb("gF", [128, 4, D])
    r_adaP = R()
    r_gF = R()
    for l in range(L):
        with ExitStack() as ph:
            def sbp(name, shape, dt=F32):
                return ph.enter_context(nc.sbuf_tensor(f"{name}_{l}", list(shape), dt))

            def psp(name, shape, dt=F32):
                return ph.enter_context(nc.psum_tensor(f"{name}_{l}", list(shape), dt))
            wa = [sbp(f"wa{i}", [128, 8, D]) for i in range(2)]
            r_wa = [R(), R()]
            badap = sbp("badap", [128, 4, 8])
            r_badap = R()
            cx.dma("sp", badap[:], b_ada_p[l], writes=[r_badap])
            bfr = sbp("bfr", [128, 4, D])
            r_bfr = R()
            GIDX = {2: 0, 5: 1, 3: 2, 4: 3}
            PIDX = {0: 0, 1: 1, 3: 2, 4: 3}
            for v, j in GIDX.items():
                cx.dma("sp", bfr[:, j, :], b_ada[l, v * D:(v + 1) * D].partition_broadcast(128), writes=[r_bfr])
            pA = psp("pA", [128, 512])
            r_pA = R(psum=True)
            pG = [psp(f"pG{i}", [128, 512]) for i in range(2)]
            r_pG = [R(psum=True), R(psum=True)]
            order = [0, 1, 3, 4, 2, 5]
            for i, v in enumerate(order):
                cx.dma("sp", wa[i % 2][:], w_ada[l, :, v * D:(v + 1) * D].rearrange("(kc p) n -> p kc n", p=128),
                       writes=[r_wa[i % 2]])
                w = wa[i % 2]
                if v in PIDX:
                    pi = PIDX[v]
                    for ncn in range(8):
                        for kc in range(8):
                            cx.op("pe", lambda e, w=w, ncn=ncn, kc=kc, pi=pi: e.matmul(
                                pA[:, pi * 8 + ncn:pi * 8 + ncn + 1], lhsT=w[:, kc, ncn * 128:(ncn + 1) * 128],
                                rhs=cact[:, kc:kc + 1], start=(kc == 0), stop=(kc == 7)),
                                reads=[r_wa[i % 2], r_cact], writes=[r_pA])
                if v in GIDX:
                    j = GIDX[v]
                    for hf in range(2):
                        for kc in range(8):
                            cx.op("pe", lambda e, w=w, hf=hf, kc=kc: e.matmul(
                                pG[hf][:, :], lhsT=cact_bc[:, kc, :], rhs=w[:, kc, hf * 512:(hf + 1) * 512],
                                start=(kc == 0), stop=(kc == 7)),
                                reads=[r_wa[i % 2], r_cbc], writes=[r_pG[hf]])
                        cx.op("dve", lambda e, hf=hf, j=j: e.tensor_tensor(
                            out=gF[:, j, hf * 512:(hf + 1) * 512], in0=pG[hf][:, :], in1=bfr[:, j, hf * 512:(hf + 1) * 512], op=ALU.add),
                            reads=[r_pG[hf], r_bfr], writes=[r_gF])
            cx.op("dve", lambda e: e.tensor_tensor(out=adaP[:].rearrange("p a b -> p (a b)"), in0=pA[:, 0:32],
                                                   in1=badap[:].rearrange("p a b -> p (a b)"), op=ALU.add),
                  reads=[r_pA, r_badap], writes=[r_adaP])
            for v in (1, 3):
                cx.op("dve", lambda e, v=v: e.tensor_scalar_add(out=adaP[:, v, :], in0=adaP[:, v, :], scalar1=1.0),
                      reads=[r_adaP], writes=[r_adaP])
            cx.op("dve", lambda e: e.tensor_scalar_add(out=gF[:, 3, :], in0=gF[:, 3, :], scalar1=1.0), reads=[r_gF], writes=[r_gF])
            cx.barrier()

        with ExitStack() as ph:
            def sbp(name, shape, dt=F32):
                return ph.enter_context(nc.sbuf_tensor(f"{name}_{l}", list(shape), dt))

            def psp(name, shape, dt=F32):
                return ph.enter_context(nc.psum_tensor(f"{name}_{l}", list(shape), dt))
            wtok = sbp("wtok", [128, 8, 768], BF16)
            wfm = sbp("wfm", [128, 8, 1024], BF16)
            r_wtok, r_wfm = R(), R()
            for kc in range(8):
                cx.dma("pool", wtok[:, kc, :], w_in_tok[l, kc * 128:(kc + 1) * 128, :], writes=[r_wtok])
                cx.dma("pool", wfm[:, kc, :], w_in_fm[l, kc * 128:(kc + 1) * 128, :], writes=[r_wfm])
            xt = [sbp(f"xt{i}", [128, D]) for i in range(2)]
            r_xt = [R(), R()]
            xn = [sbp(f"xn{i}", [128, D]) for i in range(2)]
            r_xn = [R(), R()]
            st = sbp("st", [128, 2, 6])
            mv = sbp("mv", [128, 2])
            rstd = sbp("rstd", [128, 1])
            r_st, r_mv, r_rstd = R(), R(), R()
            hT = [sbp(f"hT{i}", [128, 8, 512], BF16) for i in range(2)]
            r_hT = [R(), R()]
            pT = [psp(f"pT{i}", [128, 512]) for i in range(2)]
            r_pT = [R(psum=True), R(psum=True)]
            pU = [psp(f"pU{i}", [128, 512]) for i in range(4)]
            r_pU = [R(psum=True) for _ in range(4)]
            uo = [sbp(f"uo{i}", [128, 512]) for i in range(4)]
            r_uo = [R() for _ in range(4)]
            r_Utok, r_Ufm = R(), R()
            src = x_in if l == 0 else XL
            npu = 0
            for g in range(8):
                hTg = hT[g % 2]
                r_hTg = r_hT[g % 2]
                for tt in range(4):
                    t = g * 4 + tt
                    b = t % 2
                    cx.dma("sp", xt[b][:], src[t * 128:(t + 1) * 128, :], writes=[r_xt[b]])
                    for hf in range(2):
                        cx.op("dve", lambda e, b=b, hf=hf: e.bn_stats(out=st[:, hf, :], in_=xt[b][:, hf * 512:(hf + 1) * 512]),
                              reads=[r_xt[b]], writes=[r_st])
                    cx.op("dve", lambda e: e.bn_aggr(out=mv[:], in_=st[:].rearrange("p a b -> p (a b)")), reads=[r_st], writes=[r_mv])
                    cx.op("act", lambda e: e.activation(out=rstd[:], in_=mv[:, 1:2], func=AF.Sqrt, bias=LN_EPS, scale=1.0),
                          reads=[r_mv], writes=[r_rstd])
                    cx.op("dve", lambda e: e.reciprocal(out=rstd[:], in_=rstd[:]), reads=[r_rstd], writes=[r_rstd])
                    cx.op("dve", lambda e, b=b: e.tensor_scalar(out=xn[b][:], in0=xt[b][:], scalar1=mv[:, 0:1], scalar2=rstd[:, 0:1],
                                                                 op0=ALU.subtract, op1=ALU.mult),
                          reads=[r_xt[b], r_mv, r_rstd], writes=[r_xn[b]])
                    for q4 in range(2):
                        pt = pT[q4]
                        for k4 in range(4):
                            kc = q4 * 4 + k4
                            cx.op("pe", lambda e, pt=pt, k4=k4, kc=kc, b=b: e.transpose(
                                out=pt[:, k4 * 128:(k4 + 1) * 128], in_=xn[b][:, kc * 128:(kc + 1) * 128], identity=ident[:]),
                                reads=[r_xn[b], r_ident], writes=[r_pT[q4]])
                        for k4 in range(4):
                            kc = q4 * 4 + k4
                            cx.op("act", lambda e, pt=pt, k4=k4, kc=kc, tt=tt, hTg=hTg: e.activation(
                                out=hTg[:, kc, tt * 128:(tt + 1) * 128], in_=pt[:, k4 * 128:(k4 + 1) * 128], func=AF.Identity,
                                bias=adaP[:, 0, kc:kc + 1], scale=adaP[:, 1, kc:kc + 1]),
                                reads=[r_pT[q4], r_adaP], writes=[r_hTg])
                for tt in range(4):
                    t = g * 4 + tt
                    for hf in range(2):
                        i = npu % 4
                        npu += 1
                        for kc in range(8):
                            cx.op("pe", lambda e, i=i, kc=kc, tt=tt, hf=hf, hTg=hTg: e.matmul(
                                pU[i][:, 0:384], lhsT=hTg[:, kc, tt * 128:(tt + 1) * 128],
                                rhs=wtok[:, kc, hf * 384:(hf + 1) * 384], start=(kc == 0), stop=(kc == 7)),
                                reads=[r_hTg, r_wtok], writes=[r_pU[i]])
                        cx.op("act" if i % 2 else "dve", lambda e, i=i: (e.copy(out=uo[i][:, 0:384], in_=pU[i][:, 0:384]) if i % 2
                                                                          else e.tensor_copy(out=uo[i][:, 0:384], in_=pU[i][:, 0:384])),
                              reads=[r_pU[i]], writes=[r_uo[i]])
                        cx.dma("pool", U_tok[t * 128:(t + 1) * 128, hf * 384:(hf + 1) * 384], uo[i][:, 0:384],
                               reads=[r_uo[i]], writes=[r_Utok])
                for cb in range(8):
                    i = npu % 4
                    npu += 1
                    for kc in range(8):
                        cx.op("pe", lambda e, i=i, kc=kc, cb=cb, hTg=hTg: e.matmul(
                            pU[i][:, :], lhsT=wfm[:, kc, cb * 128:(cb + 1) * 128], rhs=hTg[:, kc, :],
                            start=(kc == 0), stop=(kc == 7)),
                            reads=[r_hTg, r_wfm], writes=[r_pU[i]])
                    cx.op("act" if i % 2 else "dve", lambda e, i=i: (e.copy(out=uo[i][:, :], in_=pU[i][:, :]) if i % 2
                                                                      else e.tensor_copy(out=uo[i][:, :], in_=pU[i][:, :])),
                          reads=[r_pU[i]], writes=[r_uo[i]])
                    cx.dma("pool", U_fm[cb * 128:(cb + 1) * 128, g * 512:(g + 1) * 512], uo[i][:, :],
                           reads=[r_uo[i]], writes=[r_Ufm])
            cx.barrier()
        if STOP_AFTER == "A":
            break
        r_mixT = R()

        def V(fn, r=(), w=()):
            return cx.op("dve", fn, r, w)

        def A(fn, r=(), w=()):
            return cx.op("act", fn, r, w)

        def P(fn, r=(), w=()):
            return cx.op("pe", fn, r, w)

        def G(fn, r=(), w=()):
            return cx.op("pool", fn, r, w)

        with ExitStack() as ph:
            def sbp(name, shape, dt=F32):
                return ph.enter_context(nc.sbuf_tensor(f"{name}_{l}", list(shape), dt))

            def psp(name, shape, dt=F32):
                return ph.enter_context(nc.psum_tensor(f"{name}_{l}", list(shape), dt))
            band = sbp("band", [128, 20, 128], BF16)
            r_band = R()
            cx.dma("pool", band[:], band_in.rearrange("g k p n -> p (g k) n"), writes=[r_band])
            wpl = sbp("wpl", [64, 4, 64], BF16)
            r_wpl = R()
            cx.dma("pool", wpl[:], w_pool_in[l].rearrange("g c d -> c g d"), writes=[r_wpl])
            spl = sbp("spl", [64, 4])
            r_spl = R()
            cx.dma("sp", spl[:], s_pool_p[l], writes=[r_spl])
            up = sbp("up", [128, NT, 256], BF16)
            r_up = R()
            for t4 in range(0, NT, 8):
                cx.dma("pool", up[:, t4:t4 + 8, :], U_tok[t4 * 128:(t4 + 8) * 128, 0:256].rearrange("(t p) c -> p t c", p=128),
                       reads=[r_Utok], writes=[r_up])
            pd = [psp(f"pd{i}", [64, 512]) for i in range(2)]
            py = [psp(f"py{i}", [64, 512]) for i in range(2)]
            r_pd = [R(psum=True), R(psum=True)]
            r_py = [R(psum=True), R(psum=True)]
            dTs = [sbp(f"dTs{i}", [64, 512], BF16) for i in range(2)]
            r_dTs = [R(), R()]
            yp = [sbp(f"yp{i}", [64, 4, 512]) for i in range(2)]
            r_yp = [R(), R()]
            n = 0
            for Gq in range(8):
                ypq = yp[Gq % 2]
                r_ypq = r_yp[Gq % 2]
                for g in range(4):
                    i = n % 2
                    n += 1
                    for tt in range(4):
                        t = Gq * 4 + tt
                        srcs = []
                        if t > 0:
                            srcs.append((t - 1, 0))
                        srcs.append((t, 3 if t == 0 else (4 if t == NT - 1 else 1)))
                        if t < NT - 1:
                            srcs.append((t + 1, 2))
                        for k, (j, typ) in enumerate(srcs):
                            P(lambda e, i=i, tt=tt, j=j, g=g, typ=typ, k=k, last=len(srcs) - 1: e.matmul(
                                pd[i][:, tt * 128:(tt + 1) * 128], lhsT=up[:, j, g * 64:(g + 1) * 64], rhs=band[:, g * 5 + typ, :],
                                start=(k == 0), stop=(k == last)), [r_up, r_band], [r_pd[i]])
                    A(lambda e, i=i: e.copy(out=dTs[i][:], in_=pd[i][:]), [r_pd[i]], [r_dTs[i]])
                    P(lambda e, i=i, g=g: e.matmul(py[i][:, :], lhsT=wpl[:, g, :], rhs=dTs[i][:], start=True, stop=True),
                      [r_wpl, r_dTs[i]], [r_py[i]])
                    V(lambda e, i=i, g=g, ypq=ypq: e.tensor_scalar(out=ypq[:, g, :], in0=py[i][:], scalar1=spl[:, g:g + 1], scalar2=None,
                                                                 op0=ALU.mult), [r_py[i], r_spl], [r_ypq])
                cx.dma("sp", mixT[0:256, Gq * 512:(Gq + 1) * 512].rearrange("(g c) t -> c g t", c=64), ypq[:],
                       reads=[r_ypq], writes=[r_mixT])
            cx.barrier()
        if STOP_AFTER == "B":
            break
        with ExitStack() as ph:
            def sbp(name, shape, dt=F32):
                return ph.enter_context(nc.sbuf_tensor(f"{name}_{l}", list(shape), dt))
            qkT = sbp("qkT", [128, 4, S], BF16)
            r_qkT = R()
            vx = sbp("vx", [128, NT, 4, 65], BF16)
            r_vx = R()
            hacc = sbp("hacc", [128, NT, 256])
            r_hacc = [R() for _ in range(NT)]
            gTcf = [sbp(f"gTcf{d}", [128, 4, NT]) for d in range(2)]
            gTcl = [sbp(f"gTcl{d}", [128, 4, NT]) for d in range(2)]
            decb = [sbp(f"decb{d}", [128, 2, NT]) for d in range(2)]
            r_gT = [R(), R()]
            maskt = sbp("maskt", [128, 2, 128])
            r_mask = R()
            cx.dma("sp", maskt[:], mask_in.rearrange("d p n -> p d n"), writes=[r_mask])
            with ExitStack() as ph2:
                def sb2(name, shape, dt=F32):
                    return ph2.enter_context(nc.sbuf_tensor(f"{name}_{l}", list(shape), dt))
                convp = sb2("convp", [128, 4, 6])
                r_convp = R()
                cx.dma("sp", convp[:], conv_p[l], writes=[r_convp])
                cin = [sb2(f"cin{i}", [128, S + 4]) for i in range(2)]
                r_cin = [R(), R()]
                acc = sb2("cacc", [128, S])
                r_acc = R()
                for i in range(2):
                    G(lambda e, i=i: e.memset(cin[i][:, 0:2], 0.0), [], [r_cin[i]])
                    G(lambda e, i=i: e.memset(cin[i][:, S + 2:S + 4], 0.0), [], [r_cin[i]])
                for ch in range(4):
                    b = ch % 2
                    cx.dma("sp", cin[b][:, 2:S + 2], U_fm[ch * 128:(ch + 1) * 128, :], reads=[r_Ufm], writes=[r_cin[b]])
                    V(lambda e, b=b, ch=ch: e.tensor_scalar(out=acc[:], in0=cin[b][:, 0:S], scalar1=convp[:, ch, 0:1], scalar2=convp[:, ch, 5:6],
                                                           op0=ALU.mult, op1=ALU.add), [r_cin[b], r_convp], [r_acc])
                    for j in range(1, 5):
                        V(lambda e, b=b, ch=ch, j=j: e.scalar_tensor_tensor(out=acc[:], in0=cin[b][:, j:j + S], scalar=convp[:, ch, j:j + 1],
                                                                          in1=acc[:], op0=ALU.mult, op1=ALU.add), [r_cin[b], r_convp, r_acc], [r_acc])
                    A(lambda e, ch=ch: e.activation(out=qkT[:, ch, :], in_=acc[:], func=AF.Silu), [r_acc], [r_qkT])
                vtmp = [sb2(f"vtmp{i}", [128, 8, 256]) for i in range(2)]
                r_vtmp = [R(), R()]
                G(lambda e: e.memset(vx[:, :, :, 64:65], 1.0), [], [r_vx])
                for i4 in range(4):
                    b = i4 % 2
                    cx.dma("sp", vtmp[b][:], U_tok[i4 * 1024:(i4 + 1) * 1024, 256:512].rearrange("(t p) c -> p t c", p=128),
                           reads=[r_Utok], writes=[r_vtmp[b]])
                    A(lambda e, b=b, i4=i4: e.copy(out=vx[:, i4 * 8:(i4 + 1) * 8, :, 0:64], in_=vtmp[b][:].rearrange("p t (h c) -> p t h c", c=64)),
                      [r_vtmp[b]], [r_vx])
                cx.barrier()
            with ExitStack() as ph2:
                def sb2(name, shape, dt=F32):
                    return ph2.enter_context(nc.sbuf_tensor(f"{name}_{l}", list(shape), dt))

                def ps2(name, shape, dt=F32):
                    return ph2.enter_context(nc.psum_tensor(f"{name}_{l}", list(shape), dt))
                gbp = sb2("gbp", [128, 4])
                ngb = sb2("ngb", [128, 4])
                r_gbp = R()
                cx.dma("sp", gbp[:], gbp_in[l], writes=[r_gbp])
                V(lambda e: e.tensor_scalar(out=ngb[:], in0=gbp[:], scalar1=-1.0, scalar2=None, op0=ALU.mult), [r_gbp], [r_gbp])
                trih = sb2("trih", [128, 2, 128])
                cmask = sb2("cmask", [128, 64])
                psel = sb2("psel", [128, 128])
                r_cst = R()
                cx.dma("sp", trih[:], trih_in.rearrange("d p n -> p d n"), writes=[r_cst])
                cx.dma("sp", cmask[:], cmask_in[:, :], writes=[r_cst])
                cx.dma("sp", psel[:], psel_in[:, :], writes=[r_cst])
                pg = ps2("pg", [128, 512])
                r_pg = R(psum=True)
                for d in range(2):
                    gi = sb2(f"gi{d}", [128, 128]); gf = sb2(f"gf{d}", [128, 128])
                    r_g = R()
                    ki, kf = 2 * d, 2 * d + 1
                    cx.dma("sp", gi[:], U_fm[896 + ki * 4:896 + ki * 4 + 4, :].rearrange("h (c l) -> (h c) l", l=128), reads=[r_Ufm], writes=[r_g])
                    cx.dma("sp", gf[:], U_fm[896 + kf * 4:896 + kf * 4 + 4, :].rearrange("h (c l) -> (h c) l", l=128), reads=[r_Ufm], writes=[r_g])
                    spt = sb2(f"spt{d}", [128, 128]); Pl = sb2(f"Pl{d}", [128, 128]); Pc = sb2(f"Pc{d}", [128, 128]); at = sb2(f"at{d}", [128, 128])
                    cols = sb2(f"cols{d}", [128, 8])
                    rows = sb2(f"rows{d}", [1, 4, 128])
                    r_w = R()
                    A(lambda e, kf=kf: e.activation(out=spt[:], in_=gf[:], func=AF.Exp, bias=ngb[:, kf:kf + 1], scale=-1.0), [r_g, r_gbp], [r_w])
                    A(lambda e: e.activation(out=spt[:], in_=spt[:], func=AF.Ln, bias=1.0, scale=1.0), [r_w], [r_w])
                    V(lambda e: e.tensor_tensor_scan(out=Pl[:], data0=spt[:], data1=spt[:], initial=0.0, op0=ALU.add, op1=ALU.max), [r_w], [r_w])
                    V(lambda e: e.tensor_copy(out=cols[:, 0:1], in_=Pl[:, 127:128]), [r_w], [r_w])
                    P(lambda e, d=d: e.matmul(pg[:, 0:1], lhsT=trih[:, d, :], rhs=cols[:, 0:1], start=True, stop=True), [r_w, r_cst], [r_pg])
                    V(lambda e: e.tensor_copy(out=cols[:, 1:2], in_=pg[:, 0:1]), [r_pg], [r_w])
                    if d == 0:
                        V(lambda e: e.tensor_scalar(out=Pc[:], in0=Pl[:], scalar1=cols[:, 1:2], scalar2=None, op0=ALU.add), [r_w], [r_w])
                    else:
                        V(lambda e: e.scalar_tensor_tensor(out=Pc[:], in0=Pl[:], scalar=-1.0, in1=spt[:], op0=ALU.mult, op1=ALU.add), [r_w], [r_w])
                        V(lambda e: e.tensor_scalar(out=Pc[:], in0=Pc[:], scalar1=cols[:, 0:1], scalar2=cols[:, 1:2], op0=ALU.add, op1=ALU.add), [r_w], [r_w])
                    V(lambda e, ki=ki: e.scalar_tensor_tensor(out=at[:], in0=gi[:], scalar=gbp[:, ki:ki + 1], in1=Pc[:], op0=ALU.add, op1=ALU.add),
                      [r_w, r_g, r_gbp], [r_w])
                    V(lambda e: e.tensor_reduce(out=cols[:, 2:3], in_=at[:], axis=AX.X, op=ALU.max), [r_w], [r_w])
                    P(lambda e: e.transpose(out=pg[0:1, 0:128], in_=cols[:, 2:3], identity=ident[:]), [r_w, r_ident], [r_pg])
                    V(lambda e: e.tensor_copy(out=rows[:, 0, :], in_=pg[0:1, 0:128]), [r_pg], [r_w])
                    cur = 0
                    for sh in (1, 2, 4, 8, 16):
                        a_ = rows[:, cur, :].rearrange("p (h c) -> p h c", c=32)
                        b_ = rows[:, 1 - cur, :].rearrange("p (h c) -> p h c", c=32)
                        if d == 0:
                            V(lambda e, a_=a_, b_=b_, sh=sh: e.tensor_tensor(out=b_[:, :, sh:], in0=a_[:, :, sh:], in1=a_[:, :, :32 - sh], op=ALU.max), [r_w], [r_w])
                            V(lambda e, a_=a_, b_=b_, sh=sh: e.tensor_copy(out=b_[:, :, :sh], in_=a_[:, :, :sh]), [r_w], [r_w])
                        else:
                            V(lambda e, a_=a_, b_=b_, sh=sh: e.tensor_tensor(out=b_[:, :, :32 - sh], in0=a_[:, :, :32 - sh], in1=a_[:, :, sh:], op=ALU.max), [r_w], [r_w])
                            V(lambda e, a_=a_, b_=b_, sh=sh: e.tensor_copy(out=b_[:, :, 32 - sh:], in_=a_[:, :, 32 - sh:]), [r_w], [r_w])
                        cur = 1 - cur
                    Mr = rows[:, cur, :].rearrange("p (h c) -> p h c", c=32)
                    dd = rows[:, 2, :].rearrange("p (h c) -> p h c", c=32)
                    V(lambda e: e.memset(rows[:, 2, :], 0.0), [r_w], [r_w])
                    if d == 0:
                        V(lambda e, Mr=Mr, dd=dd: e.tensor_tensor(out=dd[:, :, 0:31], in0=Mr[:, :, 0:31], in1=Mr[:, :, 1:32], op=ALU.subtract), [r_w], [r_w])
                    else:
                        V(lambda e, Mr=Mr, dd=dd: e.tensor_tensor(out=dd[:, :, 1:32], in0=Mr[:, :, 1:32], in1=Mr[:, :, 0:31], op=ALU.subtract), [r_w], [r_w])
                    A(lambda e: e.activation(out=rows[:, 3, :], in_=rows[:, 2, :], func=AF.Exp), [r_w], [r_w])
                    P(lambda e, cur=cur: e.transpose(out=pg[:, 0:1], in_=rows[0:1, cur, :], identity=ident[0:1, 0:1]), [r_w, r_ident], [r_pg])
                    P(lambda e: e.transpose(out=pg[:, 1:2], in_=rows[0:1, 3, :], identity=ident[0:1, 0:1]), [r_w, r_ident], [r_pg])
                    V(lambda e: e.tensor_copy(out=cols[:, 3:4], in_=pg[:, 0:1]), [r_pg], [r_w])
                    V(lambda e: e.tensor_copy(out=cols[:, 6:7], in_=pg[:, 1:2]), [r_pg], [r_w])
                    V(lambda e: e.tensor_scalar(out=cols[:, 4:5], in0=cols[:, 3:4], scalar1=-1.0, scalar2=None, op0=ALU.mult), [r_w], [r_w])
                    V(lambda e: e.tensor_scalar(out=cols[:, 5:6], in0=cols[:, 3:4], scalar1=-1.0, scalar2=-float(np.log(8.0)), op0=ALU.mult, op1=ALU.add), [r_w], [r_w])
                    A(lambda e: e.activation(out=at[:], in_=at[:], func=AF.Exp, bias=cols[:, 5:6], scale=1.0), [r_w], [r_w])
                    A(lambda e: e.activation(out=Pc[:], in_=Pc[:], func=AF.Exp, bias=cols[:, 4:5], scale=1.0), [r_w], [r_w])
                    P(lambda e: e.transpose(out=pg[:, 0:128], in_=at[:], identity=ident[:]), [r_w, r_ident], [r_pg])
                    P(lambda e: e.transpose(out=pg[:, 128:256], in_=Pc[:], identity=ident[:]), [r_w, r_ident], [r_pg])
                    V(lambda e, d=d: e.tensor_copy(out=gTcf[d][:].rearrange("p h c -> p (h c)"), in_=pg[:, 0:128]), [r_pg], [r_gT[d]])
                    V(lambda e, d=d: e.tensor_copy(out=gTcl[d][:].rearrange("p h c -> p (h c)"), in_=pg[:, 128:256]), [r_pg], [r_gT[d]])
                    V(lambda e: e.tensor_scalar(out=spt[:, 0:64], in0=cmask[:], scalar1=cols[:, 6:7], scalar2=None, op0=ALU.mult), [r_w, r_cst], [r_w])
                    P(lambda e: e.matmul(pg[:, 256:320], lhsT=psel[:], rhs=spt[:, 0:64], start=True, stop=True), [r_w, r_cst], [r_pg])
                    V(lambda e, d=d: e.tensor_copy(out=decb[d][:].rearrange("p j c -> p (j c)"), in_=pg[:, 256:320]), [r_pg], [r_gT[d]])
                cx.barrier()
            with ExitStack() as ph2:
                def sb2(name, shape, dt=F32):
                    return ph2.enter_context(nc.sbuf_tensor(f"{name}_{l}", list(shape), dt))

                def ps2(name, shape, dt=F32):
                    return ph2.enter_context(nc.psum_tensor(f"{name}_{l}", list(shape), dt))
                pS = [[ps2(f"pS{d}{par}", [128, 4, 128]) for par in range(2)] for d in range(2)]
                pnd = [ps2(f"pnd{d}", [128, 4, 128]) for d in range(2)]
                pdC1 = ps2("pdC", [128, 4, 128])
                pkt1 = ps2("pkt", [128, 1024], BF16)
                pdC = [pdC1, pdC1]
                pkt = [pkt1, pkt1]
                r_pS = [[R(psum=True), R(psum=True)], [R(psum=True), R(psum=True)]]
                r_pnd = [R(psum=True), R(psum=True)]
                r1 = R(psum=True); r2 = R(psum=True)
                r_pdC = [r1, r1]; r_pkt = [r2, r2]
                Sm = [sb2(f"Sm{d}", [128, 4, 128], BF16) for d in range(2)]
                kt = [sb2(f"kt{d}", [128, 4, 64], BF16) for d in range(2)]
                rr = [sb2(f"rr{d}", [128, 4]) for d in range(2)]
                tmph = [sb2(f"tmph{d}", [128, 4, 64]) for d in range(2)]
                Cst = [sb2(f"Cst{d}", [128, 2, 65]) for d in range(2)]
                Cstb = [sb2(f"Cstb{d}", [128, 2, 65], BF16) for d in range(2)]
                tmpC = [sb2(f"tmpC{d}", [128, 2, 65]) for d in range(2)]
                r_Sm = [R(), R()]; r_kt = [R(), R()]; r_rr = [R(), R()]; r_tmph = [R(), R()]
                r_Cst = [R(), R()]; r_Cstb = [R(), R()]; r_tmpC = [R(), R()]
                for d in range(2):
                    G(lambda e, d=d: e.memset(Cst[d][:], 0.0), [], [r_Cst[d]])
                    G(lambda e, d=d: e.memset(Cstb[d][:], 0.0), [], [r_Cstb[d]])
                done = set()
                for step in range(NT):
                    for d in range(2):
                        c = step if d == 0 else NT - 1 - step
                        last = (step == NT - 1)
                        cs = slice(c * 128, (c + 1) * 128)
                        for j in range(2):
                            P(lambda e, d=d, j=j, cs=cs: e.transpose(out=pkt[d][:, j * 128:(j + 1) * 128], in_=qkT[:, 2 + j, cs], identity=identb[:]),
                              [r_qkT, r_identb], [r_pkt[d]])
                        V(lambda e, d=d, c=c: e.tensor_tensor(out=kt[d][:], in0=pkt[d][:, 0:256].rearrange("p (h k) -> p h k", k=64),
                                                            in1=gTcf[d][:, :, c:c + 1].to_broadcast([128, 4, 64]), op=ALU.mult),
                          [r_pkt[d], r_gT[d]], [r_kt[d]])
                        for h in range(4):
                            hp, hj = h % 2, h // 2
                            P(lambda e, d=d, h=h, hp=hp, hj=hj, cs=cs: e.matmul(pS[d][hp][:, hj, :], lhsT=qkT[hp * 64:(hp + 1) * 64, 2 + hj, cs],
                                                                              rhs=qkT[hp * 64:(hp + 1) * 64, hj, cs], start=True, stop=True),
                              [r_qkT], [r_pS[d][hp]])
                        for h in range(4):
                            hp, hj = h % 2, h // 2
                            V(lambda e, d=d, h=h, c=c, hp=hp, hj=hj: e.scalar_tensor_tensor(out=Sm[d][:, h, :], in0=pS[d][hp][:, hj, :], scalar=gTcf[d][:, h, c:c + 1],
                                                                            in1=maskt[:, d, :], op0=ALU.mult, op1=ALU.mult),
                              [r_pS[d][hp], r_gT[d], r_mask], [r_Sm[d]])
                        for h in range(4):
                            hp, hj = h % 2, h // 2
                            P(lambda e, d=d, h=h, c=c: e.matmul(pnd[d][:, h, 0:65], lhsT=Sm[d][:, h, :], rhs=vx[:, c, h, :], start=True, stop=False),
                              [r_Sm[d], r_vx], [r_pnd[d]])
                            P(lambda e, d=d, h=h, hp=hp, hj=hj, cs=cs: e.matmul(pnd[d][:, h, 0:65], lhsT=qkT[hp * 64:(hp + 1) * 64, hj, cs],
                                                                              rhs=Cstb[d][hp * 64:(hp + 1) * 64, hj, :], start=False, stop=True),
                              [r_qkT, r_Cstb[d]], [r_pnd[d]])
                        A(lambda e, d=d: e.activation(out=rr[d][:].unsqueeze(2), in_=pnd[d][:, :, 64:65], func=AF.Abs),
                          [r_pnd[d]], [r_rr[d]])
                        V(lambda e, d=d, c=c: e.tensor_tensor(out=rr[d][:], in0=rr[d][:], in1=gTcl[d][:, :, c], op=ALU.max), [r_rr[d], r_gT[d]], [r_rr[d]])
                        V(lambda e, d=d: e.reciprocal(out=rr[d][:], in_=rr[d][:]), [r_rr[d]], [r_rr[d]])
                        hv = hacc[:, c, :].rearrange("p (h k) -> p h k", k=64)
                        if c not in done:
                            done.add(c)
                            V(lambda e, d=d, hv=hv: e.tensor_tensor(out=hv, in0=pnd[d][:, :, 0:64], in1=rr[d][:].unsqueeze(2).to_broadcast([128, 4, 64]), op=ALU.mult),
                              [r_pnd[d], r_rr[d]], [r_hacc[c]])
                        else:
                            V(lambda e, d=d: e.tensor_tensor(out=tmph[d][:], in0=pnd[d][:, :, 0:64], in1=rr[d][:].unsqueeze(2).to_broadcast([128, 4, 64]), op=ALU.mult),
                              [r_pnd[d], r_rr[d]], [r_tmph[d]])
                            G(lambda e, d=d, hv=hv: e.tensor_tensor(out=hv, in0=hv, in1=tmph[d][:], op=ALU.add), [r_tmph[d], r_hacc[c]], [r_hacc[c]])
                        if last:
                            continue
                        for h in range(4):
                            hj = h // 2
                            P(lambda e, d=d, h=h, hj=hj, c=c: e.matmul(pdC[d][:, h, 0:65], lhsT=kt[d][:, 2 * hj:2 * hj + 2, :].rearrange("p a k -> p (a k)"),
                                                                     rhs=vx[:, c, h, :], start=True, stop=True),
                              [r_kt[d], r_vx], [r_pdC[d]])
                        for par in range(2):
                            rs = slice(par * 64, (par + 1) * 64)
                            V(lambda e, d=d, rs=rs, par=par: e.tensor_tensor(out=tmpC[d][rs, :, :], in0=pdC[d][rs, par::2, 0:65], in1=Cst[d][rs, :, :], op=ALU.add),
                              [r_pdC[d], r_Cst[d]], [r_tmpC[d]])
                        for par in range(2):
                            rs = slice(par * 64, (par + 1) * 64)
                            V(lambda e, d=d, rs=rs, c=c: e.tensor_tensor(out=Cst[d][rs, :, :], in0=tmpC[d][rs, :, :],
                                                                       in1=decb[d][rs, :, c:c + 1].to_broadcast([64, 2, 65]), op=ALU.mult),
                              [r_tmpC[d], r_gT[d]], [r_Cst[d]])
                            G(lambda e, d=d, rs=rs, c=c: e.tensor_tensor(out=Cstb[d][rs, :, :], in0=tmpC[d][rs, :, :],
                                                                       in1=decb[d][rs, :, c:c + 1].to_broadcast([64, 2, 65]), op=ALU.mult),
                              [r_tmpC[d], r_gT[d]], [r_Cstb[d]])
                cx.barrier()
            with ExitStack() as ph2:
                def sb2(name, shape, dt=F32):
                    return ph2.enter_context(nc.sbuf_tensor(f"{name}_{l}", list(shape), dt))

                def ps2(name, shape, dt=F32):
                    return ph2.enter_context(nc.psum_tensor(f"{name}_{l}", list(shape), dt))
                gnw = sb2("gnw", [128, 256])
                r_gnw = R()
                cx.dma("sp", gnw[:], gn_w_in[l].partition_broadcast(128), writes=[r_gnw])
                uo_t = [sb2(f"uo_t{i}", [128, 256]) for i in range(2)]
                r_uot = [R(), R()]
                sq = sb2("sq", [128, 256]); hc = [sb2(f"hc{i}", [128, 256]) for i in range(2)]
                st4 = sb2("st4", [128, 4, 4])
                r_sq, r_st4 = R(), R()
                r_hc = [R(), R()]
                pyT = [ps2(f"pyT{i}", [128, 512]) for i in range(2)]
                r_pyT = [R(psum=True), R(psum=True)]
                ymT = [sb2(f"ymT{i}", [128, 2, 128]) for i in range(2)]
                r_ymT = [R(), R()]
                for t in range(NT):
                    b = t % 2
                    cx.dma("sp", uo_t[b][:], U_tok[t * 128:(t + 1) * 128, 512:768], reads=[r_Utok], writes=[r_uot[b]])
                    hv = hacc[:, t, :].rearrange("p (h k) -> p h k", k=64)
                    V(lambda e, hv=hv: e.tensor_reduce(out=st4[:, 0, :], in_=hv, axis=AX.X, op=ALU.add), [r_hacc[t]], [r_st4])
                    G(lambda e, t=t: e.tensor_tensor(out=sq[:], in0=hacc[:, t, :], in1=hacc[:, t, :], op=ALU.mult), [r_hacc[t]], [r_sq])
                    V(lambda e: e.tensor_reduce(out=st4[:, 1, :], in_=sq[:].rearrange("p (h k) -> p h k", k=64), axis=AX.X, op=ALU.add), [r_sq], [r_st4])
                    V(lambda e: e.tensor_scalar(out=st4[:, 2, :], in0=st4[:, 0, :], scalar1=1.0 / 64, scalar2=None, op0=ALU.mult), [r_st4], [r_st4])
                    V(lambda e: e.tensor_tensor(out=st4[:, 0, :], in0=st4[:, 2, :], in1=st4[:, 2, :], op=ALU.mult), [r_st4], [r_st4])
                    V(lambda e: e.scalar_tensor_tensor(out=st4[:, 3, :], in0=st4[:, 1, :], scalar=1.0 / 64, in1=st4[:, 0, :], op0=ALU.mult, op1=ALU.subtract), [r_st4], [r_st4])
                    A(lambda e: e.activation(out=st4[:, 3, :], in_=st4[:, 3, :], func=AF.Sqrt, bias=LN_EPS, scale=1.0), [r_st4], [r_st4])
                    V(lambda e: e.reciprocal(out=st4[:, 3, :], in_=st4[:, 3, :]), [r_st4], [r_st4])
                    hcv = hc[b][:].rearrange("p (h k) -> p h k", k=64)
                    V(lambda e, hv=hv, hcv=hcv: e.tensor_tensor(out=hcv, in0=hv, in1=st4[:, 2, :].unsqueeze(2).to_broadcast([128, 4, 64]), op=ALU.subtract),
                      [r_hacc[t], r_st4], [r_hc[b]])
                    V(lambda e, hcv=hcv: e.tensor_tensor(out=hcv, in0=hcv, in1=st4[:, 3, :].unsqueeze(2).to_broadcast([128, 4, 64]), op=ALU.mult),
                      [r_hc[b], r_st4], [r_hc[b]])
                    G(lambda e, b=b: e.tensor_tensor(out=hc[b][:], in0=hc[b][:], in1=gnw[:], op=ALU.mult), [r_hc[b], r_gnw], [r_hc[b]])
                    A(lambda e, b=b: e.activation(out=uo_t[b][:], in_=uo_t[b][:], func=AF.Sigmoid), [r_uot[b]], [r_uot[b]])
                    V(lambda e, b=b: e.tensor_tensor(out=hc[b][:], in0=hc[b][:], in1=uo_t[b][:], op=ALU.mult), [r_hc[b], r_uot[b]], [r_hc[b]])
                    for j in range(2):
                        P(lambda e, b=b, j=j: e.transpose(out=pyT[b][:, j * 128:(j + 1) * 128], in_=hc[b][:, j * 128:(j + 1) * 128], identity=ident[:]),
                          [r_hc[b], r_ident], [r_pyT[b]])
                    A(lambda e, b=b: e.copy(out=ymT[b][:].rearrange("p j t -> p (j t)"), in_=pyT[b][:, 0:256]), [r_pyT[b]], [r_ymT[b]])
                    cx.dma("sp", mixT[256:512, t * 128:(t + 1) * 128].rearrange("(j p) t -> p j t", p=128), ymT[b][:], reads=[r_ymT[b]], writes=[r_mixT])
                cx.barrier()
        if STOP_AFTER == "C":
            break
        with ExitStack() as ph:
            def sbp(name, shape, dt=F32):
                return ph.enter_context(nc.sbuf_tensor(f"{name}_{l}", list(shape), dt))

            def psp(name, shape, dt=F32):
                return ph.enter_context(nc.psum_tensor(f"{name}_{l}", list(shape), dt))
            SCALE = float(96 ** -0.5)
            cs2 = sbp("cs2", [128, 2, S], BF16)
            r_cs2 = R()
            for tb in range(2):
                cx.dma("pool", cs2[64:96, tb, :], ropeT[tb], reads=[r_ropeT], writes=[r_cs2])
            wuq = sbp("wuq", [128, 2, 768], BF16)
            wuqs = sbp("wuqs", [128, 2, 768], BF16)
            wuk = sbp("wuk", [128, 512], BF16)
            wuv = sbp("wuv", [128, 512], BF16)
            r_w = R()
            cx.dma("pool", wuq[:], w_uq_in[l].rearrange("(j p) n -> p j n", p=128), writes=[r_w])
            cx.dma("pool", wuqs[:], w_uq_sw_in[l].rearrange("(j p) n -> p j n", p=128), writes=[r_w])
            cx.dma("pool", wuk[:], w_uk_in[l], writes=[r_w])
            cx.dma("pool", wuv[:], w_uv_in[l], writes=[r_w])
            gqp = sbp("gqp", [128, 2]); gkvp = sbp("gkvp", [128, 1])
            r_g = R()
            cx.dma("sp", gqp[:], g_q_p[l], writes=[r_g])
            cx.dma("sp", gkvp[:], g_kv_p[l], writes=[r_g])
            sel64 = sbp("sel64", [65, 64])
            r_sel = R()
            cx.dma("sp", sel64[:], sel64_in[:, :], writes=[r_sel])
            onesb = sbp("onesb", [128, 128], BF16)
            r_ones = R()
            G(lambda e: e.memset(onesb[:], 1.0), [], [r_ones])
            qn = sbp("qn", [128, 2, S], BF16)
            ckv = sbp("ckv", [128, S], BF16)
            krope = sbp("krope", [128, S], BF16)
            vx2 = sbp("vx2", [128, NT, 8, 65], BF16)
            r_qn, r_ckv, r_krope, r_vx2 = R(), R(), R(), R()
            G(lambda e: e.memset(vx2[:, :, :, 64:65], 1.0), [], [r_vx2])
            pa = [psp(f"pa{i}", [128, 512]) for i in range(2)]
            pb_ = [psp(f"pb{i}", [128, 512]) for i in range(2)]
            pc_ = [psp(f"pc{i}", [128, 512]) for i in range(2)]
            pm = [psp(f"pm{i}", [128, 512]) for i in range(2)]
            r_pa = [R(psum=True), R(psum=True)]
            r_pb = [R(psum=True), R(psum=True)]
            r_pc = [R(psum=True), R(psum=True)]
            r_pm = [R(psum=True), R(psum=True)]
            with ExitStack() as ph2:
                def sb2(name, shape, dt=F32):
                    return ph2.enter_context(nc.sbuf_tensor(f"{name}_{l}", list(shape), dt))
                ub = [sb2(f"ub{i}", [128, 3, 512]) for i in range(2)]
                r_ub = [R(), R()]
                sqb = [sb2(f"sqb{i}", [128, 3, 512], BF16) for i in range(2)]
                r_sqb = [R(), R()]
                rs = [sb2(f"rs{i}", [128, 2, 512]) for i in range(2)]
                r_rs = [R(), R()]
                krr = sb2("krr", [128, 2, S], BF16)
                r_krr = R()
                cx.dma("pool", krr[64:96, 0, :], U_fm[912:944, :], reads=[r_Ufm], writes=[r_krr])
                cx.dma("pool", krr[64:96, 1, :], U_fm[944:976, :], reads=[r_Ufm], writes=[r_krr])
                tmpk = sb2("tmpk", [128, S])
                r_tmpk = R()
                V(lambda e: e.tensor_tensor(out=tmpk[64:96, :], in0=krr[64:96, 0, :], in1=cs2[64:96, 1, :], op=ALU.mult), [r_krr, r_cs2], [r_tmpk])
                G(lambda e: e.tensor_tensor(out=krr[64:96, 1, :], in0=krr[64:96, 1, :], in1=cs2[64:96, 0, :], op=ALU.mult), [r_krr, r_cs2], [r_krr])
                V(lambda e: e.tensor_tensor(out=krope[64:96, :], in0=tmpk[64:96, :], in1=krr[64:96, 1, :], op=ALU.add), [r_krr, r_tmpk], [r_krope])
                for blk in range(8):
                    b = blk % 2
                    bs = slice(blk * 512, (blk + 1) * 512)
                    cx.dma("sp", ub[b][:], U_fm[512:896, bs].rearrange("(j p) t -> p j t", p=128), reads=[r_Ufm], writes=[r_ub[b]])
                    A(lambda e, b=b: e.activation(out=sqb[b][:], in_=ub[b][:], func=AF.Square), [r_ub[b]], [r_sqb[b]])
                    for j in range(2):
                        P(lambda e, b=b, j=j: e.matmul(pm[0][:, :], lhsT=onesb[:], rhs=sqb[b][:, j, :], start=(j == 0), stop=(j == 1)),
                          [r_ones, r_sqb[b]], [r_pm[0]])
                    P(lambda e, b=b: e.matmul(pm[1][:, :], lhsT=onesb[:], rhs=sqb[b][:, 2, :], start=True, stop=True), [r_ones, r_sqb[b]], [r_pm[1]])
                    A(lambda e, b=b: e.activation(out=rs[b][:, 0, :], in_=pm[0][:, :], func=AF.Sqrt, bias=RMS_EPS, scale=1.0 / 256), [r_pm[0]], [r_rs[b]])
                    A(lambda e, b=b: e.activation(out=rs[b][:, 1, :], in_=pm[1][:, :], func=AF.Sqrt, bias=RMS_EPS, scale=1.0 / 128), [r_pm[1]], [r_rs[b]])
                    V(lambda e, b=b: e.reciprocal(out=rs[b][:], in_=rs[b][:]), [r_rs[b]], [r_rs[b]])
                    for j in range(2):
                        V(lambda e, b=b, j=j, bs=bs: e.scalar_tensor_tensor(out=qn[:, j, bs], in0=ub[b][:, j, :], scalar=gqp[:, j:j + 1], in1=rs[b][:, 0, :],
                                                                          op0=ALU.mult, op1=ALU.mult), [r_ub[b], r_g, r_rs[b]], [r_qn])
                    V(lambda e, b=b, bs=bs: e.scalar_tensor_tensor(out=ckv[:, bs], in0=ub[b][:, 2, :], scalar=gkvp[:, 0:1], in1=rs[b][:, 1, :],
                                                                 op0=ALU.mult, op1=ALU.mult), [r_ub[b], r_g, r_rs[b]], [r_ckv])
                for t in range(NT):
                    i = t % 2
                    P(lambda e, t=t, i=i: e.matmul(pc_[i][:, :], lhsT=ckv[:, t * 128:(t + 1) * 128], rhs=wuv[:], start=True, stop=True),
                      [r_ckv, r_w], [r_pc[i]])
                    A(lambda e, t=t, i=i: e.copy(out=vx2[:, t, :, 0:64], in_=pc_[i][:, :].rearrange("p (h c) -> p h c", c=64)), [r_pc[i]], [r_vx2])
                cx.barrier()
            kTh = [sbp(f"kTh{i}", [128, S], BF16) for i in range(2)]
            qTh = [sbp(f"qTh{i}", [128, S], BF16) for i in range(2)]
            r_kTh = [R(), R()]; r_qTh = [R(), R()]
            rt = [sbp(f"rt{i}", [128, 2, 512]) for i in range(2)]
            r_rt = [R(), R()]
            pT = [sbp(f"pTe{i}", [128, 512], BF16) for i in range(3)]
            r_pTs = [R(), R(), R()]
            osb = [sbp(f"osb{i}", [65, 512]) for i in range(2)]
            r_osb = [R(), R()]
            rec = [sbp(f"rec{i}", [64, 512]) for i in range(2)]
            r_rec = [R(), R()]
            npt = 0
            npc = 0
            for h in range(8):
                hb = h % 2
                kT, qT = kTh[hb], qTh[hb]
                G(lambda e, kT=kT: e.tensor_copy(out=kT[64:96, :], in_=krope[64:96, :]), [r_krope], [r_kTh[hb]])
                for blk in range(8):
                    bs = slice(blk * 512, (blk + 1) * 512)
                    i = npc % 2
                    npc += 1
                    P(lambda e, i=i, h=h, bs=bs: e.matmul(pc_[i][0:64, :], lhsT=wuk[:, h * 64:(h + 1) * 64], rhs=ckv[:, bs], start=True, stop=True),
                      [r_w, r_ckv], [r_pc[i]])
                    V(lambda e, i=i, kT=kT, bs=bs: e.tensor_copy(out=kT[0:64, bs], in_=pc_[i][0:64, :]), [r_pc[i]], [r_kTh[hb]])
                    i = npc % 2
                    npc += 1
                    for j in range(2):
                        P(lambda e, i=i, h=h, j=j, bs=bs: e.matmul(pc_[i][0:96, :], lhsT=wuq[:, j, h * 96:(h + 1) * 96], rhs=qn[:, j, bs],
                                                                 start=(j == 0), stop=(j == 1)), [r_w, r_qn], [r_pc[i]])
                    for j in range(2):
                        P(lambda e, i=i, h=h, j=j, bs=bs: e.matmul(pm[i][0:96, :], lhsT=wuqs[:, j, h * 96:(h + 1) * 96], rhs=qn[:, j, bs],
                                                                 start=(j == 0), stop=(j == 1)), [r_w, r_qn], [r_pm[i]])
                    A(lambda e, i=i, qT=qT, bs=bs: e.copy(out=qT[0:64, bs], in_=pc_[i][0:64, :]), [r_pc[i]], [r_qTh[hb]])
                    V(lambda e, i=i, bs=bs: e.tensor_tensor(out=rt[i][64:96, 0, :], in0=pc_[i][64:96, :], in1=cs2[64:96, 1, bs], op=ALU.mult),
                      [r_pc[i], r_cs2], [r_rt[i]])
                    V(lambda e, i=i, bs=bs: e.tensor_tensor(out=rt[i][64:96, 1, :], in0=pm[i][64:96, :], in1=cs2[64:96, 0, bs], op=ALU.mult),
                      [r_pm[i], r_cs2], [r_rt[i]])
                    G(lambda e, i=i, qT=qT, bs=bs: e.tensor_tensor(out=qT[64:96, bs], in0=rt[i][64:96, 0, :], in1=rt[i][64:96, 1, :], op=ALU.add),
                      [r_rt[i]], [r_qTh[hb]])
                for qb in range(8):
                    qs = slice(qb * 512, (qb + 1) * 512)
                    ob = qb % 2
                    for kt in range(NT):
                        i = npt % 2
                        ip = npt % 3
                        npt += 1
                        P(lambda e, i=i, kT=kT, qT=qT, kt=kt, qs=qs: e.matmul(pa[i][:, :], lhsT=kT[0:96, kt * 128:(kt + 1) * 128], rhs=qT[0:96, qs],
                                                                            start=True, stop=True), [r_kTh[hb], r_qTh[hb]], [r_pa[i]])
                        A(lambda e, i=i, ip=ip: e.activation(out=pT[ip][:], in_=pa[i][:, :], func=AF.Exp, scale=SCALE), [r_pa[i]], [r_pTs[ip]])
                        P(lambda e, ip=ip, ob=ob, kt=kt, h=h: e.matmul(pb_[ob][0:65, :], lhsT=vx2[:, kt, h, :], rhs=pT[ip][:],
                                                                     start=(kt == 0), stop=(kt == NT - 1)), [r_vx2, r_pTs[ip]], [r_pb[ob]])
                    V(lambda e, ob=ob: e.tensor_copy(out=osb[ob][:], in_=pb_[ob][0:65, :]), [r_pb[ob]], [r_osb[ob]])
                    P(lambda e, ob=ob: e.matmul(pm[ob][0:64, :], lhsT=sel64[:], rhs=osb[ob][:], start=True, stop=True), [r_sel, r_osb[ob]], [r_pm[ob]])
                    V(lambda e, ob=ob: e.reciprocal(out=rec[ob][:], in_=pm[ob][0:64, :]), [r_pm[ob]], [r_rec[ob]])
                    G(lambda e, ob=ob: e.tensor_tensor(out=rec[ob][:], in0=rec[ob][:], in1=osb[ob][0:64, :], op=ALU.mult), [r_rec[ob], r_osb[ob]], [r_rec[ob]])
                    cx.dma("sp", mixT[512 + h * 64:512 + (h + 1) * 64, qs], rec[ob][:], reads=[r_rec[ob]], writes=[r_mixT])
            cx.barrier()
        if STOP_AFTER == "D":
            break
        x_src = x_in if l == 0 else XL
        r_X1, r_H2, r_MS, r_WN, r_FFN = R(), R(), R(), R(), R()
        cnt = sb(f"cnt{l}", [128, 256])
        r_cnt = R()
        with ExitStack() as ph:
            def sbp(name, shape, dt=F32):
                return ph.enter_context(nc.sbuf_tensor(f"{name}_{l}", list(shape), dt))

            def psp(name, shape, dt=F32):
                return ph.enter_context(nc.psum_tensor(f"{name}_{l}", list(shape), dt))
            wout = sbp("wout", [128, 8, D], BF16)
            r_wout = R()
            for kc in range(8):
                cx.dma("pool", wout[:, kc, :], w_out_in[l, kc * 128:(kc + 1) * 128, :], writes=[r_wout])
            lnp = sbp("lnp", [128, 2, D])
            r_lnp = R()
            for j in range(2):
                cx.dma("sp", lnp[:, j, :], lnp_in[l, j].partition_broadcast(128), writes=[r_lnp])
            wr = sbp("wr", [128, 8, 256])
            r_wr = R()
            cx.dma("sp", wr[:], w_router_in[l].rearrange("(kc p) n -> p kc n", p=128), writes=[r_wr])
            ebias = sbp("ebias", [128, 256])
            r_eb = R()
            cx.dma("sp", ebias[:], e_bias_in[l].partition_broadcast(128), writes=[r_eb])
            ws13 = sbp("ws13", [128, 8, 512], BF16)
            ws2 = sbp("ws2", [128, 2, D], BF16)
            r_ws = R()
            for kc in range(8):
                cx.dma("pool", ws13[:, kc, :], ws13_in[l, kc * 128:(kc + 1) * 128, :], writes=[r_ws])
            for j in range(2):
                cx.dma("pool", ws2[:, j, :], ws2_in[l, j * 128:(j + 1) * 128, :], writes=[r_ws])
            onesb = sbp("onesbE", [128, 128], BF16)
            r_ones = R()
            G(lambda e: e.memset(onesb[:], 1.0), [], [r_ones])
            G(lambda e: e.memset(cnt[:], 0.0), [], [r_cnt])
            mxt = [sbp(f"mxt{i}", [128, 8, 512], BF16) for i in range(2)]
            r_mxt = [R(), R()]
            xt = [sbp(f"xtE{i}", [128, D]) for i in range(2)]
            r_xt = [R(), R()]
            z = [sbp(f"zE{i}", [128, D]) for i in range(2)]
            r_z = [R(), R()]
            x1 = [sbp(f"x1E{i}", [128, D]) for i in range(2)]
            r_x1 = [R(), R()]
            h2f = [sbp(f"h2f{i}", [128, D]) for i in range(2)]
            r_h2f = [R(), R()]
            h2b = [sbp(f"h2b{i}", [128, D], BF16) for i in range(2)]
            r_h2b = [R(), R()]
            h2T = sbp("h2T", [128, 8, 128]); h2Tb = sbp("h2Tb", [128, 8, 128], BF16)
            r_h2T, r_h2Tb = R(), R()
            st = sbp("stE", [128, 2, 6]); mv = sbp("mvE", [128, 2]); rstd = sbp("rstdE", [128, 1])
            r_st, r_mv, r_rstd = R(), R(), R()
            sc = sbp("scE", [128, 256]); sel = sbp("selE", [128, 256]); selm = sbp("selmE", [128, 256])
            m8g = sbp("m8g", [128, 8, 8]); gs = sbp("gsE", [128, 8]); m8 = sbp("m8E", [128, 8]); gm = sbp("gmE", [128, 2, 8])
            Mf = sbp("MfE", [128, 256]); Mb = [sbp(f"MbE{i}", [128, 256], BF16) for i in range(2)]
            wn = [sbp(f"wnE{i}", [128, 256]) for i in range(2)]
            ws_ = sbp("wsE", [128, 2])
            r_rt = R()
            r_Mb = [R(), R()]; r_wn = [R(), R()]
            s1 = sbp("s1E", [128, 256]); gsh = sbp("gshE", [128, 256], BF16); gT = sbp("gTE", [128, 2, 128], BF16)
            r_s1, r_gsh, r_gT = R(), R(), R()
            fo = [sbp(f"foE{i}", [128, D]) for i in range(2)]
            r_fo = [R(), R()]
            pX = [psp(f"pX{i}", [128, 512]) for i in range(2)]
            pY = [psp(f"pY{i}", [128, 512]) for i in range(2)]
            pZ = [psp(f"pZ{i}", [128, 512]) for i in range(2)]
            pW0 = psp("pW0", [128, 1024], BF16)
            pW1 = psp("pW1", [128, 512])
            r_pX = [R(psum=True), R(psum=True)]; r_pY = [R(psum=True), R(psum=True)]; r_pZ = [R(psum=True), R(psum=True)]
            r_pW0, r_pW1 = R(psum=True), R(psum=True)
            BIG = 1.0e4

            def layer_norm_stats(src, r_src):
                for hf in range(2):
                    V(lambda e, hf=hf: e.bn_stats(out=st[:, hf, :], in_=src[:, hf * 512:(hf + 1) * 512]), [r_src], [r_st])
                V(lambda e: e.bn_aggr(out=mv[:], in_=st[:].rearrange("p a b -> p (a b)")), [r_st], [r_mv])
                A(lambda e: e.activation(out=rstd[:], in_=mv[:, 1:2], func=AF.Sqrt, bias=LN_EPS, scale=1.0), [r_mv], [r_rstd])
                V(lambda e: e.reciprocal(out=rstd[:], in_=rstd[:]), [r_rstd], [r_rstd])

            for t in range(NT):
                b = t % 2
                if t % 4 == 0:
                    g4 = (t // 4) % 2
                    cx.dma("pool", mxt[g4][:], mixT[:, t * 128:(t + 4) * 128].rearrange("(kc p) t -> p kc t", p=128),
                           reads=[r_mixT], writes=[r_mxt[g4]])
                g4 = (t // 4) % 2
                tt = t % 4
                cx.dma("sp", xt[b][:], x_src[t * 128:(t + 1) * 128, :], writes=[r_xt[b]])
                for hf in range(2):
                    for kc in range(8):
                        P(lambda e, hf=hf, kc=kc, g4=g4, tt=tt: e.matmul(pX[hf][:, :], lhsT=mxt[g4][:, kc, tt * 128:(tt + 1) * 128],
                                                                      rhs=wout[:, kc, hf * 512:(hf + 1) * 512], start=(kc == 0), stop=(kc == 7)),
                          [r_mxt[g4], r_wout], [r_pX[hf]])
                    V(lambda e, hf=hf, b=b: e.tensor_tensor(out=z[b][:, hf * 512:(hf + 1) * 512], in0=pX[hf][:, :], in1=gF[:, 0, hf * 512:(hf + 1) * 512], op=ALU.mult),
                      [r_pX[hf], r_gF], [r_z[b]])
                V(lambda e, b=b: e.scalar_tensor_tensor(out=z[b][:], in0=xt[b][:], scalar=float(ALPHA), in1=z[b][:], op0=ALU.mult, op1=ALU.add),
                  [r_xt[b], r_z[b]], [r_z[b]])
                layer_norm_stats(z[b], r_z[b])
                V(lambda e, b=b: e.tensor_scalar(out=z[b][:], in0=z[b][:], scalar1=mv[:, 0:1], scalar2=rstd[:, 0:1], op0=ALU.subtract, op1=ALU.mult),
                  [r_z[b], r_mv, r_rstd], [r_z[b]])
                G(lambda e, b=b: e.tensor_tensor(out=z[b][:], in0=z[b][:], in1=lnp[:, 0, :], op=ALU.mult), [r_z[b], r_lnp], [r_z[b]])
                V(lambda e, b=b: e.tensor_tensor(out=x1[b][:], in0=z[b][:], in1=lnp[:, 1, :], op=ALU.add), [r_z[b], r_lnp], [r_x1[b]])
                cx.dma("sp", X1[t * 128:(t + 1) * 128, :], x1[b][:], reads=[r_x1[b]], writes=[r_X1])
                layer_norm_stats(x1[b], r_x1[b])
                V(lambda e, b=b: e.tensor_scalar(out=h2f[b][:], in0=x1[b][:], scalar1=mv[:, 0:1], scalar2=rstd[:, 0:1], op0=ALU.subtract, op1=ALU.mult),
                  [r_x1[b], r_mv, r_rstd], [r_h2f[b]])
                G(lambda e, b=b: e.tensor_tensor(out=h2f[b][:], in0=h2f[b][:], in1=gF[:, 3, :], op=ALU.mult), [r_h2f[b], r_gF], [r_h2f[b]])
                V(lambda e, b=b: e.tensor_tensor(out=h2f[b][:], in0=h2f[b][:], in1=gF[:, 2, :], op=ALU.add), [r_h2f[b], r_gF], [r_h2f[b]])
                A(lambda e, b=b: e.copy(out=h2b[b][:], in_=h2f[b][:]), [r_h2f[b]], [r_h2b[b]])
                cx.dma("sp", H2[t * 128:(t + 1) * 128, :], h2b[b][:], reads=[r_h2b[b]], writes=[r_H2])
                for q4 in range(2):
                    for k4 in range(4):
                        kc = q4 * 4 + k4
                        P(lambda e, q4=q4, k4=k4, kc=kc, b=b: e.transpose(out=pY[q4][:, k4 * 128:(k4 + 1) * 128], in_=h2f[b][:, kc * 128:(kc + 1) * 128], identity=ident[:]),
                          [r_h2f[b], r_ident], [r_pY[q4]])
                    V(lambda e, q4=q4: e.tensor_copy(out=h2T[:, q4 * 4:(q4 + 1) * 4, :].rearrange("p a t -> p (a t)"), in_=pY[q4][:, :]), [r_pY[q4]], [r_h2T])
                    A(lambda e, q4=q4: e.copy(out=h2Tb[:, q4 * 4:(q4 + 1) * 4, :].rearrange("p a t -> p (a t)"), in_=pY[q4][:, :]), [r_pY[q4]], [r_h2Tb])
                for kc in range(8):
                    P(lambda e, kc=kc: e.matmul(pZ[0][:, 0:256], lhsT=h2T[:, kc, :], rhs=wr[:, kc, :], start=(kc == 0), stop=(kc == 7)),
                      [r_h2T, r_wr], [r_pZ[0]])
                A(lambda e: e.activation(out=sc[:], in_=pZ[0][:, 0:256], func=AF.Sigmoid), [r_pZ[0]], [r_rt])
                V(lambda e: e.tensor_tensor(out=sel[:], in0=sc[:], in1=ebias[:], op=ALU.add), [r_rt, r_eb], [r_rt])
                for g in range(8):
                    V(lambda e, g=g: e.max(out=m8g[:, g, :], in_=sel[:, g * 32:(g + 1) * 32]), [r_rt], [r_rt])
                V(lambda e: e.tensor_tensor(out=gs[:], in0=m8g[:, :, 0], in1=m8g[:, :, 1], op=ALU.add), [r_rt], [r_rt])
                V(lambda e: e.max(out=m8[:], in_=gs[:]), [r_rt], [r_rt])
                V(lambda e: e.tensor_scalar(out=gm[:, 0, :], in0=gs[:], scalar1=m8[:, 3:4], scalar2=None, op0=ALU.is_ge), [r_rt], [r_rt])
                V(lambda e: e.tensor_scalar(out=gm[:, 1, :], in0=gm[:, 0, :], scalar1=BIG, scalar2=-BIG, op0=ALU.mult, op1=ALU.add), [r_rt], [r_rt])
                V(lambda e: e.tensor_tensor(out=selm[:].rearrange("p (g k) -> p g k", k=32), in0=sel[:].rearrange("p (g k) -> p g k", k=32),
                                            in1=gm[:, 0, :].unsqueeze(2).to_broadcast([128, 8, 32]), op=ALU.mult), [r_rt], [r_rt])
                V(lambda e: e.tensor_tensor(out=selm[:].rearrange("p (g k) -> p g k", k=32), in0=selm[:].rearrange("p (g k) -> p g k", k=32),
                                            in1=gm[:, 1, :].unsqueeze(2).to_broadcast([128, 8, 32]), op=ALU.add), [r_rt], [r_rt])
                V(lambda e: e.max(out=m8[:], in_=selm[:]), [r_rt], [r_rt])
                V(lambda e: e.tensor_scalar(out=Mf[:], in0=selm[:], scalar1=m8[:, 7:8], scalar2=None, op0=ALU.is_ge), [r_rt], [r_rt])
                G(lambda e, b=b: e.tensor_copy(out=Mb[b][:], in_=Mf[:]), [r_rt], [r_Mb[b]])
                V(lambda e: e.tensor_tensor(out=sel[:], in0=sc[:], in1=Mf[:], op=ALU.mult), [r_rt], [r_rt])
                V(lambda e: e.tensor_reduce(out=ws_[:, 0:1], in_=sel[:], axis=AX.X, op=ALU.add), [r_rt], [r_rt])
                V(lambda e: e.reciprocal(out=ws_[:, 1:2], in_=ws_[:, 0:1]), [r_rt], [r_rt])
                V(lambda e, b=b: e.tensor_scalar(out=wn[b][:], in0=sel[:], scalar1=ws_[:, 1:2], scalar2=2.5, op0=ALU.mult, op1=ALU.mult), [r_rt], [r_wn[b]])
                cx.dma("sp", MS[t * 128:(t + 1) * 128, :], Mb[b][:], reads=[r_Mb[b]], writes=[r_MS])
                cx.dma("sp", WN[t * 128:(t + 1) * 128, :], wn[b][:], reads=[r_wn[b]], writes=[r_WN])
                P(lambda e, b=b: e.matmul(pW1[:, 0:256], lhsT=onesb[:], rhs=Mb[b][:], start=True, stop=True), [r_ones, r_Mb[b]], [r_pW1])
                V(lambda e: e.tensor_tensor(out=cnt[:], in0=cnt[:], in1=pW1[:, 0:256], op=ALU.add), [r_pW1, r_cnt], [r_cnt])
                for kc in range(8):
                    P(lambda e, kc=kc: e.matmul(pZ[1][:, :], lhsT=h2Tb[:, kc, :], rhs=ws13[:, kc, :], start=(kc == 0), stop=(kc == 7)),
                      [r_h2Tb, r_ws], [r_pZ[1]])
                A(lambda e: e.activation(out=s1[:], in_=pZ[1][:, 0:256], func=AF.Silu), [r_pZ[1]], [r_s1])
                V(lambda e: e.tensor_tensor(out=gsh[:], in0=s1[:], in1=pZ[1][:, 256:512], op=ALU.mult), [r_s1, r_pZ[1]], [r_gsh])
                for j in range(2):
                    P(lambda e, j=j: e.transpose(out=pW0[:, j * 128:(j + 1) * 128], in_=gsh[:, j * 128:(j + 1) * 128], identity=identb[:]),
                      [r_gsh, r_identb], [r_pW0])
                A(lambda e: e.copy(out=gT[:].rearrange("p j t -> p (j t)"), in_=pW0[:, 0:256]), [r_pW0], [r_gT])
                for hf in range(2):
                    for j in range(2):
                        P(lambda e, hf=hf, j=j: e.matmul(pX[hf][:, :], lhsT=gT[:, j, :], rhs=ws2[:, j, hf * 512:(hf + 1) * 512], start=(j == 0), stop=(j == 1)),
                          [r_gT, r_ws], [r_pX[hf]])
                    if hf == 0:
                        A(lambda e, b=b: e.copy(out=fo[b][:, 0:512], in_=pX[0][:, :]), [r_pX[0]], [r_fo[b]])
                    else:
                        V(lambda e, b=b: e.tensor_copy(out=fo[b][:, 512:1024], in_=pX[1][:, :]), [r_pX[1]], [r_fo[b]])
                cx.dma("sp", FFN[t * 128:(t + 1) * 128, :], fo[b][:], reads=[r_fo[b]], writes=[r_FFN])
            cx.barrier()
        if STOP_AFTER == "E":
            break
        r_Xs, r_Ys = R(), R()
        idxs = sb(f"idxs{l}", [128, NT, 8], I32)
        wk = sb(f"wk{l}", [128, NT, 8])
        idxw = sb(f"idxw{l}", [128, 512], I32)
        r_idxs, r_wk, r_idxw = R(), R(), R()
        with ExitStack() as ph:
            def sbp(name, shape, dt=F32):
                return ph.enter_context(nc.sbuf_tensor(f"{name}_{l}", list(shape), dt))

            def psp(name, shape, dt=F32):
                return ph.enter_context(nc.psum_tensor(f"{name}_{l}", list(shape), dt))
            xq = sbp("xq", [128, 256]); qi = sbp("qi", [128, 256], I32); qf = sbp("qf", [128, 256]); gtm = sbp("gtm", [128, 256])
            pend = sbp("pend", [128, 256]); base = sbp("base", [128, 256])
            r_f1 = R(); r_base = R()
            V(lambda e: e.tensor_scalar(out=xq[:], in0=cnt[:], scalar1=127.0, scalar2=1.0 / 128, op0=ALU.add, op1=ALU.mult), [r_cnt], [r_f1])
            V(lambda e: e.tensor_copy(out=qi[:], in_=xq[:]), [r_f1], [r_f1])
            V(lambda e: e.tensor_copy(out=qf[:], in_=qi[:]), [r_f1], [r_f1])
            V(lambda e: e.tensor_tensor(out=gtm[:], in0=qf[:], in1=xq[:], op=ALU.is_gt), [r_f1], [r_f1])
            V(lambda e: e.tensor_tensor(out=qf[:], in0=qf[:], in1=gtm[:], op=ALU.subtract), [r_f1], [r_f1])
            V(lambda e: e.tensor_scalar(out=qf[:], in0=qf[:], scalar1=128.0, scalar2=None, op0=ALU.mult), [r_f1], [r_f1])
            V(lambda e: e.tensor_tensor_scan(out=pend[:], data0=qf[:], data1=qf[:], initial=0.0, op0=ALU.add, op1=ALU.max), [r_f1], [r_f1])
            V(lambda e: e.tensor_tensor(out=base[:], in0=pend[:], in1=qf[:], op=ALU.subtract), [r_f1], [r_base])
            V(lambda e: e.tensor_scalar(out=base[:], in0=base[:], scalar1=1.0, scalar2=None, op0=ALU.add), [r_base], [r_base])
            pF = [psp(f"pF{i}", [128, 512]) for i in range(2)]
            r_pF = [R(psum=True), R(psum=True)]
            pendT = sbp("pendT", [128, 2])
            for j in range(2):
                P(lambda e, j=j: e.transpose(out=pF[0][:, j:j + 1], in_=pend[0:1, j * 128:(j + 1) * 128], identity=ident[0:1, 0:1]), [r_f1, r_ident], [r_pF[0]])
            V(lambda e: e.tensor_copy(out=pendT[:], in_=pF[0][:, 0:2]), [r_pF[0]], [r_f1])
            blkpos = sbp("blkpos", [128, 512]); pidx = sbp("pidx", [128, 1])
            r_c2 = R()
            cx.dma("sp", blkpos[:], blkpos_in[:, :], writes=[r_c2])
            cx.dma("sp", pidx[:], pidx_in[:, :], writes=[r_c2])
            Gm = [sbp(f"Gm{j}", [128, 512], BF16) for j in range(2)]
            onesb = sbp("onesbF", [128, 128], BF16)
            ustr = sbp("ustr", [128, 128], BF16)
            r_ones = R()
            G(lambda e: e.memset(onesb[:], 1.0), [], [r_ones])
            cx.dma("pool", ustr[:], ustrict_in[:, :], writes=[r_ones])
            for j in range(2):
                V(lambda e, j=j: e.tensor_scalar(out=Gm[j][:], in0=blkpos[:], scalar1=pendT[:, j:j + 1], scalar2=None, op0=ALU.is_ge), [r_c2, r_f1], [r_f1])
            for j in range(2):
                P(lambda e, j=j: e.matmul(pF[1][:, :], lhsT=onesb[:], rhs=Gm[j][:], start=(j == 0), stop=(j == 1)), [r_ones, r_f1], [r_pF[1]])
            eall = sbp("eall", [128, 512])
            V(lambda e: e.tensor_scalar(out=eall[:], in0=pF[1][:, :], scalar1=255.0, scalar2=None, op0=ALU.min), [r_pF[1]], [r_f1])
            V(lambda e: e.tensor_scalar(out=eall[:], in0=eall[:], scalar1=128.0, scalar2=float(l * 256 * 128), op0=ALU.mult, op1=ALU.add), [r_f1], [r_f1])
            V(lambda e: e.tensor_scalar(out=idxw[:], in0=eall[:], scalar1=pidx[:, 0:1], scalar2=None, op0=ALU.add), [r_f1, r_c2], [r_idxw])
            Mt = [sbp(f"Mt{i}", [128, 256], BF16) for i in range(2)]
            wnt = [sbp(f"wnt{i}", [128, 256]) for i in range(2)]
            h2t = [sbp(f"h2t{i}", [128, D], BF16) for i in range(2)]
            r_Mt = [R(), R()]; r_wnt = [R(), R()]; r_h2t = [R(), R()]
            t1 = sbp("t1", [128, 256]); Vt = sbp("Vt", [128, 256]); junk = sbp("junk", [128, 256]); p8 = sbp("p8", [128, 8])
            r_t1, r_Vt, r_junk, r_p8 = R(), R(), R(), R()
            for t in range(NT):
                b = t % 2
                cx.dma("sp", Mt[b][:], MS[t * 128:(t + 1) * 128, :], reads=[r_MS], writes=[r_Mt[b]])
                cx.dma("sp", wnt[b][:], WN[t * 128:(t + 1) * 128, :], reads=[r_WN], writes=[r_wnt[b]])
                cx.dma("sp", h2t[b][:], H2[t * 128:(t + 1) * 128, :], reads=[r_H2], writes=[r_h2t[b]])
                P(lambda e, b=b: e.matmul(pF[0][:, 0:256], lhsT=ustr[:], rhs=Mt[b][:], start=True, stop=True), [r_ones, r_Mt[b]], [r_pF[0]])
                V(lambda e: e.tensor_tensor(out=t1[:], in0=pF[0][:, 0:256], in1=base[:], op=ALU.add), [r_pF[0], r_base], [r_t1])
                G(lambda e, b=b: e.tensor_tensor(out=Vt[:], in0=t1[:], in1=Mt[b][:], op=ALU.mult), [r_t1, r_Mt[b]], [r_Vt])
                V(lambda e: e.max(out=p8[:], in_=Vt[:]), [r_Vt], [r_p8])
                V(lambda e, t=t: e.tensor_scalar(out=idxs[:, t, :], in0=p8[:], scalar1=-1.0, scalar2=None, op0=ALU.add), [r_p8], [r_idxs])
                for k in range(8):
                    V(lambda e, t=t, k=k, b=b: e.scalar_tensor_tensor(out=junk[:], in0=Vt[:], scalar=p8[:, k:k + 1], in1=wnt[b][:], op0=ALU.is_equal, op1=ALU.mult,
                                                                    accum_out=wk[:, t, k:k + 1]), [r_Vt, r_p8, r_wnt[b]], [r_junk, r_wk])
                P(lambda e, b=b: e.matmul(pF[1][:, 0:256], lhsT=onesb[:], rhs=Mt[b][:], start=True, stop=True), [r_ones, r_Mt[b]], [r_pF[1]])
                V(lambda e: e.tensor_tensor(out=base[:], in0=base[:], in1=pF[1][:, 0:256], op=ALU.add), [r_pF[1], r_base, r_t1], [r_base])
                for k in range(8):
                    cx.dma("pool", None, None, reads=[r_h2t[b], r_idxs], writes=[r_Xs],
                           fn=lambda e, t=t, k=k, b=b: e.indirect_dma_start(
                               out=Xs[:, :], out_offset=bass.IndirectOffsetOnAxis(ap=idxs[:, t, k:k + 1].bitcast(U32), axis=0),
                               in_=h2t[b][:], in_offset=None))
            cx.barrier()
        with ExitStack() as ph:
            def sbp(name, shape, dt=F32):
                return ph.enter_context(nc.sbuf_tensor(f"{name}_{l}", list(shape), dt))

            def psp(name, shape, dt=F32):
                return ph.enter_context(nc.psum_tensor(f"{name}_{l}", list(shape), dt))
            Xb = [sbp(f"Xb{i}", [128, D], BF16) for i in range(2)]
            w1b = [sbp(f"w1b{i}", [128, 2048], BF16) for i in range(2)]
            w3b = [sbp(f"w3b{i}", [128, 2048], BF16) for i in range(2)]
            w2b = [sbp(f"w2b{i}", [128, 2048], BF16) for i in range(2)]
            xT = [sbp(f"xTb{i}", [128, 8, 128], BF16) for i in range(2)]
            s1 = [sbp(f"s1b{i}", [128, 256]) for i in range(2)]
            gb = [sbp(f"gbb{i}", [128, 256], BF16) for i in range(2)]
            gT = [sbp(f"gTb{i}", [128, 2, 128], BF16) for i in range(2)]
            Yb = [sbp(f"Yb{i}", [128, D], BF16) for i in range(2)]
            r_Xb = [R(), R()]; r_w1b = [R(), R()]; r_w3b = [R(), R()]; r_w2b = [R(), R()]; r_xT = [R(), R()]
            r_s1 = [R(), R()]; r_gb = [R(), R()]; r_gT = [R(), R()]; r_Yb = [R(), R()]
            pxT = [psp(f"pxT{i}", [128, 1024], BF16) for i in range(2)]
            phh = [psp(f"phh{i}", [128, 512]) for i in range(2)]
            pgT = psp("pgT", [128, 1024], BF16)
            pyy = [psp(f"pyy{i}", [128, 512]) for i in range(2)]
            r_pxT = [R(psum=True), R(psum=True)]; r_phh = [R(psum=True), R(psum=True)]; r_pgT = R(psum=True); r_pyy = [R(psum=True), R(psum=True)]
            for b in range(NBLK):
                i = b % 2
                cx.dma("sp", Xb[i][:], Xs[b * 128:(b + 1) * 128, :], reads=[r_Xs], writes=[r_Xb[i]])
                for wsrc, wdst, rw in ((w1_in, w1b, r_w1b), (w3_in, w3b, r_w3b), (w2_in, w2b, r_w2b)):
                    cx.dma("pool", None, None, reads=[r_idxw], writes=[rw[i]],
                           fn=lambda e, wsrc=wsrc, wdst=wdst, i=i, b=b: e.indirect_dma_start(
                               out=wdst[i][:], out_offset=None, in_=wsrc[:, :],
                               in_offset=bass.IndirectOffsetOnAxis(ap=idxw[:, b:b + 1].bitcast(U32), axis=0)))
                for j in range(8):
                    P(lambda e, i=i, j=j: e.transpose(out=pxT[i][:, j * 128:(j + 1) * 128], in_=Xb[i][:, j::8], identity=identb[:]),
                      [r_Xb[i], r_identb], [r_pxT[i]])
                A(lambda e, i=i: e.copy(out=xT[i][:, 0:4, :].rearrange("p a t -> p (a t)"), in_=pxT[i][:, 0:512]), [r_pxT[i]], [r_xT[i]])
                V(lambda e, i=i: e.tensor_copy(out=xT[i][:, 4:8, :].rearrange("p a t -> p (a t)"), in_=pxT[i][:, 512:1024]), [r_pxT[i]], [r_xT[i]])
                for j in range(8):
                    P(lambda e, i=i, j=j: e.matmul(phh[i][:, 0:256], lhsT=xT[i][:, j, :], rhs=w1b[i][:, j * 256:(j + 1) * 256], start=(j == 0), stop=(j == 7)),
                      [r_xT[i], r_w1b[i]], [r_phh[i]])
                for j in range(8):
                    P(lambda e, i=i, j=j: e.matmul(phh[i][:, 256:512], lhsT=xT[i][:, j, :], rhs=w3b[i][:, j * 256:(j + 1) * 256], start=(j == 0), stop=(j == 7)),
                      [r_xT[i], r_w3b[i]], [r_phh[i]])
                A(lambda e, i=i: e.activation(out=s1[i][:], in_=phh[i][:, 0:256], func=AF.Silu), [r_phh[i]], [r_s1[i]])
                V(lambda e, i=i: e.tensor_tensor(out=gb[i][:], in0=s1[i][:], in1=phh[i][:, 256:512], op=ALU.mult), [r_s1[i], r_phh[i]], [r_gb[i]])
                for j in range(2):
                    P(lambda e, i=i, j=j: e.transpose(out=pgT[:, j * 128:(j + 1) * 128], in_=gb[i][:, j::2], identity=identb[:]), [r_gb[i], r_identb], [r_pgT])
                A(lambda e, i=i: e.copy(out=gT[i][:].rearrange("p j t -> p (j t)"), in_=pgT[:, 0:256]), [r_pgT], [r_gT[i]])
                for hf in range(2):
                    for j in range(2):
                        P(lambda e, i=i, j=j, hf=hf: e.matmul(pyy[hf][:, :], lhsT=gT[i][:, j, :], rhs=w2b[i][:, j * 1024 + hf * 512:j * 1024 + (hf + 1) * 512],
                                                            start=(j == 0), stop=(j == 1)), [r_gT[i], r_w2b[i]], [r_pyy[hf]])
                A(lambda e, i=i: e.copy(out=Yb[i][:, 0:512], in_=pyy[0][:, :]), [r_pyy[0]], [r_Yb[i]])
                V(lambda e, i=i: e.tensor_copy(out=Yb[i][:, 512:1024], in_=pyy[1][:, :]), [r_pyy[1]], [r_Yb[i]])
                cx.dma("sp", Ys[b * 128:(b + 1) * 128, :], Yb[i][:], reads=[r_Yb[i]], writes=[r_Ys])
            cx.barrier()
        with ExitStack() as ph:
            def sbp(name, shape, dt=F32):
                return ph.enter_context(nc.sbuf_tensor(f"{name}_{l}", list(shape), dt))
            lnp2 = sbp("lnp2", [128, 2, D])
            r_lnp2 = R()
            for j in range(2):
                cx.dma("sp", lnp2[:, j, :], lnp_in[l, 2 + j].partition_broadcast(128), writes=[r_lnp2])
            yg = [sbp(f"yg{i}", [128, 8, D], BF16) for i in range(2)]
            r_yg = [R(), R()]
            acc = [sbp(f"accF{i}", [128, D]) for i in range(2)]
            r_acc = [R(), R()]
            x1t = [sbp(f"x1t{i}", [128, D]) for i in range(2)]
            r_x1t = [R(), R()]
            st = sbp("stF", [128, 2, 6]); mv = sbp("mvF", [128, 2]); rstd = sbp("rstdF", [128, 1])
            r_st, r_mv, r_rstd = R(), R(), R()
            dst = XL if l < L - 1 else y_out
            r_dst = R()
            for t in range(NT):
                b = t % 2
                for k in range(8):
                    cx.dma("pool", None, None, reads=[r_Ys, r_idxs], writes=[r_yg[b]],
                           fn=lambda e, t=t, k=k, b=b: e.indirect_dma_start(
                               out=yg[b][:, k, :], out_offset=None, in_=Ys[:, :],
                               in_offset=bass.IndirectOffsetOnAxis(ap=idxs[:, t, k:k + 1].bitcast(U32), axis=0)))
                cx.dma("sp", acc[b][:], FFN[t * 128:(t + 1) * 128, :], reads=[r_FFN], writes=[r_acc[b]])
                cx.dma("sp", x1t[b][:], X1[t * 128:(t + 1) * 128, :], reads=[r_X1], writes=[r_x1t[b]])
                for k in range(8):
                    V(lambda e, t=t, k=k, b=b: e.scalar_tensor_tensor(out=acc[b][:], in0=yg[b][:, k, :], scalar=wk[:, t, k:k + 1], in1=acc[b][:],
                                                                    op0=ALU.mult, op1=ALU.add), [r_yg[b], r_wk, r_acc[b]], [r_acc[b]])
                G(lambda e, b=b: e.tensor_tensor(out=acc[b][:], in0=acc[b][:], in1=gF[:, 1, :], op=ALU.mult), [r_acc[b], r_gF], [r_acc[b]])
                V(lambda e, b=b: e.scalar_tensor_tensor(out=acc[b][:], in0=x1t[b][:], scalar=float(ALPHA), in1=acc[b][:], op0=ALU.mult, op1=ALU.add),
                  [r_x1t[b], r_acc[b]], [r_acc[b]])
                for hf in range(2):
                    V(lambda e, hf=hf, b=b: e.bn_stats(out=st[:, hf, :], in_=acc[b][:, hf * 512:(hf + 1) * 512]), [r_acc[b]], [r_st])
                V(lambda e: e.bn_aggr(out=mv[:], in_=st[:].rearrange("p a b -> p (a b)")), [r_st], [r_mv])
                A(lambda e: e.activation(out=rstd[:], in_=mv[:, 1:2], func=AF.Sqrt, bias=LN_EPS, scale=1.0), [r_mv], [r_rstd])
                V(lambda e: e.reciprocal(out=rstd[:], in_=rstd[:]), [r_rstd], [r_rstd])
                V(lambda e, b=b: e.tensor_scalar(out=acc[b][:], in0=acc[b][:], scalar1=mv[:, 0:1], scalar2=rstd[:, 0:1], op0=ALU.subtract, op1=ALU.mult),
                  [r_acc[b], r_mv, r_rstd], [r_acc[b]])
                G(lambda e, b=b: e.tensor_tensor(out=acc[b][:], in0=acc[b][:], in1=lnp2[:, 0, :], op=ALU.mult), [r_acc[b], r_lnp2], [r_acc[b]])
                V(lambda e, b=b: e.tensor_tensor(out=x1t[b][:], in0=acc[b][:], in1=lnp2[:, 1, :], op=ALU.add), [r_acc[b], r_lnp2, r_x1t[b]], [r_x1t[b]])
                cx.dma("sp", dst[t * 128:(t + 1) * 128, :], x1t[b][:], reads=[r_x1t[b]], writes=[r_dst])
            cx.barrier()
        if STOP_AFTER == "F":
            break

    cx.finish()
    es.close()
    return nc


def prep_shared(inp):
    w_in = np.asarray(inp["w_in"], np.float32)
    o = IN_OFF
    w_tok = np.concatenate([w_in[:, :, o["pool"]:o["pool"] + 256], w_in[:, :, o["v"]:o["v"] + 256],
                            w_in[:, :, o["o"]:o["o"] + 256]], axis=2)
    kr = w_in[:, :, o["kr"]:o["kr"] + 32]
    kr_sw = np.concatenate([kr[:, :, 16:32], kr[:, :, 0:16]], axis=2)
    misc = np.concatenate([w_in[:, :, o["gate"]:o["gate"] + 16], kr, kr_sw, np.zeros((L, D, 48), np.float32)], axis=2)
    w_fm = np.concatenate([w_in[:, :, o["q"]:o["q"] + 256], w_in[:, :, o["k"]:o["k"] + 256],
                           w_in[:, :, o["dq"]:o["dq"] + 256], w_in[:, :, o["dkv"]:o["dkv"] + 128], misc], axis=2)
    b_ada = np.asarray(inp["b_ada"], np.float32)
    bp = np.stack([b_ada[:, v * D:(v + 1) * D].reshape(L, 8, 128).transpose(0, 2, 1) for v in (0, 1, 3, 4)], axis=2)
    band = np.zeros((4, 5, 128, 128), np.float32)
    for g, w in enumerate((2, 4, 8, 16)):
        A_ = np.zeros((S, S), np.float32) if False else None
        def arow(t):
            lo = max(t - w // 2, 0); hi = min(t + w // 2, S)
            return lo, hi, 1.0 / (hi - lo)
        def fill(mat, ti, tj):
            for tl in range(128):
                t = ti * 128 + tl
                lo, hi, inv = arow(t)
                for tp in range(max(lo, tj * 128), min(hi, tj * 128 + 128)):
                    mat[tp - tj * 128, tl] += inv
                if tj * 128 <= t < tj * 128 + 128:
                    mat[t - tj * 128, tl] -= 1.0
        fill(band[g, 0], 5, 4)
        fill(band[g, 1], 5, 5)
        fill(band[g, 2], 5, 6)
        fill(band[g, 3], 0, 0)
        fill(band[g, 4], NT - 1, NT - 1)
    conv_w = np.asarray(inp["conv_w"], np.float32)
    conv_b = np.asarray(inp["conv_b"], np.float32)
    conv_p = np.concatenate([conv_w.transpose(0, 2, 1), conv_b[:, :, None]], axis=2)
    conv_p = conv_p.reshape(L, 4, 128, 6).transpose(0, 2, 1, 3)
    gate_b4 = np.asarray(inp["gate_b"], np.float32).reshape(L, 4, 4).transpose(0, 2, 1)
    sel2 = np.zeros((4, 2, 128), np.float32)
    for j in range(2):
        sel2[2 * j, j, 0:64] = 1.0
        sel2[2 * j + 1, j, 64:128] = 1.0
    masks = np.zeros((2, 128, 128), np.float32)
    masks[0] = np.triu(np.ones((128, 128), np.float32))
    masks[1] = np.tril(np.ones((128, 128), np.float32))
    gb = np.asarray(inp["gate_b"], np.float32)
    gbp = np.zeros((L, 128, 4), np.float32)
    for h in range(4):
        for k in range(4):
            gbp[:, h * 32:(h + 1) * 32, k] = gb[:, k * 4 + h][:, None]
    trih = np.zeros((2, 128, 128), np.float32)
    cmask = np.zeros((128, 64), np.float32)
    psel = np.zeros((128, 128), np.float32)
    for h in range(4):
        for c in range(32):
            p = h * 32 + c
            trih[0, h * 32:h * 32 + c, p] = 1.0
            trih[1, h * 32 + c + 1:(h + 1) * 32, p] = 1.0
            cmask[p, (h // 2) * 32 + c] = 1.0
            psel[p, (h % 2) * 64:(h % 2) * 64 + 64] = 1.0
    inv_freq = (np.float32(10000.0) ** (-np.arange(0, 32, 2, dtype=np.float32) / np.float32(32))).astype(np.float32)
    ropec = np.zeros((32, 2), np.float32)
    ropec[:, 0] = np.concatenate([inv_freq, inv_freq])
    ropec[:16, 1] = -1.0
    ropec[16:, 1] = 1.0
    w_uq = np.asarray(inp["w_uq"], np.float32)
    w_uq_sw = w_uq.copy().reshape(L, 256, 8, 96)
    w_uq_sw[:, :, :, 64:80] = w_uq.reshape(L, 256, 8, 96)[:, :, :, 80:96]
    w_uq_sw[:, :, :, 80:96] = w_uq.reshape(L, 256, 8, 96)[:, :, :, 64:80]
    w_uq_sw = w_uq_sw.reshape(L, 256, 768)
    sel64 = np.zeros((65, 64), np.float32)
    sel64[64, :] = 1.0
    lnp = np.stack([np.asarray(inp[k], np.float32) for k in ("ln1_g", "ln1_b", "ln2_g", "ln2_b")], axis=1)
    ws13 = np.concatenate([np.asarray(inp["ws1"], np.float32), np.asarray(inp["ws3"], np.float32)], axis=2)
    blkpos = np.tile((np.arange(512, dtype=np.float32) * 128.0)[None, :], (128, 1))
    sh = {
        "w_out": np.ascontiguousarray(inp["w_out"], np.float32),
        "lnp": np.ascontiguousarray(lnp),
        "w_router": np.ascontiguousarray(inp["w_router"], np.float32),
        "e_bias": np.ascontiguousarray(inp["e_bias"], np.float32),
        "ws13": np.ascontiguousarray(ws13),
        "ws2": np.ascontiguousarray(inp["ws2"], np.float32),
        "w1": np.asarray(inp["w1"], np.float32).reshape(L * 256 * 128, 2048),
        "w3": np.asarray(inp["w3"], np.float32).reshape(L * 256 * 128, 2048),
        "w2": np.asarray(inp["w2"], np.float32).reshape(L * 256 * 128, 2048),
        "ustrict": np.triu(np.ones((128, 128), np.float32), 1),
        "blkpos": blkpos,
        "pidx": np.arange(128, dtype=np.float32).reshape(128, 1),
        "ropec": ropec,
        "g_q_p": np.ascontiguousarray(np.asarray(inp["g_q"], np.float32).reshape(L, 2, 128).transpose(0, 2, 1)),
        "g_kv_p": np.ascontiguousarray(np.asarray(inp["g_kv"], np.float32).reshape(L, 128, 1)),
        "w_uq": np.ascontiguousarray(w_uq), "w_uq_sw": np.ascontiguousarray(w_uq_sw),
        "w_uk": np.ascontiguousarray(inp["w_uk"], np.float32), "w_uv": np.ascontiguousarray(inp["w_uv"], np.float32),
        "sel64": sel64,
        "gbp": gbp, "trih": trih, "cmask": cmask, "psel": psel,
        "band": band,
        "w_pool": np.ascontiguousarray(inp["w_pool"], np.float32),
        "s_pool_p": np.ascontiguousarray(np.asarray(inp["s_pool"], np.float32).reshape(L, 4, 64).transpose(0, 2, 1)),
        "conv_p": np.ascontiguousarray(conv_p),
        "gate_b4": np.ascontiguousarray(gate_b4),
        "gn_w": np.ascontiguousarray(inp["gn_w"], np.float32),
        "sel2": sel2,
        "masks": masks,
        "ident": np.eye(128, dtype=np.float32),
        "w_ada": np.ascontiguousarray(inp["w_ada"], np.float32),
        "b_ada_p": np.ascontiguousarray(bp),
        "b_ada": np.ascontiguousarray(b_ada),
        "w_in_tok": np.ascontiguousarray(w_tok),
        "w_in_fm": np.ascontiguousarray(w_fm),
    }
    return sh


def prep_core(inp, b):
    x = np.asarray(inp["x"][b], np.float32)
    c = np.asarray(inp["c"][b], np.float32)
    return {"x": np.ascontiguousarray(x), "c_p": np.ascontiguousarray(c.reshape(8, 128).T),
            "pos": np.ascontiguousarray(np.asarray(inp["positions"][b], np.int32))}


def kernel(**inp):
    nc = build_program()
    sh = prep_shared(inp)
    in_maps = []
    for b in range(8):
        m = dict(sh)
        m.update(prep_core(inp, b))
        in_maps.append(m)
    res = run_bass_kernel_spmd(nc, in_maps, core_ids=list(range(8)))
    kernel.last = res
    return np.stack([r["y"] for r in res.results], axis=0)
```

```python
from contextlib import ExitStack
import numpy as np
import concourse.bass as bass
import concourse.mybir as mybir
from concourse.bass_utils import run_bass_kernel_spmd

F32 = mybir.dt.float32
F32R = mybir.dt.float32r
BF16 = mybir.dt.bfloat16
I32 = mybir.dt.int32
U32 = mybir.dt.uint32
AF = mybir.ActivationFunctionType
ALU = mybir.AluOpType
AX = mybir.AxisListType

S = 4096
D = 1024
NT = S // 128
L = 2
ALPHA = (2 * L) ** 0.25
LN_EPS = 1e-5
RMS_EPS = 1e-6

DEBUG = {}
STOP_AFTER = None
OOB_IDX = 1048576.0
NBLK = 512


class R:
    __slots__ = ("w", "r", "name", "psum")

    def __init__(self, name="", psum=False):
        self.w = {}
        self.r = {}
        self.name = name
        self.psum = psum


class EngState:
    EPOCH = 16000

    def __init__(self, ctx, name, handle):
        self.ctx = ctx
        self.name = name
        self.h = handle
        self.sem = None
        self.count = 0
        self.own = set()
        self.seen = {}
        self.nsem = 0
        self.slots = []
        self.rr = 0

    def tick(self):
        if self.sem is None or self.count >= self.EPOCH:
            self.sem = self.ctx.new_sem(f"e_{self.name}_{self.nsem}")
            self.nsem += 1
            self.count = 0
            self.own.add(self.sem)
        self.count += 1
        return self.sem, self.count


class Ctx:
    def __init__(self, nc, es):
        self.nc = nc
        self.es = es
        self.nsems = 0
        self.E = {
            "pe": EngState(self, "pe", nc.tensor),
            "act": EngState(self, "act", nc.scalar),
            "dve": EngState(self, "dve", nc.vector),
            "pool": EngState(self, "pool", nc.gpsimd),
            "sp": EngState(self, "sp", nc.sync),
        }
        for q, n in (("sp", 40), ("pool", 40), ("act", 8)):
            self.E[q].slots = [[self.new_sem(f"d_{q}_{i}"), 0] for i in range(n)]

    def new_sem(self, name):
        self.nsems += 1
        return self.es.enter_context(self.nc.semaphore(name))

    def _waits(self, E, reads, writes, skip_own=False):
        need = {}
        for t in reads:
            for s, v in t.w.items():
                if need.get(s, 0) < v:
                    need[s] = v
            if t.psum:
                for s, v in t.r.items():
                    if s not in E.own and need.get(s, 0) < v:
                        need[s] = v
        for t in writes:
            for s, v in t.w.items():
                if need.get(s, 0) < v:
                    need[s] = v
            for s, v in t.r.items():
                if need.get(s, 0) < v:
                    need[s] = v
        for s, v in need.items():
            if skip_own and s in E.own:
                continue
            if E.seen.get(s, 0) >= v:
                continue
            E.h.wait_ge(s, v)
            E.seen[s] = v

    def op(self, eng, fn, reads=(), writes=()):
        E = self.E[eng]
        self._waits(E, reads, writes, skip_own=(eng == "pe"))
        ins = fn(E.h)
        s, v = E.tick()
        ins.then_inc(s, 1)
        for t in writes:
            t.w = {s: v}
            t.r = {}
        for t in reads:
            if t.r.get(s, 0) < v:
                t.r[s] = v
        return ins

    def dma(self, q, out, in_, reads=(), writes=(), fn=None, acc=True):
        E = self.E[q]
        if acc:
            for t in writes:
                if t.r:
                    self._waits(E, (), [t])
                    t.w = {}
                    t.r = {}
            self._waits(E, reads, ())
        else:
            self._waits(E, reads, writes)
        slot = E.slots[E.rr % len(E.slots)]
        E.rr += 1
        s = slot[0]
        if slot[1] > 0 and E.seen.get(s, 0) < 16 * slot[1]:
            E.h.wait_ge(s, 16 * slot[1])
            E.seen[s] = 16 * slot[1]
        if fn is None:
            ins = E.h.dma_start(out=out, in_=in_)
        else:
            ins = fn(E.h)
        slot[1] += 1
        v = 16 * slot[1]
        ins.then_inc(s, 16)
        for t in writes:
            if acc:
                t.w[s] = v
            else:
                t.w = {s: v}
                t.r = {}
        for t in reads:
            if t.r.get(s, 0) < v:
                t.r[s] = v
        return ins

    def barrier(self):
        marks = []
        for e in self.E.values():
            if e.sem is not None and e.count > 0:
                marks.append((e.sem, e.count))
            for s, n in e.slots:
                if n > 0:
                    marks.append((s, 16 * n))
        for E in self.E.values():
            for s, v in marks:
                if s in E.own and E.name == "pe":
                    pass
                if E.seen.get(s, 0) < v:
                    E.h.wait_ge(s, v)
                    E.seen[s] = v

    def finish(self, extra=()):
        E = self.E["sp"]
        for e in self.E.values():
            if e.sem is not None and e.count > 0 and E.seen.get(e.sem, 0) < e.count:
                E.h.wait_ge(e.sem, e.count)
            for s, n in e.slots:
                if n > 0 and E.seen.get(s, 0) < 16 * n:
                    E.h.wait_ge(s, 16 * n)


def r32(ap):
    return ap.bitcast(F32R)


IN_OFF = dict(pool=0, q=256, k=512, v=768, o=1024, gate=1280, dq=1296, dkv=1552, kr=1680)


def build_program():
    nc = bass.Bass("TRN2", target_bir_lowering=False)
    es = ExitStack()
    cx = Ctx(nc, es)

    def din(name, shape, dt=F32):
        return nc.dram_tensor(name, list(shape), dt, kind="ExternalInput").ap()

    def dscr(name, shape, dt=F32):
        kind = "ExternalOutput" if DEBUG.get(name) else "Internal"
        return nc.dram_tensor(name, list(shape), dt, kind=kind).ap()

    def sb(name, shape, dt=F32):
        return es.enter_context(nc.sbuf_tensor(name, list(shape), dt))

    def ps(name, shape, dt=F32):
        return es.enter_context(nc.psum_tensor(name, list(shape), dt))

    x_in = din("x", [S, D])
    c_in = din("c_p", [128, 8])
    ident_in = din("ident", [128, 128])
    w_ada = din("w_ada", [L, D, 6 * D])
    b_ada_p = din("b_ada_p", [L, 128, 4, 8])
    b_ada = din("b_ada", [L, 6 * D])
    w_in_tok = din("w_in_tok", [L, D, 768])
    w_in_fm = din("w_in_fm", [L, D, 1024])
    band_in = din("band", [4, 5, 128, 128])
    w_pool_in = din("w_pool", [L, 4, 64, 64])
    s_pool_p = din("s_pool_p", [L, 64, 4])
    conv_p = din("conv_p", [L, 128, 4, 6])
    gate_b_in = din("gate_b4", [L, 4, 4])
    gn_w_in = din("gn_w", [L, 256])
    sel2_in = din("sel2", [4, 2, 128])
    mask_in = din("masks", [2, 128, 128])
    gbp_in = din("gbp", [L, 128, 4])
    pos_in = din("pos", [S], I32)
    ropec_in = din("ropec", [32, 2])
    g_q_p = din("g_q_p", [L, 128, 2])
    g_kv_p = din("g_kv_p", [L, 128, 1])
    w_uq_in = din("w_uq", [L, 256, 768])
    w_uq_sw_in = din("w_uq_sw", [L, 256, 768])
    w_uk_in = din("w_uk", [L, 128, 512])
    w_uv_in = din("w_uv", [L, 128, 512])
    sel64_in = din("sel64", [65, 64])
    w_out_in = din("w_out", [L, D, D])
    lnp_in = din("lnp", [L, 4, D])
    w_router_in = din("w_router", [L, D, 256])
    e_bias_in = din("e_bias", [L, 256])
    ws13_in = din("ws13", [L, D, 512])
    ws2_in = din("ws2", [L, 256, D])
    w1_in = din("w1", [L * 256 * 128, 2048])
    w3_in = din("w3", [L * 256 * 128, 2048])
    w2_in = din("w2", [L * 256 * 128, 2048])
    ustrict_in = din("ustrict", [128, 128])
    blkpos_in = din("blkpos", [128, 512])
    pidx_in = din("pidx", [128, 1])
    trih_in = din("trih", [2, 128, 128])
    cmask_in = din("cmask", [128, 64])
    psel_in = din("psel", [128, 128])
    y_out = nc.dram_tensor("y", [S, D], F32, kind="ExternalOutput").ap()

    U_tok = dscr("U_tok", [S, 768])
    U_fm = dscr("U_fm", [1024, S])
    mixT = dscr("mixT", [1024, S])
    ropeT = dscr("ropeT", [2, 32, S])
    X1 = dscr("X1", [S, D])
    XL = dscr("XL", [S, D])
    H2 = dscr("H2", [S, D], BF16)
    MS = dscr("MS", [S, 256], BF16)
    WN = dscr("WN", [S, 256])
    FFN = dscr("FFN", [S, D])
    NSLOT = 512 * 128
    Xs = dscr("Xs", [NSLOT, D], BF16)
    Ys = dscr("Ys", [NSLOT, D], BF16)

    bc_reg = nc.gpsimd.alloc_register("bc_reg")
    nc.gpsimd.reg_mov(bc_reg, L * 256 * 128 - 1)
    ident = sb("ident_sb", [128, 128])
    r_ident = R("ident")
    cx.dma("sp", ident[:], ident_in[:, :], writes=[r_ident])
    identb = sb("identb_g", [128, 128], BF16)
    r_identb = R("identb")
    cx.op("dve", lambda e: e.tensor_copy(out=identb[:], in_=ident[:]), [r_ident], [r_identb])
    cact = sb("cact", [128, 8])
    cact_bc = sb("cact_bc", [128, 8, 128])
    r_cact = R("cact")
    craw = sb("craw", [128, 8])
    r_craw = R()
    cx.dma("sp", craw[:], c_in[:, :], writes=[r_craw])
    cx.op("act", lambda e: e.activation(out=cact[:], in_=craw[:], func=AF.Silu), reads=[r_craw], writes=[r_cact])
    r_cbc = R()
    for kc in range(8):
        cx.op("dve", lambda e, kc=kc: e.tensor_copy(out=cact_bc[:, kc, :], in_=cact[:, kc:kc + 1].to_broadcast([128, 128])),
              reads=[r_cact], writes=[r_cbc])

    def V(fn, r=(), w=()):
        return cx.op("dve", fn, r, w)

    def A(fn, r=(), w=()):
        return cx.op("act", fn, r, w)

    def P(fn, r=(), w=()):
        return cx.op("pe", fn, r, w)

    def G(fn, r=(), w=()):
        return cx.op("pool", fn, r, w)

    r_ropeT = R()
    with ExitStack() as ph:
        def sbp(name, shape, dt=F32):
            return ph.enter_context(nc.sbuf_tensor(f"rp_{name}", list(shape), dt))
        posi = sbp("posi", [32, S], I32)
        ang = sbp("ang", [32, S])
        kf = sbp("kf", [32, S])
        ki = sbp("ki", [32, S], I32)
        rr_ = sbp("rr", [32, S])
        mm = sbp("mm", [32, S])
        ropec = sbp("ropec", [32, 2])
        r_rp = R()
        cx.dma("sp", posi[:], pos_in.partition_broadcast(32), writes=[r_rp])
        cx.dma("sp", ropec[:], ropec_in[:, :], writes=[r_rp])
        V(lambda e: e.tensor_copy(out=ang[:], in_=posi[:]), [r_rp], [r_rp])
        V(lambda e: e.tensor_scalar(out=ang[:], in0=ang[:], scalar1=ropec[:, 0:1], scalar2=None, op0=ALU.mult), [r_rp], [r_rp])
        TWO_PI = 2.0 * np.pi
        C1 = 6.28125
        C2 = TWO_PI - C1
        PI_LO = 3.1415925
        for tb in range(2):
            src = ang
            if tb == 1:
                V(lambda e: e.tensor_scalar(out=mm[:], in0=ang[:], scalar1=float(np.pi / 2), scalar2=None, op0=ALU.add), [r_rp], [r_rp])
                src = mm
            V(lambda e, src=src: e.tensor_scalar(out=kf[:], in0=src[:], scalar1=float(1.0 / TWO_PI), scalar2=None, op0=ALU.mult), [r_rp], [r_rp])
            V(lambda e: e.tensor_copy(out=ki[:], in_=kf[:]), [r_rp], [r_rp])
            V(lambda e: e.tensor_copy(out=kf[:], in_=ki[:]), [r_rp], [r_rp])
            V(lambda e, src=src: e.scalar_tensor_tensor(out=rr_[:], in0=kf[:], scalar=-C1, in1=src[:], op0=ALU.mult, op1=ALU.add), [r_rp], [r_rp])
            V(lambda e: e.scalar_tensor_tensor(out=rr_[:], in0=kf[:], scalar=-C2, in1=rr_[:], op0=ALU.mult, op1=ALU.add), [r_rp], [r_rp])
            V(lambda e: e.tensor_scalar(out=kf[:], in0=rr_[:], scalar1=float(np.pi), scalar2=None, op0=ALU.is_gt), [r_rp], [r_rp])
            V(lambda e: e.scalar_tensor_tensor(out=rr_[:], in0=kf[:], scalar=-TWO_PI, in1=rr_[:], op0=ALU.mult, op1=ALU.add), [r_rp], [r_rp])
            V(lambda e: e.tensor_scalar(out=kf[:], in0=rr_[:], scalar1=float(-np.pi), scalar2=None, op0=ALU.is_lt), [r_rp], [r_rp])
            V(lambda e: e.scalar_tensor_tensor(out=rr_[:], in0=kf[:], scalar=TWO_PI, in1=rr_[:], op0=ALU.mult, op1=ALU.add), [r_rp], [r_rp])
            V(lambda e: e.tensor_scalar(out=rr_[:], in0=rr_[:], scalar1=PI_LO, scalar2=-PI_LO, op0=ALU.min, op1=ALU.max), [r_rp], [r_rp])
            A(lambda e: e.activation(out=rr_[:], in_=rr_[:], func=AF.Sin), [r_rp], [r_rp])
            if tb == 0:
                V(lambda e: e.tensor_scalar(out=rr_[:], in0=rr_[:], scalar1=ropec[:, 1:2], scalar2=None, op0=ALU.mult), [r_rp], [r_rp])
            cx.dma("sp", ropeT[tb], rr_[:], reads=[r_rp], writes=[r_ropeT])
        cx.barrier()

    adaP = sb("adaP", [128, 4, 8])
    gF = sb("gF", [128, 4, D])
    r_adaP = R()
    r_gF = R()
    for l in range(L):
        with ExitStack() as ph:
            def sbp(name, shape, dt=F32):
                return ph.enter_context(nc.sbuf_tensor(f"{name}_{l}", list(shape), dt))

            def psp(name, shape, dt=F32):
                return ph.enter_context(nc.psum_tensor(f"{name}_{l}", list(shape), dt))
            wa = [sbp(f"wa{i}", [128, 8, D]) for i in range(2)]
            r_wa = [R(), R()]
            badap = sbp("badap", [128, 4, 8])
            r_badap = R()
            cx.dma("sp", badap[:], b_ada_p[l], writes=[r_badap])
            bfr = sbp("bfr", [128, 4, D])
            r_bfr = R()
            GIDX = {2: 0, 5: 1, 3: 2, 4: 3}
            PIDX = {0: 0, 1: 1, 3: 2, 4: 3}
            for v, j in GIDX.items():
                cx.dma("sp", bfr[:, j, :], b_ada[l, v * D:(v + 1) * D].partition_broadcast(128), writes=[r_bfr])
            pA = psp("pA", [128, 512])
            r_pA = R(psum=True)
            pG = [psp(f"pG{i}", [128, 512]) for i in range(2)]
            r_pG = [R(psum=True), R(psum=True)]
            order = [0, 1, 3, 4, 2, 5]
            for i, v in enumerate(order):
                cx.dma("sp", wa[i % 2][:], w_ada[l, :, v * D:(v + 1) * D].rearrange("(kc p) n -> p kc n", p=128),
                       writes=[r_wa[i % 2]])
                w = wa[i % 2]
                if v in PIDX:
                    pi = PIDX[v]
                    for ncn in range(8):
                        for kc in range(8):
                            cx.op("pe", lambda e, w=w, ncn=ncn, kc=kc, pi=pi: e.matmul(
                                pA[:, pi * 8 + ncn:pi * 8 + ncn + 1], lhsT=w[:, kc, ncn * 128:(ncn + 1) * 128],
                                rhs=cact[:, kc:kc + 1], start=(kc == 0), stop=(kc == 7)),
                                reads=[r_wa[i % 2], r_cact], writes=[r_pA])
                if v in GIDX:
                    j = GIDX[v]
                    for hf in range(2):
                        for kc in range(8):
                            cx.op("pe", lambda e, w=w, hf=hf, kc=kc: e.matmul(
                                pG[hf][:, :], lhsT=cact_bc[:, kc, :], rhs=w[:, kc, hf * 512:(hf + 1) * 512],
                                start=(kc == 0), stop=(kc == 7)),
                                reads=[r_wa[i % 2], r_cbc], writes=[r_pG[hf]])
                        cx.op("dve", lambda e, hf=hf, j=j: e.tensor_tensor(
                            out=gF[:, j, hf * 512:(hf + 1) * 512], in0=pG[hf][:, :], in1=bfr[:, j, hf * 512:(hf + 1) * 512], op=ALU.add),
                            reads=[r_pG[hf], r_bfr], writes=[r_gF])
            cx.op("dve", lambda e: e.tensor_tensor(out=adaP[:].rearrange("p a b -> p (a b)"), in0=pA[:, 0:32],
                                                   in1=badap[:].rearrange("p a b -> p (a b)"), op=ALU.add),
                  reads=[r_pA, r_badap], writes=[r_adaP])
            for v in (1, 3):
                cx.op("dve", lambda e, v=v: e.tensor_scalar_add(out=adaP[:, v, :], in0=adaP[:, v, :], scalar1=1.0),
                      reads=[r_adaP], writes=[r_adaP])
            cx.op("dve", lambda e: e.tensor_scalar_add(out=gF[:, 3, :], in0=gF[:, 3, :], scalar1=1.0), reads=[r_gF], writes=[r_gF])
            cx.barrier()

        with ExitStack() as ph:
            def sbp(name, shape, dt=F32):
                return ph.enter_context(nc.sbuf_tensor(f"{name}_{l}", list(shape), dt))

            def psp(name, shape, dt=F32):
                return ph.enter_context(nc.psum_tensor(f"{name}_{l}", list(shape), dt))
            wtok = sbp("wtok", [128, 8, 768], BF16)
            wfm = sbp("wfm", [128, 8, 1024], BF16)
            r_wtok, r_wfm = R(), R()
            for kc in range(8):
                cx.dma("pool", wtok[:, kc, :], w_in_tok[l, kc * 128:(kc + 1) * 128, :], writes=[r_wtok])
                cx.dma("pool", wfm[:, kc, :], w_in_fm[l, kc * 128:(kc + 1) * 128, :], writes=[r_wfm])
            xt = [sbp(f"xt{i}", [128, D]) for i in range(2)]
            r_xt = [R(), R()]
            xn = [sbp(f"xn{i}", [128, D]) for i in range(2)]
            r_xn = [R(), R()]
            st = sbp("st", [128, 2, 6])
            mv = sbp("mv", [128, 2])
            rstd = sbp("rstd", [128, 1])
            r_st, r_mv, r_rstd = R(), R(), R()
            hT = [sbp(f"hT{i}", [128, 8, 512], BF16) for i in range(2)]
            r_hT = [R(), R()]
            pT = [psp(f"pT{i}", [128, 512]) for i in range(2)]
            r_pT = [R(psum=True), R(psum=True)]
            pU = [psp(f"pU{i}", [128, 512]) for i in range(4)]
            r_pU = [R(psum=True) for _ in range(4)]
            uo = [sbp(f"uo{i}", [128, 512]) for i in range(4)]
            r_uo = [R() for _ in range(4)]
            r_Utok, r_Ufm = R(), R()
            src = x_in if l == 0 else XL
            npu = 0
            for g in range(8):
                hTg = hT[g % 2]
                r_hTg = r_hT[g % 2]
                for tt in range(4):
                    t = g * 4 + tt
                    b = t % 2
                    cx.dma("sp", xt[b][:], src[t * 128:(t + 1) * 128, :], writes=[r_xt[b]])
                    for hf in range(2):
                        cx.op("dve", lambda e, b=b, hf=hf: e.bn_stats(out=st[:, hf, :], in_=xt[b][:, hf * 512:(hf + 1) * 512]),
                              reads=[r_xt[b]], writes=[r_st])
                    cx.op("dve", lambda e: e.bn_aggr(out=mv[:], in_=st[:].rearrange("p a b -> p (a b)")), reads=[r_st], writes=[r_mv])
                    cx.op("act", lambda e: e.activation(out=rstd[:], in_=mv[:, 1:2], func=AF.Sqrt, bias=LN_EPS, scale=1.0),
                          reads=[r_mv], writes=[r_rstd])
                    cx.op("dve", lambda e: e.reciprocal(out=rstd[:], in_=rstd[:]), reads=[r_rstd], writes=[r_rstd])
                    cx.op("dve", lambda e, b=b: e.tensor_scalar(out=xn[b][:], in0=xt[b][:], scalar1=mv[:, 0:1], scalar2=rstd[:, 0:1],
                                                                 op0=ALU.subtract, op1=ALU.mult),
                          reads=[r_xt[b], r_mv, r_rstd], writes=[r_xn[b]])
                    for q4 in range(2):
                        pt = pT[q4]
                        for k4 in range(4):
                            kc = q4 * 4 + k4
                            cx.op("pe", lambda e, pt=pt, k4=k4, kc=kc, b=b: e.transpose(
                                out=pt[:, k4 * 128:(k4 + 1) * 128], in_=xn[b][:, kc * 128:(kc + 1) * 128], identity=ident[:]),
                                reads=[r_xn[b], r_ident], writes=[r_pT[q4]])
                        for k4 in range(4):
                            kc = q4 * 4 + k4
                            cx.op("act", lambda e, pt=pt, k4=k4, kc=kc, tt=tt, hTg=hTg: e.activation(
                                out=hTg[:, kc, tt * 128:(tt + 1) * 128], in_=pt[:, k4 * 128:(k4 + 1) * 128], func=AF.Identity,
                                bias=adaP[:, 0, kc:kc + 1], scale=adaP[:, 1, kc:kc + 1]),
                                reads=[r_pT[q4], r_adaP], writes=[r_hTg])
                for tt in range(4):
                    t = g * 4 + tt
                    for hf in range(2):
                        i = npu % 4
                        npu += 1
                        for kc in range(8):
                            cx.op("pe", lambda e, i=i, kc=kc, tt=tt, hf=hf, hTg=hTg: e.matmul(
                                pU[i][:, 0:384], lhsT=hTg[:, kc, tt * 128:(tt + 1) * 128],
                                rhs=wtok[:, kc, hf * 384:(hf + 1) * 384], start=(kc == 0), stop=(kc == 7)),
                                reads=[r_hTg, r_wtok], writes=[r_pU[i]])
                        cx.op("act" if i % 2 else "dve", lambda e, i=i: (e.copy(out=uo[i][:, 0:384], in_=pU[i][:, 0:384]) if i % 2
                                                                          else e.tensor_copy(out=uo[i][:, 0:384], in_=pU[i][:, 0:384])),
                              reads=[r_pU[i]], writes=[r_uo[i]])
                        cx.dma("pool", U_tok[t * 128:(t + 1) * 128, hf * 384:(hf + 1) * 384], uo[i][:, 0:384],
                               reads=[r_uo[i]], writes=[r_Utok])
                for cb in range(8):
                    i = npu % 4
                    npu += 1
                    for kc in range(8):
                        cx.op("pe", lambda e, i=i, kc=kc, cb=cb, hTg=hTg: e.matmul(
                            pU[i][:, :], lhsT=wfm[:, kc, cb * 128:(cb + 1) * 128], rhs=hTg[:, kc, :],
                            start=(kc == 0), stop=(kc == 7)),
                            reads=[r_hTg, r_wfm], writes=[r_pU[i]])
                    cx.op("act" if i % 2 else "dve", lambda e, i=i: (e.copy(out=uo[i][:, :], in_=pU[i][:, :]) if i % 2
                                                                      else e.tensor_copy(out=uo[i][:, :], in_=pU[i][:, :])),
                          reads=[r_pU[i]], writes=[r_uo[i]])
                    cx.dma("pool", U_fm[cb * 128:(cb + 1) * 128, g * 512:(g + 1) * 512], uo[i][:, :],
                           reads=[r_uo[i]], writes=[r_Ufm])
            cx.barrier()
        if STOP_AFTER == "A":
            break
        r_mixT = R()

        def V(fn, r=(), w=()):
            return cx.op("dve", fn, r, w)

        def A(fn, r=(), w=()):
            return cx.op("act", fn, r, w)

        def P(fn, r=(), w=()):
            return cx.op("pe", fn, r, w)

        def G(fn, r=(), w=()):
            return cx.op("pool", fn, r, w)

        with ExitStack() as ph:
            def sbp(name, shape, dt=F32):
                return ph.enter_context(nc.sbuf_tensor(f"{name}_{l}", list(shape), dt))

            def psp(name, shape, dt=F32):
                return ph.enter_context(nc.psum_tensor(f"{name}_{l}", list(shape), dt))
            band = sbp("band", [128, 20, 128], BF16)
            r_band = R()
            cx.dma("pool", band[:], band_in.rearrange("g k p n -> p (g k) n"), writes=[r_band])
            wpl = sbp("wpl", [64, 4, 64], BF16)
            r_wpl = R()
            cx.dma("pool", wpl[:], w_pool_in[l].rearrange("g c d -> c g d"), writes=[r_wpl])
            spl = sbp("spl", [64, 4])
            r_spl = R()
            cx.dma("sp", spl[:], s_pool_p[l], writes=[r_spl])
            up = sbp("up", [128, NT, 256], BF16)
            r_up = R()
            for t4 in range(0, NT, 8):
                cx.dma("pool", up[:, t4:t4 + 8, :], U_tok[t4 * 128:(t4 + 8) * 128, 0:256].rearrange("(t p) c -> p t c", p=128),
                       reads=[r_Utok], writes=[r_up])
            pd = [psp(f"pd{i}", [64, 512]) for i in range(2)]
            py = [psp(f"py{i}", [64, 512]) for i in range(2)]
            r_pd = [R(psum=True), R(psum=True)]
            r_py = [R(psum=True), R(psum=True)]
            dTs = [sbp(f"dTs{i}", [64, 512], BF16) for i in range(2)]
            r_dTs = [R(), R()]
            yp = [sbp(f"yp{i}", [64, 4, 512]) for i in range(2)]
            r_yp = [R(), R()]
            n = 0
            for Gq in range(8):
                ypq = yp[Gq % 2]
                r_ypq = r_yp[Gq % 2]
                for g in range(4):
                    i = n % 2
                    n += 1
                    for tt in range(4):
                        t = Gq * 4 + tt
                        srcs = []
                        if t > 0:
                            srcs.append((t - 1, 0))
                        srcs.append((t, 3 if t == 0 else (4 if t == NT - 1 else 1)))
                        if t < NT - 1:
                            srcs.append((t + 1, 2))
                        for k, (j, typ) in enumerate(srcs):
                            P(lambda e, i=i, tt=tt, j=j, g=g, typ=typ, k=k, last=len(srcs) - 1: e.matmul(
                                pd[i][:, tt * 128:(tt + 1) * 128], lhsT=up[:, j, g * 64:(g + 1) * 64], rhs=band[:, g * 5 + typ, :],
                                start=(k == 0), stop=(k == last)), [r_up, r_band], [r_pd[i]])
                    A(lambda e, i=i: e.copy(out=dTs[i][:], in_=pd[i][:]), [r_pd[i]], [r_dTs[i]])
                    P(lambda e, i=i, g=g: e.matmul(py[i][:, :], lhsT=wpl[:, g, :], rhs=dTs[i][:], start=True, stop=True),
                      [r_wpl, r_dTs[i]], [r_py[i]])
                    V(lambda e, i=i, g=g, ypq=ypq: e.tensor_scalar(out=ypq[:, g, :], in0=py[i][:], scalar1=spl[:, g:g + 1], scalar2=None,
                                                                 op0=ALU.mult), [r_py[i], r_spl], [r_ypq])
                cx.dma("sp", mixT[0:256, Gq * 512:(Gq + 1) * 512].rearrange("(g c) t -> c g t", c=64), ypq[:],
                       reads=[r_ypq], writes=[r_mixT])
            cx.barrier()
        if STOP_AFTER == "B":
            break
        with ExitStack() as ph:
            def sbp(name, shape, dt=F32):
                return ph.enter_context(nc.sbuf_tensor(f"{name}_{l}", list(shape), dt))
            qkT = sbp("qkT", [128, 4, S], BF16)
            r_qkT = R()
            vx = sbp("vx", [128, NT, 4, 65], BF16)
            r_vx = R()
            hacc = sbp("hacc", [128, NT, 256])
            r_hacc = [R() for _ in range(NT)]
            gTcf = [sbp(f"gTcf{d}", [128, 4, NT]) for d in range(2)]
            gTcl = [sbp(f"gTcl{d}", [128, 4, NT]) for d in range(2)]
            decb = [sbp(f"decb{d}", [128, 2, NT]) for d in range(2)]
            r_gT = [R(), R()]
            maskt = sbp("maskt", [128, 2, 128])
            r_mask = R()
            cx.dma("sp", maskt[:], mask_in.rearrange("d p n -> p d n"), writes=[r_mask])
            with ExitStack() as ph2:
                def sb2(name, shape, dt=F32):
                    return ph2.enter_context(nc.sbuf_tensor(f"{name}_{l}", list(shape), dt))
                convp = sb2("convp", [128, 4, 6])
                r_convp = R()
                cx.dma("sp", convp[:], conv_p[l], writes=[r_convp])
                cin = [sb2(f"cin{i}", [128, S + 4]) for i in range(2)]
                r_cin = [R(), R()]
                acc = sb2("cacc", [128, S])
                r_acc = R()
                for i in range(2):
                    G(lambda e, i=i: e.memset(cin[i][:, 0:2], 0.0), [], [r_cin[i]])
                    G(lambda e, i=i: e.memset(cin[i][:, S + 2:S + 4], 0.0), [], [r_cin[i]])
                for ch in range(4):
                    b = ch % 2
                    cx.dma("sp", cin[b][:, 2:S + 2], U_fm[ch * 128:(ch + 1) * 128, :], reads=[r_Ufm], writes=[r_cin[b]])
                    V(lambda e, b=b, ch=ch: e.tensor_scalar(out=acc[:], in0=cin[b][:, 0:S], scalar1=convp[:, ch, 0:1], scalar2=convp[:, ch, 5:6],
                                                           op0=ALU.mult, op1=ALU.add), [r_cin[b], r_convp], [r_acc])
                    for j in range(1, 5):
                        V(lambda e, b=b, ch=ch, j=j: e.scalar_tensor_tensor(out=acc[:], in0=cin[b][:, j:j + S], scalar=convp[:, ch, j:j + 1],
                                                                          in1=acc[:], op0=ALU.mult, op1=ALU.add), [r_cin[b], r_convp, r_acc], [r_acc])
                    A(lambda e, ch=ch: e.activation(out=qkT[:, ch, :], in_=acc[:], func=AF.Silu), [r_acc], [r_qkT])
                vtmp = [sb2(f"vtmp{i}", [128, 8, 256]) for i in range(2)]
                r_vtmp = [R(), R()]
                G(lambda e: e.memset(vx[:, :, :, 64:65], 1.0), [], [r_vx])
                for i4 in range(4):
                    b = i4 % 2
                    cx.dma("sp", vtmp[b][:], U_tok[i4 * 1024:(i4 + 1) * 1024, 256:512].rearrange("(t p) c -> p t c", p=128),
                           reads=[r_Utok], writes=[r_vtmp[b]])
                    A(lambda e, b=b, i4=i4: e.copy(out=vx[:, i4 * 8:(i4 + 1) * 8, :, 0:64], in_=vtmp[b][:].rearrange("p t (h c) -> p t h c", c=64)),
                      [r_vtmp[b]], [r_vx])
                cx.barrier()
            with ExitStack() as ph2:
                def sb2(name, shape, dt=F32):
                    return ph2.enter_context(nc.sbuf_tensor(f"{name}_{l}", list(shape), dt))

                def ps2(name, shape, dt=F32):
                    return ph2.enter_context(nc.psum_tensor(f"{name}_{l}", list(shape), dt))
                gbp = sb2("gbp", [128, 4])
                ngb = sb2("ngb", [128, 4])
                r_gbp = R()
                cx.dma("sp", gbp[:], gbp_in[l], writes=[r_gbp])
                V(lambda e: e.tensor_scalar(out=ngb[:], in0=gbp[:], scalar1=-1.0, scalar2=None, op0=ALU.mult), [r_gbp], [r_gbp])
                trih = sb2("trih", [128, 2, 128])
                cmask = sb2("cmask", [128, 64])
                psel = sb2("psel", [128, 128])
                r_cst = R()
                cx.dma("sp", trih[:], trih_in.rearrange("d p n -> p d n"), writes=[r_cst])
                cx.dma("sp", cmask[:], cmask_in[:, :], writes=[r_cst])
                cx.dma("sp", psel[:], psel_in[:, :], writes=[r_cst])
                pg = ps2("pg", [128, 512])
                r_pg = R(psum=True)
                for d in range(2):
                    gi = sb2(f"gi{d}", [128, 128]); gf = sb2(f"gf{d}", [128, 128])
                    r_g = R()
                    ki, kf = 2 * d, 2 * d + 1
                    cx.dma("sp", gi[:], U_fm[896 + ki * 4:896 + ki * 4 + 4, :].rearrange("h (c l) -> (h c) l", l=128), reads=[r_Ufm], writes=[r_g])
                    cx.dma("sp", gf[:], U_fm[896 + kf * 4:896 + kf * 4 + 4, :].rearrange("h (c l) -> (h c) l", l=128), reads=[r_Ufm], writes=[r_g])
                    spt = sb2(f"spt{d}", [128, 128]); Pl = sb2(f"Pl{d}", [128, 128]); Pc = sb2(f"Pc{d}", [128, 128]); at = sb2(f"at{d}", [128, 128])
                    cols = sb2(f"cols{d}", [128, 8])
                    rows = sb2(f"rows{d}", [1, 4, 128])
                    r_w = R()
                    A(lambda e, kf=kf: e.activation(out=spt[:], in_=gf[:], func=AF.Exp, bias=ngb[:, kf:kf + 1], scale=-1.0), [r_g, r_gbp], [r_w])
                    A(lambda e: e.activation(out=spt[:], in_=spt[:], func=AF.Ln, bias=1.0, scale=1.0), [r_w], [r_w])
                    V(lambda e: e.tensor_tensor_scan(out=Pl[:], data0=spt[:], data1=spt[:], initial=0.0, op0=ALU.add, op1=ALU.max), [r_w], [r_w])
                    V(lambda e: e.tensor_copy(out=cols[:, 0:1], in_=Pl[:, 127:128]), [r_w], [r_w])
                    P(lambda e, d=d: e.matmul(pg[:, 0:1], lhsT=trih[:, d, :], rhs=cols[:, 0:1], start=True, stop=True), [r_w, r_cst], [r_pg])
                    V(lambda e: e.tensor_copy(out=cols[:, 1:2], in_=pg[:, 0:1]), [r_pg], [r_w])
                    if d == 0:
                        V(lambda e: e.tensor_scalar(out=Pc[:], in0=Pl[:], scalar1=cols[:, 1:2], scalar2=None, op0=ALU.add), [r_w], [r_w])
                    else:
                        V(lambda e: e.scalar_tensor_tensor(out=Pc[:], in0=Pl[:], scalar=-1.0, in1=spt[:], op0=ALU.mult, op1=ALU.add), [r_w], [r_w])
                        V(lambda e: e.tensor_scalar(out=Pc[:], in0=Pc[:], scalar1=cols[:, 0:1], scalar2=cols[:, 1:2], op0=ALU.add, op1=ALU.add), [r_w], [r_w])
                    V(lambda e, ki=ki: e.scalar_tensor_tensor(out=at[:], in0=gi[:], scalar=gbp[:, ki:ki + 1], in1=Pc[:], op0=ALU.add, op1=ALU.add),
                      [r_w, r_g, r_gbp], [r_w])
                    V(lambda e: e.tensor_reduce(out=cols[:, 2:3], in_=at[:], axis=AX.X, op=ALU.max), [r_w], [r_w])
                    P(lambda e: e.transpose(out=pg[0:1, 0:128], in_=cols[:, 2:3], identity=ident[:]), [r_w, r_ident], [r_pg])
                    V(lambda e: e.tensor_copy(out=rows[:, 0, :], in_=pg[0:1, 0:128]), [r_pg], [r_w])
                    cur = 0
                    for sh in (1, 2, 4, 8, 16):
                        a_ = rows[:, cur, :].rearrange("p (h c) -> p h c", c=32)
                        b_ = rows[:, 1 - cur, :].rearrange("p (h c) -> p h c", c=32)
                        if d == 0:
                            V(lambda e, a_=a_, b_=b_, sh=sh: e.tensor_tensor(out=b_[:, :, sh:], in0=a_[:, :, sh:], in1=a_[:, :, :32 - sh], op=ALU.max), [r_w], [r_w])
                            V(lambda e, a_=a_, b_=b_, sh=sh: e.tensor_copy(out=b_[:, :, :sh], in_=a_[:, :, :sh]), [r_w], [r_w])
                        else:
                            V(lambda e, a_=a_, b_=b_, sh=sh: e.tensor_tensor(out=b_[:, :, :32 - sh], in0=a_[:, :, :32 - sh], in1=a_[:, :, sh:], op=ALU.max), [r_w], [r_w])
                            V(lambda e, a_=a_, b_=b_, sh=sh: e.tensor_copy(out=b_[:, :, 32 - sh:], in_=a_[:, :, 32 - sh:]), [r_w], [r_w])
                        cur = 1 - cur
                    Mr = rows[:, cur, :].rearrange("p (h c) -> p h c", c=32)
                    dd = rows[:, 2, :].rearrange("p (h c) -> p h c", c=32)
                    V(lambda e: e.memset(rows[:, 2, :], 0.0), [r_w], [r_w])
                    if d == 0:
                        V(lambda e, Mr=Mr, dd=dd: e.tensor_tensor(out=dd[:, :, 0:31], in0=Mr[:, :, 0:31], in1=Mr[:, :, 1:32], op=ALU.subtract), [r_w], [r_w])
                    else:
                        V(lambda e, Mr=Mr, dd=dd: e.tensor_tensor(out=dd[:, :, 1:32], in0=Mr[:, :, 1:32], in1=Mr[:, :, 0:31], op=ALU.subtract), [r_w], [r_w])
                    A(lambda e: e.activation(out=rows[:, 3, :], in_=rows[:, 2, :], func=AF.Exp), [r_w], [r_w])
                    P(lambda e, cur=cur: e.transpose(out=pg[:, 0:1], in_=rows[0:1, cur, :], identity=ident[0:1, 0:1]), [r_w, r_ident], [r_pg])
                    P(lambda e: e.transpose(out=pg[:, 1:2], in_=rows[0:1, 3, :], identity=ident[0:1, 0:1]), [r_w, r_ident], [r_pg])
                    V(lambda e: e.tensor_copy(out=cols[:, 3:4], in_=pg[:, 0:1]), [r_pg], [r_w])
                    V(lambda e: e.tensor_copy(out=cols[:, 6:7], in_=pg[:, 1:2]), [r_pg], [r_w])
                    V(lambda e: e.tensor_scalar(out=cols[:, 4:5], in0=cols[:, 3:4], scalar1=-1.0, scalar2=None, op0=ALU.mult), [r_w], [r_w])
                    V(lambda e: e.tensor_scalar(out=cols[:, 5:6], in0=cols[:, 3:4], scalar1=-1.0, scalar2=-float(np.log(8.0)), op0=ALU.mult, op1=ALU.add), [r_w], [r_w])
                    A(lambda e: e.activation(out=at[:], in_=at[:], func=AF.Exp, bias=cols[:, 5:6], scale=1.0), [r_w], [r_w])
                    A(lambda e: e.activation(out=Pc[:], in_=Pc[:], func=AF.Exp, bias=cols[:, 4:5], scale=1.0), [r_w], [r_w])
                    P(lambda e: e.transpose(out=pg[:, 0:128], in_=at[:], identity=ident[:]), [r_w, r_ident], [r_pg])
                    P(lambda e: e.transpose(out=pg[:, 128:256], in_=Pc[:], identity=ident[:]), [r_w, r_ident], [r_pg])
                    V(lambda e, d=d: e.tensor_copy(out=gTcf[d][:].rearrange("p h c -> p (h c)"), in_=pg[:, 0:128]), [r_pg], [r_gT[d]])
                    V(lambda e, d=d: e.tensor_copy(out=gTcl[d][:].rearrange("p h c -> p (h c)"), in_=pg[:, 128:256]), [r_pg], [r_gT[d]])
                    V(lambda e: e.tensor_scalar(out=spt[:, 0:64], in0=cmask[:], scalar1=cols[:, 6:7], scalar2=None, op0=ALU.mult), [r_w, r_cst], [r_w])
                    P(lambda e: e.matmul(pg[:, 256:320], lhsT=psel[:], rhs=spt[:, 0:64], start=True, stop=True), [r_w, r_cst], [r_pg])
                    V(lambda e, d=d: e.tensor_copy(out=decb[d][:].rearrange("p j c -> p (j c)"), in_=pg[:, 256:320]), [r_pg], [r_gT[d]])
                cx.barrier()
            with ExitStack() as ph2:
                def sb2(name, shape, dt=F32):
                    return ph2.enter_context(nc.sbuf_tensor(f"{name}_{l}", list(shape), dt))

                def ps2(name, shape, dt=F32):
                    return ph2.enter_context(nc.psum_tensor(f"{name}_{l}", list(shape), dt))
                pS = [[ps2(f"pS{d}{par}", [128, 4, 128]) for par in range(2)] for d in range(2)]
                pnd = [ps2(f"pnd{d}", [128, 4, 128]) for d in range(2)]
                pdC1 = ps2("pdC", [128, 4, 128])
                pkt1 = ps2("pkt", [128, 1024], BF16)
                pdC = [pdC1, pdC1]
                pkt = [pkt1, pkt1]
                r_pS = [[R(psum=True), R(psum=True)], [R(psum=True), R(psum=True)]]
                r_pnd = [R(psum=True), R(psum=True)]
                r1 = R(psum=True); r2 = R(psum=True)
                r_pdC = [r1, r1]; r_pkt = [r2, r2]
                Sm = [sb2(f"Sm{d}", [128, 4, 128], BF16) for d in range(2)]
                kt = [sb2(f"kt{d}", [128, 4, 64], BF16) for d in range(2)]
                rr = [sb2(f"rr{d}", [128, 4]) for d in range(2)]
                tmph = [sb2(f"tmph{d}", [128, 4, 64]) for d in range(2)]
                Cst = [sb2(f"Cst{d}", [128, 2, 65]) for d in range(2)]
                Cstb = [sb2(f"Cstb{d}", [128, 2, 65], BF16) for d in range(2)]
                tmpC = [sb2(f"tmpC{d}", [128, 2, 65]) for d in range(2)]
                r_Sm = [R(), R()]; r_kt = [R(), R()]; r_rr = [R(), R()]; r_tmph = [R(), R()]
                r_Cst = [R(), R()]; r_Cstb = [R(), R()]; r_tmpC = [R(), R()]
                for d in range(2):
                    G(lambda e, d=d: e.memset(Cst[d][:], 0.0), [], [r_Cst[d]])
                    G(lambda e, d=d: e.memset(Cstb[d][:], 0.0), [], [r_Cstb[d]])
                done = set()
                for step in range(NT):
                    for d in range(2):
                        c = step if d == 0 else NT - 1 - step
                        last = (step == NT - 1)
                        cs = slice(c * 128, (c + 1) * 128)
                        for j in range(2):
                            P(lambda e, d=d, j=j, cs=cs: e.transpose(out=pkt[d][:, j * 128:(j + 1) * 128], in_=qkT[:, 2 + j, cs], identity=identb[:]),
                              [r_qkT, r_identb], [r_pkt[d]])
                        V(lambda e, d=d, c=c: e.tensor_tensor(out=kt[d][:], in0=pkt[d][:, 0:256].rearrange("p (h k) -> p h k", k=64),
                                                            in1=gTcf[d][:, :, c:c + 1].to_broadcast([128, 4, 64]), op=ALU.mult),
                          [r_pkt[d], r_gT[d]], [r_kt[d]])
                        for h in range(4):
                            hp, hj = h % 2, h // 2
                            P(lambda e, d=d, h=h, hp=hp, hj=hj, cs=cs: e.matmul(pS[d][hp][:, hj, :], lhsT=qkT[hp * 64:(hp + 1) * 64, 2 + hj, cs],
                                                                              rhs=qkT[hp * 64:(hp + 1) * 64, hj, cs], start=True, stop=True),
                              [r_qkT], [r_pS[d][hp]])
                        for h in range(4):
                            hp, hj = h % 2, h // 2
                            V(lambda e, d=d, h=h, c=c, hp=hp, hj=hj: e.scalar_tensor_tensor(out=Sm[d][:, h, :], in0=pS[d][hp][:, hj, :], scalar=gTcf[d][:, h, c:c + 1],
                                                                            in1=maskt[:, d, :], op0=ALU.mult, op1=ALU.mult),
                              [r_pS[d][hp], r_gT[d], r_mask], [r_Sm[d]])
                        for h in range(4):
                            hp, hj = h % 2, h // 2
                            P(lambda e, d=d, h=h, c=c: e.matmul(pnd[d][:, h, 0:65], lhsT=Sm[d][:, h, :], rhs=vx[:, c, h, :], start=True, stop=False),
                              [r_Sm[d], r_vx], [r_pnd[d]])
                            P(lambda e, d=d, h=h, hp=hp, hj=hj, cs=cs: e.matmul(pnd[d][:, h, 0:65], lhsT=qkT[hp * 64:(hp + 1) * 64, hj, cs],
                                                                              rhs=Cstb[d][hp * 64:(hp + 1) * 64, hj, :], start=False, stop=True),
                              [r_qkT, r_Cstb[d]], [r_pnd[d]])
                        A(lambda e, d=d: e.activation(out=rr[d][:].unsqueeze(2), in_=pnd[d][:, :, 64:65], func=AF.Abs),
                          [r_pnd[d]], [r_rr[d]])
                        V(lambda e, d=d, c=c: e.tensor_tensor(out=rr[d][:], in0=rr[d][:], in1=gTcl[d][:, :, c], op=ALU.max), [r_rr[d], r_gT[d]], [r_rr[d]])
                        V(lambda e, d=d: e.reciprocal(out=rr[d][:], in_=rr[d][:]), [r_rr[d]], [r_rr[d]])
                        hv = hacc[:, c, :].rearrange("p (h k) -> p h k", k=64)
                        if c not in done:
                            done.add(c)
                            V(lambda e, d=d, hv=hv: e.tensor_tensor(out=hv, in0=pnd[d][:, :, 0:64], in1=rr[d][:].unsqueeze(2).to_broadcast([128, 4, 64]), op=ALU.mult),
                              [r_pnd[d], r_rr[d]], [r_hacc[c]])
                        else:
                            V(lambda e, d=d: e.tensor_tensor(out=tmph[d][:], in0=pnd[d][:, :, 0:64], in1=rr[d][:].unsqueeze(2).to_broadcast([128, 4, 64]), op=ALU.mult),
                              [r_pnd[d], r_rr[d]], [r_tmph[d]])
                            G(lambda e, d=d, hv=hv: e.tensor_tensor(out=hv, in0=hv, in1=tmph[d][:], op=ALU.add), [r_tmph[d], r_hacc[c]], [r_hacc[c]])
                        if last:
                            continue
                        for h in range(4):
                            hj = h // 2
                            P(lambda e, d=d, h=h, hj=hj, c=c: e.matmul(pdC[d][:, h, 0:65], lhsT=kt[d][:, 2 * hj:2 * hj + 2, :].rearrange("p a k -> p (a k)"),
                                                                     rhs=vx[:, c, h, :], start=True, stop=True),
                              [r_kt[d], r_vx], [r_pdC[d]])
                        for par in range(2):
                            rs = slice(par * 64, (par + 1) * 64)
                            V(lambda e, d=d, rs=rs, par=par: e.tensor_tensor(out=tmpC[d][rs, :, :], in0=pdC[d][rs, par::2, 0:65], in1=Cst[d][rs, :, :], op=ALU.add),
                              [r_pdC[d], r_Cst[d]], [r_tmpC[d]])
                        for par in range(2):
                            rs = slice(par * 64, (par + 1) * 64)
                            V(lambda e, d=d, rs=rs, c=c: e.tensor_tensor(out=Cst[d][rs, :, :], in0=tmpC[d][rs, :, :],
                                                                       in1=decb[d][rs, :, c:c + 1].to_broadcast([64, 2, 65]), op=ALU.mult),
                              [r_tmpC[d], r_gT[d]], [r_Cst[d]])
                            G(lambda e, d=d, rs=rs, c=c: e.tensor_tensor(out=Cstb[d][rs, :, :], in0=tmpC[d][rs, :, :],
                                                                       in1=decb[d][rs, :, c:c + 1].to_broadcast([64, 2, 65]), op=ALU.mult),
                              [r_tmpC[d], r_gT[d]], [r_Cstb[d]])
                cx.barrier()
            with ExitStack() as ph2:
                def sb2(name, shape, dt=F32):
                    return ph2.enter_context(nc.sbuf_tensor(f"{name}_{l}", list(shape), dt))

                def ps2(name, shape, dt=F32):
                    return ph2.enter_context(nc.psum_tensor(f"{name}_{l}", list(shape), dt))
                gnw = sb2("gnw", [128, 256])
                r_gnw = R()
                cx.dma("sp", gnw[:], gn_w_in[l].partition_broadcast(128), writes=[r_gnw])
                uo_t = [sb2(f"uo_t{i}", [128, 256]) for i in range(2)]
                r_uot = [R(), R()]
                sq = sb2("sq", [128, 256]); hc = [sb2(f"hc{i}", [128, 256]) for i in range(2)]
                st4 = sb2("st4", [128, 4, 4])
                r_sq, r_st4 = R(), R()
                r_hc = [R(), R()]
                pyT = [ps2(f"pyT{i}", [128, 512]) for i in range(2)]
                r_pyT = [R(psum=True), R(psum=True)]
                ymT = [sb2(f"ymT{i}", [128, 2, 128]) for i in range(2)]
                r_ymT = [R(), R()]
                for t in range(NT):
                    b = t % 2
                    cx.dma("sp", uo_t[b][:], U_tok[t * 128:(t + 1) * 128, 512:768], reads=[r_Utok], writes=[r_uot[b]])
                    hv = hacc[:, t, :].rearrange("p (h k) -> p h k", k=64)
                    V(lambda e, hv=hv: e.tensor_reduce(out=st4[:, 0, :], in_=hv, axis=AX.X, op=ALU.add), [r_hacc[t]], [r_st4])
                    G(lambda e, t=t: e.tensor_tensor(out=sq[:], in0=hacc[:, t, :], in1=hacc[:, t, :], op=ALU.mult), [r_hacc[t]], [r_sq])
                    V(lambda e: e.tensor_reduce(out=st4[:, 1, :], in_=sq[:].rearrange("p (h k) -> p h k", k=64), axis=AX.X, op=ALU.add), [r_sq], [r_st4])
                    V(lambda e: e.tensor_scalar(out=st4[:, 2, :], in0=st4[:, 0, :], scalar1=1.0 / 64, scalar2=None, op0=ALU.mult), [r_st4], [r_st4])
                    V(lambda e: e.tensor_tensor(out=st4[:, 0, :], in0=st4[:, 2, :], in1=st4[:, 2, :], op=ALU.mult), [r_st4], [r_st4])
                    V(lambda e: e.scalar_tensor_tensor(out=st4[:, 3, :], in0=st4[:, 1, :], scalar=1.0 / 64, in1=st4[:, 0, :], op0=ALU.mult, op1=ALU.subtract), [r_st4], [r_st4])
                    A(lambda e: e.activation(out=st4[:, 3, :], in_=st4[:, 3, :], func=AF.Sqrt, bias=LN_EPS, scale=1.0), [r_st4], [r_st4])
                    V(lambda e: e.reciprocal(out=st4[:, 3, :], in_=st4[:, 3, :]), [r_st4], [r_st4])
                    hcv = hc[b][:].rearrange("p (h k) -> p h k", k=64)
                    V(lambda e, hv=hv, hcv=hcv: e.tensor_tensor(out=hcv, in0=hv, in1=st4[:, 2, :].unsqueeze(2).to_broadcast([128, 4, 64]), op=ALU.subtract),
                      [r_hacc[t], r_st4], [r_hc[b]])
                    V(lambda e, hcv=hcv: e.tensor_tensor(out=hcv, in0=hcv, in1=st4[:, 3, :].unsqueeze(2).to_broadcast([128, 4, 64]), op=ALU.mult),
                      [r_hc[b], r_st4], [r_hc[b]])
                    G(lambda e, b=b: e.tensor_tensor(out=hc[b][:], in0=hc[b][:], in1=gnw[:], op=ALU.mult), [r_hc[b], r_gnw], [r_hc[b]])
                    A(lambda e, b=b: e.activation(out=uo_t[b][:], in_=uo_t[b][:], func=AF.Sigmoid), [r_uot[b]], [r_uot[b]])
                    V(lambda e, b=b: e.tensor_tensor(out=hc[b][:], in0=hc[b][:], in1=uo_t[b][:], op=ALU.mult), [r_hc[b], r_uot[b]], [r_hc[b]])
                    for j in range(2):
                        P(lambda e, b=b, j=j: e.transpose(out=pyT[b][:, j * 128:(j + 1) * 128], in_=hc[b][:, j * 128:(j + 1) * 128], identity=ident[:]),
                          [r_hc[b], r_ident], [r_pyT[b]])
                    A(lambda e, b=b: e.copy(out=ymT[b][:].rearrange("p j t -> p (j t)"), in_=pyT[b][:, 0:256]), [r_pyT[b]], [r_ymT[b]])
                    cx.dma("sp", mixT[256:512, t * 128:(t + 1) * 128].rearrange("(j p) t -> p j t", p=128), ymT[b][:], reads=[r_ymT[b]], writes=[r_mixT])
                cx.barrier()
        if STOP_AFTER == "C":
            break
        with ExitStack() as ph:
            def sbp(name, shape, dt=F32):
                return ph.enter_context(nc.sbuf_tensor(f"{name}_{l}", list(shape), dt))

            def psp(name, shape, dt=F32):
                return ph.enter_context(nc.psum_tensor(f"{name}_{l}", list(shape), dt))
            SCALE = float(96 ** -0.5)
            cs2 = sbp("cs2", [128, 2, S], BF16)
            r_cs2 = R()
            for tb in range(2):
                cx.dma("pool", cs2[64:96, tb, :], ropeT[tb], reads=[r_ropeT], writes=[r_cs2])
            wuq = sbp("wuq", [128, 2, 768], BF16)
            wuqs = sbp("wuqs", [128, 2, 768], BF16)
            wuk = sbp("wuk", [128, 512], BF16)
            wuv = sbp("wuv", [128, 512], BF16)
            r_w = R()
            cx.dma("pool", wuq[:], w_uq_in[l].rearrange("(j p) n -> p j n", p=128), writes=[r_w])
            cx.dma("pool", wuqs[:], w_uq_sw_in[l].rearrange("(j p) n -> p j n", p=128), writes=[r_w])
            cx.dma("pool", wuk[:], w_uk_in[l], writes=[r_w])
            cx.dma("pool", wuv[:], w_uv_in[l], writes=[r_w])
            gqp = sbp("gqp", [128, 2]); gkvp = sbp("gkvp", [128, 1])
            r_g = R()
            cx.dma("sp", gqp[:], g_q_p[l], writes=[r_g])
            cx.dma("sp", gkvp[:], g_kv_p[l], writes=[r_g])
            sel64 = sbp("sel64", [65, 64])
            r_sel = R()
            cx.dma("sp", sel64[:], sel64_in[:, :], writes=[r_sel])
            onesb = sbp("onesb", [128, 128], BF16)
            r_ones = R()
            G(lambda e: e.memset(onesb[:], 1.0), [], [r_ones])
            qn = sbp("qn", [128, 2, S], BF16)
            ckv = sbp("ckv", [128, S], BF16)
            krope = sbp("krope", [128, S], BF16)
            vx2 = sbp("vx2", [128, NT, 8, 65], BF16)
            r_qn, r_ckv, r_krope, r_vx2 = R(), R(), R(), R()
            G(lambda e: e.memset(vx2[:, :, :, 64:65], 1.0), [], [r_vx2])
            pa = [psp(f"pa{i}", [128, 512]) for i in range(2)]
            pb_ = [psp(f"pb{i}", [128, 512]) for i in range(2)]
            pc_ = [psp(f"pc{i}", [128, 512]) for i in range(2)]
            pm = [psp(f"pm{i}", [128, 512]) for i in range(2)]
            r_pa = [R(psum=True), R(psum=True)]
            r_pb = [R(psum=True), R(psum=True)]
            r_pc = [R(psum=True), R(psum=True)]
            r_pm = [R(psum=True), R(psum=True)]
            with ExitStack() as ph2:
                def sb2(name, shape, dt=F32):
                    return ph2.enter_context(nc.sbuf_tensor(f"{name}_{l}", list(shape), dt))
                ub = [sb2(f"ub{i}", [128, 3, 512]) for i in range(2)]
                r_ub = [R(), R()]
                sqb = [sb2(f"sqb{i}", [128, 3, 512], BF16) for i in range(2)]
                r_sqb = [R(), R()]
                rs = [sb2(f"rs{i}", [128, 2, 512]) for i in range(2)]
                r_rs = [R(), R()]
                krr = sb2("krr", [128, 2, S], BF16)
                r_krr = R()
                cx.dma("pool", krr[64:96, 0, :], U_fm[912:944, :], reads=[r_Ufm], writes=[r_krr])
                cx.dma("pool", krr[64:96, 1, :], U_fm[944:976, :], reads=[r_Ufm], writes=[r_krr])
                tmpk = sb2("tmpk", [128, S])
                r_tmpk = R()
                V(lambda e: e.tensor_tensor(out=tmpk[64:96, :], in0=krr[64:96, 0, :], in1=cs2[64:96, 1, :], op=ALU.mult), [r_krr, r_cs2], [r_tmpk])
                G(lambda e: e.tensor_tensor(out=krr[64:96, 1, :], in0=krr[64:96, 1, :], in1=cs2[64:96, 0, :], op=ALU.mult), [r_krr, r_cs2], [r_krr])
                V(lambda e: e.tensor_tensor(out=krope[64:96, :], in0=tmpk[64:96, :], in1=krr[64:96, 1, :], op=ALU.add), [r_krr, r_tmpk], [r_krope])
                for blk in range(8):
                    b = blk % 2
                    bs = slice(blk * 512, (blk + 1) * 512)
                    cx.dma("sp", ub[b][:], U_fm[512:896, bs].rearrange("(j p) t -> p j t", p=128), reads=[r_Ufm], writes=[r_ub[b]])
                    A(lambda e, b=b: e.activation(out=sqb[b][:], in_=ub[b][:], func=AF.Square), [r_ub[b]], [r_sqb[b]])
                    for j in range(2):
                        P(lambda e, b=b, j=j: e.matmul(pm[0][:, :], lhsT=onesb[:], rhs=sqb[b][:, j, :], start=(j == 0), stop=(j == 1)),
                          [r_ones, r_sqb[b]], [r_pm[0]])
                    P(lambda e, b=b: e.matmul(pm[1][:, :], lhsT=onesb[:], rhs=sqb[b][:, 2, :], start=True, stop=True), [r_ones, r_sqb[b]], [r_pm[1]])
                    A(lambda e, b=b: e.activation(out=rs[b][:, 0, :], in_=pm[0][:, :], func=AF.Sqrt, bias=RMS_EPS, scale=1.0 / 256), [r_pm[0]], [r_rs[b]])
                    A(lambda e, b=b: e.activation(out=rs[b][:, 1, :], in_=pm[1][:, :], func=AF.Sqrt, bias=RMS_EPS, scale=1.0 / 128), [r_pm[1]], [r_rs[b]])
                    V(lambda e, b=b: e.reciprocal(out=rs[b][:], in_=rs[b][:]), [r_rs[b]], [r_rs[b]])
                    for j in range(2):
                        V(lambda e, b=b, j=j, bs=bs: e.scalar_tensor_tensor(out=qn[:, j, bs], in0=ub[b][:, j, :], scalar=gqp[:, j:j + 1], in1=rs[b][:, 0, :],
                                                                          op0=ALU.mult, op1=ALU.mult), [r_ub[b], r_g, r_rs[b]], [r_qn])
                    V(lambda e, b=b, bs=bs: e.scalar_tensor_tensor(out=ckv[:, bs], in0=ub[b][:, 2, :], scalar=gkvp[:, 0:1], in1=rs[b][:, 1, :],
                                                                 op0=ALU.mult, op1=ALU.mult), [r_ub[b], r_g, r_rs[b]], [r_ckv])
                for t in range(NT):
                    i = t % 2
                    P(lambda e, t=t, i=i: e.matmul(pc_[i][:, :], lhsT=ckv[:, t * 128:(t + 1) * 128], rhs=wuv[:], start=True, stop=True),
                      [r_ckv, r_w], [r_pc[i]])
                    A(lambda e, t=t, i=i: e.copy(out=vx2[:, t, :, 0:64], in_=pc_[i][:, :].rearrange("p (h c) -> p h c", c=64)), [r_pc[i]], [r_vx2])
                cx.barrier()
            kTh = [sbp(f"kTh{i}", [128, S], BF16) for i in range(2)]
            qTh = [sbp(f"qTh{i}", [128, S], BF16) for i in range(2)]
            r_kTh = [R(), R()]; r_qTh = [R(), R()]
            rt = [sbp(f"rt{i}", [128, 2, 512]) for i in range(2)]
            r_rt = [R(), R()]
            pT = [sbp(f"pTe{i}", [128, 512], BF16) for i in range(3)]
            r_pTs = [R(), R(), R()]
            osb = [sbp(f"osb{i}", [65, 512]) for i in range(2)]
            r_osb = [R(), R()]
            rec = [sbp(f"rec{i}", [64, 512]) for i in range(2)]
            r_rec = [R(), R()]
            npt = 0
            npc = 0
            for h in range(8):
                hb = h % 2
                kT, qT = kTh[hb], qTh[hb]
                G(lambda e, kT=kT: e.tensor_copy(out=kT[64:96, :], in_=krope[64:96, :]), [r_krope], [r_kTh[hb]])
                for blk in range(8):
                    bs = slice(blk * 512, (blk + 1) * 512)
                    i = npc % 2
                    npc += 1
                    P(lambda e, i=i, h=h, bs=bs: e.matmul(pc_[i][0:64, :], lhsT=wuk[:, h * 64:(h + 1) * 64], rhs=ckv[:, bs], start=True, stop=True),
                      [r_w, r_ckv], [r_pc[i]])
                    V(lambda e, i=i, kT=kT, bs=bs: e.tensor_copy(out=kT[0:64, bs], in_=pc_[i][0:64, :]), [r_pc[i]], [r_kTh[hb]])
                    i = npc % 2
                    npc += 1
                    for j in range(2):
                        P(lambda e, i=i, h=h, j=j, bs=bs: e.matmul(pc_[i][0:96, :], lhsT=wuq[:, j, h * 96:(h + 1) * 96], rhs=qn[:, j, bs],
                                                                 start=(j == 0), stop=(j == 1)), [r_w, r_qn], [r_pc[i]])
                    for j in range(2):
                        P(lambda e, i=i, h=h, j=j, bs=bs: e.matmul(pm[i][0:96, :], lhsT=wuqs[:, j, h * 96:(h + 1) * 96], rhs=qn[:, j, bs],
                                                                 start=(j == 0), stop=(j == 1)), [r_w, r_qn], [r_pm[i]])
                    A(lambda e, i=i, qT=qT, bs=bs: e.copy(out=qT[0:64, bs], in_=pc_[i][0:64, :]), [r_pc[i]], [r_qTh[hb]])
                    V(lambda e, i=i, bs=bs: e.tensor_tensor(out=rt[i][64:96, 0, :], in0=pc_[i][64:96, :], in1=cs2[64:96, 1, bs], op=ALU.mult),
                      [r_pc[i], r_cs2], [r_rt[i]])
                    V(lambda e, i=i, bs=bs: e.tensor_tensor(out=rt[i][64:96, 1, :], in0=pm[i][64:96, :], in1=cs2[64:96, 0, bs], op=ALU.mult),
                      [r_pm[i], r_cs2], [r_rt[i]])
                    G(lambda e, i=i, qT=qT, bs=bs: e.tensor_tensor(out=qT[64:96, bs], in0=rt[i][64:96, 0, :], in1=rt[i][64:96, 1, :], op=ALU.add),
                      [r_rt[i]], [r_qTh[hb]])
                items = [(qb, kt) for qb in range(8) for kt in range(NT)]

                def emitS(n, kT=kT, qT=qT, hb=hb):
                    qb, kt = items[n]
                    i = n % 2
                    qs = slice(qb * 512, (qb + 1) * 512)
                    P(lambda e: e.matmul(pa[i][:, :], lhsT=kT[0:96, kt * 128:(kt + 1) * 128], rhs=qT[0:96, qs], start=True, stop=True),
                      [r_kTh[hb], r_qTh[hb]], [r_pa[i]])

                def post_a(qb, h=h):
                    ob = qb % 2
                    V(lambda e: e.tensor_copy(out=osb[ob][:], in_=pb_[ob][0:65, :]), [r_pb[ob]], [r_osb[ob]])

                def post_b(qb, h=h):
                    ob = qb % 2
                    qs = slice(qb * 512, (qb + 1) * 512)
                    P(lambda e: e.matmul(pm[ob][0:64, :], lhsT=sel64[:], rhs=osb[ob][:], start=True, stop=True), [r_sel, r_osb[ob]], [r_pm[ob]])
                    V(lambda e: e.reciprocal(out=rec[ob][:], in_=pm[ob][0:64, :]), [r_pm[ob]], [r_rec[ob]])
                    G(lambda e: e.tensor_tensor(out=rec[ob][:], in0=rec[ob][:], in1=osb[ob][0:64, :], op=ALU.mult), [r_rec[ob], r_osb[ob]], [r_rec[ob]])
                    cx.dma("sp", mixT[512 + h * 64:512 + (h + 1) * 64, qs], rec[ob][:], reads=[r_rec[ob]], writes=[r_mixT])

                emitS(0)
                pending = None
                for n, (qb, kt) in enumerate(items):
                    if n + 1 < len(items):
                        emitS(n + 1)
                    i = n % 2
                    ip = n % 3
                    ob = qb % 2
                    A(lambda e, i=i, ip=ip: e.activation(out=pT[ip][:], in_=pa[i][:, :], func=AF.Exp, scale=SCALE), [r_pa[i]], [r_pTs[ip]])
                    P(lambda e, ip=ip, ob=ob, kt=kt, h=h: e.matmul(pb_[ob][0:65, :], lhsT=vx2[:, kt, h, :], rhs=pT[ip][:],
                                                                 start=(kt == 0), stop=(kt == NT - 1)), [r_vx2, r_pTs[ip]], [r_pb[ob]])
                    if kt == NT - 1:
                        post_a(qb)
                        pending = (qb, n + 6)
                    if pending is not None and n >= pending[1]:
                        post_b(pending[0])
                        pending = None
                if pending is not None:
                    post_b(pending[0])
            cx.barrier()
        if STOP_AFTER == "D":
            break
        x_src = x_in if l == 0 else XL
        r_X1, r_H2, r_MS, r_WN, r_FFN = R(), R(), R(), R(), R()
        cnt = sb(f"cnt{l}", [128, 256])
        r_cnt = R()
        with ExitStack() as ph:
            def sbp(name, shape, dt=F32):
                return ph.enter_context(nc.sbuf_tensor(f"{name}_{l}", list(shape), dt))

            def psp(name, shape, dt=F32):
                return ph.enter_context(nc.psum_tensor(f"{name}_{l}", list(shape), dt))
            wout = sbp("wout", [128, 8, D], BF16)
            r_wout = R()
            for kc in range(8):
                cx.dma("pool", wout[:, kc, :], w_out_in[l, kc * 128:(kc + 1) * 128, :], writes=[r_wout])
            lnp = sbp("lnp", [128, 2, D])
            r_lnp = R()
            for j in range(2):
                cx.dma("sp", lnp[:, j, :], lnp_in[l, j].partition_broadcast(128), writes=[r_lnp])
            wr = sbp("wr", [128, 8, 256])
            r_wr = R()
            cx.dma("sp", wr[:], w_router_in[l].rearrange("(kc p) n -> p kc n", p=128), writes=[r_wr])
            ebias = sbp("ebias", [128, 256])
            r_eb = R()
            cx.dma("sp", ebias[:], e_bias_in[l].partition_broadcast(128), writes=[r_eb])
            ws13 = sbp("ws13", [128, 8, 512], BF16)
            ws2 = sbp("ws2", [128, 2, D], BF16)
            r_ws = R()
            for kc in range(8):
                cx.dma("pool", ws13[:, kc, :], ws13_in[l, kc * 128:(kc + 1) * 128, :], writes=[r_ws])
            for j in range(2):
                cx.dma("pool", ws2[:, j, :], ws2_in[l, j * 128:(j + 1) * 128, :], writes=[r_ws])
            onesb = sbp("onesbE", [128, 128], BF16)
            r_ones = R()
            G(lambda e: e.memset(onesb[:], 1.0), [], [r_ones])
            G(lambda e: e.memset(cnt[:], 0.0), [], [r_cnt])
            mxt = [sbp(f"mxt{i}", [128, 8, 512], BF16) for i in range(2)]
            r_mxt = [R(), R()]
            xt = [sbp(f"xtE{i}", [128, D]) for i in range(2)]
            r_xt = [R(), R()]
            z = [sbp(f"zE{i}", [128, D]) for i in range(2)]
            r_z = [R(), R()]
            x1 = [sbp(f"x1E{i}", [128, D]) for i in range(2)]
            r_x1 = [R(), R()]
            h2f = [sbp(f"h2f{i}", [128, D]) for i in range(2)]
            r_h2f = [R(), R()]
            h2b = [sbp(f"h2b{i}", [128, D], BF16) for i in range(2)]
            r_h2b = [R(), R()]
            h2T = sbp("h2T", [128, 8, 128]); h2Tb = sbp("h2Tb", [128, 8, 128], BF16)
            r_h2T, r_h2Tb = R(), R()
            st = sbp("stE", [128, 2, 6]); mv = sbp("mvE", [128, 2]); rstd = sbp("rstdE", [128, 1])
            r_st, r_mv, r_rstd = R(), R(), R()
            sc = sbp("scE", [128, 256]); sel = sbp("selE", [128, 256]); selm = sbp("selmE", [128, 256])
            m8g = sbp("m8g", [128, 8, 8]); gs = sbp("gsE", [128, 8]); m8 = sbp("m8E", [128, 8]); gm = sbp("gmE", [128, 2, 8])
            Mf = sbp("MfE", [128, 256]); Mb = [sbp(f"MbE{i}", [128, 256], BF16) for i in range(2)]
            wn = [sbp(f"wnE{i}", [128, 256]) for i in range(2)]
            ws_ = sbp("wsE", [128, 2])
            r_rt = R()
            r_Mb = [R(), R()]; r_wn = [R(), R()]
            s1 = sbp("s1E", [128, 256]); gsh = sbp("gshE", [128, 256], BF16); gT = sbp("gTE", [128, 2, 128], BF16)
            r_s1, r_gsh, r_gT = R(), R(), R()
            fo = [sbp(f"foE{i}", [128, D]) for i in range(2)]
            r_fo = [R(), R()]
            pX = [psp(f"pX{i}", [128, 512]) for i in range(2)]
            pY = [psp(f"pY{i}", [128, 512]) for i in range(2)]
            pZ = [psp(f"pZ{i}", [128, 512]) for i in range(2)]
            pW0 = psp("pW0", [128, 1024], BF16)
            pW1 = psp("pW1", [128, 512])
            r_pX = [R(psum=True), R(psum=True)]; r_pY = [R(psum=True), R(psum=True)]; r_pZ = [R(psum=True), R(psum=True)]
            r_pW0, r_pW1 = R(psum=True), R(psum=True)
            BIG = 1.0e4

            def layer_norm_stats(src, r_src):
                for hf in range(2):
                    V(lambda e, hf=hf: e.bn_stats(out=st[:, hf, :], in_=src[:, hf * 512:(hf + 1) * 512]), [r_src], [r_st])
                V(lambda e: e.bn_aggr(out=mv[:], in_=st[:].rearrange("p a b -> p (a b)")), [r_st], [r_mv])
                A(lambda e: e.activation(out=rstd[:], in_=mv[:, 1:2], func=AF.Sqrt, bias=LN_EPS, scale=1.0), [r_mv], [r_rstd])
                V(lambda e: e.reciprocal(out=rstd[:], in_=rstd[:]), [r_rstd], [r_rstd])

            for t in range(NT):
                b = t % 2
                if t % 4 == 0:
                    g4 = (t // 4) % 2
                    cx.dma("pool", mxt[g4][:], mixT[:, t * 128:(t + 4) * 128].rearrange("(kc p) t -> p kc t", p=128),
                           reads=[r_mixT], writes=[r_mxt[g4]])
                g4 = (t // 4) % 2
                tt = t % 4
                cx.dma("sp", xt[b][:], x_src[t * 128:(t + 1) * 128, :], writes=[r_xt[b]])
                for hf in range(2):
                    for kc in range(8):
                        P(lambda e, hf=hf, kc=kc, g4=g4, tt=tt: e.matmul(pX[hf][:, :], lhsT=mxt[g4][:, kc, tt * 128:(tt + 1) * 128],
                                                                      rhs=wout[:, kc, hf * 512:(hf + 1) * 512], start=(kc == 0), stop=(kc == 7)),
                          [r_mxt[g4], r_wout], [r_pX[hf]])
                    V(lambda e, hf=hf, b=b: e.tensor_tensor(out=z[b][:, hf * 512:(hf + 1) * 512], in0=pX[hf][:, :], in1=gF[:, 0, hf * 512:(hf + 1) * 512], op=ALU.mult),
                      [r_pX[hf], r_gF], [r_z[b]])
                V(lambda e, b=b: e.scalar_tensor_tensor(out=z[b][:], in0=xt[b][:], scalar=float(ALPHA), in1=z[b][:], op0=ALU.mult, op1=ALU.add),
                  [r_xt[b], r_z[b]], [r_z[b]])
                layer_norm_stats(z[b], r_z[b])
                V(lambda e, b=b: e.tensor_scalar(out=z[b][:], in0=z[b][:], scalar1=mv[:, 0:1], scalar2=rstd[:, 0:1], op0=ALU.subtract, op1=ALU.mult),
                  [r_z[b], r_mv, r_rstd], [r_z[b]])
                G(lambda e, b=b: e.tensor_tensor(out=z[b][:], in0=z[b][:], in1=lnp[:, 0, :], op=ALU.mult), [r_z[b], r_lnp], [r_z[b]])
                V(lambda e, b=b: e.tensor_tensor(out=x1[b][:], in0=z[b][:], in1=lnp[:, 1, :], op=ALU.add), [r_z[b], r_lnp], [r_x1[b]])
                cx.dma("sp", X1[t * 128:(t + 1) * 128, :], x1[b][:], reads=[r_x1[b]], writes=[r_X1])
                layer_norm_stats(x1[b], r_x1[b])
                V(lambda e, b=b: e.tensor_scalar(out=h2f[b][:], in0=x1[b][:], scalar1=mv[:, 0:1], scalar2=rstd[:, 0:1], op0=ALU.subtract, op1=ALU.mult),
                  [r_x1[b], r_mv, r_rstd], [r_h2f[b]])
                G(lambda e, b=b: e.tensor_tensor(out=h2f[b][:], in0=h2f[b][:], in1=gF[:, 3, :], op=ALU.mult), [r_h2f[b], r_gF], [r_h2f[b]])
                V(lambda e, b=b: e.tensor_tensor(out=h2f[b][:], in0=h2f[b][:], in1=gF[:, 2, :], op=ALU.add), [r_h2f[b], r_gF], [r_h2f[b]])
                A(lambda e, b=b: e.copy(out=h2b[b][:], in_=h2f[b][:]), [r_h2f[b]], [r_h2b[b]])
                cx.dma("sp", H2[t * 128:(t + 1) * 128, :], h2b[b][:], reads=[r_h2b[b]], writes=[r_H2])
                for q4 in range(2):
                    for k4 in range(4):
                        kc = q4 * 4 + k4
                        P(lambda e, q4=q4, k4=k4, kc=kc, b=b: e.transpose(out=pY[q4][:, k4 * 128:(k4 + 1) * 128], in_=h2f[b][:, kc * 128:(kc + 1) * 128], identity=ident[:]),
                          [r_h2f[b], r_ident], [r_pY[q4]])
                    V(lambda e, q4=q4: e.tensor_copy(out=h2T[:, q4 * 4:(q4 + 1) * 4, :].rearrange("p a t -> p (a t)"), in_=pY[q4][:, :]), [r_pY[q4]], [r_h2T])
                    A(lambda e, q4=q4: e.copy(out=h2Tb[:, q4 * 4:(q4 + 1) * 4, :].rearrange("p a t -> p (a t)"), in_=pY[q4][:, :]), [r_pY[q4]], [r_h2Tb])
                for kc in range(8):
                    P(lambda e, kc=kc: e.matmul(pZ[0][:, 0:256], lhsT=h2T[:, kc, :], rhs=wr[:, kc, :], start=(kc == 0), stop=(kc == 7)),
                      [r_h2T, r_wr], [r_pZ[0]])
                A(lambda e: e.activation(out=sc[:], in_=pZ[0][:, 0:256], func=AF.Sigmoid), [r_pZ[0]], [r_rt])
                V(lambda e: e.tensor_tensor(out=sel[:], in0=sc[:], in1=ebias[:], op=ALU.add), [r_rt, r_eb], [r_rt])
                for g in range(8):
                    V(lambda e, g=g: e.max(out=m8g[:, g, :], in_=sel[:, g * 32:(g + 1) * 32]), [r_rt], [r_rt])
                V(lambda e: e.tensor_tensor(out=gs[:], in0=m8g[:, :, 0], in1=m8g[:, :, 1], op=ALU.add), [r_rt], [r_rt])
                V(lambda e: e.max(out=m8[:], in_=gs[:]), [r_rt], [r_rt])
                V(lambda e: e.tensor_scalar(out=gm[:, 0, :], in0=gs[:], scalar1=m8[:, 3:4], scalar2=None, op0=ALU.is_ge), [r_rt], [r_rt])
                V(lambda e: e.tensor_scalar(out=gm[:, 1, :], in0=gm[:, 0, :], scalar1=BIG, scalar2=-BIG, op0=ALU.mult, op1=ALU.add), [r_rt], [r_rt])
                V(lambda e: e.tensor_tensor(out=selm[:].rearrange("p (g k) -> p g k", k=32), in0=sel[:].rearrange("p (g k) -> p g k", k=32),
                                            in1=gm[:, 0, :].unsqueeze(2).to_broadcast([128, 8, 32]), op=ALU.mult), [r_rt], [r_rt])
                V(lambda e: e.tensor_tensor(out=selm[:].rearrange("p (g k) -> p g k", k=32), in0=selm[:].rearrange("p (g k) -> p g k", k=32),
                                            in1=gm[:, 1, :].unsqueeze(2).to_broadcast([128, 8, 32]), op=ALU.add), [r_rt], [r_rt])
                V(lambda e: e.max(out=m8[:], in_=selm[:]), [r_rt], [r_rt])
                V(lambda e: e.tensor_scalar(out=Mf[:], in0=selm[:], scalar1=m8[:, 7:8], scalar2=None, op0=ALU.is_ge), [r_rt], [r_rt])
                G(lambda e, b=b: e.tensor_copy(out=Mb[b][:], in_=Mf[:]), [r_rt], [r_Mb[b]])
                V(lambda e: e.tensor_tensor(out=sel[:], in0=sc[:], in1=Mf[:], op=ALU.mult), [r_rt], [r_rt])
                V(lambda e: e.tensor_reduce(out=ws_[:, 0:1], in_=sel[:], axis=AX.X, op=ALU.add), [r_rt], [r_rt])
                V(lambda e: e.reciprocal(out=ws_[:, 1:2], in_=ws_[:, 0:1]), [r_rt], [r_rt])
                V(lambda e, b=b: e.tensor_scalar(out=wn[b][:], in0=sel[:], scalar1=ws_[:, 1:2], scalar2=2.5, op0=ALU.mult, op1=ALU.mult), [r_rt], [r_wn[b]])
                cx.dma("sp", MS[t * 128:(t + 1) * 128, :], Mb[b][:], reads=[r_Mb[b]], writes=[r_MS])
                cx.dma("sp", WN[t * 128:(t + 1) * 128, :], wn[b][:], reads=[r_wn[b]], writes=[r_WN])
                P(lambda e, b=b: e.matmul(pW1[:, 0:256], lhsT=onesb[:], rhs=Mb[b][:], start=True, stop=True), [r_ones, r_Mb[b]], [r_pW1])
                V(lambda e: e.tensor_tensor(out=cnt[:], in0=cnt[:], in1=pW1[:, 0:256], op=ALU.add), [r_pW1, r_cnt], [r_cnt])
                for kc in range(8):
                    P(lambda e, kc=kc: e.matmul(pZ[1][:, :], lhsT=h2Tb[:, kc, :], rhs=ws13[:, kc, :], start=(kc == 0), stop=(kc == 7)),
                      [r_h2Tb, r_ws], [r_pZ[1]])
                A(lambda e: e.activation(out=s1[:], in_=pZ[1][:, 0:256], func=AF.Silu), [r_pZ[1]], [r_s1])
                V(lambda e: e.tensor_tensor(out=gsh[:], in0=s1[:], in1=pZ[1][:, 256:512], op=ALU.mult), [r_s1, r_pZ[1]], [r_gsh])
                for j in range(2):
                    P(lambda e, j=j: e.transpose(out=pW0[:, j * 128:(j + 1) * 128], in_=gsh[:, j * 128:(j + 1) * 128], identity=identb[:]),
                      [r_gsh, r_identb], [r_pW0])
                A(lambda e: e.copy(out=gT[:].rearrange("p j t -> p (j t)"), in_=pW0[:, 0:256]), [r_pW0], [r_gT])
                for hf in range(2):
                    for j in range(2):
                        P(lambda e, hf=hf, j=j: e.matmul(pX[hf][:, :], lhsT=gT[:, j, :], rhs=ws2[:, j, hf * 512:(hf + 1) * 512], start=(j == 0), stop=(j == 1)),
                          [r_gT, r_ws], [r_pX[hf]])
                    if hf == 0:
                        A(lambda e, b=b: e.copy(out=fo[b][:, 0:512], in_=pX[0][:, :]), [r_pX[0]], [r_fo[b]])
                    else:
                        V(lambda e, b=b: e.tensor_copy(out=fo[b][:, 512:1024], in_=pX[1][:, :]), [r_pX[1]], [r_fo[b]])
                cx.dma("sp", FFN[t * 128:(t + 1) * 128, :], fo[b][:], reads=[r_fo[b]], writes=[r_FFN])
            cx.barrier()
        if STOP_AFTER == "E":
            break
        r_Xs, r_Ys = R(), R()
        idxs = sb(f"idxs{l}", [128, NT, 8], I32)
        wk = sb(f"wk{l}", [128, NT, 8])
        idxw = sb(f"idxw{l}", [128, 512], I32)
        r_idxs, r_wk, r_idxw = R(), R(), R()
        with ExitStack() as ph:
            def sbp(name, shape, dt=F32):
                return ph.enter_context(nc.sbuf_tensor(f"{name}_{l}", list(shape), dt))

            def psp(name, shape, dt=F32):
                return ph.enter_context(nc.psum_tensor(f"{name}_{l}", list(shape), dt))
            xq = sbp("xq", [128, 256]); qi = sbp("qi", [128, 256], I32); qf = sbp("qf", [128, 256]); gtm = sbp("gtm", [128, 256])
            pend = sbp("pend", [128, 256]); base = sbp("base", [128, 256])
            r_f1 = R(); r_base = R()
            V(lambda e: e.tensor_scalar(out=xq[:], in0=cnt[:], scalar1=127.0, scalar2=1.0 / 128, op0=ALU.add, op1=ALU.mult), [r_cnt], [r_f1])
            V(lambda e: e.tensor_copy(out=qi[:], in_=xq[:]), [r_f1], [r_f1])
            V(lambda e: e.tensor_copy(out=qf[:], in_=qi[:]), [r_f1], [r_f1])
            V(lambda e: e.tensor_tensor(out=gtm[:], in0=qf[:], in1=xq[:], op=ALU.is_gt), [r_f1], [r_f1])
            V(lambda e: e.tensor_tensor(out=qf[:], in0=qf[:], in1=gtm[:], op=ALU.subtract), [r_f1], [r_f1])
            V(lambda e: e.tensor_scalar(out=qf[:], in0=qf[:], scalar1=128.0, scalar2=None, op0=ALU.mult), [r_f1], [r_f1])
            V(lambda e: e.tensor_tensor_scan(out=pend[:], data0=qf[:], data1=qf[:], initial=0.0, op0=ALU.add, op1=ALU.max), [r_f1], [r_f1])
            V(lambda e: e.tensor_tensor(out=base[:], in0=pend[:], in1=qf[:], op=ALU.subtract), [r_f1], [r_base])
            V(lambda e: e.tensor_scalar(out=base[:], in0=base[:], scalar1=1.0, scalar2=None, op0=ALU.add), [r_base], [r_base])
            pF = [psp(f"pF{i}", [128, 512]) for i in range(2)]
            r_pF = [R(psum=True), R(psum=True)]
            pendT = sbp("pendT", [128, 2])
            for j in range(2):
                P(lambda e, j=j: e.transpose(out=pF[0][:, j:j + 1], in_=pend[0:1, j * 128:(j + 1) * 128], identity=ident[0:1, 0:1]), [r_f1, r_ident], [r_pF[0]])
            V(lambda e: e.tensor_copy(out=pendT[:], in_=pF[0][:, 0:2]), [r_pF[0]], [r_f1])
            blkpos = sbp("blkpos", [128, 512]); pidx = sbp("pidx", [128, 1])
            r_c2 = R()
            cx.dma("sp", blkpos[:], blkpos_in[:, :], writes=[r_c2])
            cx.dma("sp", pidx[:], pidx_in[:, :], writes=[r_c2])
            Gm = [sbp(f"Gm{j}", [128, 512], BF16) for j in range(2)]
            onesb = sbp("onesbF", [128, 128], BF16)
            ustr = sbp("ustr", [128, 128], BF16)
            r_ones = R()
            G(lambda e: e.memset(onesb[:], 1.0), [], [r_ones])
            cx.dma("pool", ustr[:], ustrict_in[:, :], writes=[r_ones])
            for j in range(2):
                V(lambda e, j=j: e.tensor_scalar(out=Gm[j][:], in0=blkpos[:], scalar1=pendT[:, j:j + 1], scalar2=None, op0=ALU.is_ge), [r_c2, r_f1], [r_f1])
            for j in range(2):
                P(lambda e, j=j: e.matmul(pF[1][:, :], lhsT=onesb[:], rhs=Gm[j][:], start=(j == 0), stop=(j == 1)), [r_ones, r_f1], [r_pF[1]])
            eall = sbp("eall", [128, 512])
            V(lambda e: e.tensor_scalar(out=eall[:], in0=pF[1][:, :], scalar1=255.0, scalar2=None, op0=ALU.min), [r_pF[1]], [r_f1])
            vld = sbp("vld", [128, 512]); vld2 = sbp("vld2", [128, 512])
            V(lambda e: e.tensor_scalar(out=vld[:], in0=blkpos[:], scalar1=pend[:, 255:256], scalar2=None, op0=ALU.is_lt), [r_f1, r_c2], [r_f1])
            V(lambda e: e.memset(vld2[:], 1.0), [r_f1], [r_f1])
            V(lambda e: e.tensor_tensor(out=vld2[:, 2:512], in0=eall[:, 2:512], in1=eall[:, 0:510], op=ALU.not_equal), [r_f1], [r_f1])
            V(lambda e: e.tensor_tensor(out=vld[:], in0=vld[:], in1=vld2[:], op=ALU.mult), [r_f1], [r_f1])
            V(lambda e: e.tensor_scalar(out=eall[:], in0=eall[:], scalar1=128.0, scalar2=float(l * 256 * 128) - OOB_IDX, op0=ALU.mult, op1=ALU.add), [r_f1], [r_f1])
            V(lambda e: e.tensor_scalar(out=eall[:], in0=eall[:], scalar1=pidx[:, 0:1], scalar2=None, op0=ALU.add), [r_f1, r_c2], [r_f1])
            V(lambda e: e.tensor_tensor(out=eall[:], in0=eall[:], in1=vld[:], op=ALU.mult), [r_f1], [r_f1])
            V(lambda e: e.tensor_scalar(out=idxw[:], in0=eall[:], scalar1=OOB_IDX, scalar2=None, op0=ALU.add), [r_f1], [r_idxw])
            Mt = [sbp(f"Mt{i}", [128, 256], BF16) for i in range(2)]
            wnt = [sbp(f"wnt{i}", [128, 256]) for i in range(2)]
            h2t = [sbp(f"h2t{i}", [128, D], BF16) for i in range(2)]
            r_Mt = [R(), R()]; r_wnt = [R(), R()]; r_h2t = [R(), R()]
            t1 = sbp("t1", [128, 256]); Vt = sbp("Vt", [128, 256]); junk = sbp("junk", [128, 256]); p8 = sbp("p8", [128, 8])
            r_t1, r_Vt, r_junk, r_p8 = R(), R(), R(), R()
            for t in range(NT):
                b = t % 2
                cx.dma("sp", Mt[b][:], MS[t * 128:(t + 1) * 128, :], reads=[r_MS], writes=[r_Mt[b]])
                cx.dma("sp", wnt[b][:], WN[t * 128:(t + 1) * 128, :], reads=[r_WN], writes=[r_wnt[b]])
                cx.dma("sp", h2t[b][:], H2[t * 128:(t + 1) * 128, :], reads=[r_H2], writes=[r_h2t[b]])
                P(lambda e, b=b: e.matmul(pF[0][:, 0:256], lhsT=ustr[:], rhs=Mt[b][:], start=True, stop=True), [r_ones, r_Mt[b]], [r_pF[0]])
                V(lambda e: e.tensor_tensor(out=t1[:], in0=pF[0][:, 0:256], in1=base[:], op=ALU.add), [r_pF[0], r_base], [r_t1])
                G(lambda e, b=b: e.tensor_tensor(out=Vt[:], in0=t1[:], in1=Mt[b][:], op=ALU.mult), [r_t1, r_Mt[b]], [r_Vt])
                V(lambda e: e.max(out=p8[:], in_=Vt[:]), [r_Vt], [r_p8])
                V(lambda e, t=t: e.tensor_scalar(out=idxs[:, t, :], in0=p8[:], scalar1=-1.0, scalar2=None, op0=ALU.add), [r_p8], [r_idxs])
                for k in range(8):
                    V(lambda e, t=t, k=k, b=b: e.scalar_tensor_tensor(out=junk[:], in0=Vt[:], scalar=p8[:, k:k + 1], in1=wnt[b][:], op0=ALU.is_equal, op1=ALU.mult,
                                                                    accum_out=wk[:, t, k:k + 1]), [r_Vt, r_p8, r_wnt[b]], [r_junk, r_wk])
                P(lambda e, b=b: e.matmul(pF[1][:, 0:256], lhsT=onesb[:], rhs=Mt[b][:], start=True, stop=True), [r_ones, r_Mt[b]], [r_pF[1]])
                V(lambda e: e.tensor_tensor(out=base[:], in0=base[:], in1=pF[1][:, 0:256], op=ALU.add), [r_pF[1], r_base, r_t1], [r_base])
                for k in range(8):
                    cx.dma("pool", None, None, reads=[r_h2t[b], r_idxs], writes=[r_Xs],
                           fn=lambda e, t=t, k=k, b=b: e.indirect_dma_start(
                               out=Xs[:, :], out_offset=bass.IndirectOffsetOnAxis(ap=idxs[:, t, k:k + 1].bitcast(U32), axis=0),
                               in_=h2t[b][:], in_offset=None))
            cx.barrier()
        with ExitStack() as ph:
            def sbp(name, shape, dt=F32):
                return ph.enter_context(nc.sbuf_tensor(f"{name}_{l}", list(shape), dt))

            def psp(name, shape, dt=F32):
                return ph.enter_context(nc.psum_tensor(f"{name}_{l}", list(shape), dt))
            Xb = [sbp(f"Xb{i}", [128, D], BF16) for i in range(2)]
            w1b = [sbp(f"w1b{i}", [128, 2048], BF16) for i in range(2)]
            w3b = [sbp(f"w3b{i}", [128, 2048], BF16) for i in range(2)]
            w2b = [sbp(f"w2b{i}", [128, 2048], BF16) for i in range(2)]
            xT = [sbp(f"xTb{i}", [128, 8, 128], BF16) for i in range(2)]
            s1 = [sbp(f"s1b{i}", [128, 256]) for i in range(2)]
            gb = [sbp(f"gbb{i}", [128, 256], BF16) for i in range(2)]
            gT = [sbp(f"gTb{i}", [128, 2, 128], BF16) for i in range(2)]
            Yb = [sbp(f"Yb{i}", [128, D], BF16) for i in range(2)]
            r_Xb = [R(), R()]; r_w1b = [R(), R()]; r_w3b = [R(), R()]; r_w2b = [R(), R()]; r_xT = [R(), R()]
            r_s1 = [R(), R()]; r_gb = [R(), R()]; r_gT = [R(), R()]; r_Yb = [R(), R()]
            pxT = [psp(f"pxT{i}", [128, 1024], BF16) for i in range(2)]
            phh = [psp(f"phh{i}", [128, 512]) for i in range(2)]
            pgT = psp("pgT", [128, 1024], BF16)
            pyy = [psp(f"pyy{i}", [128, 512]) for i in range(2)]
            r_pxT = [R(psum=True), R(psum=True)]; r_phh = [R(psum=True), R(psum=True)]; r_pgT = R(psum=True); r_pyy = [R(psum=True), R(psum=True)]
            for b in range(NBLK):
                i = b % 2
                cx.dma("sp", Xb[i][:], Xs[b * 128:(b + 1) * 128, :], reads=[r_Xs], writes=[r_Xb[i]])
                for wsrc, wdst, rw in ((w1_in, w1b, r_w1b), (w3_in, w3b, r_w3b), (w2_in, w2b, r_w2b)):
                    cx.dma("pool", None, None, reads=[r_idxw], writes=[rw[i]],
                           fn=lambda e, wsrc=wsrc, wdst=wdst, i=i, b=b: e.indirect_dma_start(
                               out=wdst[i][:], out_offset=None, in_=wsrc[:, :],
                               in_offset=bass.IndirectOffsetOnAxis(ap=idxw[:, b:b + 1].bitcast(U32), axis=0),
                               bounds_check=bc_reg, oob_is_err=False))
                for j in range(8):
                    P(lambda e, i=i, j=j: e.transpose(out=pxT[i][:, j * 128:(j + 1) * 128], in_=Xb[i][:, j::8], identity=identb[:]),
                      [r_Xb[i], r_identb], [r_pxT[i]])
                A(lambda e, i=i: e.copy(out=xT[i][:, 0:4, :].rearrange("p a t -> p (a t)"), in_=pxT[i][:, 0:512]), [r_pxT[i]], [r_xT[i]])
                V(lambda e, i=i: e.tensor_copy(out=xT[i][:, 4:8, :].rearrange("p a t -> p (a t)"), in_=pxT[i][:, 512:1024]), [r_pxT[i]], [r_xT[i]])
                for j in range(8):
                    P(lambda e, i=i, j=j: e.matmul(phh[i][:, 0:256], lhsT=xT[i][:, j, :], rhs=w1b[i][:, j * 256:(j + 1) * 256], start=(j == 0), stop=(j == 7)),
                      [r_xT[i], r_w1b[i]], [r_phh[i]])
                for j in range(8):
                    P(lambda e, i=i, j=j: e.matmul(phh[i][:, 256:512], lhsT=xT[i][:, j, :], rhs=w3b[i][:, j * 256:(j + 1) * 256], start=(j == 0), stop=(j == 7)),
                      [r_xT[i], r_w3b[i]], [r_phh[i]])
                A(lambda e, i=i: e.activation(out=s1[i][:], in_=phh[i][:, 0:256], func=AF.Silu), [r_phh[i]], [r_s1[i]])
                V(lambda e, i=i: e.tensor_tensor(out=gb[i][:], in0=s1[i][:], in1=phh[i][:, 256:512], op=ALU.mult), [r_s1[i], r_phh[i]], [r_gb[i]])
                for j in range(2):
                    P(lambda e, i=i, j=j: e.transpose(out=pgT[:, j * 128:(j + 1) * 128], in_=gb[i][:, j::2], identity=identb[:]), [r_gb[i], r_identb], [r_pgT])
                A(lambda e, i=i: e.copy(out=gT[i][:].rearrange("p j t -> p (j t)"), in_=pgT[:, 0:256]), [r_pgT], [r_gT[i]])
                for hf in range(2):
                    for j in range(2):
                        P(lambda e, i=i, j=j, hf=hf: e.matmul(pyy[hf][:, :], lhsT=gT[i][:, j, :], rhs=w2b[i][:, j * 1024 + hf * 512:j * 1024 + (hf + 1) * 512],
                                                            start=(j == 0), stop=(j == 1)), [r_gT[i], r_w2b[i]], [r_pyy[hf]])
                A(lambda e, i=i: e.copy(out=Yb[i][:, 0:512], in_=pyy[0][:, :]), [r_pyy[0]], [r_Yb[i]])
                V(lambda e, i=i: e.tensor_copy(out=Yb[i][:, 512:1024], in_=pyy[1][:, :]), [r_pyy[1]], [r_Yb[i]])
                cx.dma("sp", Ys[b * 128:(b + 1) * 128, :], Yb[i][:], reads=[r_Yb[i]], writes=[r_Ys])
            cx.barrier()
        with ExitStack() as ph:
            def sbp(name, shape, dt=F32):
                return ph.enter_context(nc.sbuf_tensor(f"{name}_{l}", list(shape), dt))
            lnp2 = sbp("lnp2", [128, 2, D])
            r_lnp2 = R()
            for j in range(2):
                cx.dma("sp", lnp2[:, j, :], lnp_in[l, 2 + j].partition_broadcast(128), writes=[r_lnp2])
            yg = [sbp(f"yg{i}", [128, 8, D], BF16) for i in range(2)]
            r_yg = [R(), R()]
            acc = [sbp(f"accF{i}", [128, D]) for i in range(2)]
            r_acc = [R(), R()]
            x1t = [sbp(f"x1t{i}", [128, D]) for i in range(2)]
            r_x1t = [R(), R()]
            st = sbp("stF", [128, 2, 6]); mv = sbp("mvF", [128, 2]); rstd = sbp("rstdF", [128, 1])
            r_st, r_mv, r_rstd = R(), R(), R()
            dst = XL if l < L - 1 else y_out
            r_dst = R()
            for t in range(NT):
                b = t % 2
                for k in range(8):
                    cx.dma("pool", None, None, reads=[r_Ys, r_idxs], writes=[r_yg[b]],
                           fn=lambda e, t=t, k=k, b=b: e.indirect_dma_start(
                               out=yg[b][:, k, :], out_offset=None, in_=Ys[:, :],
                               in_offset=bass.IndirectOffsetOnAxis(ap=idxs[:, t, k:k + 1].bitcast(U32), axis=0)))
                cx.dma("sp", acc[b][:], FFN[t * 128:(t + 1) * 128, :], reads=[r_FFN], writes=[r_acc[b]])
                cx.dma("sp", x1t[b][:], X1[t * 128:(t + 1) * 128, :], reads=[r_X1], writes=[r_x1t[b]])
                for k in range(8):
                    V(lambda e, t=t, k=k, b=b: e.scalar_tensor_tensor(out=acc[b][:], in0=yg[b][:, k, :], scalar=wk[:, t, k:k + 1], in1=acc[b][:],
                                                                    op0=ALU.mult, op1=ALU.add), [r_yg[b], r_wk, r_acc[b]], [r_acc[b]])
                G(lambda e, b=b: e.tensor_tensor(out=acc[b][:], in0=acc[b][:], in1=gF[:, 1, :], op=ALU.mult), [r_acc[b], r_gF], [r_acc[b]])
                V(lambda e, b=b: e.scalar_tensor_tensor(out=acc[b][:], in0=x1t[b][:], scalar=float(ALPHA), in1=acc[b][:], op0=ALU.mult, op1=ALU.add),
                  [r_x1t[b], r_acc[b]], [r_acc[b]])
                for hf in range(2):
                    V(lambda e, hf=hf, b=b: e.bn_stats(out=st[:, hf, :], in_=acc[b][:, hf * 512:(hf + 1) * 512]), [r_acc[b]], [r_st])
                V(lambda e: e.bn_aggr(out=mv[:], in_=st[:].rearrange("p a b -> p (a b)")), [r_st], [r_mv])
                A(lambda e: e.activation(out=rstd[:], in_=mv[:, 1:2], func=AF.Sqrt, bias=LN_EPS, scale=1.0), [r_mv], [r_rstd])
                V(lambda e: e.reciprocal(out=rstd[:], in_=rstd[:]), [r_rstd], [r_rstd])
                V(lambda e, b=b: e.tensor_scalar(out=acc[b][:], in0=acc[b][:], scalar1=mv[:, 0:1], scalar2=rstd[:, 0:1], op0=ALU.subtract, op1=ALU.mult),
                  [r_acc[b], r_mv, r_rstd], [r_acc[b]])
                G(lambda e, b=b: e.tensor_tensor(out=acc[b][:], in0=acc[b][:], in1=lnp2[:, 0, :], op=ALU.mult), [r_acc[b], r_lnp2], [r_acc[b]])
                V(lambda e, b=b: e.tensor_tensor(out=x1t[b][:], in0=acc[b][:], in1=lnp2[:, 1, :], op=ALU.add), [r_acc[b], r_lnp2, r_x1t[b]], [r_x1t[b]])
                cx.dma("sp", dst[t * 128:(t + 1) * 128, :], x1t[b][:], reads=[r_x1t[b]], writes=[r_dst])
            cx.barrier()
        if STOP_AFTER == "F":
            break

    cx.finish()
    es.close()
    return nc


def prep_shared(inp):
    w_in = np.asarray(inp["w_in"], np.float32)
    o = IN_OFF
    w_tok = np.concatenate([w_in[:, :, o["pool"]:o["pool"] + 256], w_in[:, :, o["v"]:o["v"] + 256],
                            w_in[:, :, o["o"]:o["o"] + 256]], axis=2)
    kr = w_in[:, :, o["kr"]:o["kr"] + 32]
    kr_sw = np.concatenate([kr[:, :, 16:32], kr[:, :, 0:16]], axis=2)
    misc = np.concatenate([w_in[:, :, o["gate"]:o["gate"] + 16], kr, kr_sw, np.zeros((L, D, 48), np.float32)], axis=2)
    w_fm = np.concatenate([w_in[:, :, o["q"]:o["q"] + 256], w_in[:, :, o["k"]:o["k"] + 256],
                           w_in[:, :, o["dq"]:o["dq"] + 256], w_in[:, :, o["dkv"]:o["dkv"] + 128], misc], axis=2)
    b_ada = np.asarray(inp["b_ada"], np.float32)
    bp = np.stack([b_ada[:, v * D:(v + 1) * D].reshape(L, 8, 128).transpose(0, 2, 1) for v in (0, 1, 3, 4)], axis=2)
    band = np.zeros((4, 5, 128, 128), np.float32)
    for g, w in enumerate((2, 4, 8, 16)):
        A_ = np.zeros((S, S), np.float32) if False else None
        def arow(t):
            lo = max(t - w // 2, 0); hi = min(t + w // 2, S)
            return lo, hi, 1.0 / (hi - lo)
        def fill(mat, ti, tj):
            for tl in range(128):
                t = ti * 128 + tl
                lo, hi, inv = arow(t)
                for tp in range(max(lo, tj * 128), min(hi, tj * 128 + 128)):
                    mat[tp - tj * 128, tl] += inv
                if tj * 128 <= t < tj * 128 + 128:
                    mat[t - tj * 128, tl] -= 1.0
        fill(band[g, 0], 5, 4)
        fill(band[g, 1], 5, 5)
        fill(band[g, 2], 5, 6)
        fill(band[g, 3], 0, 0)
        fill(band[g, 4], NT - 1, NT - 1)
    conv_w = np.asarray(inp["conv_w"], np.float32)
    conv_b = np.asarray(inp["conv_b"], np.float32)
    conv_p = np.concatenate([conv_w.transpose(0, 2, 1), conv_b[:, :, None]], axis=2)
    conv_p = conv_p.reshape(L, 4, 128, 6).transpose(0, 2, 1, 3)
    gate_b4 = np.asarray(inp["gate_b"], np.float32).reshape(L, 4, 4).transpose(0, 2, 1)
    sel2 = np.zeros((4, 2, 128), np.float32)
    for j in range(2):
        sel2[2 * j, j, 0:64] = 1.0
        sel2[2 * j + 1, j, 64:128] = 1.0
    masks = np.zeros((2, 128, 128), np.float32)
    masks[0] = np.triu(np.ones((128, 128), np.float32))
    masks[1] = np.tril(np.ones((128, 128), np.float32))
    gb = np.asarray(inp["gate_b"], np.float32)
    gbp = np.zeros((L, 128, 4), np.float32)
    for h in range(4):
        for k in range(4):
            gbp[:, h * 32:(h + 1) * 32, k] = gb[:, k * 4 + h][:, None]
    trih = np.zeros((2, 128, 128), np.float32)
    cmask = np.zeros((128, 64), np.float32)
    psel = np.zeros((128, 128), np.float32)
    for h in range(4):
        for c in range(32):
            p = h * 32 + c
            trih[0, h * 32:h * 32 + c, p] = 1.0
            trih[1, h * 32 + c + 1:(h + 1) * 32, p] = 1.0
            cmask[p, (h // 2) * 32 + c] = 1.0
            psel[p, (h % 2) * 64:(h % 2) * 64 + 64] = 1.0
    inv_freq = (np.float32(10000.0) ** (-np.arange(0, 32, 2, dtype=np.float32) / np.float32(32))).astype(np.float32)
    ropec = np.zeros((32, 2), np.float32)
    ropec[:, 0] = np.concatenate([inv_freq, inv_freq])
    ropec[:16, 1] = -1.0
    ropec[16:, 1] = 1.0
    w_uq = np.asarray(inp["w_uq"], np.float32)
    w_uq_sw = w_uq.copy().reshape(L, 256, 8, 96)
    w_uq_sw[:, :, :, 64:80] = w_uq.reshape(L, 256, 8, 96)[:, :, :, 80:96]
    w_uq_sw[:, :, :, 80:96] = w_uq.reshape(L, 256, 8, 96)[:, :, :, 64:80]
    w_uq_sw = w_uq_sw.reshape(L, 256, 768)
    sel64 = np.zeros((65, 64), np.float32)
    sel64[64, :] = 1.0
    lnp = np.stack([np.asarray(inp[k], np.float32) for k in ("ln1_g", "ln1_b", "ln2_g", "ln2_b")], axis=1)
    ws13 = np.concatenate([np.asarray(inp["ws1"], np.float32), np.asarray(inp["ws3"], np.float32)], axis=2)
    blkpos = np.tile((np.arange(512, dtype=np.float32) * 128.0)[None, :], (128, 1))
    sh = {
        "w_out": np.ascontiguousarray(inp["w_out"], np.float32),
        "lnp": np.ascontiguousarray(lnp),
        "w_router": np.ascontiguousarray(inp["w_router"], np.float32),
        "e_bias": np.ascontiguousarray(inp["e_bias"], np.float32),
        "ws13": np.ascontiguousarray(ws13),
        "ws2": np.ascontiguousarray(inp["ws2"], np.float32),
        "w1": np.asarray(inp["w1"], np.float32).reshape(L * 256 * 128, 2048),
        "w3": np.asarray(inp["w3"], np.float32).reshape(L * 256 * 128, 2048),
        "w2": np.asarray(inp["w2"], np.float32).reshape(L * 256 * 128, 2048),
        "ustrict": np.triu(np.ones((128, 128), np.float32), 1),
        "blkpos": blkpos,
        "pidx": np.arange(128, dtype=np.float32).reshape(128, 1),
        "ropec": ropec,
        "g_q_p": np.ascontiguousarray(np.asarray(inp["g_q"], np.float32).reshape(L, 2, 128).transpose(0, 2, 1)),
        "g_kv_p": np.ascontiguousarray(np.asarray(inp["g_kv"], np.float32).reshape(L, 128, 1)),
        "w_uq": np.ascontiguousarray(w_uq), "w_uq_sw": np.ascontiguousarray(w_uq_sw),
        "w_uk": np.ascontiguousarray(inp["w_uk"], np.float32), "w_uv": np.ascontiguousarray(inp["w_uv"], np.float32),
        "sel64": sel64,
        "gbp": gbp, "trih": trih, "cmask": cmask, "psel": psel,
        "band": band,
        "w_pool": np.ascontiguousarray(inp["w_pool"], np.float32),
        "s_pool_p": np.ascontiguousarray(np.asarray(inp["s_pool"], np.float32).reshape(L, 4, 64).transpose(0, 2, 1)),
        "conv_p": np.ascontiguousarray(conv_p),
        "gate_b4": np.ascontiguousarray(gate_b4),
        "gn_w": np.ascontiguousarray(inp["gn_w"], np.float32),
        "sel2": sel2,
        "masks": masks,
        "ident": np.eye(128, dtype=np.float32),
        "w_ada": np.ascontiguousarray(inp["w_ada"], np.float32),
        "b_ada_p": np.ascontiguousarray(bp),
        "b_ada": np.ascontiguousarray(b_ada),
        "w_in_tok": np.ascontiguousarray(w_tok),
        "w_in_fm": np.ascontiguousarray(w_fm),
    }
    return sh


def prep_core(inp, b):
    x = np.asarray(inp["x"][b], np.float32)
    c = np.asarray(inp["c"][b], np.float32)
    return {"x": np.ascontiguousarray(x), "c_p": np.ascontiguousarray(c.reshape(8, 128).T),
            "pos": np.ascontiguousarray(np.asarray(inp["positions"][b], np.int32))}


def kernel(**inp):
    nc = build_program()
    sh = prep_shared(inp)
    in_maps = []
    for b in range(8):
        m = dict(sh)
        m.update(prep_core(inp, b))
        in_maps.append(m)
    res = run_bass_kernel_spmd(nc, in_maps, core_ids=list(range(8)))
    kernel.last = res
    return np.stack([r["y"] for r in res.results], axis=0)
```

```python
from contextlib import ExitStack
import numpy as np
import concourse.bass as bass
import concourse.mybir as mybir
from concourse.bass_utils import run_bass_kernel_spmd

F32 = mybir.dt.float32
F32R = mybir.dt.float32r
BF16 = mybir.dt.bfloat16
I32 = mybir.dt.int32
U32 = mybir.dt.uint32
AF = mybir.ActivationFunctionType
ALU = mybir.AluOpType
AX = mybir.AxisListType

S = 4096
D = 1024
NT = S // 128
L = 2
ALPHA = (2 * L) ** 0.25
LN_EPS = 1e-5
RMS_EPS = 1e-6

DEBUG = {}
STOP_AFTER = None
OOB_IDX = 1048576.0
NBLK = 512


class R:
    __slots__ = ("w", "r", "name", "psum")

    def __init__(self, name="", psum=False):
        self.w = {}
        self.r = {}
        self.name = name
        self.psum = psum


class EngState:
    EPOCH = 16000

    def __init__(self, ctx, name, handle):
        self.ctx = ctx
        self.name = name
        self.h = handle
        self.sem = None
        self.count = 0
        self.own = set()
        self.seen = {}
        self.nsem = 0
        self.slots = []
        self.rr = 0

    def tick(self):
        if self.sem is None or self.count >= self.EPOCH:
            self.sem = self.ctx.new_sem(f"e_{self.name}_{self.nsem}")
            self.nsem += 1
            self.count = 0
            self.own.add(self.sem)
        self.count += 1
        return self.sem, self.count


class Ctx:
    def __init__(self, nc, es):
        self.nc = nc
        self.es = es
        self.nsems = 0
        self.E = {
            "pe": EngState(self, "pe", nc.tensor),
            "act": EngState(self, "act", nc.scalar),
            "dve": EngState(self, "dve", nc.vector),
            "pool": EngState(self, "pool", nc.gpsimd),
            "sp": EngState(self, "sp", nc.sync),
        }
        for q, n in (("sp", 40), ("pool", 40), ("act", 8)):
            self.E[q].slots = [[self.new_sem(f"d_{q}_{i}"), 0] for i in range(n)]

    def new_sem(self, name):
        self.nsems += 1
        return self.es.enter_context(self.nc.semaphore(name))

    def _waits(self, E, reads, writes, skip_own=False):
        need = {}
        for t in reads:
            for s, v in t.w.items():
                if need.get(s, 0) < v:
                    need[s] = v
            if t.psum:
                for s, v in t.r.items():
                    if s not in E.own and need.get(s, 0) < v:
                        need[s] = v
        for t in writes:
            for s, v in t.w.items():
                if need.get(s, 0) < v:
                    need[s] = v
            for s, v in t.r.items():
                if need.get(s, 0) < v:
                    need[s] = v
        for s, v in need.items():
            if skip_own and s in E.own:
                continue
            if E.seen.get(s, 0) >= v:
                continue
            E.h.wait_ge(s, v)
            E.seen[s] = v

    def op(self, eng, fn, reads=(), writes=()):
        E = self.E[eng]
        self._waits(E, reads, writes, skip_own=(eng == "pe"))
        ins = fn(E.h)
        s, v = E.tick()
        ins.then_inc(s, 1)
        for t in writes:
            t.w = {s: v}
            t.r = {}
        for t in reads:
            if t.r.get(s, 0) < v:
                t.r[s] = v
        return ins

    def dma(self, q, out, in_, reads=(), writes=(), fn=None, acc=True):
        E = self.E[q]
        if acc:
            for t in writes:
                if t.r:
                    self._waits(E, (), [t])
                    t.w = {}
                    t.r = {}
            self._waits(E, reads, ())
        else:
            self._waits(E, reads, writes)
        slot = E.slots[E.rr % len(E.slots)]
        E.rr += 1
        s = slot[0]
        if slot[1] > 0 and E.seen.get(s, 0) < 16 * slot[1]:
            E.h.wait_ge(s, 16 * slot[1])
            E.seen[s] = 16 * slot[1]
        if fn is None:
            ins = E.h.dma_start(out=out, in_=in_)
        else:
            ins = fn(E.h)
        slot[1] += 1
        v = 16 * slot[1]
        ins.then_inc(s, 16)
        for t in writes:
            if acc:
                t.w[s] = v
            else:
                t.w = {s: v}
                t.r = {}
        for t in reads:
            if t.r.get(s, 0) < v:
                t.r[s] = v
        return ins

    def barrier(self):
        marks = []
        for e in self.E.values():
            if e.sem is not None and e.count > 0:
                marks.append((e.sem, e.count))
            for s, n in e.slots:
                if n > 0:
                    marks.append((s, 16 * n))
        for E in self.E.values():
            for s, v in marks:
                if s in E.own and E.name == "pe":
                    pass
                if E.seen.get(s, 0) < v:
                    E.h.wait_ge(s, v)
                    E.seen[s] = v

    def finish(self, extra=()):
        E = self.E["sp"]
        for e in self.E.values():
            if e.sem is not None and e.count > 0 and E.seen.get(e.sem, 0) < e.count:
                E.h.wait_ge(e.sem, e.count)
            for s, n in e.slots:
                if n > 0 and E.seen.get(s, 0) < 16 * n:
                    E.h.wait_ge(s, 16 * n)


def r32(ap):
    return ap.bitcast(F32R)


IN_OFF = dict(pool=0, q=256, k=512, v=768, o=1024, gate=1280, dq=1296, dkv=1552, kr=1680)


def build_program():
    nc = bass.Bass("TRN2", target_bir_lowering=False)
    es = ExitStack()
    cx = Ctx(nc, es)

    def din(name, shape, dt=F32):
        return nc.dram_tensor(name, list(shape), dt, kind="ExternalInput").ap()

    def dscr(name, shape, dt=F32):
        kind = "ExternalOutput" if DEBUG.get(name) else "Internal"
        return nc.dram_tensor(name, list(shape), dt, kind=kind).ap()

    def sb(name, shape, dt=F32):
        return es.enter_context(nc.sbuf_tensor(name, list(shape), dt))

    def ps(name, shape, dt=F32):
        return es.enter_context(nc.psum_tensor(name, list(shape), dt))

    x_in = din("x", [S, D])
    c_in = din("c_p", [128, 8])
    ident_in = din("ident", [128, 128])
    w_ada = din("w_ada", [L, D, 6 * D])
    b_ada_p = din("b_ada_p", [L, 128, 4, 8])
    b_ada = din("b_ada", [L, 6 * D])
    w_in_tok = din("w_in_tok", [L, D, 768])
    w_in_fm = din("w_in_fm", [L, D, 1024])
    band_in = din("band", [4, 5, 128, 128])
    w_pool_in = din("w_pool", [L, 4, 64, 64])
    s_pool_p = din("s_pool_p", [L, 64, 4])
    conv_p = din("conv_p", [L, 128, 4, 6])
    gate_b_in = din("gate_b4", [L, 4, 4])
    gn_w_in = din("gn_w", [L, 256])
    sel2_in = din("sel2", [4, 2, 128])
    mask_in = din("masks", [2, 128, 128])
    gbp_in = din("gbp", [L, 128, 4])
    pos_in = din("pos", [S], I32)
    ropec_in = din("ropec", [32, 2])
    g_q_p = din("g_q_p", [L, 128, 2])
    g_kv_p = din("g_kv_p", [L, 128, 1])
    w_uq_in = din("w_uq", [L, 256, 768])
    w_uq_sw_in = din("w_uq_sw", [L, 256, 768])
    w_uk_in = din("w_uk", [L, 128, 512])
    w_uv_in = din("w_uv", [L, 128, 512])
    sel64_in = din("sel64", [65, 64])
    w_out_in = din("w_out", [L, D, D])
    lnp_in = din("lnp", [L, 4, D])
    w_router_in = din("w_router", [L, D, 256])
    e_bias_in = din("e_bias", [L, 256])
    ws13_in = din("ws13", [L, D, 512])
    ws2_in = din("ws2", [L, 256, D])
    w1_in = din("w1", [L * 256 * 128, 2048])
    w3_in = din("w3", [L * 256 * 128, 2048])
    w2_in = din("w2", [L * 256 * 128, 2048])
    ustrict_in = din("ustrict", [128, 128])
    blkpos_in = din("blkpos", [128, 512])
    pidx_in = din("pidx", [128, 1])
    trih_in = din("trih", [2, 128, 128])
    cmask_in = din("cmask", [128, 64])
    psel_in = din("psel", [128, 128])
    y_out = nc.dram_tensor("y", [S, D], F32, kind="ExternalOutput").ap()

    U_tok = dscr("U_tok", [S, 768])
    U_fm = dscr("U_fm", [1024, S])
    mixT = dscr("mixT", [1024, S])
    ropeT = dscr("ropeT", [2, 32, S])
    X1 = dscr("X1", [S, D])
    XL = dscr("XL", [S, D])
    H2 = dscr("H2", [S, D], BF16)
    MS = dscr("MS", [S, 256], BF16)
    WN = dscr("WN", [S, 256])
    FFN = dscr("FFN", [S, D])
    NSLOT = 512 * 128
    Xs = dscr("Xs", [NSLOT, D], BF16)
    Ys = dscr("Ys", [NSLOT, D], BF16)

    bc_reg = nc.gpsimd.alloc_register("bc_reg")
    nc.gpsimd.reg_mov(bc_reg, L * 256 * 128 - 1)
    ident = sb("ident_sb", [128, 128])
    r_ident = R("ident")
    cx.dma("sp", ident[:], ident_in[:, :], writes=[r_ident])
    identb = sb("identb_g", [128, 128], BF16)
    r_identb = R("identb")
    cx.op("dve", lambda e: e.tensor_copy(out=identb[:], in_=ident[:]), [r_ident], [r_identb])
    cact = sb("cact", [128, 8])
    cact_bc = sb("cact_bc", [128, 8, 128])
    r_cact = R("cact")
    craw = sb("craw", [128, 8])
    r_craw = R()
    cx.dma("sp", craw[:], c_in[:, :], writes=[r_craw])
    cx.op("act", lambda e: e.activation(out=cact[:], in_=craw[:], func=AF.Silu), reads=[r_craw], writes=[r_cact])
    r_cbc = R()
    for kc in range(8):
        cx.op("dve", lambda e, kc=kc: e.tensor_copy(out=cact_bc[:, kc, :], in_=cact[:, kc:kc + 1].to_broadcast([128, 128])),
              reads=[r_cact], writes=[r_cbc])

    def V(fn, r=(), w=()):
        return cx.op("dve", fn, r, w)

    def A(fn, r=(), w=()):
        return cx.op("act", fn, r, w)

    def P(fn, r=(), w=()):
        return cx.op("pe", fn, r, w)

    def G(fn, r=(), w=()):
        return cx.op("pool", fn, r, w)

    r_ropeT = R()
    with ExitStack() as ph:
        def sbp(name, shape, dt=F32):
            return ph.enter_context(nc.sbuf_tensor(f"rp_{name}", list(shape), dt))
        posi = sbp("posi", [32, S], I32)
        ang = sbp("ang", [32, S])
        kf = sbp("kf", [32, S])
        ki = sbp("ki", [32, S], I32)
        rr_ = sbp("rr", [32, S])
        mm = sbp("mm", [32, S])
        ropec = sbp("ropec", [32, 2])
        r_rp = R()
        cx.dma("sp", posi[:], pos_in.partition_broadcast(32), writes=[r_rp])
        cx.dma("sp", ropec[:], ropec_in[:, :], writes=[r_rp])
        V(lambda e: e.tensor_copy(out=ang[:], in_=posi[:]), [r_rp], [r_rp])
        V(lambda e: e.tensor_scalar(out=ang[:], in0=ang[:], scalar1=ropec[:, 0:1], scalar2=None, op0=ALU.mult), [r_rp], [r_rp])
        TWO_PI = 2.0 * np.pi
        C1 = 6.28125
        C2 = TWO_PI - C1
        PI_LO = 3.1415925
        for tb in range(2):
            src = ang
            if tb == 1:
                V(lambda e: e.tensor_scalar(out=mm[:], in0=ang[:], scalar1=float(np.pi / 2), scalar2=None, op0=ALU.add), [r_rp], [r_rp])
                src = mm
            V(lambda e, src=src: e.tensor_scalar(out=kf[:], in0=src[:], scalar1=float(1.0 / TWO_PI), scalar2=None, op0=ALU.mult), [r_rp], [r_rp])
            V(lambda e: e.tensor_copy(out=ki[:], in_=kf[:]), [r_rp], [r_rp])
            V(lambda e: e.tensor_copy(out=kf[:], in_=ki[:]), [r_rp], [r_rp])
            V(lambda e, src=src: e.scalar_tensor_tensor(out=rr_[:], in0=kf[:], scalar=-C1, in1=src[:], op0=ALU.mult, op1=ALU.add), [r_rp], [r_rp])
            V(lambda e: e.scalar_tensor_tensor(out=rr_[:], in0=kf[:], scalar=-C2, in1=rr_[:], op0=ALU.mult, op1=ALU.add), [r_rp], [r_rp])
            V(lambda e: e.tensor_scalar(out=kf[:], in0=rr_[:], scalar1=float(np.pi), scalar2=None, op0=ALU.is_gt), [r_rp], [r_rp])
            V(lambda e: e.scalar_tensor_tensor(out=rr_[:], in0=kf[:], scalar=-TWO_PI, in1=rr_[:], op0=ALU.mult, op1=ALU.add), [r_rp], [r_rp])
            V(lambda e: e.tensor_scalar(out=kf[:], in0=rr_[:], scalar1=float(-np.pi), scalar2=None, op0=ALU.is_lt), [r_rp], [r_rp])
            V(lambda e: e.scalar_tensor_tensor(out=rr_[:], in0=kf[:], scalar=TWO_PI, in1=rr_[:], op0=ALU.mult, op1=ALU.add), [r_rp], [r_rp])
            V(lambda e: e.tensor_scalar(out=rr_[:], in0=rr_[:], scalar1=PI_LO, scalar2=-PI_LO, op0=ALU.min, op1=ALU.max), [r_rp], [r_rp])
            A(lambda e: e.activation(out=rr_[:], in_=rr_[:], func=AF.Sin), [r_rp], [r_rp])
            if tb == 0:
                V(lambda e: e.tensor_scalar(out=rr_[:], in0=rr_[:], scalar1=ropec[:, 1:2], scalar2=None, op0=ALU.mult), [r_rp], [r_rp])
            cx.dma("sp", ropeT[tb], rr_[:], reads=[r_rp], writes=[r_ropeT])
        cx.barrier()

    adaP = sb("adaP", [128, 4, 8])
    gF = sb("gF", [128, 4, D])
    r_adaP = R()
    r_gF = R()
    for l in range(L):
        with ExitStack() as ph:
            def sbp(name, shape, dt=F32):
                return ph.enter_context(nc.sbuf_tensor(f"{name}_{l}", list(shape), dt))

            def psp(name, shape, dt=F32):
                return ph.enter_context(nc.psum_tensor(f"{name}_{l}", list(shape), dt))
            wa = [sbp(f"wa{i}", [128, 8, D]) for i in range(2)]
            r_wa = [R(), R()]
            badap = sbp("badap", [128, 4, 8])
            r_badap = R()
            cx.dma("sp", badap[:], b_ada_p[l], writes=[r_badap])
            bfr = sbp("bfr", [128, 4, D])
            r_bfr = R()
            GIDX = {2: 0, 5: 1, 3: 2, 4: 3}
            PIDX = {0: 0, 1: 1, 3: 2, 4: 3}
            for v, j in GIDX.items():
                cx.dma("sp", bfr[:, j, :], b_ada[l, v * D:(v + 1) * D].partition_broadcast(128), writes=[r_bfr])
            pA = psp("pA", [128, 512])
            r_pA = R(psum=True)
            pG = [psp(f"pG{i}", [128, 512]) for i in range(2)]
            r_pG = [R(psum=True), R(psum=True)]
            order = [0, 1, 3, 4, 2, 5]
            for i, v in enumerate(order):
                cx.dma("sp", wa[i % 2][:], w_ada[l, :, v * D:(v + 1) * D].rearrange("(kc p) n -> p kc n", p=128),
                       writes=[r_wa[i % 2]])
                w = wa[i % 2]
                if v in PIDX:
                    pi = PIDX[v]
                    for ncn in range(8):
                        for kc in range(8):
                            cx.op("pe", lambda e, w=w, ncn=ncn, kc=kc, pi=pi: e.matmul(
                                pA[:, pi * 8 + ncn:pi * 8 + ncn + 1], lhsT=w[:, kc, ncn * 128:(ncn + 1) * 128],
                                rhs=cact[:, kc:kc + 1], start=(kc == 0), stop=(kc == 7)),
                                reads=[r_wa[i % 2], r_cact], writes=[r_pA])
                if v in GIDX:
                    j = GIDX[v]
                    for hf in range(2):
                        for kc in range(8):
                            cx.op("pe", lambda e, w=w, hf=hf, kc=kc: e.matmul(
                                pG[hf][:, :], lhsT=cact_bc[:, kc, :], rhs=w[:, kc, hf * 512:(hf + 1) * 512],
                                start=(kc == 0), stop=(kc == 7)),
                                reads=[r_wa[i % 2], r_cbc], writes=[r_pG[hf]])
                        cx.op("dve", lambda e, hf=hf, j=j: e.tensor_tensor(
                            out=gF[:, j, hf * 512:(hf + 1) * 512], in0=pG[hf][:, :], in1=bfr[:, j, hf * 512:(hf + 1) * 512], op=ALU.add),
                            reads=[r_pG[hf], r_bfr], writes=[r_gF])
            cx.op("dve", lambda e: e.tensor_tensor(out=adaP[:].rearrange("p a b -> p (a b)"), in0=pA[:, 0:32],
                                                   in1=badap[:].rearrange("p a b -> p (a b)"), op=ALU.add),
                  reads=[r_pA, r_badap], writes=[r_adaP])
            for v in (1, 3):
                cx.op("dve", lambda e, v=v: e.tensor_scalar_add(out=adaP[:, v, :], in0=adaP[:, v, :], scalar1=1.0),
                      reads=[r_adaP], writes=[r_adaP])
            cx.op("dve", lambda e: e.tensor_scalar_add(out=gF[:, 3, :], in0=gF[:, 3, :], scalar1=1.0), reads=[r_gF], writes=[r_gF])
            cx.barrier()

        with ExitStack() as ph:
            def sbp(name, shape, dt=F32):
                return ph.enter_context(nc.sbuf_tensor(f"{name}_{l}", list(shape), dt))

            def psp(name, shape, dt=F32):
                return ph.enter_context(nc.psum_tensor(f"{name}_{l}", list(shape), dt))
            wtok = sbp("wtok", [128, 8, 768], BF16)
            wfm = sbp("wfm", [128, 8, 1024], BF16)
            r_wtok, r_wfm = R(), R()
            for kc in range(8):
                cx.dma("pool", wtok[:, kc, :], w_in_tok[l, kc * 128:(kc + 1) * 128, :], writes=[r_wtok])
                cx.dma("pool", wfm[:, kc, :], w_in_fm[l, kc * 128:(kc + 1) * 128, :], writes=[r_wfm])
            xt = [sbp(f"xt{i}", [128, D]) for i in range(2)]
            r_xt = [R(), R()]
            xn = [sbp(f"xn{i}", [128, D]) for i in range(2)]
            r_xn = [R(), R()]
            st = sbp("st", [128, 2, 6])
            mv = sbp("mv", [128, 2])
            rstd = sbp("rstd", [128, 1])
            r_st, r_mv, r_rstd = R(), R(), R()
            hT = [sbp(f"hT{i}", [128, 8, 512], BF16) for i in range(2)]
            r_hT = [R(), R()]
            pT = [psp(f"pT{i}", [128, 512]) for i in range(2)]
            r_pT = [R(psum=True), R(psum=True)]
            pU = [psp(f"pU{i}", [128, 512]) for i in range(4)]
            r_pU = [R(psum=True) for _ in range(4)]
            uo = [sbp(f"uo{i}", [128, 512]) for i in range(4)]
            r_uo = [R() for _ in range(4)]
            r_Utok, r_Ufm = R(), R()
            src = x_in if l == 0 else XL
            npu = 0
            for g in range(8):
                hTg = hT[g % 2]
                r_hTg = r_hT[g % 2]
                for tt in range(4):
                    t = g * 4 + tt
                    b = t % 2
                    cx.dma("sp", xt[b][:], src[t * 128:(t + 1) * 128, :], writes=[r_xt[b]])
                    for hf in range(2):
                        cx.op("dve", lambda e, b=b, hf=hf: e.bn_stats(out=st[:, hf, :], in_=xt[b][:, hf * 512:(hf + 1) * 512]),
                              reads=[r_xt[b]], writes=[r_st])
                    cx.op("dve", lambda e: e.bn_aggr(out=mv[:], in_=st[:].rearrange("p a b -> p (a b)")), reads=[r_st], writes=[r_mv])
                    cx.op("act", lambda e: e.activation(out=rstd[:], in_=mv[:, 1:2], func=AF.Sqrt, bias=LN_EPS, scale=1.0),
                          reads=[r_mv], writes=[r_rstd])
                    cx.op("dve", lambda e: e.reciprocal(out=rstd[:], in_=rstd[:]), reads=[r_rstd], writes=[r_rstd])
                    cx.op("dve", lambda e, b=b: e.tensor_scalar(out=xn[b][:], in0=xt[b][:], scalar1=mv[:, 0:1], scalar2=rstd[:, 0:1],
                                                                 op0=ALU.subtract, op1=ALU.mult),
                          reads=[r_xt[b], r_mv, r_rstd], writes=[r_xn[b]])
                    for q4 in range(2):
                        pt = pT[q4]
                        for k4 in range(4):
                            kc = q4 * 4 + k4
                            cx.op("pe", lambda e, pt=pt, k4=k4, kc=kc, b=b: e.transpose(
                                out=pt[:, k4 * 128:(k4 + 1) * 128], in_=xn[b][:, kc * 128:(kc + 1) * 128], identity=ident[:]),
                                reads=[r_xn[b], r_ident], writes=[r_pT[q4]])
                        for k4 in range(4):
                            kc = q4 * 4 + k4
                            cx.op("act", lambda e, pt=pt, k4=k4, kc=kc, tt=tt, hTg=hTg: e.activation(
                                out=hTg[:, kc, tt * 128:(tt + 1) * 128], in_=pt[:, k4 * 128:(k4 + 1) * 128], func=AF.Identity,
                                bias=adaP[:, 0, kc:kc + 1], scale=adaP[:, 1, kc:kc + 1]),
                                reads=[r_pT[q4], r_adaP], writes=[r_hTg])
                for tt in range(4):
                    t = g * 4 + tt
                    for hf in range(2):
                        i = npu % 4
                        npu += 1
                        for kc in range(8):
                            cx.op("pe", lambda e, i=i, kc=kc, tt=tt, hf=hf, hTg=hTg: e.matmul(
                                pU[i][:, 0:384], lhsT=hTg[:, kc, tt * 128:(tt + 1) * 128],
                                rhs=wtok[:, kc, hf * 384:(hf + 1) * 384], start=(kc == 0), stop=(kc == 7)),
                                reads=[r_hTg, r_wtok], writes=[r_pU[i]])
                        cx.op("act" if i % 2 else "dve", lambda e, i=i: (e.copy(out=uo[i][:, 0:384], in_=pU[i][:, 0:384]) if i % 2
                                                                          else e.tensor_copy(out=uo[i][:, 0:384], in_=pU[i][:, 0:384])),
                              reads=[r_pU[i]], writes=[r_uo[i]])
                        cx.dma("pool", U_tok[t * 128:(t + 1) * 128, hf * 384:(hf + 1) * 384], uo[i][:, 0:384],
                               reads=[r_uo[i]], writes=[r_Utok])
                for cb in range(8):
                    i = npu % 4
                    npu += 1
                    for kc in range(8):
                        cx.op("pe", lambda e, i=i, kc=kc, cb=cb, hTg=hTg: e.matmul(
                            pU[i][:, :], lhsT=wfm[:, kc, cb * 128:(cb + 1) * 128], rhs=hTg[:, kc, :],
                            start=(kc == 0), stop=(kc == 7)),
                            reads=[r_hTg, r_wfm], writes=[r_pU[i]])
                    cx.op("act" if i % 2 else "dve", lambda e, i=i: (e.copy(out=uo[i][:, :], in_=pU[i][:, :]) if i % 2
                                                                      else e.tensor_copy(out=uo[i][:, :], in_=pU[i][:, :])),
                          reads=[r_pU[i]], writes=[r_uo[i]])
                    cx.dma("pool", U_fm[cb * 128:(cb + 1) * 128, g * 512:(g + 1) * 512], uo[i][:, :],
                           reads=[r_uo[i]], writes=[r_Ufm])
            cx.barrier()
        if STOP_AFTER == "A":
            break
        r_mixT = R()

        def V(fn, r=(), w=()):
            return cx.op("dve", fn, r, w)

        def A(fn, r=(), w=()):
            return cx.op("act", fn, r, w)

        def P(fn, r=(), w=()):
            return cx.op("pe", fn, r, w)

        def G(fn, r=(), w=()):
            return cx.op("pool", fn, r, w)

        with ExitStack() as ph:
            def sbp(name, shape, dt=F32):
                return ph.enter_context(nc.sbuf_tensor(f"{name}_{l}", list(shape), dt))

            def psp(name, shape, dt=F32):
                return ph.enter_context(nc.psum_tensor(f"{name}_{l}", list(shape), dt))
            band = sbp("band", [128, 20, 128], BF16)
            r_band = R()
            cx.dma("pool", band[:], band_in.rearrange("g k p n -> p (g k) n"), writes=[r_band])
            wpl = sbp("wpl", [64, 4, 64], BF16)
            r_wpl = R()
            cx.dma("pool", wpl[:], w_pool_in[l].rearrange("g c d -> c g d"), writes=[r_wpl])
            spl = sbp("spl", [64, 4])
            r_spl = R()
            cx.dma("sp", spl[:], s_pool_p[l], writes=[r_spl])
            up = sbp("up", [128, NT, 256], BF16)
            r_up = R()
            for t4 in range(0, NT, 8):
                cx.dma("pool", up[:, t4:t4 + 8, :], U_tok[t4 * 128:(t4 + 8) * 128, 0:256].rearrange("(t p) c -> p t c", p=128),
                       reads=[r_Utok], writes=[r_up])
            pd = [psp(f"pd{i}", [64, 512]) for i in range(2)]
            py = [psp(f"py{i}", [64, 512]) for i in range(2)]
            r_pd = [R(psum=True), R(psum=True)]
            r_py = [R(psum=True), R(psum=True)]
            dTs = [sbp(f"dTs{i}", [64, 512], BF16) for i in range(2)]
            r_dTs = [R(), R()]
            yp = [sbp(f"yp{i}", [64, 4, 512]) for i in range(2)]
            r_yp = [R(), R()]
            n = 0
            for Gq in range(8):
                ypq = yp[Gq % 2]
                r_ypq = r_yp[Gq % 2]
                for g in range(4):
                    i = n % 2
                    n += 1
                    for tt in range(4):
                        t = Gq * 4 + tt
                        srcs = []
                        if t > 0:
                            srcs.append((t - 1, 0))
                        srcs.append((t, 3 if t == 0 else (4 if t == NT - 1 else 1)))
                        if t < NT - 1:
                            srcs.append((t + 1, 2))
                        for k, (j, typ) in enumerate(srcs):
                            P(lambda e, i=i, tt=tt, j=j, g=g, typ=typ, k=k, last=len(srcs) - 1: e.matmul(
                                pd[i][:, tt * 128:(tt + 1) * 128], lhsT=up[:, j, g * 64:(g + 1) * 64], rhs=band[:, g * 5 + typ, :],
                                start=(k == 0), stop=(k == last)), [r_up, r_band], [r_pd[i]])
                    A(lambda e, i=i: e.copy(out=dTs[i][:], in_=pd[i][:]), [r_pd[i]], [r_dTs[i]])
                    P(lambda e, i=i, g=g: e.matmul(py[i][:, :], lhsT=wpl[:, g, :], rhs=dTs[i][:], start=True, stop=True),
                      [r_wpl, r_dTs[i]], [r_py[i]])
                    V(lambda e, i=i, g=g, ypq=ypq: e.tensor_scalar(out=ypq[:, g, :], in0=py[i][:], scalar1=spl[:, g:g + 1], scalar2=None,
                                                                 op0=ALU.mult), [r_py[i], r_spl], [r_ypq])
                cx.dma("sp", mixT[0:256, Gq * 512:(Gq + 1) * 512].rearrange("(g c) t -> c g t", c=64), ypq[:],
                       reads=[r_ypq], writes=[r_mixT])
            cx.barrier()
        if STOP_AFTER == "B":
            break
        with ExitStack() as ph:
            def sbp(name, shape, dt=F32):
                return ph.enter_context(nc.sbuf_tensor(f"{name}_{l}", list(shape), dt))
            qkT = sbp("qkT", [128, 4, S], BF16)
            r_qkT = R()
            vx = sbp("vx", [128, NT, 4, 65], BF16)
            r_vx = R()
            hacc = sbp("hacc", [128, NT, 256])
            r_hacc = [R() for _ in range(NT)]
            gTcf = [sbp(f"gTcf{d}", [128, 4, NT]) for d in range(2)]
            gTcl = [sbp(f"gTcl{d}", [128, 4, NT]) for d in range(2)]
            decb = [sbp(f"decb{d}", [128, 2, NT]) for d in range(2)]
            r_gT = [R(), R()]
            maskt = sbp("maskt", [128, 2, 128])
            r_mask = R()
            cx.dma("sp", maskt[:], mask_in.rearrange("d p n -> p d n"), writes=[r_mask])
            with ExitStack() as ph2:
                def sb2(name, shape, dt=F32):
                    return ph2.enter_context(nc.sbuf_tensor(f"{name}_{l}", list(shape), dt))
                convp = sb2("convp", [128, 4, 6])
                r_convp = R()
                cx.dma("sp", convp[:], conv_p[l], writes=[r_convp])
                cin = [sb2(f"cin{i}", [128, S + 4]) for i in range(2)]
                r_cin = [R(), R()]
                acc = sb2("cacc", [128, S])
                r_acc = R()
                for i in range(2):
                    G(lambda e, i=i: e.memset(cin[i][:, 0:2], 0.0), [], [r_cin[i]])
                    G(lambda e, i=i: e.memset(cin[i][:, S + 2:S + 4], 0.0), [], [r_cin[i]])
                for ch in range(4):
                    b = ch % 2
                    cx.dma("sp", cin[b][:, 2:S + 2], U_fm[ch * 128:(ch + 1) * 128, :], reads=[r_Ufm], writes=[r_cin[b]])
                    V(lambda e, b=b, ch=ch: e.tensor_scalar(out=acc[:], in0=cin[b][:, 0:S], scalar1=convp[:, ch, 0:1], scalar2=convp[:, ch, 5:6],
                                                           op0=ALU.mult, op1=ALU.add), [r_cin[b], r_convp], [r_acc])
                    for j in range(1, 5):
                        V(lambda e, b=b, ch=ch, j=j: e.scalar_tensor_tensor(out=acc[:], in0=cin[b][:, j:j + S], scalar=convp[:, ch, j:j + 1],
                                                                          in1=acc[:], op0=ALU.mult, op1=ALU.add), [r_cin[b], r_convp, r_acc], [r_acc])
                    A(lambda e, ch=ch: e.activation(out=qkT[:, ch, :], in_=acc[:], func=AF.Silu), [r_acc], [r_qkT])
                vtmp = [sb2(f"vtmp{i}", [128, 8, 256]) for i in range(2)]
                r_vtmp = [R(), R()]
                G(lambda e: e.memset(vx[:, :, :, 64:65], 1.0), [], [r_vx])
                for i4 in range(4):
                    b = i4 % 2
                    cx.dma("sp", vtmp[b][:], U_tok[i4 * 1024:(i4 + 1) * 1024, 256:512].rearrange("(t p) c -> p t c", p=128),
                           reads=[r_Utok], writes=[r_vtmp[b]])
                    A(lambda e, b=b, i4=i4: e.copy(out=vx[:, i4 * 8:(i4 + 1) * 8, :, 0:64], in_=vtmp[b][:].rearrange("p t (h c) -> p t h c", c=64)),
                      [r_vtmp[b]], [r_vx])
                cx.barrier()
            with ExitStack() as ph2:
                def sb2(name, shape, dt=F32):
                    return ph2.enter_context(nc.sbuf_tensor(f"{name}_{l}", list(shape), dt))

                def ps2(name, shape, dt=F32):
                    return ph2.enter_context(nc.psum_tensor(f"{name}_{l}", list(shape), dt))
                gbp = sb2("gbp", [128, 4])
                ngb = sb2("ngb", [128, 4])
                r_gbp = R()
                cx.dma("sp", gbp[:], gbp_in[l], writes=[r_gbp])
                V(lambda e: e.tensor_scalar(out=ngb[:], in0=gbp[:], scalar1=-1.0, scalar2=None, op0=ALU.mult), [r_gbp], [r_gbp])
                trih = sb2("trih", [128, 2, 128])
                cmask = sb2("cmask", [128, 64])
                psel = sb2("psel", [128, 128])
                r_cst = R()
                cx.dma("sp", trih[:], trih_in.rearrange("d p n -> p d n"), writes=[r_cst])
                cx.dma("sp", cmask[:], cmask_in[:, :], writes=[r_cst])
                cx.dma("sp", psel[:], psel_in[:, :], writes=[r_cst])
                pg = ps2("pg", [128, 512])
                r_pg = R(psum=True)
                for d in range(2):
                    gi = sb2(f"gi{d}", [128, 128]); gf = sb2(f"gf{d}", [128, 128])
                    r_g = R()
                    ki, kf = 2 * d, 2 * d + 1
                    cx.dma("sp", gi[:], U_fm[896 + ki * 4:896 + ki * 4 + 4, :].rearrange("h (c l) -> (h c) l", l=128), reads=[r_Ufm], writes=[r_g])
                    cx.dma("sp", gf[:], U_fm[896 + kf * 4:896 + kf * 4 + 4, :].rearrange("h (c l) -> (h c) l", l=128), reads=[r_Ufm], writes=[r_g])
                    spt = sb2(f"spt{d}", [128, 128]); Pl = sb2(f"Pl{d}", [128, 128]); Pc = sb2(f"Pc{d}", [128, 128]); at = sb2(f"at{d}", [128, 128])
                    cols = sb2(f"cols{d}", [128, 8])
                    rows = sb2(f"rows{d}", [1, 4, 128])
                    r_w = R()
                    A(lambda e, kf=kf: e.activation(out=spt[:], in_=gf[:], func=AF.Exp, bias=ngb[:, kf:kf + 1], scale=-1.0), [r_g, r_gbp], [r_w])
                    A(lambda e: e.activation(out=spt[:], in_=spt[:], func=AF.Ln, bias=1.0, scale=1.0), [r_w], [r_w])
                    V(lambda e: e.tensor_tensor_scan(out=Pl[:], data0=spt[:], data1=spt[:], initial=0.0, op0=ALU.add, op1=ALU.max), [r_w], [r_w])
                    V(lambda e: e.tensor_copy(out=cols[:, 0:1], in_=Pl[:, 127:128]), [r_w], [r_w])
                    P(lambda e, d=d: e.matmul(pg[:, 0:1], lhsT=trih[:, d, :], rhs=cols[:, 0:1], start=True, stop=True), [r_w, r_cst], [r_pg])
                    V(lambda e: e.tensor_copy(out=cols[:, 1:2], in_=pg[:, 0:1]), [r_pg], [r_w])
                    if d == 0:
                        V(lambda e: e.tensor_scalar(out=Pc[:], in0=Pl[:], scalar1=cols[:, 1:2], scalar2=None, op0=ALU.add), [r_w], [r_w])
                    else:
                        V(lambda e: e.scalar_tensor_tensor(out=Pc[:], in0=Pl[:], scalar=-1.0, in1=spt[:], op0=ALU.mult, op1=ALU.add), [r_w], [r_w])
                        V(lambda e: e.tensor_scalar(out=Pc[:], in0=Pc[:], scalar1=cols[:, 0:1], scalar2=cols[:, 1:2], op0=ALU.add, op1=ALU.add), [r_w], [r_w])
                    V(lambda e, ki=ki: e.scalar_tensor_tensor(out=at[:], in0=gi[:], scalar=gbp[:, ki:ki + 1], in1=Pc[:], op0=ALU.add, op1=ALU.add),
                      [r_w, r_g, r_gbp], [r_w])
                    V(lambda e: e.tensor_reduce(out=cols[:, 2:3], in_=at[:], axis=AX.X, op=ALU.max), [r_w], [r_w])
                    P(lambda e: e.transpose(out=pg[0:1, 0:128], in_=cols[:, 2:3], identity=ident[:]), [r_w, r_ident], [r_pg])
                    V(lambda e: e.tensor_copy(out=rows[:, 0, :], in_=pg[0:1, 0:128]), [r_pg], [r_w])
                    cur = 0
                    for sh in (1, 2, 4, 8, 16):
                        a_ = rows[:, cur, :].rearrange("p (h c) -> p h c", c=32)
                        b_ = rows[:, 1 - cur, :].rearrange("p (h c) -> p h c", c=32)
                        if d == 0:
                            V(lambda e, a_=a_, b_=b_, sh=sh: e.tensor_tensor(out=b_[:, :, sh:], in0=a_[:, :, sh:], in1=a_[:, :, :32 - sh], op=ALU.max), [r_w], [r_w])
                            V(lambda e, a_=a_, b_=b_, sh=sh: e.tensor_copy(out=b_[:, :, :sh], in_=a_[:, :, :sh]), [r_w], [r_w])
                        else:
                            V(lambda e, a_=a_, b_=b_, sh=sh: e.tensor_tensor(out=b_[:, :, :32 - sh], in0=a_[:, :, :32 - sh], in1=a_[:, :, sh:], op=ALU.max), [r_w], [r_w])
                            V(lambda e, a_=a_, b_=b_, sh=sh: e.tensor_copy(out=b_[:, :, 32 - sh:], in_=a_[:, :, 32 - sh:]), [r_w], [r_w])
                        cur = 1 - cur
                    Mr = rows[:, cur, :].rearrange("p (h c) -> p h c", c=32)
                    dd = rows[:, 2, :].rearrange("p (h c) -> p h c", c=32)
                    V(lambda e: e.memset(rows[:, 2, :], 0.0), [r_w], [r_w])
                    if d == 0:
                        V(lambda e, Mr=Mr, dd=dd: e.tensor_tensor(out=dd[:, :, 0:31], in0=Mr[:, :, 0:31], in1=Mr[:, :, 1:32], op=ALU.subtract), [r_w], [r_w])
                    else:
                        V(lambda e, Mr=Mr, dd=dd: e.tensor_tensor(out=dd[:, :, 1:32], in0=Mr[:, :, 1:32], in1=Mr[:, :, 0:31], op=ALU.subtract), [r_w], [r_w])
                    A(lambda e: e.activation(out=rows[:, 3, :], in_=rows[:, 2, :], func=AF.Exp), [r_w], [r_w])
                    P(lambda e, cur=cur: e.transpose(out=pg[:, 0:1], in_=rows[0:1, cur, :], identity=ident[0:1, 0:1]), [r_w, r_ident], [r_pg])
                    P(lambda e: e.transpose(out=pg[:, 1:2], in_=rows[0:1, 3, :], identity=ident[0:1, 0:1]), [r_w, r_ident], [r_pg])
                    V(lambda e: e.tensor_copy(out=cols[:, 3:4], in_=pg[:, 0:1]), [r_pg], [r_w])
                    V(lambda e: e.tensor_copy(out=cols[:, 6:7], in_=pg[:, 1:2]), [r_pg], [r_w])
                    V(lambda e: e.tensor_scalar(out=cols[:, 4:5], in0=cols[:, 3:4], scalar1=-1.0, scalar2=None, op0=ALU.mult), [r_w], [r_w])
                    V(lambda e: e.tensor_scalar(out=cols[:, 5:6], in0=cols[:, 3:4], scalar1=-1.0, scalar2=-float(np.log(8.0)), op0=ALU.mult, op1=ALU.add), [r_w], [r_w])
                    A(lambda e: e.activation(out=at[:], in_=at[:], func=AF.Exp, bias=cols[:, 5:6], scale=1.0), [r_w], [r_w])
                    A(lambda e: e.activation(out=Pc[:], in_=Pc[:], func=AF.Exp, bias=cols[:, 4:5], scale=1.0), [r_w], [r_w])
                    P(lambda e: e.transpose(out=pg[:, 0:128], in_=at[:], identity=ident[:]), [r_w, r_ident], [r_pg])
                    P(lambda e: e.transpose(out=pg[:, 128:256], in_=Pc[:], identity=ident[:]), [r_w, r_ident], [r_pg])
                    V(lambda e, d=d: e.tensor_copy(out=gTcf[d][:].rearrange("p h c -> p (h c)"), in_=pg[:, 0:128]), [r_pg], [r_gT[d]])
                    V(lambda e, d=d: e.tensor_copy(out=gTcl[d][:].rearrange("p h c -> p (h c)"), in_=pg[:, 128:256]), [r_pg], [r_gT[d]])
                    V(lambda e: e.tensor_scalar(out=spt[:, 0:64], in0=cmask[:], scalar1=cols[:, 6:7], scalar2=None, op0=ALU.mult), [r_w, r_cst], [r_w])
                    P(lambda e: e.matmul(pg[:, 256:320], lhsT=psel[:], rhs=spt[:, 0:64], start=True, stop=True), [r_w, r_cst], [r_pg])
                    V(lambda e, d=d: e.tensor_copy(out=decb[d][:].rearrange("p j c -> p (j c)"), in_=pg[:, 256:320]), [r_pg], [r_gT[d]])
                cx.barrier()
            with ExitStack() as ph2:
                def sb2(name, shape, dt=F32):
                    return ph2.enter_context(nc.sbuf_tensor(f"{name}_{l}", list(shape), dt))

                def ps2(name, shape, dt=F32):
                    return ph2.enter_context(nc.psum_tensor(f"{name}_{l}", list(shape), dt))
                pS = [[ps2(f"pS{d}{par}", [128, 4, 128]) for par in range(2)] for d in range(2)]
                pnd = [ps2(f"pnd{d}", [128, 4, 128]) for d in range(2)]
                pdC1 = ps2("pdC", [128, 4, 128])
                pkt1 = ps2("pkt", [128, 1024], BF16)
                pdC = [pdC1, pdC1]
                pkt = [pkt1, pkt1]
                r_pS = [[R(psum=True), R(psum=True)], [R(psum=True), R(psum=True)]]
                r_pnd = [R(psum=True), R(psum=True)]
                r1 = R(psum=True); r2 = R(psum=True)
                r_pdC = [r1, r1]; r_pkt = [r2, r2]
                Sm = [sb2(f"Sm{d}", [128, 4, 128], BF16) for d in range(2)]
                kt = [sb2(f"kt{d}", [128, 4, 64], BF16) for d in range(2)]
                rr = [sb2(f"rr{d}", [128, 4]) for d in range(2)]
                tmph = [sb2(f"tmph{d}", [128, 4, 64]) for d in range(2)]
                Cst = [sb2(f"Cst{d}", [128, 2, 65]) for d in range(2)]
                Cstb = [sb2(f"Cstb{d}", [128, 2, 65], BF16) for d in range(2)]
                tmpC = [sb2(f"tmpC{d}", [128, 2, 65]) for d in range(2)]
                r_Sm = [R(), R()]; r_kt = [R(), R()]; r_rr = [R(), R()]; r_tmph = [R(), R()]
                r_Cst = [R(), R()]; r_Cstb = [R(), R()]; r_tmpC = [R(), R()]
                for d in range(2):
                    G(lambda e, d=d: e.memset(Cst[d][:], 0.0), [], [r_Cst[d]])
                    G(lambda e, d=d: e.memset(Cstb[d][:], 0.0), [], [r_Cstb[d]])
                done = set()
                for step in range(NT):
                    for d in range(2):
                        c = step if d == 0 else NT - 1 - step
                        last = (step == NT - 1)
                        cs = slice(c * 128, (c + 1) * 128)
                        for j in range(2):
                            P(lambda e, d=d, j=j, cs=cs: e.transpose(out=pkt[d][:, j * 128:(j + 1) * 128], in_=qkT[:, 2 + j, cs], identity=identb[:]),
                              [r_qkT, r_identb], [r_pkt[d]])
                        V(lambda e, d=d, c=c: e.tensor_tensor(out=kt[d][:], in0=pkt[d][:, 0:256].rearrange("p (h k) -> p h k", k=64),
                                                            in1=gTcf[d][:, :, c:c + 1].to_broadcast([128, 4, 64]), op=ALU.mult),
                          [r_pkt[d], r_gT[d]], [r_kt[d]])
                        for h in range(4):
                            hp, hj = h % 2, h // 2
                            P(lambda e, d=d, h=h, hp=hp, hj=hj, cs=cs: e.matmul(pS[d][hp][:, hj, :], lhsT=qkT[hp * 64:(hp + 1) * 64, 2 + hj, cs],
                                                                              rhs=qkT[hp * 64:(hp + 1) * 64, hj, cs], start=True, stop=True),
                              [r_qkT], [r_pS[d][hp]])
                        for h in range(4):
                            hp, hj = h % 2, h // 2
                            V(lambda e, d=d, h=h, c=c, hp=hp, hj=hj: e.scalar_tensor_tensor(out=Sm[d][:, h, :], in0=pS[d][hp][:, hj, :], scalar=gTcf[d][:, h, c:c + 1],
                                                                            in1=maskt[:, d, :], op0=ALU.mult, op1=ALU.mult),
                              [r_pS[d][hp], r_gT[d], r_mask], [r_Sm[d]])
                        for h in range(4):
                            hp, hj = h % 2, h // 2
                            P(lambda e, d=d, h=h, c=c: e.matmul(pnd[d][:, h, 0:65], lhsT=Sm[d][:, h, :], rhs=vx[:, c, h, :], start=True, stop=False),
                              [r_Sm[d], r_vx], [r_pnd[d]])
                            P(lambda e, d=d, h=h, hp=hp, hj=hj, cs=cs: e.matmul(pnd[d][:, h, 0:65], lhsT=qkT[hp * 64:(hp + 1) * 64, hj, cs],
                                                                              rhs=Cstb[d][hp * 64:(hp + 1) * 64, hj, :], start=False, stop=True),
                              [r_qkT, r_Cstb[d]], [r_pnd[d]])
                        A(lambda e, d=d: e.activation(out=rr[d][:].unsqueeze(2), in_=pnd[d][:, :, 64:65], func=AF.Abs),
                          [r_pnd[d]], [r_rr[d]])
                        V(lambda e, d=d, c=c: e.tensor_tensor(out=rr[d][:], in0=rr[d][:], in1=gTcl[d][:, :, c], op=ALU.max), [r_rr[d], r_gT[d]], [r_rr[d]])
                        V(lambda e, d=d: e.reciprocal(out=rr[d][:], in_=rr[d][:]), [r_rr[d]], [r_rr[d]])
                        hv = hacc[:, c, :].rearrange("p (h k) -> p h k", k=64)
                        if c not in done:
                            done.add(c)
                            V(lambda e, d=d, hv=hv: e.tensor_tensor(out=hv, in0=pnd[d][:, :, 0:64], in1=rr[d][:].unsqueeze(2).to_broadcast([128, 4, 64]), op=ALU.mult),
                              [r_pnd[d], r_rr[d]], [r_hacc[c]])
                        else:
                            V(lambda e, d=d: e.tensor_tensor(out=tmph[d][:], in0=pnd[d][:, :, 0:64], in1=rr[d][:].unsqueeze(2).to_broadcast([128, 4, 64]), op=ALU.mult),
                              [r_pnd[d], r_rr[d]], [r_tmph[d]])
                            G(lambda e, d=d, hv=hv: e.tensor_tensor(out=hv, in0=hv, in1=tmph[d][:], op=ALU.add), [r_tmph[d], r_hacc[c]], [r_hacc[c]])
                        if last:
                            continue
                        for h in range(4):
                            hj = h // 2
                            P(lambda e, d=d, h=h, hj=hj, c=c: e.matmul(pdC[d][:, h, 0:65], lhsT=kt[d][:, 2 * hj:2 * hj + 2, :].rearrange("p a k -> p (a k)"),
                                                                     rhs=vx[:, c, h, :], start=True, stop=True),
                              [r_kt[d], r_vx], [r_pdC[d]])
                        for par in range(2):
                            rs = slice(par * 64, (par + 1) * 64)
                            V(lambda e, d=d, rs=rs, par=par: e.tensor_tensor(out=tmpC[d][rs, :, :], in0=pdC[d][rs, par::2, 0:65], in1=Cst[d][rs, :, :], op=ALU.add),
                              [r_pdC[d], r_Cst[d]], [r_tmpC[d]])
                        for par in range(2):
                            rs = slice(par * 64, (par + 1) * 64)
                            V(lambda e, d=d, rs=rs, c=c: e.tensor_tensor(out=Cst[d][rs, :, :], in0=tmpC[d][rs, :, :],
                                                                       in1=decb[d][rs, :, c:c + 1].to_broadcast([64, 2, 65]), op=ALU.mult),
                              [r_tmpC[d], r_gT[d]], [r_Cst[d]])
                            G(lambda e, d=d, rs=rs, c=c: e.tensor_tensor(out=Cstb[d][rs, :, :], in0=tmpC[d][rs, :, :],
                                                                       in1=decb[d][rs, :, c:c + 1].to_broadcast([64, 2, 65]), op=ALU.mult),
                              [r_tmpC[d], r_gT[d]], [r_Cstb[d]])
                cx.barrier()
            with ExitStack() as ph2:
                def sb2(name, shape, dt=F32):
                    return ph2.enter_context(nc.sbuf_tensor(f"{name}_{l}", list(shape), dt))

                def ps2(name, shape, dt=F32):
                    return ph2.enter_context(nc.psum_tensor(f"{name}_{l}", list(shape), dt))
                gnw = sb2("gnw", [128, 256])
                r_gnw = R()
                cx.dma("sp", gnw[:], gn_w_in[l].partition_broadcast(128), writes=[r_gnw])
                uo_t = [sb2(f"uo_t{i}", [128, 256]) for i in range(2)]
                r_uot = [R(), R()]
                sq = sb2("sq", [128, 256]); hc = [sb2(f"hc{i}", [128, 256]) for i in range(2)]
                st4 = sb2("st4", [128, 4, 4])
                r_sq, r_st4 = R(), R()
                r_hc = [R(), R()]
                pyT = [ps2(f"pyT{i}", [128, 512]) for i in range(2)]
                r_pyT = [R(psum=True), R(psum=True)]
                ymT = [sb2(f"ymT{i}", [128, 2, 128]) for i in range(2)]
                r_ymT = [R(), R()]
                for t in range(NT):
                    b = t % 2
                    cx.dma("sp", uo_t[b][:], U_tok[t * 128:(t + 1) * 128, 512:768], reads=[r_Utok], writes=[r_uot[b]])
                    hv = hacc[:, t, :].rearrange("p (h k) -> p h k", k=64)
                    V(lambda e, hv=hv: e.tensor_reduce(out=st4[:, 0, :], in_=hv, axis=AX.X, op=ALU.add), [r_hacc[t]], [r_st4])
                    G(lambda e, t=t: e.tensor_tensor(out=sq[:], in0=hacc[:, t, :], in1=hacc[:, t, :], op=ALU.mult), [r_hacc[t]], [r_sq])
                    V(lambda e: e.tensor_reduce(out=st4[:, 1, :], in_=sq[:].rearrange("p (h k) -> p h k", k=64), axis=AX.X, op=ALU.add), [r_sq], [r_st4])
                    V(lambda e: e.tensor_scalar(out=st4[:, 2, :], in0=st4[:, 0, :], scalar1=1.0 / 64, scalar2=None, op0=ALU.mult), [r_st4], [r_st4])
                    V(lambda e: e.tensor_tensor(out=st4[:, 0, :], in0=st4[:, 2, :], in1=st4[:, 2, :], op=ALU.mult), [r_st4], [r_st4])
                    V(lambda e: e.scalar_tensor_tensor(out=st4[:, 3, :], in0=st4[:, 1, :], scalar=1.0 / 64, in1=st4[:, 0, :], op0=ALU.mult, op1=ALU.subtract), [r_st4], [r_st4])
                    A(lambda e: e.activation(out=st4[:, 3, :], in_=st4[:, 3, :], func=AF.Sqrt, bias=LN_EPS, scale=1.0), [r_st4], [r_st4])
                    V(lambda e: e.reciprocal(out=st4[:, 3, :], in_=st4[:, 3, :]), [r_st4], [r_st4])
                    hcv = hc[b][:].rearrange("p (h k) -> p h k", k=64)
                    V(lambda e, hv=hv, hcv=hcv: e.tensor_tensor(out=hcv, in0=hv, in1=st4[:, 2, :].unsqueeze(2).to_broadcast([128, 4, 64]), op=ALU.subtract),
                      [r_hacc[t], r_st4], [r_hc[b]])
                    V(lambda e, hcv=hcv: e.tensor_tensor(out=hcv, in0=hcv, in1=st4[:, 3, :].unsqueeze(2).to_broadcast([128, 4, 64]), op=ALU.mult),
                      [r_hc[b], r_st4], [r_hc[b]])
                    G(lambda e, b=b: e.tensor_tensor(out=hc[b][:], in0=hc[b][:], in1=gnw[:], op=ALU.mult), [r_hc[b], r_gnw], [r_hc[b]])
                    A(lambda e, b=b: e.activation(out=uo_t[b][:], in_=uo_t[b][:], func=AF.Sigmoid), [r_uot[b]], [r_uot[b]])
                    V(lambda e, b=b: e.tensor_tensor(out=hc[b][:], in0=hc[b][:], in1=uo_t[b][:], op=ALU.mult), [r_hc[b], r_uot[b]], [r_hc[b]])
                    for j in range(2):
                        P(lambda e, b=b, j=j: e.transpose(out=pyT[b][:, j * 128:(j + 1) * 128], in_=hc[b][:, j * 128:(j + 1) * 128], identity=ident[:]),
                          [r_hc[b], r_ident], [r_pyT[b]])
                    A(lambda e, b=b: e.copy(out=ymT[b][:].rearrange("p j t -> p (j t)"), in_=pyT[b][:, 0:256]), [r_pyT[b]], [r_ymT[b]])
                    cx.dma("sp", mixT[256:512, t * 128:(t + 1) * 128].rearrange("(j p) t -> p j t", p=128), ymT[b][:], reads=[r_ymT[b]], writes=[r_mixT])
                cx.barrier()
        if STOP_AFTER == "C":
            break
        with ExitStack() as ph:
            def sbp(name, shape, dt=F32):
                return ph.enter_context(nc.sbuf_tensor(f"{name}_{l}", list(shape), dt))

            def psp(name, shape, dt=F32):
                return ph.enter_context(nc.psum_tensor(f"{name}_{l}", list(shape), dt))
            SCALE = float(96 ** -0.5)
            cs2 = sbp("cs2", [128, 2, S], BF16)
            r_cs2 = R()
            for tb in range(2):
                cx.dma("pool", cs2[64:96, tb, :], ropeT[tb], reads=[r_ropeT], writes=[r_cs2])
            wuq = sbp("wuq", [128, 2, 768], BF16)
            wuqs = sbp("wuqs", [128, 2, 768], BF16)
            wuk = sbp("wuk", [128, 512], BF16)
            wuv = sbp("wuv", [128, 512], BF16)
            r_w = R()
            cx.dma("pool", wuq[:], w_uq_in[l].rearrange("(j p) n -> p j n", p=128), writes=[r_w])
            cx.dma("pool", wuqs[:], w_uq_sw_in[l].rearrange("(j p) n -> p j n", p=128), writes=[r_w])
            cx.dma("pool", wuk[:], w_uk_in[l], writes=[r_w])
            cx.dma("pool", wuv[:], w_uv_in[l], writes=[r_w])
            gqp = sbp("gqp", [128, 2]); gkvp = sbp("gkvp", [128, 1])
            r_g = R()
            cx.dma("sp", gqp[:], g_q_p[l], writes=[r_g])
            cx.dma("sp", gkvp[:], g_kv_p[l], writes=[r_g])
            sel64 = sbp("sel64", [65, 64])
            r_sel = R()
            cx.dma("sp", sel64[:], sel64_in[:, :], writes=[r_sel])
            onesb = sbp("onesb", [128, 128], BF16)
            r_ones = R()
            G(lambda e: e.memset(onesb[:], 1.0), [], [r_ones])
            qn = sbp("qn", [128, 2, S], BF16)
            ckv = sbp("ckv", [128, S], BF16)
            krope = sbp("krope", [128, S], BF16)
            vx2 = sbp("vx2", [128, NT, 8, 65], BF16)
            r_qn, r_ckv, r_krope, r_vx2 = R(), R(), R(), R()
            G(lambda e: e.memset(vx2[:, :, :, 64:65], 1.0), [], [r_vx2])
            pa = [psp(f"pa{i}", [128, 512]) for i in range(2)]
            pb_ = [psp(f"pb{i}", [128, 512]) for i in range(2)]
            pc_ = [psp(f"pc{i}", [128, 512]) for i in range(2)]
            pm = [psp(f"pm{i}", [128, 512]) for i in range(2)]
            r_pa = [R(psum=True), R(psum=True)]
            r_pb = [R(psum=True), R(psum=True)]
            r_pc = [R(psum=True), R(psum=True)]
            r_pm = [R(psum=True), R(psum=True)]
            with ExitStack() as ph2:
                def sb2(name, shape, dt=F32):
                    return ph2.enter_context(nc.sbuf_tensor(f"{name}_{l}", list(shape), dt))
                ub = [sb2(f"ub{i}", [128, 3, 512]) for i in range(2)]
                r_ub = [R(), R()]
                sqb = [sb2(f"sqb{i}", [128, 3, 512], BF16) for i in range(2)]
                r_sqb = [R(), R()]
                rs = [sb2(f"rs{i}", [128, 2, 512]) for i in range(2)]
                r_rs = [R(), R()]
                krr = sb2("krr", [128, 2, S], BF16)
                r_krr = R()
                cx.dma("pool", krr[64:96, 0, :], U_fm[912:944, :], reads=[r_Ufm], writes=[r_krr])
                cx.dma("pool", krr[64:96, 1, :], U_fm[944:976, :], reads=[r_Ufm], writes=[r_krr])
                tmpk = sb2("tmpk", [128, S])
                r_tmpk = R()
                V(lambda e: e.tensor_tensor(out=tmpk[64:96, :], in0=krr[64:96, 0, :], in1=cs2[64:96, 1, :], op=ALU.mult), [r_krr, r_cs2], [r_tmpk])
                G(lambda e: e.tensor_tensor(out=krr[64:96, 1, :], in0=krr[64:96, 1, :], in1=cs2[64:96, 0, :], op=ALU.mult), [r_krr, r_cs2], [r_krr])
                V(lambda e: e.tensor_tensor(out=krope[64:96, :], in0=tmpk[64:96, :], in1=krr[64:96, 1, :], op=ALU.add), [r_krr, r_tmpk], [r_krope])
                for blk in range(8):
                    b = blk % 2
                    bs = slice(blk * 512, (blk + 1) * 512)
                    cx.dma("sp", ub[b][:], U_fm[512:896, bs].rearrange("(j p) t -> p j t", p=128), reads=[r_Ufm], writes=[r_ub[b]])
                    A(lambda e, b=b: e.activation(out=sqb[b][:], in_=ub[b][:], func=AF.Square), [r_ub[b]], [r_sqb[b]])
                    for j in range(2):
                        P(lambda e, b=b, j=j: e.matmul(pm[0][:, :], lhsT=onesb[:], rhs=sqb[b][:, j, :], start=(j == 0), stop=(j == 1)),
                          [r_ones, r_sqb[b]], [r_pm[0]])
                    P(lambda e, b=b: e.matmul(pm[1][:, :], lhsT=onesb[:], rhs=sqb[b][:, 2, :], start=True, stop=True), [r_ones, r_sqb[b]], [r_pm[1]])
                    A(lambda e, b=b: e.activation(out=rs[b][:, 0, :], in_=pm[0][:, :], func=AF.Sqrt, bias=RMS_EPS, scale=1.0 / 256), [r_pm[0]], [r_rs[b]])
                    A(lambda e, b=b: e.activation(out=rs[b][:, 1, :], in_=pm[1][:, :], func=AF.Sqrt, bias=RMS_EPS, scale=1.0 / 128), [r_pm[1]], [r_rs[b]])
                    V(lambda e, b=b: e.reciprocal(out=rs[b][:], in_=rs[b][:]), [r_rs[b]], [r_rs[b]])
                    for j in range(2):
                        V(lambda e, b=b, j=j, bs=bs: e.scalar_tensor_tensor(out=qn[:, j, bs], in0=ub[b][:, j, :], scalar=gqp[:, j:j + 1], in1=rs[b][:, 0, :],
                                                                          op0=ALU.mult, op1=ALU.mult), [r_ub[b], r_g, r_rs[b]], [r_qn])
                    V(lambda e, b=b, bs=bs: e.scalar_tensor_tensor(out=ckv[:, bs], in0=ub[b][:, 2, :], scalar=gkvp[:, 0:1], in1=rs[b][:, 1, :],
                                                                 op0=ALU.mult, op1=ALU.mult), [r_ub[b], r_g, r_rs[b]], [r_ckv])
                for t in range(NT):
                    i = t % 2
                    P(lambda e, t=t, i=i: e.matmul(pc_[i][:, :], lhsT=ckv[:, t * 128:(t + 1) * 128], rhs=wuv[:], start=True, stop=True),
                      [r_ckv, r_w], [r_pc[i]])
                    A(lambda e, t=t, i=i: e.copy(out=vx2[:, t, :, 0:64], in_=pc_[i][:, :].rearrange("p (h c) -> p h c", c=64)), [r_pc[i]], [r_vx2])
                cx.barrier()
            kTh = [sbp(f"kTh{i}", [128, S], BF16) for i in range(2)]
            qTh = [sbp(f"qTh{i}", [128, S], BF16) for i in range(2)]
            r_kTh = [R(), R()]; r_qTh = [R(), R()]
            rt = [sbp(f"rt{i}", [128, 2, 512]) for i in range(2)]
            r_rt = [R(), R()]
            pT = [sbp(f"pTe{i}", [128, 512], BF16) for i in range(3)]
            r_pTs = [R(), R(), R()]
            osb = [sbp(f"osb{i}", [65, 512]) for i in range(2)]
            r_osb = [R(), R()]
            rec = [sbp(f"rec{i}", [64, 512]) for i in range(2)]
            r_rec = [R(), R()]
            npt = 0
            npc = 0
            for h in range(8):
                hb = h % 2
                kT, qT = kTh[hb], qTh[hb]
                G(lambda e, kT=kT: e.tensor_copy(out=kT[64:96, :], in_=krope[64:96, :]), [r_krope], [r_kTh[hb]])
                for blk in range(8):
                    bs = slice(blk * 512, (blk + 1) * 512)
                    i = npc % 2
                    npc += 1
                    P(lambda e, i=i, h=h, bs=bs: e.matmul(pc_[i][0:64, :], lhsT=wuk[:, h * 64:(h + 1) * 64], rhs=ckv[:, bs], start=True, stop=True),
                      [r_w, r_ckv], [r_pc[i]])
                    V(lambda e, i=i, kT=kT, bs=bs: e.tensor_copy(out=kT[0:64, bs], in_=pc_[i][0:64, :]), [r_pc[i]], [r_kTh[hb]])
                    i = npc % 2
                    npc += 1
                    for j in range(2):
                        P(lambda e, i=i, h=h, j=j, bs=bs: e.matmul(pc_[i][0:96, :], lhsT=wuq[:, j, h * 96:(h + 1) * 96], rhs=qn[:, j, bs],
                                                                 start=(j == 0), stop=(j == 1)), [r_w, r_qn], [r_pc[i]])
                    for j in range(2):
                        P(lambda e, i=i, h=h, j=j, bs=bs: e.matmul(pm[i][0:96, :], lhsT=wuqs[:, j, h * 96:(h + 1) * 96], rhs=qn[:, j, bs],
                                                                 start=(j == 0), stop=(j == 1)), [r_w, r_qn], [r_pm[i]])
                    A(lambda e, i=i, qT=qT, bs=bs: e.copy(out=qT[0:64, bs], in_=pc_[i][0:64, :]), [r_pc[i]], [r_qTh[hb]])
                    V(lambda e, i=i, bs=bs: e.tensor_tensor(out=rt[i][64:96, 0, :], in0=pc_[i][64:96, :], in1=cs2[64:96, 1, bs], op=ALU.mult),
                      [r_pc[i], r_cs2], [r_rt[i]])
                    V(lambda e, i=i, bs=bs: e.tensor_tensor(out=rt[i][64:96, 1, :], in0=pm[i][64:96, :], in1=cs2[64:96, 0, bs], op=ALU.mult),
                      [r_pm[i], r_cs2], [r_rt[i]])
                    G(lambda e, i=i, qT=qT, bs=bs: e.tensor_tensor(out=qT[64:96, bs], in0=rt[i][64:96, 0, :], in1=rt[i][64:96, 1, :], op=ALU.add),
                      [r_rt[i]], [r_qTh[hb]])
                items = [(qb, kt) for qb in range(8) for kt in range(NT)]

                def emitS(n, kT=kT, qT=qT, hb=hb):
                    qb, kt = items[n]
                    i = n % 2
                    qs = slice(qb * 512, (qb + 1) * 512)
                    P(lambda e: e.matmul(pa[i][:, :], lhsT=kT[0:96, kt * 128:(kt + 1) * 128], rhs=qT[0:96, qs], start=True, stop=True),
                      [r_kTh[hb], r_qTh[hb]], [r_pa[i]])

                def post_a(qb, h=h):
                    ob = qb % 2
                    V(lambda e: e.tensor_copy(out=osb[ob][:], in_=pb_[ob][0:65, :]), [r_pb[ob]], [r_osb[ob]])

                def post_b(qb, h=h):
                    ob = qb % 2
                    qs = slice(qb * 512, (qb + 1) * 512)
                    P(lambda e: e.matmul(pm[ob][0:64, :], lhsT=sel64[:], rhs=osb[ob][:], start=True, stop=True), [r_sel, r_osb[ob]], [r_pm[ob]])
                    V(lambda e: e.reciprocal(out=rec[ob][:], in_=pm[ob][0:64, :]), [r_pm[ob]], [r_rec[ob]])
                    G(lambda e: e.tensor_tensor(out=rec[ob][:], in0=rec[ob][:], in1=osb[ob][0:64, :], op=ALU.mult), [r_rec[ob], r_osb[ob]], [r_rec[ob]])
                    cx.dma("sp", mixT[512 + h * 64:512 + (h + 1) * 64, qs], rec[ob][:], reads=[r_rec[ob]], writes=[r_mixT])

                emitS(0)
                pending = None
                for n, (qb, kt) in enumerate(items):
                    if n + 1 < len(items):
                        emitS(n + 1)
                    i = n % 2
                    ip = n % 3
                    ob = qb % 2
                    A(lambda e, i=i, ip=ip: e.activation(out=pT[ip][:], in_=pa[i][:, :], func=AF.Exp, scale=SCALE), [r_pa[i]], [r_pTs[ip]])
                    P(lambda e, ip=ip, ob=ob, kt=kt, h=h: e.matmul(pb_[ob][0:65, :], lhsT=vx2[:, kt, h, :], rhs=pT[ip][:],
                                                                 start=(kt == 0), stop=(kt == NT - 1)), [r_vx2, r_pTs[ip]], [r_pb[ob]])
                    if kt == NT - 1:
                        post_a(qb)
                        pending = (qb, n + 6)
                    if pending is not None and n >= pending[1]:
                        post_b(pending[0])
                        pending = None
                if pending is not None:
                    post_b(pending[0])
            cx.barrier()
        if STOP_AFTER == "D":
            break
        x_src = x_in if l == 0 else XL
        r_X1, r_H2, r_MS, r_WN, r_FFN = R(), R(), R(), R(), R()
        cnt = sb(f"cnt{l}", [128, 256])
        r_cnt = R()
        with ExitStack() as ph:
            def sbp(name, shape, dt=F32):
                return ph.enter_context(nc.sbuf_tensor(f"{name}_{l}", list(shape), dt))

            def psp(name, shape, dt=F32):
                return ph.enter_context(nc.psum_tensor(f"{name}_{l}", list(shape), dt))
            wout = sbp("wout", [128, 8, D], BF16)
            r_wout = R()
            for kc in range(8):
                cx.dma("pool", wout[:, kc, :], w_out_in[l, kc * 128:(kc + 1) * 128, :], writes=[r_wout])
            lnp = sbp("lnp", [128, 2, D])
            r_lnp = R()
            for j in range(2):
                cx.dma("sp", lnp[:, j, :], lnp_in[l, j].partition_broadcast(128), writes=[r_lnp])
            wr = sbp("wr", [128, 8, 256])
            r_wr = R()
            cx.dma("sp", wr[:], w_router_in[l].rearrange("(kc p) n -> p kc n", p=128), writes=[r_wr])
            ebias = sbp("ebias", [128, 256])
            r_eb = R()
            cx.dma("sp", ebias[:], e_bias_in[l].partition_broadcast(128), writes=[r_eb])
            ws13 = sbp("ws13", [128, 8, 512], BF16)
            ws2 = sbp("ws2", [128, 2, D], BF16)
            r_ws = R()
            for kc in range(8):
                cx.dma("pool", ws13[:, kc, :], ws13_in[l, kc * 128:(kc + 1) * 128, :], writes=[r_ws])
            for j in range(2):
                cx.dma("pool", ws2[:, j, :], ws2_in[l, j * 128:(j + 1) * 128, :], writes=[r_ws])
            onesb = sbp("onesbE", [128, 128], BF16)
            r_ones = R()
            G(lambda e: e.memset(onesb[:], 1.0), [], [r_ones])
            G(lambda e: e.memset(cnt[:], 0.0), [], [r_cnt])
            mxt = [sbp(f"mxt{i}", [128, 8, 512], BF16) for i in range(2)]
            r_mxt = [R(), R()]
            xt = [sbp(f"xtE{i}", [128, D]) for i in range(2)]
            r_xt = [R(), R()]
            z = [sbp(f"zE{i}", [128, D]) for i in range(2)]
            r_z = [R(), R()]
            x1 = [sbp(f"x1E{i}", [128, D]) for i in range(2)]
            r_x1 = [R(), R()]
            h2f = [sbp(f"h2f{i}", [128, D]) for i in range(2)]
            r_h2f = [R(), R()]
            h2b = [sbp(f"h2b{i}", [128, D], BF16) for i in range(2)]
            r_h2b = [R(), R()]
            h2T = sbp("h2T", [128, 8, 128]); h2Tb = sbp("h2Tb", [128, 8, 128], BF16)
            r_h2T, r_h2Tb = R(), R()
            st = sbp("stE", [128, 2, 6]); mv = sbp("mvE", [128, 2]); rstd = sbp("rstdE", [128, 1])
            r_st, r_mv, r_rstd = R(), R(), R()
            sc = sbp("scE", [128, 256]); sel = sbp("selE", [128, 256]); selm = sbp("selmE", [128, 256])
            m8g = sbp("m8g", [128, 8, 8]); gs = sbp("gsE", [128, 8]); m8 = sbp("m8E", [128, 8]); gm = sbp("gmE", [128, 2, 8])
            Mf = sbp("MfE", [128, 256]); Mb = [sbp(f"MbE{i}", [128, 256], BF16) for i in range(2)]
            wn = [sbp(f"wnE{i}", [128, 256]) for i in range(2)]
            ws_ = sbp("wsE", [128, 2])
            r_rt = R()
            r_Mb = [R(), R()]; r_wn = [R(), R()]
            s1 = sbp("s1E", [128, 256]); gsh = sbp("gshE", [128, 256], BF16); gT = sbp("gTE", [128, 2, 128], BF16)
            r_s1, r_gsh, r_gT = R(), R(), R()
            fo = [sbp(f"foE{i}", [128, D]) for i in range(2)]
            r_fo = [R(), R()]
            pX = [psp(f"pX{i}", [128, 512]) for i in range(2)]
            pY = [psp(f"pY{i}", [128, 512]) for i in range(2)]
            pZ = [psp(f"pZ{i}", [128, 512]) for i in range(2)]
            pW0 = psp("pW0", [128, 1024], BF16)
            pW1 = psp("pW1", [128, 512])
            r_pX = [R(psum=True), R(psum=True)]; r_pY = [R(psum=True), R(psum=True)]; r_pZ = [R(psum=True), R(psum=True)]
            r_pW0, r_pW1 = R(psum=True), R(psum=True)
            BIG = 1.0e4

            def layer_norm_stats(src, r_src):
                for hf in range(2):
                    V(lambda e, hf=hf: e.bn_stats(out=st[:, hf, :], in_=src[:, hf * 512:(hf + 1) * 512]), [r_src], [r_st])
                V(lambda e: e.bn_aggr(out=mv[:], in_=st[:].rearrange("p a b -> p (a b)")), [r_st], [r_mv])
                A(lambda e: e.activation(out=rstd[:], in_=mv[:, 1:2], func=AF.Sqrt, bias=LN_EPS, scale=1.0), [r_mv], [r_rstd])
                V(lambda e: e.reciprocal(out=rstd[:], in_=rstd[:]), [r_rstd], [r_rstd])

            for t in range(NT):
                b = t % 2
                if t % 4 == 0:
                    g4 = (t // 4) % 2
                    cx.dma("pool", mxt[g4][:], mixT[:, t * 128:(t + 4) * 128].rearrange("(kc p) t -> p kc t", p=128),
                           reads=[r_mixT], writes=[r_mxt[g4]])
                g4 = (t // 4) % 2
                tt = t % 4
                cx.dma("sp", xt[b][:], x_src[t * 128:(t + 1) * 128, :], writes=[r_xt[b]])
                for hf in range(2):
                    for kc in range(8):
                        P(lambda e, hf=hf, kc=kc, g4=g4, tt=tt: e.matmul(pX[hf][:, :], lhsT=mxt[g4][:, kc, tt * 128:(tt + 1) * 128],
                                                                      rhs=wout[:, kc, hf * 512:(hf + 1) * 512], start=(kc == 0), stop=(kc == 7)),
                          [r_mxt[g4], r_wout], [r_pX[hf]])
                    V(lambda e, hf=hf, b=b: e.tensor_tensor(out=z[b][:, hf * 512:(hf + 1) * 512], in0=pX[hf][:, :], in1=gF[:, 0, hf * 512:(hf + 1) * 512], op=ALU.mult),
                      [r_pX[hf], r_gF], [r_z[b]])
                V(lambda e, b=b: e.scalar_tensor_tensor(out=z[b][:], in0=xt[b][:], scalar=float(ALPHA), in1=z[b][:], op0=ALU.mult, op1=ALU.add),
                  [r_xt[b], r_z[b]], [r_z[b]])
                layer_norm_stats(z[b], r_z[b])
                V(lambda e, b=b: e.tensor_scalar(out=z[b][:], in0=z[b][:], scalar1=mv[:, 0:1], scalar2=rstd[:, 0:1], op0=ALU.subtract, op1=ALU.mult),
                  [r_z[b], r_mv, r_rstd], [r_z[b]])
                G(lambda e, b=b: e.tensor_tensor(out=z[b][:], in0=z[b][:], in1=lnp[:, 0, :], op=ALU.mult), [r_z[b], r_lnp], [r_z[b]])
                V(lambda e, b=b: e.tensor_tensor(out=x1[b][:], in0=z[b][:], in1=lnp[:, 1, :], op=ALU.add), [r_z[b], r_lnp], [r_x1[b]])
                cx.dma("sp", X1[t * 128:(t + 1) * 128, :], x1[b][:], reads=[r_x1[b]], writes=[r_X1])
                layer_norm_stats(x1[b], r_x1[b])
                V(lambda e, b=b: e.tensor_scalar(out=h2f[b][:], in0=x1[b][:], scalar1=mv[:, 0:1], scalar2=rstd[:, 0:1], op0=ALU.subtract, op1=ALU.mult),
                  [r_x1[b], r_mv, r_rstd], [r_h2f[b]])
                G(lambda e, b=b: e.tensor_tensor(out=h2f[b][:], in0=h2f[b][:], in1=gF[:, 3, :], op=ALU.mult), [r_h2f[b], r_gF], [r_h2f[b]])
                V(lambda e, b=b: e.tensor_tensor(out=h2f[b][:], in0=h2f[b][:], in1=gF[:, 2, :], op=ALU.add), [r_h2f[b], r_gF], [r_h2f[b]])
                A(lambda e, b=b: e.copy(out=h2b[b][:], in_=h2f[b][:]), [r_h2f[b]], [r_h2b[b]])
                cx.dma("sp", H2[t * 128:(t + 1) * 128, :], h2b[b][:], reads=[r_h2b[b]], writes=[r_H2])
                for q4 in range(2):
                    for k4 in range(4):
                        kc = q4 * 4 + k4
                        P(lambda e, q4=q4, k4=k4, kc=kc, b=b: e.transpose(out=pY[q4][:, k4 * 128:(k4 + 1) * 128], in_=h2f[b][:, kc * 128:(kc + 1) * 128], identity=ident[:]),
                          [r_h2f[b], r_ident], [r_pY[q4]])
                    V(lambda e, q4=q4: e.tensor_copy(out=h2T[:, q4 * 4:(q4 + 1) * 4, :].rearrange("p a t -> p (a t)"), in_=pY[q4][:, :]), [r_pY[q4]], [r_h2T])
                    A(lambda e, q4=q4: e.copy(out=h2Tb[:, q4 * 4:(q4 + 1) * 4, :].rearrange("p a t -> p (a t)"), in_=pY[q4][:, :]), [r_pY[q4]], [r_h2Tb])
                for kc in range(8):
                    P(lambda e, kc=kc: e.matmul(pZ[0][:, 0:256], lhsT=h2T[:, kc, :], rhs=wr[:, kc, :], start=(kc == 0), stop=(kc == 7)),
                      [r_h2T, r_wr], [r_pZ[0]])
                A(lambda e: e.activation(out=sc[:], in_=pZ[0][:, 0:256], func=AF.Sigmoid), [r_pZ[0]], [r_rt])
                V(lambda e: e.tensor_tensor(out=sel[:], in0=sc[:], in1=ebias[:], op=ALU.add), [r_rt, r_eb], [r_rt])
                for g in range(8):
                    V(lambda e, g=g: e.max(out=m8g[:, g, :], in_=sel[:, g * 32:(g + 1) * 32]), [r_rt], [r_rt])
                V(lambda e: e.tensor_tensor(out=gs[:], in0=m8g[:, :, 0], in1=m8g[:, :, 1], op=ALU.add), [r_rt], [r_rt])
                V(lambda e: e.max(out=m8[:], in_=gs[:]), [r_rt], [r_rt])
                V(lambda e: e.tensor_scalar(out=gm[:, 0, :], in0=gs[:], scalar1=m8[:, 3:4], scalar2=None, op0=ALU.is_ge), [r_rt], [r_rt])
                V(lambda e: e.tensor_scalar(out=gm[:, 1, :], in0=gm[:, 0, :], scalar1=BIG, scalar2=-BIG, op0=ALU.mult, op1=ALU.add), [r_rt], [r_rt])
                V(lambda e: e.tensor_tensor(out=selm[:].rearrange("p (g k) -> p g k", k=32), in0=sel[:].rearrange("p (g k) -> p g k", k=32),
                                            in1=gm[:, 0, :].unsqueeze(2).to_broadcast([128, 8, 32]), op=ALU.mult), [r_rt], [r_rt])
                V(lambda e: e.tensor_tensor(out=selm[:].rearrange("p (g k) -> p g k", k=32), in0=selm[:].rearrange("p (g k) -> p g k", k=32),
                                            in1=gm[:, 1, :].unsqueeze(2).to_broadcast([128, 8, 32]), op=ALU.add), [r_rt], [r_rt])
                V(lambda e: e.max(out=m8[:], in_=selm[:]), [r_rt], [r_rt])
                V(lambda e: e.tensor_scalar(out=Mf[:], in0=selm[:], scalar1=m8[:, 7:8], scalar2=None, op0=ALU.is_ge), [r_rt], [r_rt])
                G(lambda e, b=b: e.tensor_copy(out=Mb[b][:], in_=Mf[:]), [r_rt], [r_Mb[b]])
                V(lambda e: e.tensor_tensor(out=sel[:], in0=sc[:], in1=Mf[:], op=ALU.mult), [r_rt], [r_rt])
                V(lambda e: e.tensor_reduce(out=ws_[:, 0:1], in_=sel[:], axis=AX.X, op=ALU.add), [r_rt], [r_rt])
                V(lambda e: e.reciprocal(out=ws_[:, 1:2], in_=ws_[:, 0:1]), [r_rt], [r_rt])
                V(lambda e, b=b: e.tensor_scalar(out=wn[b][:], in0=sel[:], scalar1=ws_[:, 1:2], scalar2=2.5, op0=ALU.mult, op1=ALU.mult), [r_rt], [r_wn[b]])
                cx.dma("sp", MS[t * 128:(t + 1) * 128, :], Mb[b][:], reads=[r_Mb[b]], writes=[r_MS])
                cx.dma("sp", WN[t * 128:(t + 1) * 128, :], wn[b][:], reads=[r_wn[b]], writes=[r_WN])
                P(lambda e, b=b: e.matmul(pW1[:, 0:256], lhsT=onesb[:], rhs=Mb[b][:], start=True, stop=True), [r_ones, r_Mb[b]], [r_pW1])
                V(lambda e: e.tensor_tensor(out=cnt[:], in0=cnt[:], in1=pW1[:, 0:256], op=ALU.add), [r_pW1, r_cnt], [r_cnt])
                for kc in range(8):
                    P(lambda e, kc=kc: e.matmul(pZ[1][:, :], lhsT=h2Tb[:, kc, :], rhs=ws13[:, kc, :], start=(kc == 0), stop=(kc == 7)),
                      [r_h2Tb, r_ws], [r_pZ[1]])
                A(lambda e: e.activation(out=s1[:], in_=pZ[1][:, 0:256], func=AF.Silu), [r_pZ[1]], [r_s1])
                V(lambda e: e.tensor_tensor(out=gsh[:], in0=s1[:], in1=pZ[1][:, 256:512], op=ALU.mult), [r_s1, r_pZ[1]], [r_gsh])
                for j in range(2):
                    P(lambda e, j=j: e.transpose(out=pW0[:, j * 128:(j + 1) * 128], in_=gsh[:, j * 128:(j + 1) * 128], identity=identb[:]),
                      [r_gsh, r_identb], [r_pW0])
                A(lambda e: e.copy(out=gT[:].rearrange("p j t -> p (j t)"), in_=pW0[:, 0:256]), [r_pW0], [r_gT])
                for hf in range(2):
                    for j in range(2):
                        P(lambda e, hf=hf, j=j: e.matmul(pX[hf][:, :], lhsT=gT[:, j, :], rhs=ws2[:, j, hf * 512:(hf + 1) * 512], start=(j == 0), stop=(j == 1)),
                          [r_gT, r_ws], [r_pX[hf]])
                    if hf == 0:
                        A(lambda e, b=b: e.copy(out=fo[b][:, 0:512], in_=pX[0][:, :]), [r_pX[0]], [r_fo[b]])
                    else:
                        V(lambda e, b=b: e.tensor_copy(out=fo[b][:, 512:1024], in_=pX[1][:, :]), [r_pX[1]], [r_fo[b]])
                cx.dma("sp", FFN[t * 128:(t + 1) * 128, :], fo[b][:], reads=[r_fo[b]], writes=[r_FFN])
            cx.barrier()
        if STOP_AFTER == "E":
            break
        r_Xs, r_Ys = R(), R()
        idxs = sb(f"idxs{l}", [128, NT, 8], I32)
        wk = sb(f"wk{l}", [128, NT, 8])
        idxw = sb(f"idxw{l}", [128, 512], I32)
        r_idxs, r_wk, r_idxw = R(), R(), R()
        with ExitStack() as ph:
            def sbp(name, shape, dt=F32):
                return ph.enter_context(nc.sbuf_tensor(f"{name}_{l}", list(shape), dt))

            def psp(name, shape, dt=F32):
                return ph.enter_context(nc.psum_tensor(f"{name}_{l}", list(shape), dt))
            xq = sbp("xq", [128, 256]); qi = sbp("qi", [128, 256], I32); qf = sbp("qf", [128, 256]); gtm = sbp("gtm", [128, 256])
            pend = sbp("pend", [128, 256]); base = sbp("base", [128, 256])
            r_f1 = R(); r_base = R()
            V(lambda e: e.tensor_scalar(out=xq[:], in0=cnt[:], scalar1=127.0, scalar2=1.0 / 128, op0=ALU.add, op1=ALU.mult), [r_cnt], [r_f1])
            V(lambda e: e.tensor_copy(out=qi[:], in_=xq[:]), [r_f1], [r_f1])
            V(lambda e: e.tensor_copy(out=qf[:], in_=qi[:]), [r_f1], [r_f1])
            V(lambda e: e.tensor_tensor(out=gtm[:], in0=qf[:], in1=xq[:], op=ALU.is_gt), [r_f1], [r_f1])
            V(lambda e: e.tensor_tensor(out=qf[:], in0=qf[:], in1=gtm[:], op=ALU.subtract), [r_f1], [r_f1])
            V(lambda e: e.tensor_scalar(out=qf[:], in0=qf[:], scalar1=128.0, scalar2=None, op0=ALU.mult), [r_f1], [r_f1])
            V(lambda e: e.tensor_tensor_scan(out=pend[:], data0=qf[:], data1=qf[:], initial=0.0, op0=ALU.add, op1=ALU.max), [r_f1], [r_f1])
            V(lambda e: e.tensor_tensor(out=base[:], in0=pend[:], in1=qf[:], op=ALU.subtract), [r_f1], [r_base])
            V(lambda e: e.tensor_scalar(out=base[:], in0=base[:], scalar1=1.0, scalar2=None, op0=ALU.add), [r_base], [r_base])
            pF = [psp(f"pF{i}", [128, 512]) for i in range(2)]
            r_pF = [R(psum=True), R(psum=True)]
            pendT = sbp("pendT", [128, 2])
            for j in range(2):
                P(lambda e, j=j: e.transpose(out=pF[0][:, j:j + 1], in_=pend[0:1, j * 128:(j + 1) * 128], identity=ident[0:1, 0:1]), [r_f1, r_ident], [r_pF[0]])
            V(lambda e: e.tensor_copy(out=pendT[:], in_=pF[0][:, 0:2]), [r_pF[0]], [r_f1])
            blkpos = sbp("blkpos", [128, 512]); pidx = sbp("pidx", [128, 1])
            r_c2 = R()
            cx.dma("sp", blkpos[:], blkpos_in[:, :], writes=[r_c2])
            cx.dma("sp", pidx[:], pidx_in[:, :], writes=[r_c2])
            Gm = [sbp(f"Gm{j}", [128, 512], BF16) for j in range(2)]
            onesb = sbp("onesbF", [128, 128], BF16)
            ustr = sbp("ustr", [128, 128], BF16)
            r_ones = R()
            G(lambda e: e.memset(onesb[:], 1.0), [], [r_ones])
            cx.dma("pool", ustr[:], ustrict_in[:, :], writes=[r_ones])
            for j in range(2):
                V(lambda e, j=j: e.tensor_scalar(out=Gm[j][:], in0=blkpos[:], scalar1=pendT[:, j:j + 1], scalar2=None, op0=ALU.is_ge), [r_c2, r_f1], [r_f1])
            for j in range(2):
                P(lambda e, j=j: e.matmul(pF[1][:, :], lhsT=onesb[:], rhs=Gm[j][:], start=(j == 0), stop=(j == 1)), [r_ones, r_f1], [r_pF[1]])
            eall = sbp("eall", [128, 512])
            V(lambda e: e.tensor_scalar(out=eall[:], in0=pF[1][:, :], scalar1=255.0, scalar2=None, op0=ALU.min), [r_pF[1]], [r_f1])
            vld = sbp("vld", [128, 512]); vld2 = sbp("vld2", [128, 512])
            V(lambda e: e.tensor_scalar(out=vld[:], in0=blkpos[:], scalar1=pend[:, 255:256], scalar2=None, op0=ALU.is_lt), [r_f1, r_c2], [r_f1])
            V(lambda e: e.memset(vld2[:], 1.0), [r_f1], [r_f1])
            V(lambda e: e.tensor_tensor(out=vld2[:, 3:512], in0=eall[:, 3:512], in1=eall[:, 0:509], op=ALU.not_equal), [r_f1], [r_f1])
            V(lambda e: e.tensor_tensor(out=vld[:], in0=vld[:], in1=vld2[:], op=ALU.mult), [r_f1], [r_f1])
            V(lambda e: e.tensor_scalar(out=eall[:], in0=eall[:], scalar1=128.0, scalar2=float(l * 256 * 128) - OOB_IDX, op0=ALU.mult, op1=ALU.add), [r_f1], [r_f1])
            V(lambda e: e.tensor_scalar(out=eall[:], in0=eall[:], scalar1=pidx[:, 0:1], scalar2=None, op0=ALU.add), [r_f1, r_c2], [r_f1])
            V(lambda e: e.tensor_tensor(out=eall[:], in0=eall[:], in1=vld[:], op=ALU.mult), [r_f1], [r_f1])
            V(lambda e: e.tensor_scalar(out=idxw[:], in0=eall[:], scalar1=OOB_IDX, scalar2=None, op0=ALU.add), [r_f1], [r_idxw])
            Mt = [sbp(f"Mt{i}", [128, 256], BF16) for i in range(2)]
            wnt = [sbp(f"wnt{i}", [128, 256]) for i in range(2)]
            h2t = [sbp(f"h2t{i}", [128, D], BF16) for i in range(2)]
            r_Mt = [R(), R()]; r_wnt = [R(), R()]; r_h2t = [R(), R()]
            t1 = sbp("t1", [128, 256]); Vt = sbp("Vt", [128, 256]); junk = sbp("junk", [128, 256]); p8 = sbp("p8", [128, 8])
            r_t1, r_Vt, r_junk, r_p8 = R(), R(), R(), R()
            for t in range(NT):
                b = t % 2
                cx.dma("sp", Mt[b][:], MS[t * 128:(t + 1) * 128, :], reads=[r_MS], writes=[r_Mt[b]])
                cx.dma("sp", wnt[b][:], WN[t * 128:(t + 1) * 128, :], reads=[r_WN], writes=[r_wnt[b]])
                cx.dma("sp", h2t[b][:], H2[t * 128:(t + 1) * 128, :], reads=[r_H2], writes=[r_h2t[b]])
                P(lambda e, b=b: e.matmul(pF[0][:, 0:256], lhsT=ustr[:], rhs=Mt[b][:], start=True, stop=True), [r_ones, r_Mt[b]], [r_pF[0]])
                V(lambda e: e.tensor_tensor(out=t1[:], in0=pF[0][:, 0:256], in1=base[:], op=ALU.add), [r_pF[0], r_base], [r_t1])
                G(lambda e, b=b: e.tensor_tensor(out=Vt[:], in0=t1[:], in1=Mt[b][:], op=ALU.mult), [r_t1, r_Mt[b]], [r_Vt])
                V(lambda e: e.max(out=p8[:], in_=Vt[:]), [r_Vt], [r_p8])
                V(lambda e, t=t: e.tensor_scalar(out=idxs[:, t, :], in0=p8[:], scalar1=-1.0, scalar2=None, op0=ALU.add), [r_p8], [r_idxs])
                for k in range(8):
                    V(lambda e, t=t, k=k, b=b: e.scalar_tensor_tensor(out=junk[:], in0=Vt[:], scalar=p8[:, k:k + 1], in1=wnt[b][:], op0=ALU.is_equal, op1=ALU.mult,
                                                                    accum_out=wk[:, t, k:k + 1]), [r_Vt, r_p8, r_wnt[b]], [r_junk, r_wk])
                P(lambda e, b=b: e.matmul(pF[1][:, 0:256], lhsT=onesb[:], rhs=Mt[b][:], start=True, stop=True), [r_ones, r_Mt[b]], [r_pF[1]])
                V(lambda e: e.tensor_tensor(out=base[:], in0=base[:], in1=pF[1][:, 0:256], op=ALU.add), [r_pF[1], r_base, r_t1], [r_base])
                for k in range(8):
                    cx.dma("pool", None, None, reads=[r_h2t[b], r_idxs], writes=[r_Xs],
                           fn=lambda e, t=t, k=k, b=b: e.indirect_dma_start(
                               out=Xs[:, :], out_offset=bass.IndirectOffsetOnAxis(ap=idxs[:, t, k:k + 1].bitcast(U32), axis=0),
                               in_=h2t[b][:], in_offset=None))
            cx.barrier()
        with ExitStack() as ph:
            def sbp(name, shape, dt=F32):
                return ph.enter_context(nc.sbuf_tensor(f"{name}_{l}", list(shape), dt))

            def psp(name, shape, dt=F32):
                return ph.enter_context(nc.psum_tensor(f"{name}_{l}", list(shape), dt))
            NW = 3
            Xb = [sbp(f"Xb{i}", [128, D], BF16) for i in range(2)]
            w1b = [sbp(f"w1b{i}", [128, 2048], BF16) for i in range(NW)]
            w3b = [sbp(f"w3b{i}", [128, 2048], BF16) for i in range(NW)]
            w2b = [sbp(f"w2b{i}", [128, 2048], BF16) for i in range(NW)]
            xT = [sbp(f"xTb{i}", [128, 8, 128], BF16) for i in range(2)]
            s1 = [sbp(f"s1b{i}", [128, 256]) for i in range(2)]
            gb = [sbp(f"gbb{i}", [128, 256], BF16) for i in range(2)]
            gT = [sbp(f"gTb{i}", [128, 2, 128], BF16) for i in range(2)]
            Yb = [sbp(f"Yb{i}", [128, D], BF16) for i in range(2)]
            r_Xb = [R(), R()]; r_xT = [R(), R()]
            r_w1b = [R() for _ in range(NW)]; r_w3b = [R() for _ in range(NW)]; r_w2b = [R() for _ in range(NW)]
            r_s1 = [R(), R()]; r_gb = [R(), R()]; r_gT = [R(), R()]; r_Yb = [R(), R()]
            pxT = [psp(f"pxT{i}", [128, 1024], BF16) for i in range(2)]
            phh = [psp(f"phh{i}", [128, 512]) for i in range(2)]
            pgT = psp("pgT", [128, 1024], BF16)
            pyy = [psp(f"pyy{i}", [128, 512]) for i in range(2)]
            r_pxT = [R(psum=True), R(psum=True)]; r_phh = [R(psum=True), R(psum=True)]; r_pgT = R(psum=True); r_pyy = [R(psum=True), R(psum=True)]

            def st_load(b):
                i = b % 2
                iw = b % NW
                cx.dma("sp", Xb[i][:], Xs[b * 128:(b + 1) * 128, :], reads=[r_Xs], writes=[r_Xb[i]])
                for wsrc, wdst, rw in ((w1_in, w1b, r_w1b), (w3_in, w3b, r_w3b), (w2_in, w2b, r_w2b)):
                    cx.dma("pool", None, None, reads=[r_idxw], writes=[rw[iw]],
                           fn=lambda e, wsrc=wsrc, wdst=wdst, iw=iw, b=b: e.indirect_dma_start(
                               out=wdst[iw][:], out_offset=None, in_=wsrc[:, :],
                               in_offset=bass.IndirectOffsetOnAxis(ap=idxw[:, b:b + 1].bitcast(U32), axis=0),
                               bounds_check=bc_reg, oob_is_err=False))

            def st_T(b):
                i = b % 2
                for j in range(8):
                    P(lambda e, i=i, j=j: e.transpose(out=pxT[i][:, j * 128:(j + 1) * 128], in_=Xb[i][:, j::8], identity=identb[:]),
                      [r_Xb[i], r_identb], [r_pxT[i]])
                A(lambda e, i=i: e.copy(out=xT[i][:, 0:4, :].rearrange("p a t -> p (a t)"), in_=pxT[i][:, 0:512]), [r_pxT[i]], [r_xT[i]])
                V(lambda e, i=i: e.tensor_copy(out=xT[i][:, 4:8, :].rearrange("p a t -> p (a t)"), in_=pxT[i][:, 512:1024]), [r_pxT[i]], [r_xT[i]])

            def st_H(b):
                i = b % 2
                iw = b % NW
                for j in range(8):
                    P(lambda e, i=i, iw=iw, j=j: e.matmul(phh[i][:, 0:256], lhsT=xT[i][:, j, :], rhs=w1b[iw][:, j * 256:(j + 1) * 256], start=(j == 0), stop=(j == 7)),
                      [r_xT[i], r_w1b[iw]], [r_phh[i]])
                for j in range(8):
                    P(lambda e, i=i, iw=iw, j=j: e.matmul(phh[i][:, 256:512], lhsT=xT[i][:, j, :], rhs=w3b[iw][:, j * 256:(j + 1) * 256], start=(j == 0), stop=(j == 7)),
                      [r_xT[i], r_w3b[iw]], [r_phh[i]])
                A(lambda e, i=i: e.activation(out=s1[i][:], in_=phh[i][:, 0:256], func=AF.Silu), [r_phh[i]], [r_s1[i]])
                V(lambda e, i=i: e.tensor_tensor(out=gb[i][:], in0=s1[i][:], in1=phh[i][:, 256:512], op=ALU.mult), [r_s1[i], r_phh[i]], [r_gb[i]])

            def st_GT(b):
                i = b % 2
                for j in range(2):
                    P(lambda e, i=i, j=j: e.transpose(out=pgT[:, j * 128:(j + 1) * 128], in_=gb[i][:, j::2], identity=identb[:]), [r_gb[i], r_identb], [r_pgT])
                A(lambda e, i=i: e.copy(out=gT[i][:].rearrange("p j t -> p (j t)"), in_=pgT[:, 0:256]), [r_pgT], [r_gT[i]])

            def st_Y(b):
                i = b % 2
                iw = b % NW
                for hf in range(2):
                    for j in range(2):
                        P(lambda e, i=i, iw=iw, j=j, hf=hf: e.matmul(pyy[hf][:, :], lhsT=gT[i][:, j, :], rhs=w2b[iw][:, j * 1024 + hf * 512:j * 1024 + (hf + 1) * 512],
                                                                   start=(j == 0), stop=(j == 1)), [r_gT[i], r_w2b[iw]], [r_pyy[hf]])
                A(lambda e, i=i: e.copy(out=Yb[i][:, 0:512], in_=pyy[0][:, :]), [r_pyy[0]], [r_Yb[i]])
                V(lambda e, i=i: e.tensor_copy(out=Yb[i][:, 512:1024], in_=pyy[1][:, :]), [r_pyy[1]], [r_Yb[i]])
                cx.dma("sp", Ys[b * 128:(b + 1) * 128, :], Yb[i][:], reads=[r_Yb[i]], writes=[r_Ys])

            st_load(0)
            st_load(1)
            st_T(0)
            for n in range(NBLK + 1):
                if n < NBLK:
                    st_H(n)
                if 1 <= n:
                    st_GT(n - 1)
                if n + 1 < NBLK:
                    st_T(n + 1)
                if 1 <= n:
                    st_Y(n - 1)
                if n + 2 < NBLK:
                    st_load(n + 2)
            cx.barrier()
        with ExitStack() as ph:
            def sbp(name, shape, dt=F32):
                return ph.enter_context(nc.sbuf_tensor(f"{name}_{l}", list(shape), dt))
            lnp2 = sbp("lnp2", [128, 2, D])
            r_lnp2 = R()
            for j in range(2):
                cx.dma("sp", lnp2[:, j, :], lnp_in[l, 2 + j].partition_broadcast(128), writes=[r_lnp2])
            yg = [sbp(f"yg{i}", [128, 8, D], BF16) for i in range(2)]
            r_yg = [R(), R()]
            acc = [sbp(f"accF{i}", [128, D]) for i in range(2)]
            r_acc = [R(), R()]
            x1t = [sbp(f"x1t{i}", [128, D]) for i in range(2)]
            r_x1t = [R(), R()]
            st = sbp("stF", [128, 2, 6]); mv = sbp("mvF", [128, 2]); rstd = sbp("rstdF", [128, 1])
            r_st, r_mv, r_rstd = R(), R(), R()
            dst = XL if l < L - 1 else y_out
            r_dst = R()
            for t in range(NT):
                b = t % 2
                for k in range(8):
                    cx.dma("pool", None, None, reads=[r_Ys, r_idxs], writes=[r_yg[b]],
                           fn=lambda e, t=t, k=k, b=b: e.indirect_dma_start(
                               out=yg[b][:, k, :], out_offset=None, in_=Ys[:, :],
                               in_offset=bass.IndirectOffsetOnAxis(ap=idxs[:, t, k:k + 1].bitcast(U32), axis=0)))
                cx.dma("sp", acc[b][:], FFN[t * 128:(t + 1) * 128, :], reads=[r_FFN], writes=[r_acc[b]])
                cx.dma("sp", x1t[b][:], X1[t * 128:(t + 1) * 128, :], reads=[r_X1], writes=[r_x1t[b]])
                for k in range(8):
                    V(lambda e, t=t, k=k, b=b: e.scalar_tensor_tensor(out=acc[b][:], in0=yg[b][:, k, :], scalar=wk[:, t, k:k + 1], in1=acc[b][:],
                                                                    op0=ALU.mult, op1=ALU.add), [r_yg[b], r_wk, r_acc[b]], [r_acc[b]])
                G(lambda e, b=b: e.tensor_tensor(out=acc[b][:], in0=acc[b][:], in1=gF[:, 1, :], op=ALU.mult), [r_acc[b], r_gF], [r_acc[b]])
                V(lambda e, b=b: e.scalar_tensor_tensor(out=acc[b][:], in0=x1t[b][:], scalar=float(ALPHA), in1=acc[b][:], op0=ALU.mult, op1=ALU.add),
                  [r_x1t[b], r_acc[b]], [r_acc[b]])
                for hf in range(2):
                    V(lambda e, hf=hf, b=b: e.bn_stats(out=st[:, hf, :], in_=acc[b][:, hf * 512:(hf + 1) * 512]), [r_acc[b]], [r_st])
                V(lambda e: e.bn_aggr(out=mv[:], in_=st[:].rearrange("p a b -> p (a b)")), [r_st], [r_mv])
                A(lambda e: e.activation(out=rstd[:], in_=mv[:, 1:2], func=AF.Sqrt, bias=LN_EPS, scale=1.0), [r_mv], [r_rstd])
                V(lambda e: e.reciprocal(out=rstd[:], in_=rstd[:]), [r_rstd], [r_rstd])
                V(lambda e, b=b: e.tensor_scalar(out=acc[b][:], in0=acc[b][:], scalar1=mv[:, 0:1], scalar2=rstd[:, 0:1], op0=ALU.subtract, op1=ALU.mult),
                  [r_acc[b], r_mv, r_rstd], [r_acc[b]])
                G(lambda e, b=b: e.tensor_tensor(out=acc[b][:], in0=acc[b][:], in1=lnp2[:, 0, :], op=ALU.mult), [r_acc[b], r_lnp2], [r_acc[b]])
                V(lambda e, b=b: e.tensor_tensor(out=x1t[b][:], in0=acc[b][:], in1=lnp2[:, 1, :], op=ALU.add), [r_acc[b], r_lnp2, r_x1t[b]], [r_x1t[b]])
                cx.dma("sp", dst[t * 128:(t + 1) * 128, :], x1t[b][:], reads=[r_x1t[b]], writes=[r_dst])
            cx.barrier()
        if STOP_AFTER == "F":
            break

    cx.finish()
    es.close()
    return nc


def prep_shared(inp):
    w_in = np.asarray(inp["w_in"], np.float32)
    o = IN_OFF
    w_tok = np.concatenate([w_in[:, :, o["pool"]:o["pool"] + 256], w_in[:, :, o["v"]:o["v"] + 256],
                            w_in[:, :, o["o"]:o["o"] + 256]], axis=2)
    kr = w_in[:, :, o["kr"]:o["kr"] + 32]
    kr_sw = np.concatenate([kr[:, :, 16:32], kr[:, :, 0:16]], axis=2)
    misc = np.concatenate([w_in[:, :, o["gate"]:o["gate"] + 16], kr, kr_sw, np.zeros((L, D, 48), np.float32)], axis=2)
    w_fm = np.concatenate([w_in[:, :, o["q"]:o["q"] + 256], w_in[:, :, o["k"]:o["k"] + 256],
                           w_in[:, :, o["dq"]:o["dq"] + 256], w_in[:, :, o["dkv"]:o["dkv"] + 128], misc], axis=2)
    b_ada = np.asarray(inp["b_ada"], np.float32)
    bp = np.stack([b_ada[:, v * D:(v + 1) * D].reshape(L, 8, 128).transpose(0, 2, 1) for v in (0, 1, 3, 4)], axis=2)
    band = np.zeros((4, 5, 128, 128), np.float32)
    for g, w in enumerate((2, 4, 8, 16)):
        A_ = np.zeros((S, S), np.float32) if False else None
        def arow(t):
            lo = max(t - w // 2, 0); hi = min(t + w // 2, S)
            return lo, hi, 1.0 / (hi - lo)
        def fill(mat, ti, tj):
            for tl in range(128):
                t = ti * 128 + tl
                lo, hi, inv = arow(t)
                for tp in range(max(lo, tj * 128), min(hi, tj * 128 + 128)):
                    mat[tp - tj * 128, tl] += inv
                if tj * 128 <= t < tj * 128 + 128:
                    mat[t - tj * 128, tl] -= 1.0
        fill(band[g, 0], 5, 4)
        fill(band[g, 1], 5, 5)
        fill(band[g, 2], 5, 6)
        fill(band[g, 3], 0, 0)
        fill(band[g, 4], NT - 1, NT - 1)
    conv_w = np.asarray(inp["conv_w"], np.float32)
    conv_b = np.asarray(inp["conv_b"], np.float32)
    conv_p = np.concatenate([conv_w.transpose(0, 2, 1), conv_b[:, :, None]], axis=2)
    conv_p = conv_p.reshape(L, 4, 128, 6).transpose(0, 2, 1, 3)
    gate_b4 = np.asarray(inp["gate_b"], np.float32).reshape(L, 4, 4).transpose(0, 2, 1)
    sel2 = np.zeros((4, 2, 128), np.float32)
    for j in range(2):
        sel2[2 * j, j, 0:64] = 1.0
        sel2[2 * j + 1, j, 64:128] = 1.0
    masks = np.zeros((2, 128, 128), np.float32)
    masks[0] = np.triu(np.ones((128, 128), np.float32))
    masks[1] = np.tril(np.ones((128, 128), np.float32))
    gb = np.asarray(inp["gate_b"], np.float32)
    gbp = np.zeros((L, 128, 4), np.float32)
    for h in range(4):
        for k in range(4):
            gbp[:, h * 32:(h + 1) * 32, k] = gb[:, k * 4 + h][:, None]
    trih = np.zeros((2, 128, 128), np.float32)
    cmask = np.zeros((128, 64), np.float32)
    psel = np.zeros((128, 128), np.float32)
    for h in range(4):
        for c in range(32):
            p = h * 32 + c
            trih[0, h * 32:h * 32 + c, p] = 1.0
            trih[1, h * 32 + c + 1:(h + 1) * 32, p] = 1.0
            cmask[p, (h // 2) * 32 + c] = 1.0
            psel[p, (h % 2) * 64:(h % 2) * 64 + 64] = 1.0
    inv_freq = (np.float32(10000.0) ** (-np.arange(0, 32, 2, dtype=np.float32) / np.float32(32))).astype(np.float32)
    ropec = np.zeros((32, 2), np.float32)
    ropec[:, 0] = np.concatenate([inv_freq, inv_freq])
    ropec[:16, 1] = -1.0
    ropec[16:, 1] = 1.0
    w_uq = np.asarray(inp["w_uq"], np.float32)
    w_uq_sw = w_uq.copy().reshape(L, 256, 8, 96)
    w_uq_sw[:, :, :, 64:80] = w_uq.reshape(L, 256, 8, 96)[:, :, :, 80:96]
    w_uq_sw[:, :, :, 80:96] = w_uq.reshape(L, 256, 8, 96)[:, :, :, 64:80]
    w_uq_sw = w_uq_sw.reshape(L, 256, 768)
    sel64 = np.zeros((65, 64), np.float32)
    sel64[64, :] = 1.0
    lnp = np.stack([np.asarray(inp[k], np.float32) for k in ("ln1_g", "ln1_b", "ln2_g", "ln2_b")], axis=1)
    ws13 = np.concatenate([np.asarray(inp["ws1"], np.float32), np.asarray(inp["ws3"], np.float32)], axis=2)
    blkpos = np.tile((np.arange(512, dtype=np.float32) * 128.0)[None, :], (128, 1))
    sh = {
        "w_out": np.ascontiguousarray(inp["w_out"], np.float32),
        "lnp": np.ascontiguousarray(lnp),
        "w_router": np.ascontiguousarray(inp["w_router"], np.float32),
        "e_bias": np.ascontiguousarray(inp["e_bias"], np.float32),
        "ws13": np.ascontiguousarray(ws13),
        "ws2": np.ascontiguousarray(inp["ws2"], np.float32),
        "w1": np.asarray(inp["w1"], np.float32).reshape(L * 256 * 128, 2048),
        "w3": np.asarray(inp["w3"], np.float32).reshape(L * 256 * 128, 2048),
        "w2": np.asarray(inp["w2"], np.float32).reshape(L * 256 * 128, 2048),
        "ustrict": np.triu(np.ones((128, 128), np.float32), 1),
        "blkpos": blkpos,
        "pidx": np.arange(128, dtype=np.float32).reshape(128, 1),
        "ropec": ropec,
        "g_q_p": np.ascontiguousarray(np.asarray(inp["g_q"], np.float32).reshape(L, 2, 128).transpose(0, 2, 1)),
        "g_kv_p": np.ascontiguousarray(np.asarray(inp["g_kv"], np.float32).reshape(L, 128, 1)),
        "w_uq": np.ascontiguousarray(w_uq), "w_uq_sw": np.ascontiguousarray(w_uq_sw),
        "w_uk": np.ascontiguousarray(inp["w_uk"], np.float32), "w_uv": np.ascontiguousarray(inp["w_uv"], np.float32),
        "sel64": sel64,
        "gbp": gbp, "trih": trih, "cmask": cmask, "psel": psel,
        "band": band,
        "w_pool": np.ascontiguousarray(inp["w_pool"], np.float32),
        "s_pool_p": np.ascontiguousarray(np.asarray(inp["s_pool"], np.float32).reshape(L, 4, 64).transpose(0, 2, 1)),
        "conv_p": np.ascontiguousarray(conv_p),
        "gate_b4": np.ascontiguousarray(gate_b4),
        "gn_w": np.ascontiguousarray(inp["gn_w"], np.float32),
        "sel2": sel2,
        "masks": masks,
        "ident": np.eye(128, dtype=np.float32),
        "w_ada": np.ascontiguousarray(inp["w_ada"], np.float32),
        "b_ada_p": np.ascontiguousarray(bp),
        "b_ada": np.ascontiguousarray(b_ada),
        "w_in_tok": np.ascontiguousarray(w_tok),
        "w_in_fm": np.ascontiguousarray(w_fm),
    }
    return sh


def prep_core(inp, b):
    x = np.asarray(inp["x"][b], np.float32)
    c = np.asarray(inp["c"][b], np.float32)
    return {"x": np.ascontiguousarray(x), "c_p": np.ascontiguousarray(c.reshape(8, 128).T),
            "pos": np.ascontiguousarray(np.asarray(inp["positions"][b], np.int32))}


def kernel(**inp):
    nc = build_program()
    sh = prep_shared(inp)
    in_maps = []
    for b in range(8):
        m = dict(sh)
        m.update(prep_core(inp, b))
        in_maps.append(m)
    res = run_bass_kernel_spmd(nc, in_maps, core_ids=list(range(8)))
    kernel.last = res
    return np.stack([r["y"] for r in res.results], axis=0)
```

```python
from contextlib import ExitStack
import numpy as np
import concourse.bass as bass
import concourse.mybir as mybir
from concourse.bass_utils import run_bass_kernel_spmd

F32 = mybir.dt.float32
F32R = mybir.dt.float32r
BF16 = mybir.dt.bfloat16
I32 = mybir.dt.int32
U32 = mybir.dt.uint32
AF = mybir.ActivationFunctionType
ALU = mybir.AluOpType
AX = mybir.AxisListType

S = 4096
D = 1024
NT = S // 128
L = 2
ALPHA = (2 * L) ** 0.25
LN_EPS = 1e-5
RMS_EPS = 1e-6

DEBUG = {}
STOP_AFTER = None
OOB_IDX = 1048576.0
NBLK = 512


class R:
    __slots__ = ("w", "r", "name", "psum")

    def __init__(self, name="", psum=False):
        self.w = {}
        self.r = {}
        self.name = name
        self.psum = psum


class EngState:
    EPOCH = 16000

    def __init__(self, ctx, name, handle):
        self.ctx = ctx
        self.name = name
        self.h = handle
        self.sem = None
        self.count = 0
        self.own = set()
        self.seen = {}
        self.nsem = 0
        self.slots = []
        self.rr = 0

    def tick(self):
        if self.sem is None or self.count >= self.EPOCH:
            self.sem = self.ctx.new_sem(f"e_{self.name}_{self.nsem}")
            self.nsem += 1
            self.count = 0
            self.own.add(self.sem)
        self.count += 1
        return self.sem, self.count


class Ctx:
    def __init__(self, nc, es):
        self.nc = nc
        self.es = es
        self.nsems = 0
        self.E = {
            "pe": EngState(self, "pe", nc.tensor),
            "act": EngState(self, "act", nc.scalar),
            "dve": EngState(self, "dve", nc.vector),
            "pool": EngState(self, "pool", nc.gpsimd),
            "sp": EngState(self, "sp", nc.sync),
        }
        for q, n in (("sp", 40), ("pool", 40), ("act", 8)):
            self.E[q].slots = [[self.new_sem(f"d_{q}_{i}"), 0] for i in range(n)]

    def new_sem(self, name):
        self.nsems += 1
        return self.es.enter_context(self.nc.semaphore(name))

    def _waits(self, E, reads, writes, skip_own=False):
        need = {}
        for t in reads:
            for s, v in t.w.items():
                if need.get(s, 0) < v:
                    need[s] = v
            if t.psum:
                for s, v in t.r.items():
                    if s not in E.own and need.get(s, 0) < v:
                        need[s] = v
        for t in writes:
            for s, v in t.w.items():
                if need.get(s, 0) < v:
                    need[s] = v
            for s, v in t.r.items():
                if need.get(s, 0) < v:
                    need[s] = v
        for s, v in need.items():
            if skip_own and s in E.own:
                continue
            if E.seen.get(s, 0) >= v:
                continue
            E.h.wait_ge(s, v)
            E.seen[s] = v

    def op(self, eng, fn, reads=(), writes=()):
        E = self.E[eng]
        self._waits(E, reads, writes, skip_own=(eng == "pe"))
        ins = fn(E.h)
        s, v = E.tick()
        ins.then_inc(s, 1)
        for t in writes:
            t.w = {s: v}
            t.r = {}
        for t in reads:
            if t.r.get(s, 0) < v:
                t.r[s] = v
        return ins

    def dma(self, q, out, in_, reads=(), writes=(), fn=None, acc=True):
        E = self.E[q]
        if acc:
            for t in writes:
                if t.r:
                    self._waits(E, (), [t])
                    t.w = {}
                    t.r = {}
            self._waits(E, reads, ())
        else:
            self._waits(E, reads, writes)
        slot = E.slots[E.rr % len(E.slots)]
        E.rr += 1
        s = slot[0]
        if slot[1] > 0 and E.seen.get(s, 0) < 16 * slot[1]:
            E.h.wait_ge(s, 16 * slot[1])
            E.seen[s] = 16 * slot[1]
        if fn is None:
            ins = E.h.dma_start(out=out, in_=in_)
        else:
            ins = fn(E.h)
        slot[1] += 1
        v = 16 * slot[1]
        ins.then_inc(s, 16)
        for t in writes:
            if acc:
                t.w[s] = v
            else:
                t.w = {s: v}
                t.r = {}
        for t in reads:
            if t.r.get(s, 0) < v:
                t.r[s] = v
        return ins

    def barrier(self):
        marks = []
        for e in self.E.values():
            if e.sem is not None and e.count > 0:
                marks.append((e.sem, e.count))
            for s, n in e.slots:
                if n > 0:
                    marks.append((s, 16 * n))
        for E in self.E.values():
            for s, v in marks:
                if s in E.own and E.name == "pe":
                    pass
                if E.seen.get(s, 0) < v:
                    E.h.wait_ge(s, v)
                    E.seen[s] = v

    def finish(self, extra=()):
        E = self.E["sp"]
        for e in self.E.values():
            if e.sem is not None and e.count > 0 and E.seen.get(e.sem, 0) < e.count:
                E.h.wait_ge(e.sem, e.count)
            for s, n in e.slots:
                if n > 0 and E.seen.get(s, 0) < 16 * n:
                    E.h.wait_ge(s, 16 * n)


def r32(ap):
    return ap.bitcast(F32R)


IN_OFF = dict(pool=0, q=256, k=512, v=768, o=1024, gate=1280, dq=1296, dkv=1552, kr=1680)


def build_program():
    nc = bass.Bass("TRN2", target_bir_lowering=False)
    es = ExitStack()
    cx = Ctx(nc, es)

    def din(name, shape, dt=F32):
        return nc.dram_tensor(name, list(shape), dt, kind="ExternalInput").ap()

    def dscr(name, shape, dt=F32):
        kind = "ExternalOutput" if DEBUG.get(name) else "Internal"
        return nc.dram_tensor(name, list(shape), dt, kind=kind).ap()

    def sb(name, shape, dt=F32):
        return es.enter_context(nc.sbuf_tensor(name, list(shape), dt))

    def ps(name, shape, dt=F32):
        return es.enter_context(nc.psum_tensor(name, list(shape), dt))

    x_in = din("x", [S, D])
    c_in = din("c_p", [128, 8])
    ident_in = din("ident", [128, 128])
    w_ada = din("w_ada", [L, D, 6 * D])
    b_ada_p = din("b_ada_p", [L, 128, 4, 8])
    b_ada = din("b_ada", [L, 6 * D])
    w_in_tok = din("w_in_tok", [L, D, 768])
    w_in_fm = din("w_in_fm", [L, D, 1024])
    band_in = din("band", [4, 5, 128, 128])
    w_pool_in = din("w_pool", [L, 4, 64, 64])
    s_pool_p = din("s_pool_p", [L, 64, 4])
    conv_p = din("conv_p", [L, 128, 4, 6])
    gate_b_in = din("gate_b4", [L, 4, 4])
    gn_w_in = din("gn_w", [L, 256])
    sel2_in = din("sel2", [4, 2, 128])
    mask_in = din("masks", [2, 128, 128])
    gbp_in = din("gbp", [L, 128, 4])
    pos_in = din("pos", [S], I32)
    ropec_in = din("ropec", [32, 2])
    g_q_p = din("g_q_p", [L, 128, 2])
    g_kv_p = din("g_kv_p", [L, 128, 1])
    w_uq_in = din("w_uq", [L, 256, 768])
    w_uq_sw_in = din("w_uq_sw", [L, 256, 768])
    w_uk_in = din("w_uk", [L, 128, 512])
    w_uv_in = din("w_uv", [L, 128, 512])
    sel64_in = din("sel64", [65, 64])
    w_out_in = din("w_out", [L, D, D])
    lnp_in = din("lnp", [L, 4, D])
    w_router_in = din("w_router", [L, D, 256])
    e_bias_in = din("e_bias", [L, 256])
    ws13_in = din("ws13", [L, D, 512])
    ws2_in = din("ws2", [L, 256, D])
    w1_in = din("w1", [L * 256 * 128, 2048])
    w3_in = din("w3", [L * 256 * 128, 2048])
    w2_in = din("w2", [L * 256 * 128, 2048])
    ustrict_in = din("ustrict", [128, 128])
    blkpos_in = din("blkpos", [128, 512])
    pidx_in = din("pidx", [128, 1])
    trih_in = din("trih", [2, 128, 128])
    cmask_in = din("cmask", [128, 64])
    psel_in = din("psel", [128, 128])
    y_out = nc.dram_tensor("y", [S, D], F32, kind="ExternalOutput").ap()

    U_tok = dscr("U_tok", [S, 768])
    U_fm = dscr("U_fm", [1024, S])
    mixT = dscr("mixT", [1024, S])
    ropeT = dscr("ropeT", [2, 32, S])
    X1 = dscr("X1", [S, D])
    XL = dscr("XL", [S, D])
    H2 = dscr("H2", [S, D], BF16)
    MS = dscr("MS", [S, 256], BF16)
    WN = dscr("WN", [S, 256])
    FFN = dscr("FFN", [S, D])
    NSLOT = 512 * 128
    Xs = dscr("Xs", [NSLOT, D], BF16)
    Ys = dscr("Ys", [NSLOT, D], BF16)

    bc_reg = nc.gpsimd.alloc_register("bc_reg")
    nc.gpsimd.reg_mov(bc_reg, L * 256 * 128 - 1)
    ident = sb("ident_sb", [128, 128])
    r_ident = R("ident")
    cx.dma("sp", ident[:], ident_in[:, :], writes=[r_ident])
    identb = sb("identb_g", [128, 128], BF16)
    r_identb = R("identb")
    cx.op("dve", lambda e: e.tensor_copy(out=identb[:], in_=ident[:]), [r_ident], [r_identb])
    cact = sb("cact", [128, 8])
    cact_bc = sb("cact_bc", [128, 8, 128])
    r_cact = R("cact")
    craw = sb("craw", [128, 8])
    r_craw = R()
    cx.dma("sp", craw[:], c_in[:, :], writes=[r_craw])
    cx.op("act", lambda e: e.activation(out=cact[:], in_=craw[:], func=AF.Silu), reads=[r_craw], writes=[r_cact])
    r_cbc = R()
    for kc in range(8):
        cx.op("dve", lambda e, kc=kc: e.tensor_copy(out=cact_bc[:, kc, :], in_=cact[:, kc:kc + 1].to_broadcast([128, 128])),
              reads=[r_cact], writes=[r_cbc])

    def V(fn, r=(), w=()):
        return cx.op("dve", fn, r, w)

    def A(fn, r=(), w=()):
        return cx.op("act", fn, r, w)

    def P(fn, r=(), w=()):
        return cx.op("pe", fn, r, w)

    def G(fn, r=(), w=()):
        return cx.op("pool", fn, r, w)

    r_ropeT = R()
    with ExitStack() as ph:
        def sbp(name, shape, dt=F32):
            return ph.enter_context(nc.sbuf_tensor(f"rp_{name}", list(shape), dt))
        posi = sbp("posi", [32, S], I32)
        ang = sbp("ang", [32, S])
        kf = sbp("kf", [32, S])
        ki = sbp("ki", [32, S], I32)
        rr_ = sbp("rr", [32, S])
        mm = sbp("mm", [32, S])
        ropec = sbp("ropec", [32, 2])
        r_rp = R()
        cx.dma("sp", posi[:], pos_in.partition_broadcast(32), writes=[r_rp])
        cx.dma("sp", ropec[:], ropec_in[:, :], writes=[r_rp])
        V(lambda e: e.tensor_copy(out=ang[:], in_=posi[:]), [r_rp], [r_rp])
        V(lambda e: e.tensor_scalar(out=ang[:], in0=ang[:], scalar1=ropec[:, 0:1], scalar2=None, op0=ALU.mult), [r_rp], [r_rp])
        TWO_PI = 2.0 * np.pi
        C1 = 6.28125
        C2 = TWO_PI - C1
        PI_LO = 3.1415925
        for tb in range(2):
            src = ang
            if tb == 1:
                V(lambda e: e.tensor_scalar(out=mm[:], in0=ang[:], scalar1=float(np.pi / 2), scalar2=None, op0=ALU.add), [r_rp], [r_rp])
                src = mm
            V(lambda e, src=src: e.tensor_scalar(out=kf[:], in0=src[:], scalar1=float(1.0 / TWO_PI), scalar2=None, op0=ALU.mult), [r_rp], [r_rp])
            V(lambda e: e.tensor_copy(out=ki[:], in_=kf[:]), [r_rp], [r_rp])
            V(lambda e: e.tensor_copy(out=kf[:], in_=ki[:]), [r_rp], [r_rp])
            V(lambda e, src=src: e.scalar_tensor_tensor(out=rr_[:], in0=kf[:], scalar=-C1, in1=src[:], op0=ALU.mult, op1=ALU.add), [r_rp], [r_rp])
            V(lambda e: e.scalar_tensor_tensor(out=rr_[:], in0=kf[:], scalar=-C2, in1=rr_[:], op0=ALU.mult, op1=ALU.add), [r_rp], [r_rp])
            V(lambda e: e.tensor_scalar(out=kf[:], in0=rr_[:], scalar1=float(np.pi), scalar2=None, op0=ALU.is_gt), [r_rp], [r_rp])
            V(lambda e: e.scalar_tensor_tensor(out=rr_[:], in0=kf[:], scalar=-TWO_PI, in1=rr_[:], op0=ALU.mult, op1=ALU.add), [r_rp], [r_rp])
            V(lambda e: e.tensor_scalar(out=kf[:], in0=rr_[:], scalar1=float(-np.pi), scalar2=None, op0=ALU.is_lt), [r_rp], [r_rp])
            V(lambda e: e.scalar_tensor_tensor(out=rr_[:], in0=kf[:], scalar=TWO_PI, in1=rr_[:], op0=ALU.mult, op1=ALU.add), [r_rp], [r_rp])
            V(lambda e: e.tensor_scalar(out=rr_[:], in0=rr_[:], scalar1=PI_LO, scalar2=-PI_LO, op0=ALU.min, op1=ALU.max), [r_rp], [r_rp])
            A(lambda e: e.activation(out=rr_[:], in_=rr_[:], func=AF.Sin), [r_rp], [r_rp])
            if tb == 0:
                V(lambda e: e.tensor_scalar(out=rr_[:], in0=rr_[:], scalar1=ropec[:, 1:2], scalar2=None, op0=ALU.mult), [r_rp], [r_rp])
            cx.dma("sp", ropeT[tb], rr_[:], reads=[r_rp], writes=[r_ropeT])
        cx.barrier()

    adaP = sb("adaP", [128, 4, 8])
    gF = sb("gF", [128, 4, D])
    r_adaP = R()
    r_gF = R()
    for l in range(L):
        with ExitStack() as ph:
            def sbp(name, shape, dt=F32):
                return ph.enter_context(nc.sbuf_tensor(f"{name}_{l}", list(shape), dt))

            def psp(name, shape, dt=F32):
                return ph.enter_context(nc.psum_tensor(f"{name}_{l}", list(shape), dt))
            wa = [sbp(f"wa{i}", [128, 8, D]) for i in range(2)]
            r_wa = [R(), R()]
            badap = sbp("badap", [128, 4, 8])
            r_badap = R()
            cx.dma("sp", badap[:], b_ada_p[l], writes=[r_badap])
            bfr = sbp("bfr", [128, 4, D])
            r_bfr = R()
            GIDX = {2: 0, 5: 1, 3: 2, 4: 3}
            PIDX = {0: 0, 1: 1, 3: 2, 4: 3}
            for v, j in GIDX.items():
                cx.dma("sp", bfr[:, j, :], b_ada[l, v * D:(v + 1) * D].partition_broadcast(128), writes=[r_bfr])
            pA = psp("pA", [128, 512])
            r_pA = R(psum=True)
            pG = [psp(f"pG{i}", [128, 512]) for i in range(2)]
            r_pG = [R(psum=True), R(psum=True)]
            order = [0, 1, 3, 4, 2, 5]
            for i, v in enumerate(order):
                cx.dma("sp", wa[i % 2][:], w_ada[l, :, v * D:(v + 1) * D].rearrange("(kc p) n -> p kc n", p=128),
                       writes=[r_wa[i % 2]])
                w = wa[i % 2]
                if v in PIDX:
                    pi = PIDX[v]
                    for ncn in range(8):
                        for kc in range(8):
                            cx.op("pe", lambda e, w=w, ncn=ncn, kc=kc, pi=pi: e.matmul(
                                pA[:, pi * 8 + ncn:pi * 8 + ncn + 1], lhsT=w[:, kc, ncn * 128:(ncn + 1) * 128],
                                rhs=cact[:, kc:kc + 1], start=(kc == 0), stop=(kc == 7)),
                                reads=[r_wa[i % 2], r_cact], writes=[r_pA])
                if v in GIDX:
                    j = GIDX[v]
                    for hf in range(2):
                        for kc in range(8):
                            cx.op("pe", lambda e, w=w, hf=hf, kc=kc: e.matmul(
                                pG[hf][:, :], lhsT=cact_bc[:, kc, :], rhs=w[:, kc, hf * 512:(hf + 1) * 512],
                                start=(kc == 0), stop=(kc == 7)),
                                reads=[r_wa[i % 2], r_cbc], writes=[r_pG[hf]])
                        cx.op("dve", lambda e, hf=hf, j=j: e.tensor_tensor(
                            out=gF[:, j, hf * 512:(hf + 1) * 512], in0=pG[hf][:, :], in1=bfr[:, j, hf * 512:(hf + 1) * 512], op=ALU.add),
                            reads=[r_pG[hf], r_bfr], writes=[r_gF])
            cx.op("dve", lambda e: e.tensor_tensor(out=adaP[:].rearrange("p a b -> p (a b)"), in0=pA[:, 0:32],
                                                   in1=badap[:].rearrange("p a b -> p (a b)"), op=ALU.add),
                  reads=[r_pA, r_badap], writes=[r_adaP])
            for v in (1, 3):
                cx.op("dve", lambda e, v=v: e.tensor_scalar_add(out=adaP[:, v, :], in0=adaP[:, v, :], scalar1=1.0),
                      reads=[r_adaP], writes=[r_adaP])
            cx.op("dve", lambda e: e.tensor_scalar_add(out=gF[:, 3, :], in0=gF[:, 3, :], scalar1=1.0), reads=[r_gF], writes=[r_gF])
            cx.barrier()

        with ExitStack() as ph:
            def sbp(name, shape, dt=F32):
                return ph.enter_context(nc.sbuf_tensor(f"{name}_{l}", list(shape), dt))

            def psp(name, shape, dt=F32):
                return ph.enter_context(nc.psum_tensor(f"{name}_{l}", list(shape), dt))
            wtok = sbp("wtok", [128, 8, 768], BF16)
            wfm = sbp("wfm", [128, 8, 1024], BF16)
            r_wtok, r_wfm = R(), R()
            for kc in range(8):
                cx.dma("pool", wtok[:, kc, :], w_in_tok[l, kc * 128:(kc + 1) * 128, :], writes=[r_wtok])
                cx.dma("pool", wfm[:, kc, :], w_in_fm[l, kc * 128:(kc + 1) * 128, :], writes=[r_wfm])
            xt = [sbp(f"xt{i}", [128, D]) for i in range(2)]
            r_xt = [R(), R()]
            xn = [sbp(f"xn{i}", [128, D]) for i in range(2)]
            r_xn = [R(), R()]
            st = sbp("st", [128, 2, 6])
            mv = sbp("mv", [128, 2])
            rstd = sbp("rstd", [128, 1])
            r_st, r_mv, r_rstd = R(), R(), R()
            hT = [sbp(f"hT{i}", [128, 8, 512], BF16) for i in range(2)]
            r_hT = [R(), R()]
            pT = [psp(f"pT{i}", [128, 512]) for i in range(2)]
            r_pT = [R(psum=True), R(psum=True)]
            pU = [psp(f"pU{i}", [128, 512]) for i in range(4)]
            r_pU = [R(psum=True) for _ in range(4)]
            uo = [sbp(f"uo{i}", [128, 512]) for i in range(4)]
            r_uo = [R() for _ in range(4)]
            r_Utok, r_Ufm = R(), R()
            src = x_in if l == 0 else XL
            npu = 0
            for g in range(8):
                hTg = hT[g % 2]
                r_hTg = r_hT[g % 2]
                for tt in range(4):
                    t = g * 4 + tt
                    b = t % 2
                    cx.dma("sp", xt[b][:], src[t * 128:(t + 1) * 128, :], writes=[r_xt[b]])
                    for hf in range(2):
                        cx.op("dve", lambda e, b=b, hf=hf: e.bn_stats(out=st[:, hf, :], in_=xt[b][:, hf * 512:(hf + 1) * 512]),
                              reads=[r_xt[b]], writes=[r_st])
                    cx.op("dve", lambda e: e.bn_aggr(out=mv[:], in_=st[:].rearrange("p a b -> p (a b)")), reads=[r_st], writes=[r_mv])
                    cx.op("act", lambda e: e.activation(out=rstd[:], in_=mv[:, 1:2], func=AF.Sqrt, bias=LN_EPS, scale=1.0),
                          reads=[r_mv], writes=[r_rstd])
                    cx.op("dve", lambda e: e.reciprocal(out=rstd[:], in_=rstd[:]), reads=[r_rstd], writes=[r_rstd])
                    cx.op("dve", lambda e, b=b: e.tensor_scalar(out=xn[b][:], in0=xt[b][:], scalar1=mv[:, 0:1], scalar2=rstd[:, 0:1],
                                                                 op0=ALU.subtract, op1=ALU.mult),
                          reads=[r_xt[b], r_mv, r_rstd], writes=[r_xn[b]])
                    for q4 in range(2):
                        pt = pT[q4]
                        for k4 in range(4):
                            kc = q4 * 4 + k4
                            cx.op("pe", lambda e, pt=pt, k4=k4, kc=kc, b=b: e.transpose(
                                out=pt[:, k4 * 128:(k4 + 1) * 128], in_=xn[b][:, kc * 128:(kc + 1) * 128], identity=ident[:]),
                                reads=[r_xn[b], r_ident], writes=[r_pT[q4]])
                        for k4 in range(4):
                            kc = q4 * 4 + k4
                            cx.op("act", lambda e, pt=pt, k4=k4, kc=kc, tt=tt, hTg=hTg: e.activation(
                                out=hTg[:, kc, tt * 128:(tt + 1) * 128], in_=pt[:, k4 * 128:(k4 + 1) * 128], func=AF.Identity,
                                bias=adaP[:, 0, kc:kc + 1], scale=adaP[:, 1, kc:kc + 1]),
                                reads=[r_pT[q4], r_adaP], writes=[r_hTg])
                for tt in range(4):
                    t = g * 4 + tt
                    for hf in range(2):
                        i = npu % 4
                        npu += 1
                        for kc in range(8):
                            cx.op("pe", lambda e, i=i, kc=kc, tt=tt, hf=hf, hTg=hTg: e.matmul(
                                pU[i][:, 0:384], lhsT=hTg[:, kc, tt * 128:(tt + 1) * 128],
                                rhs=wtok[:, kc, hf * 384:(hf + 1) * 384], start=(kc == 0), stop=(kc == 7)),
                                reads=[r_hTg, r_wtok], writes=[r_pU[i]])
                        cx.op("act" if i % 2 else "dve", lambda e, i=i: (e.copy(out=uo[i][:, 0:384], in_=pU[i][:, 0:384]) if i % 2
                                                                          else e.tensor_copy(out=uo[i][:, 0:384], in_=pU[i][:, 0:384])),
                              reads=[r_pU[i]], writes=[r_uo[i]])
                        cx.dma("pool", U_tok[t * 128:(t + 1) * 128, hf * 384:(hf + 1) * 384], uo[i][:, 0:384],
                               reads=[r_uo[i]], writes=[r_Utok])
                for cb in range(8):
                    i = npu % 4
                    npu += 1
                    for kc in range(8):
                        cx.op("pe", lambda e, i=i, kc=kc, cb=cb, hTg=hTg: e.matmul(
                            pU[i][:, :], lhsT=wfm[:, kc, cb * 128:(cb + 1) * 128], rhs=hTg[:, kc, :],
                            start=(kc == 0), stop=(kc == 7)),
                            reads=[r_hTg, r_wfm], writes=[r_pU[i]])
                    cx.op("act" if i % 2 else "dve", lambda e, i=i: (e.copy(out=uo[i][:, :], in_=pU[i][:, :]) if i % 2
                                                                      else e.tensor_copy(out=uo[i][:, :], in_=pU[i][:, :])),
                          reads=[r_pU[i]], writes=[r_uo[i]])
                    cx.dma("pool", U_fm[cb * 128:(cb + 1) * 128, g * 512:(g + 1) * 512], uo[i][:, :],
                           reads=[r_uo[i]], writes=[r_Ufm])
            cx.barrier()
        if STOP_AFTER == "A":
            break
        r_mixT = R()

        def V(fn, r=(), w=()):
            return cx.op("dve", fn, r, w)

        def A(fn, r=(), w=()):
            return cx.op("act", fn, r, w)

        def P(fn, r=(), w=()):
            return cx.op("pe", fn, r, w)

        def G(fn, r=(), w=()):
            return cx.op("pool", fn, r, w)

        with ExitStack() as ph:
            def sbp(name, shape, dt=F32):
                return ph.enter_context(nc.sbuf_tensor(f"{name}_{l}", list(shape), dt))

            def psp(name, shape, dt=F32):
                return ph.enter_context(nc.psum_tensor(f"{name}_{l}", list(shape), dt))
            band = sbp("band", [128, 20, 128], BF16)
            r_band = R()
            cx.dma("pool", band[:], band_in.rearrange("g k p n -> p (g k) n"), writes=[r_band])
            wpl = sbp("wpl", [64, 4, 64], BF16)
            r_wpl = R()
            cx.dma("pool", wpl[:], w_pool_in[l].rearrange("g c d -> c g d"), writes=[r_wpl])
            spl = sbp("spl", [64, 4])
            r_spl = R()
            cx.dma("sp", spl[:], s_pool_p[l], writes=[r_spl])
            up = sbp("up", [128, NT, 256], BF16)
            r_up = R()
            for t4 in range(0, NT, 8):
                cx.dma("pool", up[:, t4:t4 + 8, :], U_tok[t4 * 128:(t4 + 8) * 128, 0:256].rearrange("(t p) c -> p t c", p=128),
                       reads=[r_Utok], writes=[r_up])
            pd = [psp(f"pd{i}", [64, 512]) for i in range(2)]
            py = [psp(f"py{i}", [64, 512]) for i in range(2)]
            r_pd = [R(psum=True), R(psum=True)]
            r_py = [R(psum=True), R(psum=True)]
            dTs = [sbp(f"dTs{i}", [64, 512], BF16) for i in range(2)]
            r_dTs = [R(), R()]
            yp = [sbp(f"yp{i}", [64, 4, 512]) for i in range(2)]
            r_yp = [R(), R()]
            n = 0
            for Gq in range(8):
                ypq = yp[Gq % 2]
                r_ypq = r_yp[Gq % 2]
                for g in range(4):
                    i = n % 2
                    n += 1
                    for tt in range(4):
                        t = Gq * 4 + tt
                        srcs = []
                        if t > 0:
                            srcs.append((t - 1, 0))
                        srcs.append((t, 3 if t == 0 else (4 if t == NT - 1 else 1)))
                        if t < NT - 1:
                            srcs.append((t + 1, 2))
                        for k, (j, typ) in enumerate(srcs):
                            P(lambda e, i=i, tt=tt, j=j, g=g, typ=typ, k=k, last=len(srcs) - 1: e.matmul(
                                pd[i][:, tt * 128:(tt + 1) * 128], lhsT=up[:, j, g * 64:(g + 1) * 64], rhs=band[:, g * 5 + typ, :],
                                start=(k == 0), stop=(k == last)), [r_up, r_band], [r_pd[i]])
                    A(lambda e, i=i: e.copy(out=dTs[i][:], in_=pd[i][:]), [r_pd[i]], [r_dTs[i]])
                    P(lambda e, i=i, g=g: e.matmul(py[i][:, :], lhsT=wpl[:, g, :], rhs=dTs[i][:], start=True, stop=True),
                      [r_wpl, r_dTs[i]], [r_py[i]])
                    V(lambda e, i=i, g=g, ypq=ypq: e.tensor_scalar(out=ypq[:, g, :], in0=py[i][:], scalar1=spl[:, g:g + 1], scalar2=None,
                                                                 op0=ALU.mult), [r_py[i], r_spl], [r_ypq])
                cx.dma("sp", mixT[0:256, Gq * 512:(Gq + 1) * 512].rearrange("(g c) t -> c g t", c=64), ypq[:],
                       reads=[r_ypq], writes=[r_mixT])
            cx.barrier()
        if STOP_AFTER == "B":
            break
        with ExitStack() as ph:
            def sbp(name, shape, dt=F32):
                return ph.enter_context(nc.sbuf_tensor(f"{name}_{l}", list(shape), dt))
            qkT = sbp("qkT", [128, 4, S], BF16)
            r_qkT = R()
            vx = sbp("vx", [128, NT, 4, 65], BF16)
            r_vx = R()
            hacc = sbp("hacc", [128, NT, 256])
            r_hacc = [R() for _ in range(NT)]
            gTcf = [sbp(f"gTcf{d}", [128, 4, NT]) for d in range(2)]
            gTcl = [sbp(f"gTcl{d}", [128, 4, NT]) for d in range(2)]
            decb = [sbp(f"decb{d}", [128, 2, NT]) for d in range(2)]
            r_gT = [R(), R()]
            maskt = sbp("maskt", [128, 2, 128])
            r_mask = R()
            cx.dma("sp", maskt[:], mask_in.rearrange("d p n -> p d n"), writes=[r_mask])
            with ExitStack() as ph2:
                def sb2(name, shape, dt=F32):
                    return ph2.enter_context(nc.sbuf_tensor(f"{name}_{l}", list(shape), dt))
                convp = sb2("convp", [128, 4, 6])
                r_convp = R()
                cx.dma("sp", convp[:], conv_p[l], writes=[r_convp])
                cin = [sb2(f"cin{i}", [128, S + 4]) for i in range(2)]
                r_cin = [R(), R()]
                acc = sb2("cacc", [128, S])
                r_acc = R()
                for i in range(2):
                    G(lambda e, i=i: e.memset(cin[i][:, 0:2], 0.0), [], [r_cin[i]])
                    G(lambda e, i=i: e.memset(cin[i][:, S + 2:S + 4], 0.0), [], [r_cin[i]])
                for ch in range(4):
                    b = ch % 2
                    cx.dma("sp", cin[b][:, 2:S + 2], U_fm[ch * 128:(ch + 1) * 128, :], reads=[r_Ufm], writes=[r_cin[b]])
                    V(lambda e, b=b, ch=ch: e.tensor_scalar(out=acc[:], in0=cin[b][:, 0:S], scalar1=convp[:, ch, 0:1], scalar2=convp[:, ch, 5:6],
                                                           op0=ALU.mult, op1=ALU.add), [r_cin[b], r_convp], [r_acc])
                    for j in range(1, 5):
                        V(lambda e, b=b, ch=ch, j=j: e.scalar_tensor_tensor(out=acc[:], in0=cin[b][:, j:j + S], scalar=convp[:, ch, j:j + 1],
                                                                          in1=acc[:], op0=ALU.mult, op1=ALU.add), [r_cin[b], r_convp, r_acc], [r_acc])
                    A(lambda e, ch=ch: e.activation(out=qkT[:, ch, :], in_=acc[:], func=AF.Silu), [r_acc], [r_qkT])
                vtmp = [sb2(f"vtmp{i}", [128, 8, 256]) for i in range(2)]
                r_vtmp = [R(), R()]
                G(lambda e: e.memset(vx[:, :, :, 64:65], 1.0), [], [r_vx])
                for i4 in range(4):
                    b = i4 % 2
                    cx.dma("sp", vtmp[b][:], U_tok[i4 * 1024:(i4 + 1) * 1024, 256:512].rearrange("(t p) c -> p t c", p=128),
                           reads=[r_Utok], writes=[r_vtmp[b]])
                    A(lambda e, b=b, i4=i4: e.copy(out=vx[:, i4 * 8:(i4 + 1) * 8, :, 0:64], in_=vtmp[b][:].rearrange("p t (h c) -> p t h c", c=64)),
                      [r_vtmp[b]], [r_vx])
                cx.barrier()
            with ExitStack() as ph2:
                def sb2(name, shape, dt=F32):
                    return ph2.enter_context(nc.sbuf_tensor(f"{name}_{l}", list(shape), dt))

                def ps2(name, shape, dt=F32):
                    return ph2.enter_context(nc.psum_tensor(f"{name}_{l}", list(shape), dt))
                gbp = sb2("gbp", [128, 4])
                ngb = sb2("ngb", [128, 4])
                r_gbp = R()
                cx.dma("sp", gbp[:], gbp_in[l], writes=[r_gbp])
                V(lambda e: e.tensor_scalar(out=ngb[:], in0=gbp[:], scalar1=-1.0, scalar2=None, op0=ALU.mult), [r_gbp], [r_gbp])
                trih = sb2("trih", [128, 2, 128])
                cmask = sb2("cmask", [128, 64])
                psel = sb2("psel", [128, 128])
                r_cst = R()
                cx.dma("sp", trih[:], trih_in.rearrange("d p n -> p d n"), writes=[r_cst])
                cx.dma("sp", cmask[:], cmask_in[:, :], writes=[r_cst])
                cx.dma("sp", psel[:], psel_in[:, :], writes=[r_cst])
                pg = ps2("pg", [128, 512])
                r_pg = R(psum=True)
                for d in range(2):
                    gi = sb2(f"gi{d}", [128, 128]); gf = sb2(f"gf{d}", [128, 128])
                    r_g = R()
                    ki, kf = 2 * d, 2 * d + 1
                    cx.dma("sp", gi[:], U_fm[896 + ki * 4:896 + ki * 4 + 4, :].rearrange("h (c l) -> (h c) l", l=128), reads=[r_Ufm], writes=[r_g])
                    cx.dma("sp", gf[:], U_fm[896 + kf * 4:896 + kf * 4 + 4, :].rearrange("h (c l) -> (h c) l", l=128), reads=[r_Ufm], writes=[r_g])
                    spt = sb2(f"spt{d}", [128, 128]); Pl = sb2(f"Pl{d}", [128, 128]); Pc = sb2(f"Pc{d}", [128, 128]); at = sb2(f"at{d}", [128, 128])
                    cols = sb2(f"cols{d}", [128, 8])
                    rows = sb2(f"rows{d}", [1, 4, 128])
                    r_w = R()
                    A(lambda e, kf=kf: e.activation(out=spt[:], in_=gf[:], func=AF.Exp, bias=ngb[:, kf:kf + 1], scale=-1.0), [r_g, r_gbp], [r_w])
                    A(lambda e: e.activation(out=spt[:], in_=spt[:], func=AF.Ln, bias=1.0, scale=1.0), [r_w], [r_w])
                    V(lambda e: e.tensor_tensor_scan(out=Pl[:], data0=spt[:], data1=spt[:], initial=0.0, op0=ALU.add, op1=ALU.max), [r_w], [r_w])
                    V(lambda e: e.tensor_copy(out=cols[:, 0:1], in_=Pl[:, 127:128]), [r_w], [r_w])
                    P(lambda e, d=d: e.matmul(pg[:, 0:1], lhsT=trih[:, d, :], rhs=cols[:, 0:1], start=True, stop=True), [r_w, r_cst], [r_pg])
                    V(lambda e: e.tensor_copy(out=cols[:, 1:2], in_=pg[:, 0:1]), [r_pg], [r_w])
                    if d == 0:
                        V(lambda e: e.tensor_scalar(out=Pc[:], in0=Pl[:], scalar1=cols[:, 1:2], scalar2=None, op0=ALU.add), [r_w], [r_w])
                    else:
                        V(lambda e: e.scalar_tensor_tensor(out=Pc[:], in0=Pl[:], scalar=-1.0, in1=spt[:], op0=ALU.mult, op1=ALU.add), [r_w], [r_w])
                        V(lambda e: e.tensor_scalar(out=Pc[:], in0=Pc[:], scalar1=cols[:, 0:1], scalar2=cols[:, 1:2], op0=ALU.add, op1=ALU.add), [r_w], [r_w])
                    V(lambda e, ki=ki: e.scalar_tensor_tensor(out=at[:], in0=gi[:], scalar=gbp[:, ki:ki + 1], in1=Pc[:], op0=ALU.add, op1=ALU.add),
                      [r_w, r_g, r_gbp], [r_w])
                    V(lambda e: e.tensor_reduce(out=cols[:, 2:3], in_=at[:], axis=AX.X, op=ALU.max), [r_w], [r_w])
                    P(lambda e: e.transpose(out=pg[0:1, 0:128], in_=cols[:, 2:3], identity=ident[:]), [r_w, r_ident], [r_pg])
                    V(lambda e: e.tensor_copy(out=rows[:, 0, :], in_=pg[0:1, 0:128]), [r_pg], [r_w])
                    cur = 0
                    for sh in (1, 2, 4, 8, 16):
                        a_ = rows[:, cur, :].rearrange("p (h c) -> p h c", c=32)
                        b_ = rows[:, 1 - cur, :].rearrange("p (h c) -> p h c", c=32)
                        if d == 0:
                            V(lambda e, a_=a_, b_=b_, sh=sh: e.tensor_tensor(out=b_[:, :, sh:], in0=a_[:, :, sh:], in1=a_[:, :, :32 - sh], op=ALU.max), [r_w], [r_w])
                            V(lambda e, a_=a_, b_=b_, sh=sh: e.tensor_copy(out=b_[:, :, :sh], in_=a_[:, :, :sh]), [r_w], [r_w])
                        else:
                            V(lambda e, a_=a_, b_=b_, sh=sh: e.tensor_tensor(out=b_[:, :, :32 - sh], in0=a_[:, :, :32 - sh], in1=a_[:, :, sh:], op=ALU.max), [r_w], [r_w])
                            V(lambda e, a_=a_, b_=b_, sh=sh: e.tensor_copy(out=b_[:, :, 32 - sh:], in_=a_[:, :, 32 - sh:]), [r_w], [r_w])
                        cur = 1 - cur
                    Mr = rows[:, cur, :].rearrange("p (h c) -> p h c", c=32)
                    dd = rows[:, 2, :].rearrange("p (h c) -> p h c", c=32)
                    V(lambda e: e.memset(rows[:, 2, :], 0.0), [r_w], [r_w])
                    if d == 0:
                        V(lambda e, Mr=Mr, dd=dd: e.tensor_tensor(out=dd[:, :, 0:31], in0=Mr[:, :, 0:31], in1=Mr[:, :, 1:32], op=ALU.subtract), [r_w], [r_w])
                    else:
                        V(lambda e, Mr=Mr, dd=dd: e.tensor_tensor(out=dd[:, :, 1:32], in0=Mr[:, :, 1:32], in1=Mr[:, :, 0:31], op=ALU.subtract), [r_w], [r_w])
                    A(lambda e: e.activation(out=rows[:, 3, :], in_=rows[:, 2, :], func=AF.Exp), [r_w], [r_w])
                    P(lambda e, cur=cur: e.transpose(out=pg[:, 0:1], in_=rows[0:1, cur, :], identity=ident[0:1, 0:1]), [r_w, r_ident], [r_pg])
                    P(lambda e: e.transpose(out=pg[:, 1:2], in_=rows[0:1, 3, :], identity=ident[0:1, 0:1]), [r_w, r_ident], [r_pg])
                    V(lambda e: e.tensor_copy(out=cols[:, 3:4], in_=pg[:, 0:1]), [r_pg], [r_w])
                    V(lambda e: e.tensor_copy(out=cols[:, 6:7], in_=pg[:, 1:2]), [r_pg], [r_w])
                    V(lambda e: e.tensor_scalar(out=cols[:, 4:5], in0=cols[:, 3:4], scalar1=-1.0, scalar2=None, op0=ALU.mult), [r_w], [r_w])
                    V(lambda e: e.tensor_scalar(out=cols[:, 5:6], in0=cols[:, 3:4], scalar1=-1.0, scalar2=-float(np.log(8.0)), op0=ALU.mult, op1=ALU.add), [r_w], [r_w])
                    A(lambda e: e.activation(out=at[:], in_=at[:], func=AF.Exp, bias=cols[:, 5:6], scale=1.0), [r_w], [r_w])
                    A(lambda e: e.activation(out=Pc[:], in_=Pc[:], func=AF.Exp, bias=cols[:, 4:5], scale=1.0), [r_w], [r_w])
                    P(lambda e: e.transpose(out=pg[:, 0:128], in_=at[:], identity=ident[:]), [r_w, r_ident], [r_pg])
                    P(lambda e: e.transpose(out=pg[:, 128:256], in_=Pc[:], identity=ident[:]), [r_w, r_ident], [r_pg])
                    V(lambda e, d=d: e.tensor_copy(out=gTcf[d][:].rearrange("p h c -> p (h c)"), in_=pg[:, 0:128]), [r_pg], [r_gT[d]])
                    V(lambda e, d=d: e.tensor_copy(out=gTcl[d][:].rearrange("p h c -> p (h c)"), in_=pg[:, 128:256]), [r_pg], [r_gT[d]])
                    V(lambda e: e.tensor_scalar(out=spt[:, 0:64], in0=cmask[:], scalar1=cols[:, 6:7], scalar2=None, op0=ALU.mult), [r_w, r_cst], [r_w])
                    P(lambda e: e.matmul(pg[:, 256:320], lhsT=psel[:], rhs=spt[:, 0:64], start=True, stop=True), [r_w, r_cst], [r_pg])
                    V(lambda e, d=d: e.tensor_copy(out=decb[d][:].rearrange("p j c -> p (j c)"), in_=pg[:, 256:320]), [r_pg], [r_gT[d]])
                cx.barrier()
            with ExitStack() as ph2:
                def sb2(name, shape, dt=F32):
                    return ph2.enter_context(nc.sbuf_tensor(f"{name}_{l}", list(shape), dt))

                def ps2(name, shape, dt=F32):
                    return ph2.enter_context(nc.psum_tensor(f"{name}_{l}", list(shape), dt))
                pS = [[ps2(f"pS{d}{par}", [128, 4, 128]) for par in range(2)] for d in range(2)]
                pnd = [ps2(f"pnd{d}", [128, 4, 128]) for d in range(2)]
                pdC1 = ps2("pdC", [128, 4, 128])
                pkt1 = ps2("pkt", [128, 1024], BF16)
                pdC = [pdC1, pdC1]
                pkt = [pkt1, pkt1]
                r_pS = [[R(psum=True), R(psum=True)], [R(psum=True), R(psum=True)]]
                r_pnd = [R(psum=True), R(psum=True)]
                r1 = R(psum=True); r2 = R(psum=True)
                r_pdC = [r1, r1]; r_pkt = [r2, r2]
                Sm = [sb2(f"Sm{d}", [128, 4, 128], BF16) for d in range(2)]
                kt = [sb2(f"kt{d}", [128, 4, 64], BF16) for d in range(2)]
                rr = [sb2(f"rr{d}", [128, 4]) for d in range(2)]
                tmph = [sb2(f"tmph{d}", [128, 4, 64]) for d in range(2)]
                Cst = [sb2(f"Cst{d}", [128, 2, 65]) for d in range(2)]
                Cstb = [sb2(f"Cstb{d}", [128, 2, 65], BF16) for d in range(2)]
                tmpC = [sb2(f"tmpC{d}", [128, 2, 65]) for d in range(2)]
                r_Sm = [R(), R()]; r_kt = [R(), R()]; r_rr = [R(), R()]; r_tmph = [R(), R()]
                r_Cst = [R(), R()]; r_Cstb = [R(), R()]; r_tmpC = [R(), R()]
                for d in range(2):
                    G(lambda e, d=d: e.memset(Cst[d][:], 0.0), [], [r_Cst[d]])
                    G(lambda e, d=d: e.memset(Cstb[d][:], 0.0), [], [r_Cstb[d]])
                done = set()
                for step in range(NT):
                    for d in range(2):
                        c = step if d == 0 else NT - 1 - step
                        last = (step == NT - 1)
                        cs = slice(c * 128, (c + 1) * 128)
                        for j in range(2):
                            P(lambda e, d=d, j=j, cs=cs: e.transpose(out=pkt[d][:, j * 128:(j + 1) * 128], in_=qkT[:, 2 + j, cs], identity=identb[:]),
                              [r_qkT, r_identb], [r_pkt[d]])
                        V(lambda e, d=d, c=c: e.tensor_tensor(out=kt[d][:], in0=pkt[d][:, 0:256].rearrange("p (h k) -> p h k", k=64),
                                                            in1=gTcf[d][:, :, c:c + 1].to_broadcast([128, 4, 64]), op=ALU.mult),
                          [r_pkt[d], r_gT[d]], [r_kt[d]])
                        for h in range(4):
                            hp, hj = h % 2, h // 2
                            P(lambda e, d=d, h=h, hp=hp, hj=hj, cs=cs: e.matmul(pS[d][hp][:, hj, :], lhsT=qkT[hp * 64:(hp + 1) * 64, 2 + hj, cs],
                                                                              rhs=qkT[hp * 64:(hp + 1) * 64, hj, cs], start=True, stop=True),
                              [r_qkT], [r_pS[d][hp]])
                        for h in range(4):
                            hp, hj = h % 2, h // 2
                            V(lambda e, d=d, h=h, c=c, hp=hp, hj=hj: e.scalar_tensor_tensor(out=Sm[d][:, h, :], in0=pS[d][hp][:, hj, :], scalar=gTcf[d][:, h, c:c + 1],
                                                                            in1=maskt[:, d, :], op0=ALU.mult, op1=ALU.mult),
                              [r_pS[d][hp], r_gT[d], r_mask], [r_Sm[d]])
                        for h in range(4):
                            hp, hj = h % 2, h // 2
                            P(lambda e, d=d, h=h, c=c: e.matmul(pnd[d][:, h, 0:65], lhsT=Sm[d][:, h, :], rhs=vx[:, c, h, :], start=True, stop=False),
                              [r_Sm[d], r_vx], [r_pnd[d]])
                            P(lambda e, d=d, h=h, hp=hp, hj=hj, cs=cs: e.matmul(pnd[d][:, h, 0:65], lhsT=qkT[hp * 64:(hp + 1) * 64, hj, cs],
                                                                              rhs=Cstb[d][hp * 64:(hp + 1) * 64, hj, :], start=False, stop=True),
                              [r_qkT, r_Cstb[d]], [r_pnd[d]])
                        A(lambda e, d=d: e.activation(out=rr[d][:].unsqueeze(2), in_=pnd[d][:, :, 64:65], func=AF.Abs),
                          [r_pnd[d]], [r_rr[d]])
                        V(lambda e, d=d, c=c: e.tensor_tensor(out=rr[d][:], in0=rr[d][:], in1=gTcl[d][:, :, c], op=ALU.max), [r_rr[d], r_gT[d]], [r_rr[d]])
                        V(lambda e, d=d: e.reciprocal(out=rr[d][:], in_=rr[d][:]), [r_rr[d]], [r_rr[d]])
                        hv = hacc[:, c, :].rearrange("p (h k) -> p h k", k=64)
                        if c not in done:
                            done.add(c)
                            V(lambda e, d=d, hv=hv: e.tensor_tensor(out=hv, in0=pnd[d][:, :, 0:64], in1=rr[d][:].unsqueeze(2).to_broadcast([128, 4, 64]), op=ALU.mult),
                              [r_pnd[d], r_rr[d]], [r_hacc[c]])
                        else:
                            V(lambda e, d=d: e.tensor_tensor(out=tmph[d][:], in0=pnd[d][:, :, 0:64], in1=rr[d][:].unsqueeze(2).to_broadcast([128, 4, 64]), op=ALU.mult),
                              [r_pnd[d], r_rr[d]], [r_tmph[d]])
                            G(lambda e, d=d, hv=hv: e.tensor_tensor(out=hv, in0=hv, in1=tmph[d][:], op=ALU.add), [r_tmph[d], r_hacc[c]], [r_hacc[c]])
                        if last:
                            continue
                        for h in range(4):
                            hj = h // 2
                            P(lambda e, d=d, h=h, hj=hj, c=c: e.matmul(pdC[d][:, h, 0:65], lhsT=kt[d][:, 2 * hj:2 * hj + 2, :].rearrange("p a k -> p (a k)"),
                                                                     rhs=vx[:, c, h, :], start=True, stop=True),
                              [r_kt[d], r_vx], [r_pdC[d]])
                        for par in range(2):
                            rs = slice(par * 64, (par + 1) * 64)
                            V(lambda e, d=d, rs=rs, par=par: e.tensor_tensor(out=tmpC[d][rs, :, :], in0=pdC[d][rs, par::2, 0:65], in1=Cst[d][rs, :, :], op=ALU.add),
                              [r_pdC[d], r_Cst[d]], [r_tmpC[d]])
                        for par in range(2):
                            rs = slice(par * 64, (par + 1) * 64)
                            V(lambda e, d=d, rs=rs, c=c: e.tensor_tensor(out=Cst[d][rs, :, :], in0=tmpC[d][rs, :, :],
                                                                       in1=decb[d][rs, :, c:c + 1].to_broadcast([64, 2, 65]), op=ALU.mult),
                              [r_tmpC[d], r_gT[d]], [r_Cst[d]])
                            G(lambda e, d=d, rs=rs, c=c: e.tensor_tensor(out=Cstb[d][rs, :, :], in0=tmpC[d][rs, :, :],
                                                                       in1=decb[d][rs, :, c:c + 1].to_broadcast([64, 2, 65]), op=ALU.mult),
                              [r_tmpC[d], r_gT[d]], [r_Cstb[d]])
                cx.barrier()
            with ExitStack() as ph2:
                def sb2(name, shape, dt=F32):
                    return ph2.enter_context(nc.sbuf_tensor(f"{name}_{l}", list(shape), dt))

                def ps2(name, shape, dt=F32):
                    return ph2.enter_context(nc.psum_tensor(f"{name}_{l}", list(shape), dt))
                gnw = sb2("gnw", [128, 256])
                r_gnw = R()
                cx.dma("sp", gnw[:], gn_w_in[l].partition_broadcast(128), writes=[r_gnw])
                uo_t = [sb2(f"uo_t{i}", [128, 256]) for i in range(2)]
                r_uot = [R(), R()]
                sq = sb2("sq", [128, 256]); hc = [sb2(f"hc{i}", [128, 256]) for i in range(2)]
                st4 = sb2("st4", [128, 4, 4])
                r_sq, r_st4 = R(), R()
                r_hc = [R(), R()]
                pyT = [ps2(f"pyT{i}", [128, 512]) for i in range(2)]
                r_pyT = [R(psum=True), R(psum=True)]
                ymT = [sb2(f"ymT{i}", [128, 2, 128]) for i in range(2)]
                r_ymT = [R(), R()]
                for t in range(NT):
                    b = t % 2
                    cx.dma("sp", uo_t[b][:], U_tok[t * 128:(t + 1) * 128, 512:768], reads=[r_Utok], writes=[r_uot[b]])
                    hv = hacc[:, t, :].rearrange("p (h k) -> p h k", k=64)
                    V(lambda e, hv=hv: e.tensor_reduce(out=st4[:, 0, :], in_=hv, axis=AX.X, op=ALU.add), [r_hacc[t]], [r_st4])
                    G(lambda e, t=t: e.tensor_tensor(out=sq[:], in0=hacc[:, t, :], in1=hacc[:, t, :], op=ALU.mult), [r_hacc[t]], [r_sq])
                    V(lambda e: e.tensor_reduce(out=st4[:, 1, :], in_=sq[:].rearrange("p (h k) -> p h k", k=64), axis=AX.X, op=ALU.add), [r_sq], [r_st4])
                    V(lambda e: e.tensor_scalar(out=st4[:, 2, :], in0=st4[:, 0, :], scalar1=1.0 / 64, scalar2=None, op0=ALU.mult), [r_st4], [r_st4])
                    V(lambda e: e.tensor_tensor(out=st4[:, 0, :], in0=st4[:, 2, :], in1=st4[:, 2, :], op=ALU.mult), [r_st4], [r_st4])
                    V(lambda e: e.scalar_tensor_tensor(out=st4[:, 3, :], in0=st4[:, 1, :], scalar=1.0 / 64, in1=st4[:, 0, :], op0=ALU.mult, op1=ALU.subtract), [r_st4], [r_st4])
                    A(lambda e: e.activation(out=st4[:, 3, :], in_=st4[:, 3, :], func=AF.Sqrt, bias=LN_EPS, scale=1.0), [r_st4], [r_st4])
                    V(lambda e: e.reciprocal(out=st4[:, 3, :], in_=st4[:, 3, :]), [r_st4], [r_st4])
                    hcv = hc[b][:].rearrange("p (h k) -> p h k", k=64)
                    V(lambda e, hv=hv, hcv=hcv: e.tensor_tensor(out=hcv, in0=hv, in1=st4[:, 2, :].unsqueeze(2).to_broadcast([128, 4, 64]), op=ALU.subtract),
                      [r_hacc[t], r_st4], [r_hc[b]])
                    V(lambda e, hcv=hcv: e.tensor_tensor(out=hcv, in0=hcv, in1=st4[:, 3, :].unsqueeze(2).to_broadcast([128, 4, 64]), op=ALU.mult),
                      [r_hc[b], r_st4], [r_hc[b]])
                    G(lambda e, b=b: e.tensor_tensor(out=hc[b][:], in0=hc[b][:], in1=gnw[:], op=ALU.mult), [r_hc[b], r_gnw], [r_hc[b]])
                    A(lambda e, b=b: e.activation(out=uo_t[b][:], in_=uo_t[b][:], func=AF.Sigmoid), [r_uot[b]], [r_uot[b]])
                    V(lambda e, b=b: e.tensor_tensor(out=hc[b][:], in0=hc[b][:], in1=uo_t[b][:], op=ALU.mult), [r_hc[b], r_uot[b]], [r_hc[b]])
                    for j in range(2):
                        P(lambda e, b=b, j=j: e.transpose(out=pyT[b][:, j * 128:(j + 1) * 128], in_=hc[b][:, j * 128:(j + 1) * 128], identity=ident[:]),
                          [r_hc[b], r_ident], [r_pyT[b]])
                    A(lambda e, b=b: e.copy(out=ymT[b][:].rearrange("p j t -> p (j t)"), in_=pyT[b][:, 0:256]), [r_pyT[b]], [r_ymT[b]])
                    cx.dma("sp", mixT[256:512, t * 128:(t + 1) * 128].rearrange("(j p) t -> p j t", p=128), ymT[b][:], reads=[r_ymT[b]], writes=[r_mixT])
                cx.barrier()
        if STOP_AFTER == "C":
            break
        with ExitStack() as ph:
            def sbp(name, shape, dt=F32):
                return ph.enter_context(nc.sbuf_tensor(f"{name}_{l}", list(shape), dt))

            def psp(name, shape, dt=F32):
                return ph.enter_context(nc.psum_tensor(f"{name}_{l}", list(shape), dt))
            SCALE = float(96 ** -0.5)
            cs2 = sbp("cs2", [128, 2, S], BF16)
            r_cs2 = R()
            for tb in range(2):
                cx.dma("pool", cs2[64:96, tb, :], ropeT[tb], reads=[r_ropeT], writes=[r_cs2])
            wuq = sbp("wuq", [128, 2, 768], BF16)
            wuqs = sbp("wuqs", [128, 2, 768], BF16)
            wuk = sbp("wuk", [128, 512], BF16)
            wuv = sbp("wuv", [128, 512], BF16)
            r_w = R()
            cx.dma("pool", wuq[:], w_uq_in[l].rearrange("(j p) n -> p j n", p=128), writes=[r_w])
            cx.dma("pool", wuqs[:], w_uq_sw_in[l].rearrange("(j p) n -> p j n", p=128), writes=[r_w])
            cx.dma("pool", wuk[:], w_uk_in[l], writes=[r_w])
            cx.dma("pool", wuv[:], w_uv_in[l], writes=[r_w])
            gqp = sbp("gqp", [128, 2]); gkvp = sbp("gkvp", [128, 1])
            r_g = R()
            cx.dma("sp", gqp[:], g_q_p[l], writes=[r_g])
            cx.dma("sp", gkvp[:], g_kv_p[l], writes=[r_g])
            sel64 = sbp("sel64", [65, 64])
            r_sel = R()
            cx.dma("sp", sel64[:], sel64_in[:, :], writes=[r_sel])
            onesb = sbp("onesb", [128, 128], BF16)
            r_ones = R()
            G(lambda e: e.memset(onesb[:], 1.0), [], [r_ones])
            qn = sbp("qn", [128, 2, S], BF16)
            ckv = sbp("ckv", [128, S], BF16)
            krope = sbp("krope", [128, S], BF16)
            vx2 = sbp("vx2", [128, NT, 8, 65], BF16)
            r_qn, r_ckv, r_krope, r_vx2 = R(), R(), R(), R()
            G(lambda e: e.memset(vx2[:, :, :, 64:65], 1.0), [], [r_vx2])
            pa = [psp(f"pa{i}", [128, 512]) for i in range(2)]
            pb_ = [psp(f"pb{i}", [128, 512]) for i in range(2)]
            pc_ = [psp(f"pc{i}", [128, 512]) for i in range(2)]
            pm = [psp(f"pm{i}", [128, 512]) for i in range(2)]
            r_pa = [R(psum=True), R(psum=True)]
            r_pb = [R(psum=True), R(psum=True)]
            r_pc = [R(psum=True), R(psum=True)]
            r_pm = [R(psum=True), R(psum=True)]
            with ExitStack() as ph2:
                def sb2(name, shape, dt=F32):
                    return ph2.enter_context(nc.sbuf_tensor(f"{name}_{l}", list(shape), dt))
                ub = [sb2(f"ub{i}", [128, 3, 512]) for i in range(2)]
                r_ub = [R(), R()]
                sqb = [sb2(f"sqb{i}", [128, 3, 512], BF16) for i in range(2)]
                r_sqb = [R(), R()]
                rs = [sb2(f"rs{i}", [128, 2, 512]) for i in range(2)]
                r_rs = [R(), R()]
                krr = sb2("krr", [128, 2, S], BF16)
                r_krr = R()
                cx.dma("pool", krr[64:96, 0, :], U_fm[912:944, :], reads=[r_Ufm], writes=[r_krr])
                cx.dma("pool", krr[64:96, 1, :], U_fm[944:976, :], reads=[r_Ufm], writes=[r_krr])
                tmpk = sb2("tmpk", [128, S])
                r_tmpk = R()
                V(lambda e: e.tensor_tensor(out=tmpk[64:96, :], in0=krr[64:96, 0, :], in1=cs2[64:96, 1, :], op=ALU.mult), [r_krr, r_cs2], [r_tmpk])
                G(lambda e: e.tensor_tensor(out=krr[64:96, 1, :], in0=krr[64:96, 1, :], in1=cs2[64:96, 0, :], op=ALU.mult), [r_krr, r_cs2], [r_krr])
                V(lambda e: e.tensor_tensor(out=krope[64:96, :], in0=tmpk[64:96, :], in1=krr[64:96, 1, :], op=ALU.add), [r_krr, r_tmpk], [r_krope])
                for blk in range(8):
                    b = blk % 2
                    bs = slice(blk * 512, (blk + 1) * 512)
                    cx.dma("sp", ub[b][:], U_fm[512:896, bs].rearrange("(j p) t -> p j t", p=128), reads=[r_Ufm], writes=[r_ub[b]])
                    A(lambda e, b=b: e.activation(out=sqb[b][:], in_=ub[b][:], func=AF.Square), [r_ub[b]], [r_sqb[b]])
                    for j in range(2):
                        P(lambda e, b=b, j=j: e.matmul(pm[0][:, :], lhsT=onesb[:], rhs=sqb[b][:, j, :], start=(j == 0), stop=(j == 1)),
                          [r_ones, r_sqb[b]], [r_pm[0]])
                    P(lambda e, b=b: e.matmul(pm[1][:, :], lhsT=onesb[:], rhs=sqb[b][:, 2, :], start=True, stop=True), [r_ones, r_sqb[b]], [r_pm[1]])
                    A(lambda e, b=b: e.activation(out=rs[b][:, 0, :], in_=pm[0][:, :], func=AF.Sqrt, bias=RMS_EPS, scale=1.0 / 256), [r_pm[0]], [r_rs[b]])
                    A(lambda e, b=b: e.activation(out=rs[b][:, 1, :], in_=pm[1][:, :], func=AF.Sqrt, bias=RMS_EPS, scale=1.0 / 128), [r_pm[1]], [r_rs[b]])
                    V(lambda e, b=b: e.reciprocal(out=rs[b][:], in_=rs[b][:]), [r_rs[b]], [r_rs[b]])
                    for j in range(2):
                        V(lambda e, b=b, j=j, bs=bs: e.scalar_tensor_tensor(out=qn[:, j, bs], in0=ub[b][:, j, :], scalar=gqp[:, j:j + 1], in1=rs[b][:, 0, :],
                                                                          op0=ALU.mult, op1=ALU.mult), [r_ub[b], r_g, r_rs[b]], [r_qn])
                    V(lambda e, b=b, bs=bs: e.scalar_tensor_tensor(out=ckv[:, bs], in0=ub[b][:, 2, :], scalar=gkvp[:, 0:1], in1=rs[b][:, 1, :],
                                                                 op0=ALU.mult, op1=ALU.mult), [r_ub[b], r_g, r_rs[b]], [r_ckv])
                for t in range(NT):
                    i = t % 2
                    P(lambda e, t=t, i=i: e.matmul(pc_[i][:, :], lhsT=ckv[:, t * 128:(t + 1) * 128], rhs=wuv[:], start=True, stop=True),
                      [r_ckv, r_w], [r_pc[i]])
                    A(lambda e, t=t, i=i: e.copy(out=vx2[:, t, :, 0:64], in_=pc_[i][:, :].rearrange("p (h c) -> p h c", c=64)), [r_pc[i]], [r_vx2])
                cx.barrier()
            kTh = [sbp(f"kTh{i}", [128, S], BF16) for i in range(2)]
            qTh = [sbp(f"qTh{i}", [128, S], BF16) for i in range(2)]
            r_kTh = [R(), R()]; r_qTh = [R(), R()]
            rt = [sbp(f"rt{i}", [128, 2, 512]) for i in range(2)]
            r_rt = [R(), R()]
            pT = [sbp(f"pTe{i}", [128, 512], BF16) for i in range(3)]
            r_pTs = [R(), R(), R()]
            osb = [sbp(f"osb{i}", [65, 512]) for i in range(2)]
            r_osb = [R(), R()]
            rec = [sbp(f"rec{i}", [64, 512]) for i in range(2)]
            r_rec = [R(), R()]
            npt = 0
            npc = 0
            pcnt = [0]

            def proj(h, blk):
                hb = h % 2
                kT, qT = kTh[hb], qTh[hb]
                if blk == 0:
                    G(lambda e: e.tensor_copy(out=kT[64:96, :], in_=krope[64:96, :]), [r_krope], [r_kTh[hb]])
                bs = slice(blk * 512, (blk + 1) * 512)
                i = pcnt[0] % 2
                pcnt[0] += 1
                P(lambda e: e.matmul(pc_[i][0:64, :], lhsT=wuk[:, h * 64:(h + 1) * 64], rhs=ckv[:, bs], start=True, stop=True),
                  [r_w, r_ckv], [r_pc[i]])
                V(lambda e: e.tensor_copy(out=kT[0:64, bs], in_=pc_[i][0:64, :]), [r_pc[i]], [r_kTh[hb]])
                i2 = pcnt[0] % 2
                pcnt[0] += 1
                for j in range(2):
                    P(lambda e, j=j: e.matmul(pc_[i2][0:96, :], lhsT=wuq[:, j, h * 96:(h + 1) * 96], rhs=qn[:, j, bs],
                                              start=(j == 0), stop=(j == 1)), [r_w, r_qn], [r_pc[i2]])
                for j in range(2):
                    P(lambda e, j=j: e.matmul(pm[i2][0:96, :], lhsT=wuqs[:, j, h * 96:(h + 1) * 96], rhs=qn[:, j, bs],
                                              start=(j == 0), stop=(j == 1)), [r_w, r_qn], [r_pm[i2]])
                A(lambda e: e.copy(out=qT[0:64, bs], in_=pc_[i2][0:64, :]), [r_pc[i2]], [r_qTh[hb]])
                V(lambda e: e.tensor_tensor(out=rt[i2][64:96, 0, :], in0=pc_[i2][64:96, :], in1=cs2[64:96, 1, bs], op=ALU.mult),
                  [r_pc[i2], r_cs2], [r_rt[i2]])
                V(lambda e: e.tensor_tensor(out=rt[i2][64:96, 1, :], in0=pm[i2][64:96, :], in1=cs2[64:96, 0, bs], op=ALU.mult),
                  [r_pm[i2], r_cs2], [r_rt[i2]])
                G(lambda e: e.tensor_tensor(out=qT[64:96, bs], in0=rt[i2][64:96, 0, :], in1=rt[i2][64:96, 1, :], op=ALU.add),
                  [r_rt[i2]], [r_qTh[hb]])

            for blk in range(8):
                proj(0, blk)
            for h in range(8):
                hb = h % 2
                kT, qT = kTh[hb], qTh[hb]
                items = [(qb, kt) for qb in range(8) for kt in range(NT)]

                def emitS(n, kT=kT, qT=qT, hb=hb):
                    qb, kt = items[n]
                    i = n % 2
                    qs = slice(qb * 512, (qb + 1) * 512)
                    P(lambda e: e.matmul(pa[i][:, :], lhsT=kT[0:96, kt * 128:(kt + 1) * 128], rhs=qT[0:96, qs], start=True, stop=True),
                      [r_kTh[hb], r_qTh[hb]], [r_pa[i]])

                def post_a(qb, h=h):
                    ob = qb % 2
                    V(lambda e: e.tensor_copy(out=osb[ob][:], in_=pb_[ob][0:65, :]), [r_pb[ob]], [r_osb[ob]])

                def post_b(qb, h=h):
                    ob = qb % 2
                    qs = slice(qb * 512, (qb + 1) * 512)
                    P(lambda e: e.matmul(pm[ob][0:64, :], lhsT=sel64[:], rhs=osb[ob][:], start=True, stop=True), [r_sel, r_osb[ob]], [r_pm[ob]])
                    V(lambda e: e.reciprocal(out=rec[ob][:], in_=pm[ob][0:64, :]), [r_pm[ob]], [r_rec[ob]])
                    G(lambda e: e.tensor_tensor(out=rec[ob][:], in0=rec[ob][:], in1=osb[ob][0:64, :], op=ALU.mult), [r_rec[ob], r_osb[ob]], [r_rec[ob]])
                    cx.dma("sp", mixT[512 + h * 64:512 + (h + 1) * 64, qs], rec[ob][:], reads=[r_rec[ob]], writes=[r_mixT])

                emitS(0)
                pending = None
                for n, (qb, kt) in enumerate(items):
                    if n + 1 < len(items):
                        emitS(n + 1)
                    i = n % 2
                    ip = n % 3
                    ob = qb % 2
                    A(lambda e, i=i, ip=ip: e.activation(out=pT[ip][:], in_=pa[i][:, :], func=AF.Exp, scale=SCALE), [r_pa[i]], [r_pTs[ip]])
                    P(lambda e, ip=ip, ob=ob, kt=kt, h=h: e.matmul(pb_[ob][0:65, :], lhsT=vx2[:, kt, h, :], rhs=pT[ip][:],
                                                                 start=(kt == 0), stop=(kt == NT - 1)), [r_vx2, r_pTs[ip]], [r_pb[ob]])
                    if h + 1 < 8 and n % 32 == 12:
                        proj(h + 1, n // 32)
                    if kt == NT - 1:
                        post_a(qb)
                        pending = (qb, n + 6)
                    if pending is not None and n >= pending[1]:
                        post_b(pending[0])
                        pending = None
                if pending is not None:
                    post_b(pending[0])
            cx.barrier()
        if STOP_AFTER == "D":
            break
        x_src = x_in if l == 0 else XL
        r_X1, r_H2, r_MS, r_WN, r_FFN = R(), R(), R(), R(), R()
        cnt = sb(f"cnt{l}", [128, 256])
        r_cnt = R()
        with ExitStack() as ph:
            def sbp(name, shape, dt=F32):
                return ph.enter_context(nc.sbuf_tensor(f"{name}_{l}", list(shape), dt))

            def psp(name, shape, dt=F32):
                return ph.enter_context(nc.psum_tensor(f"{name}_{l}", list(shape), dt))
            wout = sbp("wout", [128, 8, D], BF16)
            r_wout = R()
            for kc in range(8):
                cx.dma("pool", wout[:, kc, :], w_out_in[l, kc * 128:(kc + 1) * 128, :], writes=[r_wout])
            lnp = sbp("lnp", [128, 2, D])
            r_lnp = R()
            for j in range(2):
                cx.dma("sp", lnp[:, j, :], lnp_in[l, j].partition_broadcast(128), writes=[r_lnp])
            wr = sbp("wr", [128, 8, 256])
            r_wr = R()
            cx.dma("sp", wr[:], w_router_in[l].rearrange("(kc p) n -> p kc n", p=128), writes=[r_wr])
            ebias = sbp("ebias", [128, 256])
            r_eb = R()
            cx.dma("sp", ebias[:], e_bias_in[l].partition_broadcast(128), writes=[r_eb])
            ws13 = sbp("ws13", [128, 8, 512], BF16)
            ws2 = sbp("ws2", [128, 2, D], BF16)
            r_ws = R()
            for kc in range(8):
                cx.dma("pool", ws13[:, kc, :], ws13_in[l, kc * 128:(kc + 1) * 128, :], writes=[r_ws])
            for j in range(2):
                cx.dma("pool", ws2[:, j, :], ws2_in[l, j * 128:(j + 1) * 128, :], writes=[r_ws])
            onesb = sbp("onesbE", [128, 128], BF16)
            r_ones = R()
            G(lambda e: e.memset(onesb[:], 1.0), [], [r_ones])
            G(lambda e: e.memset(cnt[:], 0.0), [], [r_cnt])
            mxt = [sbp(f"mxt{i}", [128, 8, 512], BF16) for i in range(2)]
            r_mxt = [R(), R()]
            xt = [sbp(f"xtE{i}", [128, D]) for i in range(2)]
            r_xt = [R(), R()]
            z = [sbp(f"zE{i}", [128, D]) for i in range(2)]
            r_z = [R(), R()]
            x1 = [sbp(f"x1E{i}", [128, D]) for i in range(2)]
            r_x1 = [R(), R()]
            h2f = [sbp(f"h2f{i}", [128, D]) for i in range(2)]
            r_h2f = [R(), R()]
            h2b = [sbp(f"h2b{i}", [128, D], BF16) for i in range(2)]
            r_h2b = [R(), R()]
            h2T = sbp("h2T", [128, 8, 128]); h2Tb = sbp("h2Tb", [128, 8, 128], BF16)
            r_h2T, r_h2Tb = R(), R()
            st = sbp("stE", [128, 2, 6]); mv = sbp("mvE", [128, 2]); rstd = sbp("rstdE", [128, 1])
            r_st, r_mv, r_rstd = R(), R(), R()
            sc = sbp("scE", [128, 256]); sel = sbp("selE", [128, 256]); selm = sbp("selmE", [128, 256])
            m8g = sbp("m8g", [128, 8, 8]); gs = sbp("gsE", [128, 8]); m8 = sbp("m8E", [128, 8]); gm = sbp("gmE", [128, 2, 8])
            Mf = sbp("MfE", [128, 256]); Mb = [sbp(f"MbE{i}", [128, 256], BF16) for i in range(2)]
            wn = [sbp(f"wnE{i}", [128, 256]) for i in range(2)]
            ws_ = sbp("wsE", [128, 2])
            r_rt = R()
            r_Mb = [R(), R()]; r_wn = [R(), R()]
            s1 = sbp("s1E", [128, 256]); gsh = sbp("gshE", [128, 256], BF16); gT = sbp("gTE", [128, 2, 128], BF16)
            r_s1, r_gsh, r_gT = R(), R(), R()
            fo = [sbp(f"foE{i}", [128, D]) for i in range(2)]
            r_fo = [R(), R()]
            pX = [psp(f"pX{i}", [128, 512]) for i in range(2)]
            pY = [psp(f"pY{i}", [128, 512]) for i in range(2)]
            pZ = [psp(f"pZ{i}", [128, 512]) for i in range(2)]
            pW0 = psp("pW0", [128, 1024], BF16)
            pW1 = psp("pW1", [128, 512])
            r_pX = [R(psum=True), R(psum=True)]; r_pY = [R(psum=True), R(psum=True)]; r_pZ = [R(psum=True), R(psum=True)]
            r_pW0, r_pW1 = R(psum=True), R(psum=True)
            BIG = 1.0e4

            def layer_norm_stats(src, r_src):
                for hf in range(2):
                    V(lambda e, hf=hf: e.bn_stats(out=st[:, hf, :], in_=src[:, hf * 512:(hf + 1) * 512]), [r_src], [r_st])
                V(lambda e: e.bn_aggr(out=mv[:], in_=st[:].rearrange("p a b -> p (a b)")), [r_st], [r_mv])
                A(lambda e: e.activation(out=rstd[:], in_=mv[:, 1:2], func=AF.Sqrt, bias=LN_EPS, scale=1.0), [r_mv], [r_rstd])
                V(lambda e: e.reciprocal(out=rstd[:], in_=rstd[:]), [r_rstd], [r_rstd])

            for t in range(NT):
                b = t % 2
                if t % 4 == 0:
                    g4 = (t // 4) % 2
                    cx.dma("pool", mxt[g4][:], mixT[:, t * 128:(t + 4) * 128].rearrange("(kc p) t -> p kc t", p=128),
                           reads=[r_mixT], writes=[r_mxt[g4]])
                g4 = (t // 4) % 2
                tt = t % 4
                cx.dma("sp", xt[b][:], x_src[t * 128:(t + 1) * 128, :], writes=[r_xt[b]])
                for hf in range(2):
                    for kc in range(8):
                        P(lambda e, hf=hf, kc=kc, g4=g4, tt=tt: e.matmul(pX[hf][:, :], lhsT=mxt[g4][:, kc, tt * 128:(tt + 1) * 128],
                                                                      rhs=wout[:, kc, hf * 512:(hf + 1) * 512], start=(kc == 0), stop=(kc == 7)),
                          [r_mxt[g4], r_wout], [r_pX[hf]])
                    V(lambda e, hf=hf, b=b: e.tensor_tensor(out=z[b][:, hf * 512:(hf + 1) * 512], in0=pX[hf][:, :], in1=gF[:, 0, hf * 512:(hf + 1) * 512], op=ALU.mult),
                      [r_pX[hf], r_gF], [r_z[b]])
                V(lambda e, b=b: e.scalar_tensor_tensor(out=z[b][:], in0=xt[b][:], scalar=float(ALPHA), in1=z[b][:], op0=ALU.mult, op1=ALU.add),
                  [r_xt[b], r_z[b]], [r_z[b]])
                layer_norm_stats(z[b], r_z[b])
                V(lambda e, b=b: e.tensor_scalar(out=z[b][:], in0=z[b][:], scalar1=mv[:, 0:1], scalar2=rstd[:, 0:1], op0=ALU.subtract, op1=ALU.mult),
                  [r_z[b], r_mv, r_rstd], [r_z[b]])
                G(lambda e, b=b: e.tensor_tensor(out=z[b][:], in0=z[b][:], in1=lnp[:, 0, :], op=ALU.mult), [r_z[b], r_lnp], [r_z[b]])
                V(lambda e, b=b: e.tensor_tensor(out=x1[b][:], in0=z[b][:], in1=lnp[:, 1, :], op=ALU.add), [r_z[b], r_lnp], [r_x1[b]])
                cx.dma("sp", X1[t * 128:(t + 1) * 128, :], x1[b][:], reads=[r_x1[b]], writes=[r_X1])
                layer_norm_stats(x1[b], r_x1[b])
                V(lambda e, b=b: e.tensor_scalar(out=h2f[b][:], in0=x1[b][:], scalar1=mv[:, 0:1], scalar2=rstd[:, 0:1], op0=ALU.subtract, op1=ALU.mult),
                  [r_x1[b], r_mv, r_rstd], [r_h2f[b]])
                G(lambda e, b=b: e.tensor_tensor(out=h2f[b][:], in0=h2f[b][:], in1=gF[:, 3, :], op=ALU.mult), [r_h2f[b], r_gF], [r_h2f[b]])
                V(lambda e, b=b: e.tensor_tensor(out=h2f[b][:], in0=h2f[b][:], in1=gF[:, 2, :], op=ALU.add), [r_h2f[b], r_gF], [r_h2f[b]])
                A(lambda e, b=b: e.copy(out=h2b[b][:], in_=h2f[b][:]), [r_h2f[b]], [r_h2b[b]])
                cx.dma("sp", H2[t * 128:(t + 1) * 128, :], h2b[b][:], reads=[r_h2b[b]], writes=[r_H2])
                for q4 in range(2):
                    for k4 in range(4):
                        kc = q4 * 4 + k4
                        P(lambda e, q4=q4, k4=k4, kc=kc, b=b: e.transpose(out=pY[q4][:, k4 * 128:(k4 + 1) * 128], in_=h2f[b][:, kc * 128:(kc + 1) * 128], identity=ident[:]),
                          [r_h2f[b], r_ident], [r_pY[q4]])
                    V(lambda e, q4=q4: e.tensor_copy(out=h2T[:, q4 * 4:(q4 + 1) * 4, :].rearrange("p a t -> p (a t)"), in_=pY[q4][:, :]), [r_pY[q4]], [r_h2T])
                    A(lambda e, q4=q4: e.copy(out=h2Tb[:, q4 * 4:(q4 + 1) * 4, :].rearrange("p a t -> p (a t)"), in_=pY[q4][:, :]), [r_pY[q4]], [r_h2Tb])
                for kc in range(8):
                    P(lambda e, kc=kc: e.matmul(pZ[0][:, 0:256], lhsT=h2T[:, kc, :], rhs=wr[:, kc, :], start=(kc == 0), stop=(kc == 7)),
                      [r_h2T, r_wr], [r_pZ[0]])
                A(lambda e: e.activation(out=sc[:], in_=pZ[0][:, 0:256], func=AF.Sigmoid), [r_pZ[0]], [r_rt])
                V(lambda e: e.tensor_tensor(out=sel[:], in0=sc[:], in1=ebias[:], op=ALU.add), [r_rt, r_eb], [r_rt])
                for g in range(8):
                    V(lambda e, g=g: e.max(out=m8g[:, g, :], in_=sel[:, g * 32:(g + 1) * 32]), [r_rt], [r_rt])
                V(lambda e: e.tensor_tensor(out=gs[:], in0=m8g[:, :, 0], in1=m8g[:, :, 1], op=ALU.add), [r_rt], [r_rt])
                V(lambda e: e.max(out=m8[:], in_=gs[:]), [r_rt], [r_rt])
                V(lambda e: e.tensor_scalar(out=gm[:, 0, :], in0=gs[:], scalar1=m8[:, 3:4], scalar2=None, op0=ALU.is_ge), [r_rt], [r_rt])
                V(lambda e: e.tensor_scalar(out=gm[:, 1, :], in0=gm[:, 0, :], scalar1=BIG, scalar2=-BIG, op0=ALU.mult, op1=ALU.add), [r_rt], [r_rt])
                V(lambda e: e.tensor_tensor(out=selm[:].rearrange("p (g k) -> p g k", k=32), in0=sel[:].rearrange("p (g k) -> p g k", k=32),
                                            in1=gm[:, 0, :].unsqueeze(2).to_broadcast([128, 8, 32]), op=ALU.mult), [r_rt], [r_rt])
                V(lambda e: e.tensor_tensor(out=selm[:].rearrange("p (g k) -> p g k", k=32), in0=selm[:].rearrange("p (g k) -> p g k", k=32),
                                            in1=gm[:, 1, :].unsqueeze(2).to_broadcast([128, 8, 32]), op=ALU.add), [r_rt], [r_rt])
                V(lambda e: e.max(out=m8[:], in_=selm[:]), [r_rt], [r_rt])
                V(lambda e: e.tensor_scalar(out=Mf[:], in0=selm[:], scalar1=m8[:, 7:8], scalar2=None, op0=ALU.is_ge), [r_rt], [r_rt])
                G(lambda e, b=b: e.tensor_copy(out=Mb[b][:], in_=Mf[:]), [r_rt], [r_Mb[b]])
                V(lambda e: e.tensor_tensor(out=sel[:], in0=sc[:], in1=Mf[:], op=ALU.mult), [r_rt], [r_rt])
                V(lambda e: e.tensor_reduce(out=ws_[:, 0:1], in_=sel[:], axis=AX.X, op=ALU.add), [r_rt], [r_rt])
                V(lambda e: e.reciprocal(out=ws_[:, 1:2], in_=ws_[:, 0:1]), [r_rt], [r_rt])
                V(lambda e, b=b: e.tensor_scalar(out=wn[b][:], in0=sel[:], scalar1=ws_[:, 1:2], scalar2=2.5, op0=ALU.mult, op1=ALU.mult), [r_rt], [r_wn[b]])
                cx.dma("sp", MS[t * 128:(t + 1) * 128, :], Mb[b][:], reads=[r_Mb[b]], writes=[r_MS])
                cx.dma("sp", WN[t * 128:(t + 1) * 128, :], wn[b][:], reads=[r_wn[b]], writes=[r_WN])
                P(lambda e, b=b: e.matmul(pW1[:, 0:256], lhsT=onesb[:], rhs=Mb[b][:], start=True, stop=True), [r_ones, r_Mb[b]], [r_pW1])
                V(lambda e: e.tensor_tensor(out=cnt[:], in0=cnt[:], in1=pW1[:, 0:256], op=ALU.add), [r_pW1, r_cnt], [r_cnt])
                for kc in range(8):
                    P(lambda e, kc=kc: e.matmul(pZ[1][:, :], lhsT=h2Tb[:, kc, :], rhs=ws13[:, kc, :], start=(kc == 0), stop=(kc == 7)),
                      [r_h2Tb, r_ws], [r_pZ[1]])
                A(lambda e: e.activation(out=s1[:], in_=pZ[1][:, 0:256], func=AF.Silu), [r_pZ[1]], [r_s1])
                V(lambda e: e.tensor_tensor(out=gsh[:], in0=s1[:], in1=pZ[1][:, 256:512], op=ALU.mult), [r_s1, r_pZ[1]], [r_gsh])
                for j in range(2):
                    P(lambda e, j=j: e.transpose(out=pW0[:, j * 128:(j + 1) * 128], in_=gsh[:, j * 128:(j + 1) * 128], identity=identb[:]),
                      [r_gsh, r_identb], [r_pW0])
                A(lambda e: e.copy(out=gT[:].rearrange("p j t -> p (j t)"), in_=pW0[:, 0:256]), [r_pW0], [r_gT])
                for hf in range(2):
                    for j in range(2):
                        P(lambda e, hf=hf, j=j: e.matmul(pX[hf][:, :], lhsT=gT[:, j, :], rhs=ws2[:, j, hf * 512:(hf + 1) * 512], start=(j == 0), stop=(j == 1)),
                          [r_gT, r_ws], [r_pX[hf]])
                    if hf == 0:
                        A(lambda e, b=b: e.copy(out=fo[b][:, 0:512], in_=pX[0][:, :]), [r_pX[0]], [r_fo[b]])
                    else:
                        V(lambda e, b=b: e.tensor_copy(out=fo[b][:, 512:1024], in_=pX[1][:, :]), [r_pX[1]], [r_fo[b]])
                cx.dma("sp", FFN[t * 128:(t + 1) * 128, :], fo[b][:], reads=[r_fo[b]], writes=[r_FFN])
            cx.barrier()
        if STOP_AFTER == "E":
            break
        r_Xs, r_Ys = R(), R()
        idxs = sb(f"idxs{l}", [128, NT, 8], I32)
        wk = sb(f"wk{l}", [128, NT, 8])
        idxw = sb(f"idxw{l}", [128, 512], I32)
        r_idxs, r_wk, r_idxw = R(), R(), R()
        with ExitStack() as ph:
            def sbp(name, shape, dt=F32):
                return ph.enter_context(nc.sbuf_tensor(f"{name}_{l}", list(shape), dt))

            def psp(name, shape, dt=F32):
                return ph.enter_context(nc.psum_tensor(f"{name}_{l}", list(shape), dt))
            xq = sbp("xq", [128, 256]); qi = sbp("qi", [128, 256], I32); qf = sbp("qf", [128, 256]); gtm = sbp("gtm", [128, 256])
            pend = sbp("pend", [128, 256]); base = sbp("base", [128, 256])
            r_f1 = R(); r_base = R()
            V(lambda e: e.tensor_scalar(out=xq[:], in0=cnt[:], scalar1=127.0, scalar2=1.0 / 128, op0=ALU.add, op1=ALU.mult), [r_cnt], [r_f1])
            V(lambda e: e.tensor_copy(out=qi[:], in_=xq[:]), [r_f1], [r_f1])
            V(lambda e: e.tensor_copy(out=qf[:], in_=qi[:]), [r_f1], [r_f1])
            V(lambda e: e.tensor_tensor(out=gtm[:], in0=qf[:], in1=xq[:], op=ALU.is_gt), [r_f1], [r_f1])
            V(lambda e: e.tensor_tensor(out=qf[:], in0=qf[:], in1=gtm[:], op=ALU.subtract), [r_f1], [r_f1])
            V(lambda e: e.tensor_scalar(out=qf[:], in0=qf[:], scalar1=128.0, scalar2=None, op0=ALU.mult), [r_f1], [r_f1])
            V(lambda e: e.tensor_tensor_scan(out=pend[:], data0=qf[:], data1=qf[:], initial=0.0, op0=ALU.add, op1=ALU.max), [r_f1], [r_f1])
            V(lambda e: e.tensor_tensor(out=base[:], in0=pend[:], in1=qf[:], op=ALU.subtract), [r_f1], [r_base])
            V(lambda e: e.tensor_scalar(out=base[:], in0=base[:], scalar1=1.0, scalar2=None, op0=ALU.add), [r_base], [r_base])
            pF = [psp(f"pF{i}", [128, 512]) for i in range(2)]
            r_pF = [R(psum=True), R(psum=True)]
            pendT = sbp("pendT", [128, 2])
            for j in range(2):
                P(lambda e, j=j: e.transpose(out=pF[0][:, j:j + 1], in_=pend[0:1, j * 128:(j + 1) * 128], identity=ident[0:1, 0:1]), [r_f1, r_ident], [r_pF[0]])
            V(lambda e: e.tensor_copy(out=pendT[:], in_=pF[0][:, 0:2]), [r_pF[0]], [r_f1])
            blkpos = sbp("blkpos", [128, 512]); pidx = sbp("pidx", [128, 1])
            r_c2 = R()
            cx.dma("sp", blkpos[:], blkpos_in[:, :], writes=[r_c2])
            cx.dma("sp", pidx[:], pidx_in[:, :], writes=[r_c2])
            Gm = [sbp(f"Gm{j}", [128, 512], BF16) for j in range(2)]
            onesb = sbp("onesbF", [128, 128], BF16)
            ustr = sbp("ustr", [128, 128], BF16)
            r_ones = R()
            G(lambda e: e.memset(onesb[:], 1.0), [], [r_ones])
            cx.dma("pool", ustr[:], ustrict_in[:, :], writes=[r_ones])
            for j in range(2):
                V(lambda e, j=j: e.tensor_scalar(out=Gm[j][:], in0=blkpos[:], scalar1=pendT[:, j:j + 1], scalar2=None, op0=ALU.is_ge), [r_c2, r_f1], [r_f1])
            for j in range(2):
                P(lambda e, j=j: e.matmul(pF[1][:, :], lhsT=onesb[:], rhs=Gm[j][:], start=(j == 0), stop=(j == 1)), [r_ones, r_f1], [r_pF[1]])
            eall = sbp("eall", [128, 512])
            V(lambda e: e.tensor_scalar(out=eall[:], in0=pF[1][:, :], scalar1=255.0, scalar2=None, op0=ALU.min), [r_pF[1]], [r_f1])
            vld = sbp("vld", [128, 512]); vld2 = sbp("vld2", [128, 512])
            V(lambda e: e.tensor_scalar(out=vld[:], in0=blkpos[:], scalar1=pend[:, 255:256], scalar2=None, op0=ALU.is_lt), [r_f1, r_c2], [r_f1])
            V(lambda e: e.memset(vld2[:], 1.0), [r_f1], [r_f1])
            V(lambda e: e.tensor_tensor(out=vld2[:, 3:512], in0=eall[:, 3:512], in1=eall[:, 0:509], op=ALU.not_equal), [r_f1], [r_f1])
            V(lambda e: e.tensor_tensor(out=vld[:], in0=vld[:], in1=vld2[:], op=ALU.mult), [r_f1], [r_f1])
            V(lambda e: e.tensor_scalar(out=eall[:], in0=eall[:], scalar1=128.0, scalar2=float(l * 256 * 128) - OOB_IDX, op0=ALU.mult, op1=ALU.add), [r_f1], [r_f1])
            V(lambda e: e.tensor_scalar(out=eall[:], in0=eall[:], scalar1=pidx[:, 0:1], scalar2=None, op0=ALU.add), [r_f1, r_c2], [r_f1])
            V(lambda e: e.tensor_tensor(out=eall[:], in0=eall[:], in1=vld[:], op=ALU.mult), [r_f1], [r_f1])
            V(lambda e: e.tensor_scalar(out=idxw[:], in0=eall[:], scalar1=OOB_IDX, scalar2=None, op0=ALU.add), [r_f1], [r_idxw])
            Mt = [sbp(f"Mt{i}", [128, 256], BF16) for i in range(2)]
            wnt = [sbp(f"wnt{i}", [128, 256]) for i in range(2)]
            h2t = [sbp(f"h2t{i}", [128, D], BF16) for i in range(2)]
            r_Mt = [R(), R()]; r_wnt = [R(), R()]; r_h2t = [R(), R()]
            t1 = sbp("t1", [128, 256]); Vt = sbp("Vt", [128, 256]); junk = sbp("junk", [128, 256]); p8 = sbp("p8", [128, 8])
            r_t1, r_Vt, r_junk, r_p8 = R(), R(), R(), R()
            for t in range(NT):
                b = t % 2
                cx.dma("sp", Mt[b][:], MS[t * 128:(t + 1) * 128, :], reads=[r_MS], writes=[r_Mt[b]])
                cx.dma("sp", wnt[b][:], WN[t * 128:(t + 1) * 128, :], reads=[r_WN], writes=[r_wnt[b]])
                cx.dma("sp", h2t[b][:], H2[t * 128:(t + 1) * 128, :], reads=[r_H2], writes=[r_h2t[b]])
                P(lambda e, b=b: e.matmul(pF[0][:, 0:256], lhsT=ustr[:], rhs=Mt[b][:], start=True, stop=True), [r_ones, r_Mt[b]], [r_pF[0]])
                V(lambda e: e.tensor_tensor(out=t1[:], in0=pF[0][:, 0:256], in1=base[:], op=ALU.add), [r_pF[0], r_base], [r_t1])
                G(lambda e, b=b: e.tensor_tensor(out=Vt[:], in0=t1[:], in1=Mt[b][:], op=ALU.mult), [r_t1, r_Mt[b]], [r_Vt])
                V(lambda e: e.max(out=p8[:], in_=Vt[:]), [r_Vt], [r_p8])
                V(lambda e, t=t: e.tensor_scalar(out=idxs[:, t, :], in0=p8[:], scalar1=-1.0, scalar2=None, op0=ALU.add), [r_p8], [r_idxs])
                for k in range(8):
                    V(lambda e, t=t, k=k, b=b: e.scalar_tensor_tensor(out=junk[:], in0=Vt[:], scalar=p8[:, k:k + 1], in1=wnt[b][:], op0=ALU.is_equal, op1=ALU.mult,
                                                                    accum_out=wk[:, t, k:k + 1]), [r_Vt, r_p8, r_wnt[b]], [r_junk, r_wk])
                P(lambda e, b=b: e.matmul(pF[1][:, 0:256], lhsT=onesb[:], rhs=Mt[b][:], start=True, stop=True), [r_ones, r_Mt[b]], [r_pF[1]])
                V(lambda e: e.tensor_tensor(out=base[:], in0=base[:], in1=pF[1][:, 0:256], op=ALU.add), [r_pF[1], r_base, r_t1], [r_base])
                for k in range(8):
                    cx.dma("pool", None, None, reads=[r_h2t[b], r_idxs], writes=[r_Xs],
                           fn=lambda e, t=t, k=k, b=b: e.indirect_dma_start(
                               out=Xs[:, :], out_offset=bass.IndirectOffsetOnAxis(ap=idxs[:, t, k:k + 1].bitcast(U32), axis=0),
                               in_=h2t[b][:], in_offset=None))
            cx.barrier()
        with ExitStack() as ph:
            def sbp(name, shape, dt=F32):
                return ph.enter_context(nc.sbuf_tensor(f"{name}_{l}", list(shape), dt))

            def psp(name, shape, dt=F32):
                return ph.enter_context(nc.psum_tensor(f"{name}_{l}", list(shape), dt))
            NW = 3
            Xb = [sbp(f"Xb{i}", [128, D], BF16) for i in range(2)]
            w1b = [sbp(f"w1b{i}", [128, 2048], BF16) for i in range(NW)]
            w3b = [sbp(f"w3b{i}", [128, 2048], BF16) for i in range(NW)]
            w2b = [sbp(f"w2b{i}", [128, 2048], BF16) for i in range(NW)]
            xT = [sbp(f"xTb{i}", [128, 8, 128], BF16) for i in range(2)]
            s1 = [sbp(f"s1b{i}", [128, 256]) for i in range(2)]
            gb = [sbp(f"gbb{i}", [128, 256], BF16) for i in range(2)]
            gT = [sbp(f"gTb{i}", [128, 2, 128], BF16) for i in range(2)]
            Yb = [sbp(f"Yb{i}", [128, D], BF16) for i in range(2)]
            r_Xb = [R(), R()]; r_xT = [R(), R()]
            r_w1b = [R() for _ in range(NW)]; r_w3b = [R() for _ in range(NW)]; r_w2b = [R() for _ in range(NW)]
            r_s1 = [R(), R()]; r_gb = [R(), R()]; r_gT = [R(), R()]; r_Yb = [R(), R()]
            pxT = [psp(f"pxT{i}", [128, 1024], BF16) for i in range(2)]
            phh = [psp(f"phh{i}", [128, 512]) for i in range(2)]
            pgT = psp("pgT", [128, 1024], BF16)
            pyy = [psp(f"pyy{i}", [128, 512]) for i in range(2)]
            r_pxT = [R(psum=True), R(psum=True)]; r_phh = [R(psum=True), R(psum=True)]; r_pgT = R(psum=True); r_pyy = [R(psum=True), R(psum=True)]

            def st_load(b):
                i = b % 2
                iw = b % NW
                cx.dma("sp", Xb[i][:], Xs[b * 128:(b + 1) * 128, :], reads=[r_Xs], writes=[r_Xb[i]])
                for wsrc, wdst, rw in ((w1_in, w1b, r_w1b), (w3_in, w3b, r_w3b), (w2_in, w2b, r_w2b)):
                    cx.dma("pool", None, None, reads=[r_idxw], writes=[rw[iw]],
                           fn=lambda e, wsrc=wsrc, wdst=wdst, iw=iw, b=b: e.indirect_dma_start(
                               out=wdst[iw][:], out_offset=None, in_=wsrc[:, :],
                               in_offset=bass.IndirectOffsetOnAxis(ap=idxw[:, b:b + 1].bitcast(U32), axis=0),
                               bounds_check=bc_reg, oob_is_err=False))

            def st_T(b):
                i = b % 2
                for j in range(8):
                    P(lambda e, i=i, j=j: e.transpose(out=pxT[i][:, j * 128:(j + 1) * 128], in_=Xb[i][:, j::8], identity=identb[:]),
                      [r_Xb[i], r_identb], [r_pxT[i]])
                A(lambda e, i=i: e.copy(out=xT[i][:, 0:4, :].rearrange("p a t -> p (a t)"), in_=pxT[i][:, 0:512]), [r_pxT[i]], [r_xT[i]])
                V(lambda e, i=i: e.tensor_copy(out=xT[i][:, 4:8, :].rearrange("p a t -> p (a t)"), in_=pxT[i][:, 512:1024]), [r_pxT[i]], [r_xT[i]])

            def st_H(b):
                i = b % 2
                iw = b % NW
                for j in range(8):
                    P(lambda e, i=i, iw=iw, j=j: e.matmul(phh[i][:, 0:256], lhsT=xT[i][:, j, :], rhs=w1b[iw][:, j * 256:(j + 1) * 256], start=(j == 0), stop=(j == 7)),
                      [r_xT[i], r_w1b[iw]], [r_phh[i]])
                for j in range(8):
                    P(lambda e, i=i, iw=iw, j=j: e.matmul(phh[i][:, 256:512], lhsT=xT[i][:, j, :], rhs=w3b[iw][:, j * 256:(j + 1) * 256], start=(j == 0), stop=(j == 7)),
                      [r_xT[i], r_w3b[iw]], [r_phh[i]])
                A(lambda e, i=i: e.activation(out=s1[i][:], in_=phh[i][:, 0:256], func=AF.Silu), [r_phh[i]], [r_s1[i]])
                V(lambda e, i=i: e.tensor_tensor(out=gb[i][:], in0=s1[i][:], in1=phh[i][:, 256:512], op=ALU.mult), [r_s1[i], r_phh[i]], [r_gb[i]])

            def st_GT(b):
                i = b % 2
                for j in range(2):
                    P(lambda e, i=i, j=j: e.transpose(out=pgT[:, j * 128:(j + 1) * 128], in_=gb[i][:, j::2], identity=identb[:]), [r_gb[i], r_identb], [r_pgT])
                A(lambda e, i=i: e.copy(out=gT[i][:].rearrange("p j t -> p (j t)"), in_=pgT[:, 0:256]), [r_pgT], [r_gT[i]])

            def st_Y(b):
                i = b % 2
                iw = b % NW
                for hf in range(2):
                    for j in range(2):
                        P(lambda e, i=i, iw=iw, j=j, hf=hf: e.matmul(pyy[hf][:, :], lhsT=gT[i][:, j, :], rhs=w2b[iw][:, j * 1024 + hf * 512:j * 1024 + (hf + 1) * 512],
                                                                   start=(j == 0), stop=(j == 1)), [r_gT[i], r_w2b[iw]], [r_pyy[hf]])
                A(lambda e, i=i: e.copy(out=Yb[i][:, 0:512], in_=pyy[0][:, :]), [r_pyy[0]], [r_Yb[i]])
                V(lambda e, i=i: e.tensor_copy(out=Yb[i][:, 512:1024], in_=pyy[1][:, :]), [r_pyy[1]], [r_Yb[i]])
                cx.dma("sp", Ys[b * 128:(b + 1) * 128, :], Yb[i][:], reads=[r_Yb[i]], writes=[r_Ys])

            st_load(0)
            st_load(1)
            st_T(0)
            for n in range(NBLK + 1):
                if n < NBLK:
                    st_H(n)
                if 1 <= n:
                    st_GT(n - 1)
                if n + 1 < NBLK:
                    st_T(n + 1)
                if 1 <= n:
                    st_Y(n - 1)
                if n + 2 < NBLK:
                    st_load(n + 2)
            cx.barrier()
        with ExitStack() as ph:
            def sbp(name, shape, dt=F32):
                return ph.enter_context(nc.sbuf_tensor(f"{name}_{l}", list(shape), dt))
            lnp2 = sbp("lnp2", [128, 2, D])
            r_lnp2 = R()
            for j in range(2):
                cx.dma("sp", lnp2[:, j, :], lnp_in[l, 2 + j].partition_broadcast(128), writes=[r_lnp2])
            yg = [sbp(f"yg{i}", [128, 8, D], BF16) for i in range(2)]
            r_yg = [R(), R()]
            acc = [sbp(f"accF{i}", [128, D]) for i in range(2)]
            r_acc = [R(), R()]
            x1t = [sbp(f"x1t{i}", [128, D]) for i in range(2)]
            r_x1t = [R(), R()]
            st = sbp("stF", [128, 2, 6]); mv = sbp("mvF", [128, 2]); rstd = sbp("rstdF", [128, 1])
            r_st, r_mv, r_rstd = R(), R(), R()
            dst = XL if l < L - 1 else y_out
            r_dst = R()
            def f4_loads(t):
                b = t % 2
                for k in range(8):
                    cx.dma("pool", None, None, reads=[r_Ys, r_idxs], writes=[r_yg[b]],
                           fn=lambda e, t=t, k=k, b=b: e.indirect_dma_start(
                               out=yg[b][:, k, :], out_offset=None, in_=Ys[:, :],
                               in_offset=bass.IndirectOffsetOnAxis(ap=idxs[:, t, k:k + 1].bitcast(U32), axis=0)))
                cx.dma("sp", acc[b][:], FFN[t * 128:(t + 1) * 128, :], reads=[r_FFN], writes=[r_acc[b]])
                cx.dma("sp", x1t[b][:], X1[t * 128:(t + 1) * 128, :], reads=[r_X1], writes=[r_x1t[b]])

            f4_loads(0)
            for t in range(NT):
                b = t % 2
                if t + 1 < NT:
                    f4_loads(t + 1)
                for k in range(8):
                    V(lambda e, t=t, k=k, b=b: e.scalar_tensor_tensor(out=acc[b][:], in0=yg[b][:, k, :], scalar=wk[:, t, k:k + 1], in1=acc[b][:],
                                                                    op0=ALU.mult, op1=ALU.add), [r_yg[b], r_wk, r_acc[b]], [r_acc[b]])
                G(lambda e, b=b: e.tensor_tensor(out=acc[b][:], in0=acc[b][:], in1=gF[:, 1, :], op=ALU.mult), [r_acc[b], r_gF], [r_acc[b]])
                V(lambda e, b=b: e.scalar_tensor_tensor(out=acc[b][:], in0=x1t[b][:], scalar=float(ALPHA), in1=acc[b][:], op0=ALU.mult, op1=ALU.add),
                  [r_x1t[b], r_acc[b]], [r_acc[b]])
                for hf in range(2):
                    V(lambda e, hf=hf, b=b: e.bn_stats(out=st[:, hf, :], in_=acc[b][:, hf * 512:(hf + 1) * 512]), [r_acc[b]], [r_st])
                V(lambda e: e.bn_aggr(out=mv[:], in_=st[:].rearrange("p a b -> p (a b)")), [r_st], [r_mv])
                A(lambda e: e.activation(out=rstd[:], in_=mv[:, 1:2], func=AF.Sqrt, bias=LN_EPS, scale=1.0), [r_mv], [r_rstd])
                V(lambda e: e.reciprocal(out=rstd[:], in_=rstd[:]), [r_rstd], [r_rstd])
                V(lambda e, b=b: e.tensor_scalar(out=acc[b][:], in0=acc[b][:], scalar1=mv[:, 0:1], scalar2=rstd[:, 0:1], op0=ALU.subtract, op1=ALU.mult),
                  [r_acc[b], r_mv, r_rstd], [r_acc[b]])
                G(lambda e, b=b: e.tensor_tensor(out=acc[b][:], in0=acc[b][:], in1=lnp2[:, 0, :], op=ALU.mult), [r_acc[b], r_lnp2], [r_acc[b]])
                V(lambda e, b=b: e.tensor_tensor(out=x1t[b][:], in0=acc[b][:], in1=lnp2[:, 1, :], op=ALU.add), [r_acc[b], r_lnp2, r_x1t[b]], [r_x1t[b]])
                cx.dma("sp", dst[t * 128:(t + 1) * 128, :], x1t[b][:], reads=[r_x1t[b]], writes=[r_dst])
            cx.barrier()
        if STOP_AFTER == "F":
            break

    cx.finish()
    es.close()
    return nc


def prep_shared(inp):
    w_in = np.asarray(inp["w_in"], np.float32)
    o = IN_OFF
    w_tok = np.concatenate([w_in[:, :, o["pool"]:o["pool"] + 256], w_in[:, :, o["v"]:o["v"] + 256],
                            w_in[:, :, o["o"]:o["o"] + 256]], axis=2)
    kr = w_in[:, :, o["kr"]:o["kr"] + 32]
    kr_sw = np.concatenate([kr[:, :, 16:32], kr[:, :, 0:16]], axis=2)
    misc = np.concatenate([w_in[:, :, o["gate"]:o["gate"] + 16], kr, kr_sw, np.zeros((L, D, 48), np.float32)], axis=2)
    w_fm = np.concatenate([w_in[:, :, o["q"]:o["q"] + 256], w_in[:, :, o["k"]:o["k"] + 256],
                           w_in[:, :, o["dq"]:o["dq"] + 256], w_in[:, :, o["dkv"]:o["dkv"] + 128], misc], axis=2)
    b_ada = np.asarray(inp["b_ada"], np.float32)
    bp = np.stack([b_ada[:, v * D:(v + 1) * D].reshape(L, 8, 128).transpose(0, 2, 1) for v in (0, 1, 3, 4)], axis=2)
    band = np.zeros((4, 5, 128, 128), np.float32)
    for g, w in enumerate((2, 4, 8, 16)):
        A_ = np.zeros((S, S), np.float32) if False else None
        def arow(t):
            lo = max(t - w // 2, 0); hi = min(t + w // 2, S)
            return lo, hi, 1.0 / (hi - lo)
        def fill(mat, ti, tj):
            for tl in range(128):
                t = ti * 128 + tl
                lo, hi, inv = arow(t)
                for tp in range(max(lo, tj * 128), min(hi, tj * 128 + 128)):
                    mat[tp - tj * 128, tl] += inv
                if tj * 128 <= t < tj * 128 + 128:
                    mat[t - tj * 128, tl] -= 1.0
        fill(band[g, 0], 5, 4)
        fill(band[g, 1], 5, 5)
        fill(band[g, 2], 5, 6)
        fill(band[g, 3], 0, 0)
        fill(band[g, 4], NT - 1, NT - 1)
    conv_w = np.asarray(inp["conv_w"], np.float32)
    conv_b = np.asarray(inp["conv_b"], np.float32)
    conv_p = np.concatenate([conv_w.transpose(0, 2, 1), conv_b[:, :, None]], axis=2)
    conv_p = conv_p.reshape(L, 4, 128, 6).transpose(0, 2, 1, 3)
    gate_b4 = np.asarray(inp["gate_b"], np.float32).reshape(L, 4, 4).transpose(0, 2, 1)
    sel2 = np.zeros((4, 2, 128), np.float32)
    for j in range(2):
        sel2[2 * j, j, 0:64] = 1.0
        sel2[2 * j + 1, j, 64:128] = 1.0
    masks = np.zeros((2, 128, 128), np.float32)
    masks[0] = np.triu(np.ones((128, 128), np.float32))
    masks[1] = np.tril(np.ones((128, 128), np.float32))
    gb = np.asarray(inp["gate_b"], np.float32)
    gbp = np.zeros((L, 128, 4), np.float32)
    for h in range(4):
        for k in range(4):
            gbp[:, h * 32:(h + 1) * 32, k] = gb[:, k * 4 + h][:, None]
    trih = np.zeros((2, 128, 128), np.float32)
    cmask = np.zeros((128, 64), np.float32)
    psel = np.zeros((128, 128), np.float32)
    for h in range(4):
        for c in range(32):
            p = h * 32 + c
            trih[0, h * 32:h * 32 + c, p] = 1.0
            trih[1, h * 32 + c + 1:(h + 1) * 32, p] = 1.0
            cmask[p, (h // 2) * 32 + c] = 1.0
            psel[p, (h % 2) * 64:(h % 2) * 64 + 64] = 1.0
    inv_freq = (np.float32(10000.0) ** (-np.arange(0, 32, 2, dtype=np.float32) / np.float32(32))).astype(np.float32)
    ropec = np.zeros((32, 2), np.float32)
    ropec[:, 0] = np.concatenate([inv_freq, inv_freq])
    ropec[:16, 1] = -1.0
    ropec[16:, 1] = 1.0
    w_uq = np.asarray(inp["w_uq"], np.float32)
    w_uq_sw = w_uq.copy().reshape(L, 256, 8, 96)
    w_uq_sw[:, :, :, 64:80] = w_uq.reshape(L, 256, 8, 96)[:, :, :, 80:96]
    w_uq_sw[:, :, :, 80:96] = w_uq.reshape(L, 256, 8, 96)[:, :, :, 64:80]
    w_uq_sw = w_uq_sw.reshape(L, 256, 768)
    sel64 = np.zeros((65, 64), np.float32)
    sel64[64, :] = 1.0
    lnp = np.stack([np.asarray(inp[k], np.float32) for k in ("ln1_g", "ln1_b", "ln2_g", "ln2_b")], axis=1)
    ws13 = np.concatenate([np.asarray(inp["ws1"], np.float32), np.asarray(inp["ws3"], np.float32)], axis=2)
    blkpos = np.tile((np.arange(512, dtype=np.float32) * 128.0)[None, :], (128, 1))
    sh = {
        "w_out": np.ascontiguousarray(inp["w_out"], np.float32),
        "lnp": np.ascontiguousarray(lnp),
        "w_router": np.ascontiguousarray(inp["w_router"], np.float32),
        "e_bias": np.ascontiguousarray(inp["e_bias"], np.float32),
        "ws13": np.ascontiguousarray(ws13),
        "ws2": np.ascontiguousarray(inp["ws2"], np.float32),
        "w1": np.asarray(inp["w1"], np.float32).reshape(L * 256 * 128, 2048),
        "w3": np.asarray(inp["w3"], np.float32).reshape(L * 256 * 128, 2048),
        "w2": np.asarray(inp["w2"], np.float32).reshape(L * 256 * 128, 2048),
        "ustrict": np.triu(np.ones((128, 128), np.float32), 1),
        "blkpos": blkpos,
        "pidx": np.arange(128, dtype=np.float32).reshape(128, 1),
        "ropec": ropec,
        "g_q_p": np.ascontiguousarray(np.asarray(inp["g_q"], np.float32).reshape(L, 2, 128).transpose(0, 2, 1)),
        "g_kv_p": np.ascontiguousarray(np.asarray(inp["g_kv"], np.float32).reshape(L, 128, 1)),
        "w_uq": np.ascontiguousarray(w_uq), "w_uq_sw": np.ascontiguousarray(w_uq_sw),
        "w_uk": np.ascontiguousarray(inp["w_uk"], np.float32), "w_uv": np.ascontiguousarray(inp["w_uv"], np.float32),
        "sel64": sel64,
        "gbp": gbp, "trih": trih, "cmask": cmask, "psel": psel,
        "band": band,
        "w_pool": np.ascontiguousarray(inp["w_pool"], np.float32),
        "s_pool_p": np.ascontiguousarray(np.asarray(inp["s_pool"], np.float32).reshape(L, 4, 64).transpose(0, 2, 1)),
        "conv_p": np.ascontiguousarray(conv_p),
        "gate_b4": np.ascontiguousarray(gate_b4),
        "gn_w": np.ascontiguousarray(inp["gn_w"], np.float32),
        "sel2": sel2,
        "masks": masks,
        "ident": np.eye(128, dtype=np.float32),
        "w_ada": np.ascontiguousarray(inp["w_ada"], np.float32),
        "b_ada_p": np.ascontiguousarray(bp),
        "b_ada": np.ascontiguousarray(b_ada),
        "w_in_tok": np.ascontiguousarray(w_tok),
        "w_in_fm": np.ascontiguousarray(w_fm),
    }
    return sh


def prep_core(inp, b):
    x = np.asarray(inp["x"][b], np.float32)
    c = np.asarray(inp["c"][b], np.float32)
    return {"x": np.ascontiguousarray(x), "c_p": np.ascontiguousarray(c.reshape(8, 128).T),
            "pos": np.ascontiguousarray(np.asarray(inp["positions"][b], np.int32))}


def kernel(**inp):
    nc = build_program()
    sh = prep_shared(inp)
    in_maps = []
    for b in range(8):
        m = dict(sh)
        m.update(prep_core(inp, b))
        in_maps.append(m)
    res = run_bass_kernel_spmd(nc, in_maps, core_ids=list(range(8)))
    kernel.last = res
    return np.stack([r["y"] for r in res.results], axis=0)
```

```python
from contextlib import ExitStack
import numpy as np
import concourse.bass as bass
import concourse.mybir as mybir
from concourse.bass_utils import run_bass_kernel_spmd

F32 = mybir.dt.float32
F32R = mybir.dt.float32r
BF16 = mybir.dt.bfloat16
I32 = mybir.dt.int32
U32 = mybir.dt.uint32
AF = mybir.ActivationFunctionType
ALU = mybir.AluOpType
AX = mybir.AxisListType

S = 4096
D = 1024
NT = S // 128
L = 2
ALPHA = (2 * L) ** 0.25
LN_EPS = 1e-5
RMS_EPS = 1e-6

DEBUG = {}
STOP_AFTER = None
OOB_IDX = 1048576.0
NBLK = 512


class R:
    __slots__ = ("w", "r", "name", "psum")

    def __init__(self, name="", psum=False):
        self.w = {}
        self.r = {}
        self.name = name
        self.psum = psum


class EngState:
    EPOCH = 16000

    def __init__(self, ctx, name, handle):
        self.ctx = ctx
        self.name = name
        self.h = handle
        self.sem = None
        self.count = 0
        self.own = set()
        self.seen = {}
        self.nsem = 0
        self.slots = []
        self.rr = 0

    def tick(self):
        if self.sem is None or self.count >= self.EPOCH:
            self.sem = self.ctx.new_sem(f"e_{self.name}_{self.nsem}")
            self.nsem += 1
            self.count = 0
            self.own.add(self.sem)
        self.count += 1
        return self.sem, self.count


class Ctx:
    def __init__(self, nc, es):
        self.nc = nc
        self.es = es
        self.nsems = 0
        self.E = {
            "pe": EngState(self, "pe", nc.tensor),
            "act": EngState(self, "act", nc.scalar),
            "dve": EngState(self, "dve", nc.vector),
            "pool": EngState(self, "pool", nc.gpsimd),
            "sp": EngState(self, "sp", nc.sync),
        }
        for q, n in (("sp", 40), ("pool", 40), ("act", 8)):
            self.E[q].slots = [[self.new_sem(f"d_{q}_{i}"), 0] for i in range(n)]

    def new_sem(self, name):
        self.nsems += 1
        return self.es.enter_context(self.nc.semaphore(name))

    def _waits(self, E, reads, writes, skip_own=False):
        need = {}
        for t in reads:
            for s, v in t.w.items():
                if need.get(s, 0) < v:
                    need[s] = v
            if t.psum:
                for s, v in t.r.items():
                    if s not in E.own and need.get(s, 0) < v:
                        need[s] = v
        for t in writes:
            for s, v in t.w.items():
                if need.get(s, 0) < v:
                    need[s] = v
            for s, v in t.r.items():
                if need.get(s, 0) < v:
                    need[s] = v
        for s, v in need.items():
            if skip_own and s in E.own:
                continue
            if E.seen.get(s, 0) >= v:
                continue
            E.h.wait_ge(s, v)
            E.seen[s] = v

    def op(self, eng, fn, reads=(), writes=()):
        E = self.E[eng]
        self._waits(E, reads, writes, skip_own=(eng == "pe"))
        ins = fn(E.h)
        s, v = E.tick()
        ins.then_inc(s, 1)
        for t in writes:
            t.w = {s: v}
            t.r = {}
        for t in reads:
            if t.r.get(s, 0) < v:
                t.r[s] = v
        return ins

    def dma(self, q, out, in_, reads=(), writes=(), fn=None, acc=True):
        E = self.E[q]
        if acc:
            for t in writes:
                if t.r:
                    self._waits(E, (), [t])
                    t.w = {}
                    t.r = {}
            self._waits(E, reads, ())
        else:
            self._waits(E, reads, writes)
        slot = E.slots[E.rr % len(E.slots)]
        E.rr += 1
        s = slot[0]
        if slot[1] > 0 and E.seen.get(s, 0) < 16 * slot[1]:
            E.h.wait_ge(s, 16 * slot[1])
            E.seen[s] = 16 * slot[1]
        if fn is None:
            ins = E.h.dma_start(out=out, in_=in_)
        else:
            ins = fn(E.h)
        slot[1] += 1
        v = 16 * slot[1]
        ins.then_inc(s, 16)
        for t in writes:
            if acc:
                t.w[s] = v
            else:
                t.w = {s: v}
                t.r = {}
        for t in reads:
            if t.r.get(s, 0) < v:
                t.r[s] = v
        return ins

    def barrier(self):
        marks = []
        for e in self.E.values():
            if e.sem is not None and e.count > 0:
                marks.append((e.sem, e.count))
            for s, n in e.slots:
                if n > 0:
                    marks.append((s, 16 * n))
        for E in self.E.values():
            for s, v in marks:
                if s in E.own and E.name == "pe":
                    pass
                if E.seen.get(s, 0) < v:
                    E.h.wait_ge(s, v)
                    E.seen[s] = v

    def finish(self, extra=()):
        E = self.E["sp"]
        for e in self.E.values():
            if e.sem is not None and e.count > 0 and E.seen.get(e.sem, 0) < e.count:
                E.h.wait_ge(e.sem, e.count)
            for s, n in e.slots:
                if n > 0 and E.seen.get(s, 0) < 16 * n:
                    E.h.wait_ge(s, 16 * n)


def r32(ap):
    return ap.bitcast(F32R)


IN_OFF = dict(pool=0, q=256, k=512, v=768, o=1024, gate=1280, dq=1296, dkv=1552, kr=1680)


def build_program():
    nc = bass.Bass("TRN2", target_bir_lowering=False)
    es = ExitStack()
    cx = Ctx(nc, es)

    def din(name, shape, dt=F32):
        return nc.dram_tensor(name, list(shape), dt, kind="ExternalInput").ap()

    def dscr(name, shape, dt=F32):
        kind = "ExternalOutput" if DEBUG.get(name) else "Internal"
        return nc.dram_tensor(name, list(shape), dt, kind=kind).ap()

    def sb(name, shape, dt=F32):
        return es.enter_context(nc.sbuf_tensor(name, list(shape), dt))

    def ps(name, shape, dt=F32):
        return es.enter_context(nc.psum_tensor(name, list(shape), dt))

    x_in = din("x", [S, D])
    c_in = din("c_p", [128, 8])
    ident_in = din("ident", [128, 128])
    w_ada = din("w_ada", [L, D, 6 * D])
    b_ada_p = din("b_ada_p", [L, 128, 4, 8])
    b_ada = din("b_ada", [L, 6 * D])
    w_in_tok = din("w_in_tok", [L, D, 768])
    w_in_fm = din("w_in_fm", [L, D, 1024])
    band_in = din("band", [4, 5, 128, 128])
    w_pool_in = din("w_pool", [L, 4, 64, 64])
    s_pool_p = din("s_pool_p", [L, 64, 4])
    conv_p = din("conv_p", [L, 128, 4, 6])
    gate_b_in = din("gate_b4", [L, 4, 4])
    gn_w_in = din("gn_w", [L, 256])
    sel2_in = din("sel2", [4, 2, 128])
    mask_in = din("masks", [2, 128, 128])
    gbp_in = din("gbp", [L, 128, 4])
    pos_in = din("pos", [S], I32)
    ropec_in = din("ropec", [32, 2])
    g_q_p = din("g_q_p", [L, 128, 2])
    g_kv_p = din("g_kv_p", [L, 128, 1])
    w_uq_in = din("w_uq", [L, 256, 768])
    w_uq_sw_in = din("w_uq_sw", [L, 256, 768])
    w_uk_in = din("w_uk", [L, 128, 512])
    w_uv_in = din("w_uv", [L, 128, 512])
    sel64_in = din("sel64", [65, 64])
    w_out_in = din("w_out", [L, D, D])
    lnp_in = din("lnp", [L, 4, D])
    w_router_in = din("w_router", [L, D, 256])
    e_bias_in = din("e_bias", [L, 256])
    ws13_in = din("ws13", [L, D, 512])
    ws2_in = din("ws2", [L, 256, D])
    w1_in = din("w1", [L * 256 * 128, 2048])
    w3_in = din("w3", [L * 256 * 128, 2048])
    w2_in = din("w2", [L * 256 * 128, 2048])
    ustrict_in = din("ustrict", [128, 128])
    blkpos_in = din("blkpos", [128, 512])
    pidx_in = din("pidx", [128, 1])
    trih_in = din("trih", [2, 128, 128])
    cmask_in = din("cmask", [128, 64])
    psel_in = din("psel", [128, 128])
    y_out = nc.dram_tensor("y", [S, D], F32, kind="ExternalOutput").ap()

    U_tok = dscr("U_tok", [S, 768])
    U_fm = dscr("U_fm", [1024, S])
    mixT = dscr("mixT", [1024, S])
    ropeT = dscr("ropeT", [2, 32, S])
    X1 = dscr("X1", [S, D])
    XL = dscr("XL", [S, D])
    H2 = dscr("H2", [S, D], BF16)
    MS = dscr("MS", [S, 256], BF16)
    WN = dscr("WN", [S, 256])
    FFN = dscr("FFN", [S, D])
    NSLOT = 512 * 128
    Xs = dscr("Xs", [NSLOT, D], BF16)
    Ys = dscr("Ys", [NSLOT, D], BF16)

    bc_reg = nc.gpsimd.alloc_register("bc_reg")
    nc.gpsimd.reg_mov(bc_reg, L * 256 * 128 - 1)
    ident = sb("ident_sb", [128, 128])
    r_ident = R("ident")
    cx.dma("sp", ident[:], ident_in[:, :], writes=[r_ident])
    identb = sb("identb_g", [128, 128], BF16)
    r_identb = R("identb")
    cx.op("dve", lambda e: e.tensor_copy(out=identb[:], in_=ident[:]), [r_ident], [r_identb])
    cact = sb("cact", [128, 8])
    cact_bc = sb("cact_bc", [128, 8, 128])
    r_cact = R("cact")
    craw = sb("craw", [128, 8])
    r_craw = R()
    cx.dma("sp", craw[:], c_in[:, :], writes=[r_craw])
    cx.op("act", lambda e: e.activation(out=cact[:], in_=craw[:], func=AF.Silu), reads=[r_craw], writes=[r_cact])
    r_cbc = R()
    for kc in range(8):
        cx.op("dve", lambda e, kc=kc: e.tensor_copy(out=cact_bc[:, kc, :], in_=cact[:, kc:kc + 1].to_broadcast([128, 128])),
              reads=[r_cact], writes=[r_cbc])

    def V(fn, r=(), w=()):
        return cx.op("dve", fn, r, w)

    def A(fn, r=(), w=()):
        return cx.op("act", fn, r, w)

    def P(fn, r=(), w=()):
        return cx.op("pe", fn, r, w)

    def G(fn, r=(), w=()):
        return cx.op("pool", fn, r, w)

    r_ropeT = R()
    with ExitStack() as ph:
        def sbp(name, shape, dt=F32):
            return ph.enter_context(nc.sbuf_tensor(f"rp_{name}", list(shape), dt))
        posi = sbp("posi", [32, S], I32)
        ang = sbp("ang", [32, S])
        kf = sbp("kf", [32, S])
        ki = sbp("ki", [32, S], I32)
        rr_ = sbp("rr", [32, S])
        mm = sbp("mm", [32, S])
        ropec = sbp("ropec", [32, 2])
        r_rp = R()
        cx.dma("sp", posi[:], pos_in.partition_broadcast(32), writes=[r_rp])
        cx.dma("sp", ropec[:], ropec_in[:, :], writes=[r_rp])
        V(lambda e: e.tensor_copy(out=ang[:], in_=posi[:]), [r_rp], [r_rp])
        V(lambda e: e.tensor_scalar(out=ang[:], in0=ang[:], scalar1=ropec[:, 0:1], scalar2=None, op0=ALU.mult), [r_rp], [r_rp])
        TWO_PI = 2.0 * np.pi
        C1 = 6.28125
        C2 = TWO_PI - C1
        PI_LO = 3.1415925
        for tb in range(2):
            src = ang
            if tb == 1:
                V(lambda e: e.tensor_scalar(out=mm[:], in0=ang[:], scalar1=float(np.pi / 2), scalar2=None, op0=ALU.add), [r_rp], [r_rp])
                src = mm
            V(lambda e, src=src: e.tensor_scalar(out=kf[:], in0=src[:], scalar1=float(1.0 / TWO_PI), scalar2=None, op0=ALU.mult), [r_rp], [r_rp])
            V(lambda e: e.tensor_copy(out=ki[:], in_=kf[:]), [r_rp], [r_rp])
            V(lambda e: e.tensor_copy(out=kf[:], in_=ki[:]), [r_rp], [r_rp])
            V(lambda e, src=src: e.scalar_tensor_tensor(out=rr_[:], in0=kf[:], scalar=-C1, in1=src[:], op0=ALU.mult, op1=ALU.add), [r_rp], [r_rp])
            V(lambda e: e.scalar_tensor_tensor(out=rr_[:], in0=kf[:], scalar=-C2, in1=rr_[:], op0=ALU.mult, op1=ALU.add), [r_rp], [r_rp])
            V(lambda e: e.tensor_scalar(out=kf[:], in0=rr_[:], scalar1=float(np.pi), scalar2=None, op0=ALU.is_gt), [r_rp], [r_rp])
            V(lambda e: e.scalar_tensor_tensor(out=rr_[:], in0=kf[:], scalar=-TWO_PI, in1=rr_[:], op0=ALU.mult, op1=ALU.add), [r_rp], [r_rp])
            V(lambda e: e.tensor_scalar(out=kf[:], in0=rr_[:], scalar1=float(-np.pi), scalar2=None, op0=ALU.is_lt), [r_rp], [r_rp])
            V(lambda e: e.scalar_tensor_tensor(out=rr_[:], in0=kf[:], scalar=TWO_PI, in1=rr_[:], op0=ALU.mult, op1=ALU.add), [r_rp], [r_rp])
            V(lambda e: e.tensor_scalar(out=rr_[:], in0=rr_[:], scalar1=PI_LO, scalar2=-PI_LO, op0=ALU.min, op1=ALU.max), [r_rp], [r_rp])
            A(lambda e: e.activation(out=rr_[:], in_=rr_[:], func=AF.Sin), [r_rp], [r_rp])
            if tb == 0:
                V(lambda e: e.tensor_scalar(out=rr_[:], in0=rr_[:], scalar1=ropec[:, 1:2], scalar2=None, op0=ALU.mult), [r_rp], [r_rp])
            cx.dma("sp", ropeT[tb], rr_[:], reads=[r_rp], writes=[r_ropeT])
        cx.barrier()

    adaP = sb("adaP", [128, 4, 8])
    gF = sb("gF", [128, 4, D])
    r_adaP = R()
    r_gF = R()
    for l in range(L):
        with ExitStack() as ph:
            def sbp(name, shape, dt=F32):
                return ph.enter_context(nc.sbuf_tensor(f"{name}_{l}", list(shape), dt))

            def psp(name, shape, dt=F32):
                return ph.enter_context(nc.psum_tensor(f"{name}_{l}", list(shape), dt))
            wa = [sbp(f"wa{i}", [128, 8, D]) for i in range(2)]
            r_wa = [R(), R()]
            badap = sbp("badap", [128, 4, 8])
            r_badap = R()
            cx.dma("sp", badap[:], b_ada_p[l], writes=[r_badap])
            bfr = sbp("bfr", [128, 4, D])
            r_bfr = R()
            GIDX = {2: 0, 5: 1, 3: 2, 4: 3}
            PIDX = {0: 0, 1: 1, 3: 2, 4: 3}
            for v, j in GIDX.items():
                cx.dma("sp", bfr[:, j, :], b_ada[l, v * D:(v + 1) * D].partition_broadcast(128), writes=[r_bfr])
            pA = psp("pA", [128, 512])
            r_pA = R(psum=True)
            pG = [psp(f"pG{i}", [128, 512]) for i in range(2)]
            r_pG = [R(psum=True), R(psum=True)]
            order = [0, 1, 3, 4, 2, 5]
            for i, v in enumerate(order):
                cx.dma("sp", wa[i % 2][:], w_ada[l, :, v * D:(v + 1) * D].rearrange("(kc p) n -> p kc n", p=128),
                       writes=[r_wa[i % 2]])
                w = wa[i % 2]
                if v in PIDX:
                    pi = PIDX[v]
                    for ncn in range(8):
                        for kc in range(8):
                            cx.op("pe", lambda e, w=w, ncn=ncn, kc=kc, pi=pi: e.matmul(
                                pA[:, pi * 8 + ncn:pi * 8 + ncn + 1], lhsT=w[:, kc, ncn * 128:(ncn + 1) * 128],
                                rhs=cact[:, kc:kc + 1], start=(kc == 0), stop=(kc == 7)),
                                reads=[r_wa[i % 2], r_cact], writes=[r_pA])
                if v in GIDX:
                    j = GIDX[v]
                    for hf in range(2):
                        for kc in range(8):
                            cx.op("pe", lambda e, w=w, hf=hf, kc=kc: e.matmul(
                                pG[hf][:, :], lhsT=cact_bc[:, kc, :], rhs=w[:, kc, hf * 512:(hf + 1) * 512],
                                start=(kc == 0), stop=(kc == 7)),
                                reads=[r_wa[i % 2], r_cbc], writes=[r_pG[hf]])
                        cx.op("dve", lambda e, hf=hf, j=j: e.tensor_tensor(
                            out=gF[:, j, hf * 512:(hf + 1) * 512], in0=pG[hf][:, :], in1=bfr[:, j, hf * 512:(hf + 1) * 512], op=ALU.add),
                            reads=[r_pG[hf], r_bfr], writes=[r_gF])
            cx.op("dve", lambda e: e.tensor_tensor(out=adaP[:].rearrange("p a b -> p (a b)"), in0=pA[:, 0:32],
                                                   in1=badap[:].rearrange("p a b -> p (a b)"), op=ALU.add),
                  reads=[r_pA, r_badap], writes=[r_adaP])
            for v in (1, 3):
                cx.op("dve", lambda e, v=v: e.tensor_scalar_add(out=adaP[:, v, :], in0=adaP[:, v, :], scalar1=1.0),
                      reads=[r_adaP], writes=[r_adaP])
            cx.op("dve", lambda e: e.tensor_scalar_add(out=gF[:, 3, :], in0=gF[:, 3, :], scalar1=1.0), reads=[r_gF], writes=[r_gF])
            cx.barrier()

        with ExitStack() as ph:
            def sbp(name, shape, dt=F32):
                return ph.enter_context(nc.sbuf_tensor(f"{name}_{l}", list(shape), dt))

            def psp(name, shape, dt=F32):
                return ph.enter_context(nc.psum_tensor(f"{name}_{l}", list(shape), dt))
            wtok = sbp("wtok", [128, 8, 768], BF16)
            wfm = sbp("wfm", [128, 8, 1024], BF16)
            r_wtok, r_wfm = R(), R()
            for kc in range(8):
                cx.dma("pool", wtok[:, kc, :], w_in_tok[l, kc * 128:(kc + 1) * 128, :], writes=[r_wtok])
                cx.dma("pool", wfm[:, kc, :], w_in_fm[l, kc * 128:(kc + 1) * 128, :], writes=[r_wfm])
            xt = [sbp(f"xt{i}", [128, D]) for i in range(2)]
            r_xt = [R(), R()]
            xn = [sbp(f"xn{i}", [128, D]) for i in range(2)]
            r_xn = [R(), R()]
            st = sbp("st", [128, 2, 6])
            mv = sbp("mv", [128, 2])
            rstd = sbp("rstd", [128, 1])
            r_st, r_mv, r_rstd = R(), R(), R()
            hT = [sbp(f"hT{i}", [128, 8, 512], BF16) for i in range(2)]
            r_hT = [R(), R()]
            pT = [psp(f"pT{i}", [128, 512]) for i in range(2)]
            r_pT = [R(psum=True), R(psum=True)]
            pU = [psp(f"pU{i}", [128, 512]) for i in range(4)]
            r_pU = [R(psum=True) for _ in range(4)]
            uo = [sbp(f"uo{i}", [128, 512]) for i in range(4)]
            r_uo = [R() for _ in range(4)]
            r_Utok, r_Ufm = R(), R()
            src = x_in if l == 0 else XL
            npu = 0
            for g in range(8):
                hTg = hT[g % 2]
                r_hTg = r_hT[g % 2]
                for tt in range(4):
                    t = g * 4 + tt
                    b = t % 2
                    cx.dma("sp", xt[b][:], src[t * 128:(t + 1) * 128, :], writes=[r_xt[b]])
                    for hf in range(2):
                        cx.op("dve", lambda e, b=b, hf=hf: e.bn_stats(out=st[:, hf, :], in_=xt[b][:, hf * 512:(hf + 1) * 512]),
                              reads=[r_xt[b]], writes=[r_st])
                    cx.op("dve", lambda e: e.bn_aggr(out=mv[:], in_=st[:].rearrange("p a b -> p (a b)")), reads=[r_st], writes=[r_mv])
                    cx.op("act", lambda e: e.activation(out=rstd[:], in_=mv[:, 1:2], func=AF.Sqrt, bias=LN_EPS, scale=1.0),
                          reads=[r_mv], writes=[r_rstd])
                    cx.op("dve", lambda e: e.reciprocal(out=rstd[:], in_=rstd[:]), reads=[r_rstd], writes=[r_rstd])
                    cx.op("dve", lambda e, b=b: e.tensor_scalar(out=xn[b][:], in0=xt[b][:], scalar1=mv[:, 0:1], scalar2=rstd[:, 0:1],
                                                                 op0=ALU.subtract, op1=ALU.mult),
                          reads=[r_xt[b], r_mv, r_rstd], writes=[r_xn[b]])
                    for q4 in range(2):
                        pt = pT[q4]
                        for k4 in range(4):
                            kc = q4 * 4 + k4
                            cx.op("pe", lambda e, pt=pt, k4=k4, kc=kc, b=b: e.transpose(
                                out=pt[:, k4 * 128:(k4 + 1) * 128], in_=xn[b][:, kc * 128:(kc + 1) * 128], identity=ident[:]),
                                reads=[r_xn[b], r_ident], writes=[r_pT[q4]])
                        for k4 in range(4):
                            kc = q4 * 4 + k4
                            cx.op("act", lambda e, pt=pt, k4=k4, kc=kc, tt=tt, hTg=hTg: e.activation(
                                out=hTg[:, kc, tt * 128:(tt + 1) * 128], in_=pt[:, k4 * 128:(k4 + 1) * 128], func=AF.Identity,
                                bias=adaP[:, 0, kc:kc + 1], scale=adaP[:, 1, kc:kc + 1]),
                                reads=[r_pT[q4], r_adaP], writes=[r_hTg])
                for tt in range(4):
                    t = g * 4 + tt
                    for hf in range(2):
                        i = npu % 4
                        npu += 1
                        for kc in range(8):
                            cx.op("pe", lambda e, i=i, kc=kc, tt=tt, hf=hf, hTg=hTg: e.matmul(
                                pU[i][:, 0:384], lhsT=hTg[:, kc, tt * 128:(tt + 1) * 128],
                                rhs=wtok[:, kc, hf * 384:(hf + 1) * 384], start=(kc == 0), stop=(kc == 7)),
                                reads=[r_hTg, r_wtok], writes=[r_pU[i]])
                        cx.op("act" if i % 2 else "dve", lambda e, i=i: (e.copy(out=uo[i][:, 0:384], in_=pU[i][:, 0:384]) if i % 2
                                                                          else e.tensor_copy(out=uo[i][:, 0:384], in_=pU[i][:, 0:384])),
                              reads=[r_pU[i]], writes=[r_uo[i]])
                        cx.dma("pool", U_tok[t * 128:(t + 1) * 128, hf * 384:(hf + 1) * 384], uo[i][:, 0:384],
                               reads=[r_uo[i]], writes=[r_Utok])
                for cb in range(8):
                    i = npu % 4
                    npu += 1
                    for kc in range(8):
                        cx.op("pe", lambda e, i=i, kc=kc, cb=cb, hTg=hTg: e.matmul(
                            pU[i][:, :], lhsT=wfm[:, kc, cb * 128:(cb + 1) * 128], rhs=hTg[:, kc, :],
                            start=(kc == 0), stop=(kc == 7)),
                            reads=[r_hTg, r_wfm], writes=[r_pU[i]])
                    cx.op("act" if i % 2 else "dve", lambda e, i=i: (e.copy(out=uo[i][:, :], in_=pU[i][:, :]) if i % 2
                                                                      else e.tensor_copy(out=uo[i][:, :], in_=pU[i][:, :])),
                          reads=[r_pU[i]], writes=[r_uo[i]])
                    cx.dma("pool", U_fm[cb * 128:(cb + 1) * 128, g * 512:(g + 1) * 512], uo[i][:, :],
                           reads=[r_uo[i]], writes=[r_Ufm])
            cx.barrier()
        if STOP_AFTER == "A":
            break
        r_mixT = R()

        def V(fn, r=(), w=()):
            return cx.op("dve", fn, r, w)

        def A(fn, r=(), w=()):
            return cx.op("act", fn, r, w)

        def P(fn, r=(), w=()):
            return cx.op("pe", fn, r, w)

        def G(fn, r=(), w=()):
            return cx.op("pool", fn, r, w)

        with ExitStack() as ph:
            def sbp(name, shape, dt=F32):
                return ph.enter_context(nc.sbuf_tensor(f"{name}_{l}", list(shape), dt))

            def psp(name, shape, dt=F32):
                return ph.enter_context(nc.psum_tensor(f"{name}_{l}", list(shape), dt))
            band = sbp("band", [128, 20, 128], BF16)
            r_band = R()
            cx.dma("pool", band[:], band_in.rearrange("g k p n -> p (g k) n"), writes=[r_band])
            wpl = sbp("wpl", [64, 4, 64], BF16)
            r_wpl = R()
            cx.dma("pool", wpl[:], w_pool_in[l].rearrange("g c d -> c g d"), writes=[r_wpl])
            spl = sbp("spl", [64, 4])
            r_spl = R()
            cx.dma("sp", spl[:], s_pool_p[l], writes=[r_spl])
            up = sbp("up", [128, NT, 256], BF16)
            r_up = R()
            for t4 in range(0, NT, 8):
                cx.dma("pool", up[:, t4:t4 + 8, :], U_tok[t4 * 128:(t4 + 8) * 128, 0:256].rearrange("(t p) c -> p t c", p=128),
                       reads=[r_Utok], writes=[r_up])
            pd = [psp(f"pd{i}", [64, 512]) for i in range(2)]
            py = [psp(f"py{i}", [64, 512]) for i in range(2)]
            r_pd = [R(psum=True), R(psum=True)]
            r_py = [R(psum=True), R(psum=True)]
            dTs = [sbp(f"dTs{i}", [64, 512], BF16) for i in range(2)]
            r_dTs = [R(), R()]
            yp = [sbp(f"yp{i}", [64, 4, 512]) for i in range(2)]
            r_yp = [R(), R()]
            n = 0
            for Gq in range(8):
                ypq = yp[Gq % 2]
                r_ypq = r_yp[Gq % 2]
                for g in range(4):
                    i = n % 2
                    n += 1
                    for tt in range(4):
                        t = Gq * 4 + tt
                        srcs = []
                        if t > 0:
                            srcs.append((t - 1, 0))
                        srcs.append((t, 3 if t == 0 else (4 if t == NT - 1 else 1)))
                        if t < NT - 1:
                            srcs.append((t + 1, 2))
                        for k, (j, typ) in enumerate(srcs):
                            P(lambda e, i=i, tt=tt, j=j, g=g, typ=typ, k=k, last=len(srcs) - 1: e.matmul(
                                pd[i][:, tt * 128:(tt + 1) * 128], lhsT=up[:, j, g * 64:(g + 1) * 64], rhs=band[:, g * 5 + typ, :],
                                start=(k == 0), stop=(k == last)), [r_up, r_band], [r_pd[i]])
                    A(lambda e, i=i: e.copy(out=dTs[i][:], in_=pd[i][:]), [r_pd[i]], [r_dTs[i]])
                    P(lambda e, i=i, g=g: e.matmul(py[i][:, :], lhsT=wpl[:, g, :], rhs=dTs[i][:], start=True, stop=True),
                      [r_wpl, r_dTs[i]], [r_py[i]])
                    V(lambda e, i=i, g=g, ypq=ypq: e.tensor_scalar(out=ypq[:, g, :], in0=py[i][:], scalar1=spl[:, g:g + 1], scalar2=None,
                                                                 op0=ALU.mult), [r_py[i], r_spl], [r_ypq])
                cx.dma("sp", mixT[0:256, Gq * 512:(Gq + 1) * 512].rearrange("(g c) t -> c g t", c=64), ypq[:],
                       reads=[r_ypq], writes=[r_mixT])
            cx.barrier()
        if STOP_AFTER == "B":
            break
        with ExitStack() as ph:
            def sbp(name, shape, dt=F32):
                return ph.enter_context(nc.sbuf_tensor(f"{name}_{l}", list(shape), dt))
            qkT = sbp("qkT", [128, 4, S], BF16)
            r_qkT = R()
            vx = sbp("vx", [128, NT, 4, 65], BF16)
            r_vx = R()
            hacc = sbp("hacc", [128, NT, 256])
            r_hacc = [R() for _ in range(NT)]
            gTcf = [sbp(f"gTcf{d}", [128, 4, NT]) for d in range(2)]
            gTcl = [sbp(f"gTcl{d}", [128, 4, NT]) for d in range(2)]
            decb = [sbp(f"decb{d}", [128, 2, NT]) for d in range(2)]
            r_gT = [R(), R()]
            maskt = sbp("maskt", [128, 2, 128])
            r_mask = R()
            cx.dma("sp", maskt[:], mask_in.rearrange("d p n -> p d n"), writes=[r_mask])
            with ExitStack() as ph2:
                def sb2(name, shape, dt=F32):
                    return ph2.enter_context(nc.sbuf_tensor(f"{name}_{l}", list(shape), dt))
                convp = sb2("convp", [128, 4, 6])
                r_convp = R()
                cx.dma("sp", convp[:], conv_p[l], writes=[r_convp])
                cin = [sb2(f"cin{i}", [128, S + 4]) for i in range(2)]
                r_cin = [R(), R()]
                acc = sb2("cacc", [128, S])
                r_acc = R()
                for i in range(2):
                    G(lambda e, i=i: e.memset(cin[i][:, 0:2], 0.0), [], [r_cin[i]])
                    G(lambda e, i=i: e.memset(cin[i][:, S + 2:S + 4], 0.0), [], [r_cin[i]])
                for ch in range(4):
                    b = ch % 2
                    cx.dma("sp", cin[b][:, 2:S + 2], U_fm[ch * 128:(ch + 1) * 128, :], reads=[r_Ufm], writes=[r_cin[b]])
                    V(lambda e, b=b, ch=ch: e.tensor_scalar(out=acc[:], in0=cin[b][:, 0:S], scalar1=convp[:, ch, 0:1], scalar2=convp[:, ch, 5:6],
                                                           op0=ALU.mult, op1=ALU.add), [r_cin[b], r_convp], [r_acc])
                    for j in range(1, 5):
                        V(lambda e, b=b, ch=ch, j=j: e.scalar_tensor_tensor(out=acc[:], in0=cin[b][:, j:j + S], scalar=convp[:, ch, j:j + 1],
                                                                          in1=acc[:], op0=ALU.mult, op1=ALU.add), [r_cin[b], r_convp, r_acc], [r_acc])
                    A(lambda e, ch=ch: e.activation(out=qkT[:, ch, :], in_=acc[:], func=AF.Silu), [r_acc], [r_qkT])
                vtmp = [sb2(f"vtmp{i}", [128, 8, 256]) for i in range(2)]
                r_vtmp = [R(), R()]
                G(lambda e: e.memset(vx[:, :, :, 64:65], 1.0), [], [r_vx])
                for i4 in range(4):
                    b = i4 % 2
                    cx.dma("sp", vtmp[b][:], U_tok[i4 * 1024:(i4 + 1) * 1024, 256:512].rearrange("(t p) c -> p t c", p=128),
                           reads=[r_Utok], writes=[r_vtmp[b]])
                    A(lambda e, b=b, i4=i4: e.copy(out=vx[:, i4 * 8:(i4 + 1) * 8, :, 0:64], in_=vtmp[b][:].rearrange("p t (h c) -> p t h c", c=64)),
                      [r_vtmp[b]], [r_vx])
                cx.barrier()
            with ExitStack() as ph2:
                def sb2(name, shape, dt=F32):
                    return ph2.enter_context(nc.sbuf_tensor(f"{name}_{l}", list(shape), dt))

                def ps2(name, shape, dt=F32):
                    return ph2.enter_context(nc.psum_tensor(f"{name}_{l}", list(shape), dt))
                gbp = sb2("gbp", [128, 4])
                ngb = sb2("ngb", [128, 4])
                r_gbp = R()
                cx.dma("sp", gbp[:], gbp_in[l], writes=[r_gbp])
                V(lambda e: e.tensor_scalar(out=ngb[:], in0=gbp[:], scalar1=-1.0, scalar2=None, op0=ALU.mult), [r_gbp], [r_gbp])
                trih = sb2("trih", [128, 2, 128])
                cmask = sb2("cmask", [128, 64])
                psel = sb2("psel", [128, 128])
                r_cst = R()
                cx.dma("sp", trih[:], trih_in.rearrange("d p n -> p d n"), writes=[r_cst])
                cx.dma("sp", cmask[:], cmask_in[:, :], writes=[r_cst])
                cx.dma("sp", psel[:], psel_in[:, :], writes=[r_cst])
                pg = ps2("pg", [128, 512])
                r_pg = R(psum=True)
                for d in range(2):
                    gi = sb2(f"gi{d}", [128, 128]); gf = sb2(f"gf{d}", [128, 128])
                    r_g = R()
                    ki, kf = 2 * d, 2 * d + 1
                    cx.dma("sp", gi[:], U_fm[896 + ki * 4:896 + ki * 4 + 4, :].rearrange("h (c l) -> (h c) l", l=128), reads=[r_Ufm], writes=[r_g])
                    cx.dma("sp", gf[:], U_fm[896 + kf * 4:896 + kf * 4 + 4, :].rearrange("h (c l) -> (h c) l", l=128), reads=[r_Ufm], writes=[r_g])
                    spt = sb2(f"spt{d}", [128, 128]); Pl = sb2(f"Pl{d}", [128, 128]); Pc = sb2(f"Pc{d}", [128, 128]); at = sb2(f"at{d}", [128, 128])
                    cols = sb2(f"cols{d}", [128, 8])
                    rows = sb2(f"rows{d}", [1, 4, 128])
                    r_w = R()
                    A(lambda e, kf=kf: e.activation(out=spt[:], in_=gf[:], func=AF.Exp, bias=ngb[:, kf:kf + 1], scale=-1.0), [r_g, r_gbp], [r_w])
                    A(lambda e: e.activation(out=spt[:], in_=spt[:], func=AF.Ln, bias=1.0, scale=1.0), [r_w], [r_w])
                    V(lambda e: e.tensor_tensor_scan(out=Pl[:], data0=spt[:], data1=spt[:], initial=0.0, op0=ALU.add, op1=ALU.max), [r_w], [r_w])
                    V(lambda e: e.tensor_copy(out=cols[:, 0:1], in_=Pl[:, 127:128]), [r_w], [r_w])
                    P(lambda e, d=d: e.matmul(pg[:, 0:1], lhsT=trih[:, d, :], rhs=cols[:, 0:1], start=True, stop=True), [r_w, r_cst], [r_pg])
                    V(lambda e: e.tensor_copy(out=cols[:, 1:2], in_=pg[:, 0:1]), [r_pg], [r_w])
                    if d == 0:
                        V(lambda e: e.tensor_scalar(out=Pc[:], in0=Pl[:], scalar1=cols[:, 1:2], scalar2=None, op0=ALU.add), [r_w], [r_w])
                    else:
                        V(lambda e: e.scalar_tensor_tensor(out=Pc[:], in0=Pl[:], scalar=-1.0, in1=spt[:], op0=ALU.mult, op1=ALU.add), [r_w], [r_w])
                        V(lambda e: e.tensor_scalar(out=Pc[:], in0=Pc[:], scalar1=cols[:, 0:1], scalar2=cols[:, 1:2], op0=ALU.add, op1=ALU.add), [r_w], [r_w])
                    V(lambda e, ki=ki: e.scalar_tensor_tensor(out=at[:], in0=gi[:], scalar=gbp[:, ki:ki + 1], in1=Pc[:], op0=ALU.add, op1=ALU.add),
                      [r_w, r_g, r_gbp], [r_w])
                    V(lambda e: e.tensor_reduce(out=cols[:, 2:3], in_=at[:], axis=AX.X, op=ALU.max), [r_w], [r_w])
                    P(lambda e: e.transpose(out=pg[0:1, 0:128], in_=cols[:, 2:3], identity=ident[:]), [r_w, r_ident], [r_pg])
                    V(lambda e: e.tensor_copy(out=rows[:, 0, :], in_=pg[0:1, 0:128]), [r_pg], [r_w])
                    cur = 0
                    for sh in (1, 2, 4, 8, 16):
                        a_ = rows[:, cur, :].rearrange("p (h c) -> p h c", c=32)
                        b_ = rows[:, 1 - cur, :].rearrange("p (h c) -> p h c", c=32)
                        if d == 0:
                            V(lambda e, a_=a_, b_=b_, sh=sh: e.tensor_tensor(out=b_[:, :, sh:], in0=a_[:, :, sh:], in1=a_[:, :, :32 - sh], op=ALU.max), [r_w], [r_w])
                            V(lambda e, a_=a_, b_=b_, sh=sh: e.tensor_copy(out=b_[:, :, :sh], in_=a_[:, :, :sh]), [r_w], [r_w])
                        else:
                            V(lambda e, a_=a_, b_=b_, sh=sh: e.tensor_tensor(out=b_[:, :, :32 - sh], in0=a_[:, :, :32 - sh], in1=a_[:, :, sh:], op=ALU.max), [r_w], [r_w])
                            V(lambda e, a_=a_, b_=b_, sh=sh: e.tensor_copy(out=b_[:, :, 32 - sh:], in_=a_[:, :, 32 - sh:]), [r_w], [r_w])
                        cur = 1 - cur
                    Mr = rows[:, cur, :].rearrange("p (h c) -> p h c", c=32)
                    dd = rows[:, 2, :].rearrange("p (h c) -> p h c", c=32)
                    V(lambda e: e.memset(rows[:, 2, :], 0.0), [r_w], [r_w])
                    if d == 0:
                        V(lambda e, Mr=Mr, dd=dd: e.tensor_tensor(out=dd[:, :, 0:31], in0=Mr[:, :, 0:31], in1=Mr[:, :, 1:32], op=ALU.subtract), [r_w], [r_w])
                    else:
                        V(lambda e, Mr=Mr, dd=dd: e.tensor_tensor(out=dd[:, :, 1:32], in0=Mr[:, :, 1:32], in1=Mr[:, :, 0:31], op=ALU.subtract), [r_w], [r_w])
                    A(lambda e: e.activation(out=rows[:, 3, :], in_=rows[:, 2, :], func=AF.Exp), [r_w], [r_w])
                    P(lambda e, cur=cur: e.transpose(out=pg[:, 0:1], in_=rows[0:1, cur, :], identity=ident[0:1, 0:1]), [r_w, r_ident], [r_pg])
                    P(lambda e: e.transpose(out=pg[:, 1:2], in_=rows[0:1, 3, :], identity=ident[0:1, 0:1]), [r_w, r_ident], [r_pg])
                    V(lambda e: e.tensor_copy(out=cols[:, 3:4], in_=pg[:, 0:1]), [r_pg], [r_w])
                    V(lambda e: e.tensor_copy(out=cols[:, 6:7], in_=pg[:, 1:2]), [r_pg], [r_w])
                    V(lambda e: e.tensor_scalar(out=cols[:, 4:5], in0=cols[:, 3:4], scalar1=-1.0, scalar2=None, op0=ALU.mult), [r_w], [r_w])
                    V(lambda e: e.tensor_scalar(out=cols[:, 5:6], in0=cols[:, 3:4], scalar1=-1.0, scalar2=-float(np.log(8.0)), op0=ALU.mult, op1=ALU.add), [r_w], [r_w])
                    A(lambda e: e.activation(out=at[:], in_=at[:], func=AF.Exp, bias=cols[:, 5:6], scale=1.0), [r_w], [r_w])
                    A(lambda e: e.activation(out=Pc[:], in_=Pc[:], func=AF.Exp, bias=cols[:, 4:5], scale=1.0), [r_w], [r_w])
                    P(lambda e: e.transpose(out=pg[:, 0:128], in_=at[:], identity=ident[:]), [r_w, r_ident], [r_pg])
                    P(lambda e: e.transpose(out=pg[:, 128:256], in_=Pc[:], identity=ident[:]), [r_w, r_ident], [r_pg])
                    V(lambda e, d=d: e.tensor_copy(out=gTcf[d][:].rearrange("p h c -> p (h c)"), in_=pg[:, 0:128]), [r_pg], [r_gT[d]])
                    V(lambda e, d=d: e.tensor_copy(out=gTcl[d][:].rearrange("p h c -> p (h c)"), in_=pg[:, 128:256]), [r_pg], [r_gT[d]])
                    V(lambda e: e.tensor_scalar(out=spt[:, 0:64], in0=cmask[:], scalar1=cols[:, 6:7], scalar2=None, op0=ALU.mult), [r_w, r_cst], [r_w])
                    P(lambda e: e.matmul(pg[:, 256:320], lhsT=psel[:], rhs=spt[:, 0:64], start=True, stop=True), [r_w, r_cst], [r_pg])
                    V(lambda e, d=d: e.tensor_copy(out=decb[d][:].rearrange("p j c -> p (j c)"), in_=pg[:, 256:320]), [r_pg], [r_gT[d]])
                cx.barrier()
            with ExitStack() as ph2:
                def sb2(name, shape, dt=F32):
                    return ph2.enter_context(nc.sbuf_tensor(f"{name}_{l}", list(shape), dt))

                def ps2(name, shape, dt=F32):
                    return ph2.enter_context(nc.psum_tensor(f"{name}_{l}", list(shape), dt))
                pS = [[ps2(f"pS{d}{par}", [128, 4, 128]) for par in range(2)] for d in range(2)]
                pnd = [ps2(f"pnd{d}", [128, 4, 128]) for d in range(2)]
                pdC1 = ps2("pdC", [128, 4, 128])
                pkt1 = ps2("pkt", [128, 1024], BF16)
                pdC = [pdC1, pdC1]
                pkt = [pkt1, pkt1]
                r_pS = [[R(psum=True), R(psum=True)], [R(psum=True), R(psum=True)]]
                r_pnd = [R(psum=True), R(psum=True)]
                r1 = R(psum=True); r2 = R(psum=True)
                r_pdC = [r1, r1]; r_pkt = [r2, r2]
                Sm = [sb2(f"Sm{d}", [128, 4, 128], BF16) for d in range(2)]
                kt = [sb2(f"kt{d}", [128, 4, 64], BF16) for d in range(2)]
                rr = [sb2(f"rr{d}", [128, 4]) for d in range(2)]
                tmph = [sb2(f"tmph{d}", [128, 4, 64]) for d in range(2)]
                Cst = [sb2(f"Cst{d}", [128, 2, 65]) for d in range(2)]
                Cstb = [sb2(f"Cstb{d}", [128, 2, 65], BF16) for d in range(2)]
                tmpC = [sb2(f"tmpC{d}", [128, 2, 65]) for d in range(2)]
                r_Sm = [R(), R()]; r_kt = [R(), R()]; r_rr = [R(), R()]; r_tmph = [R(), R()]
                r_Cst = [R(), R()]; r_Cstb = [R(), R()]; r_tmpC = [R(), R()]
                for d in range(2):
                    G(lambda e, d=d: e.memset(Cst[d][:], 0.0), [], [r_Cst[d]])
                    G(lambda e, d=d: e.memset(Cstb[d][:], 0.0), [], [r_Cstb[d]])
                done = set()
                for step in range(NT):
                    for d in range(2):
                        c = step if d == 0 else NT - 1 - step
                        last = (step == NT - 1)
                        cs = slice(c * 128, (c + 1) * 128)
                        for j in range(2):
                            P(lambda e, d=d, j=j, cs=cs: e.transpose(out=pkt[d][:, j * 128:(j + 1) * 128], in_=qkT[:, 2 + j, cs], identity=identb[:]),
                              [r_qkT, r_identb], [r_pkt[d]])
                        V(lambda e, d=d, c=c: e.tensor_tensor(out=kt[d][:], in0=pkt[d][:, 0:256].rearrange("p (h k) -> p h k", k=64),
                                                            in1=gTcf[d][:, :, c:c + 1].to_broadcast([128, 4, 64]), op=ALU.mult),
                          [r_pkt[d], r_gT[d]], [r_kt[d]])
                        for h in range(4):
                            hp, hj = h % 2, h // 2
                            P(lambda e, d=d, h=h, hp=hp, hj=hj, cs=cs: e.matmul(pS[d][hp][:, hj, :], lhsT=qkT[hp * 64:(hp + 1) * 64, 2 + hj, cs],
                                                                              rhs=qkT[hp * 64:(hp + 1) * 64, hj, cs], start=True, stop=True),
                              [r_qkT], [r_pS[d][hp]])
                        for h in range(4):
                            hp, hj = h % 2, h // 2
                            V(lambda e, d=d, h=h, c=c, hp=hp, hj=hj: e.scalar_tensor_tensor(out=Sm[d][:, h, :], in0=pS[d][hp][:, hj, :], scalar=gTcf[d][:, h, c:c + 1],
                                                                            in1=maskt[:, d, :], op0=ALU.mult, op1=ALU.mult),
                              [r_pS[d][hp], r_gT[d], r_mask], [r_Sm[d]])
                        for h in range(4):
                            hp, hj = h % 2, h // 2
                            P(lambda e, d=d, h=h, c=c: e.matmul(pnd[d][:, h, 0:65], lhsT=Sm[d][:, h, :], rhs=vx[:, c, h, :], start=True, stop=False),
                              [r_Sm[d], r_vx], [r_pnd[d]])
                            P(lambda e, d=d, h=h, hp=hp, hj=hj, cs=cs: e.matmul(pnd[d][:, h, 0:65], lhsT=qkT[hp * 64:(hp + 1) * 64, hj, cs],
                                                                              rhs=Cstb[d][hp * 64:(hp + 1) * 64, hj, :], start=False, stop=True),
                              [r_qkT, r_Cstb[d]], [r_pnd[d]])
                        A(lambda e, d=d: e.activation(out=rr[d][:].unsqueeze(2), in_=pnd[d][:, :, 64:65], func=AF.Abs),
                          [r_pnd[d]], [r_rr[d]])
                        V(lambda e, d=d, c=c: e.tensor_tensor(out=rr[d][:], in0=rr[d][:], in1=gTcl[d][:, :, c], op=ALU.max), [r_rr[d], r_gT[d]], [r_rr[d]])
                        V(lambda e, d=d: e.reciprocal(out=rr[d][:], in_=rr[d][:]), [r_rr[d]], [r_rr[d]])
                        hv = hacc[:, c, :].rearrange("p (h k) -> p h k", k=64)
                        if c not in done:
                            done.add(c)
                            V(lambda e, d=d, hv=hv: e.tensor_tensor(out=hv, in0=pnd[d][:, :, 0:64], in1=rr[d][:].unsqueeze(2).to_broadcast([128, 4, 64]), op=ALU.mult),
                              [r_pnd[d], r_rr[d]], [r_hacc[c]])
                        else:
                            V(lambda e, d=d: e.tensor_tensor(out=tmph[d][:], in0=pnd[d][:, :, 0:64], in1=rr[d][:].unsqueeze(2).to_broadcast([128, 4, 64]), op=ALU.mult),
                              [r_pnd[d], r_rr[d]], [r_tmph[d]])
                            G(lambda e, d=d, hv=hv: e.tensor_tensor(out=hv, in0=hv, in1=tmph[d][:], op=ALU.add), [r_tmph[d], r_hacc[c]], [r_hacc[c]])
                        if last:
                            continue
                        for h in range(4):
                            hj = h // 2
                            P(lambda e, d=d, h=h, hj=hj, c=c: e.matmul(pdC[d][:, h, 0:65], lhsT=kt[d][:, 2 * hj:2 * hj + 2, :].rearrange("p a k -> p (a k)"),
                                                                     rhs=vx[:, c, h, :], start=True, stop=True),
                              [r_kt[d], r_vx], [r_pdC[d]])
                        for par in range(2):
                            rs = slice(par * 64, (par + 1) * 64)
                            V(lambda e, d=d, rs=rs, par=par: e.tensor_tensor(out=tmpC[d][rs, :, :], in0=pdC[d][rs, par::2, 0:65], in1=Cst[d][rs, :, :], op=ALU.add),
                              [r_pdC[d], r_Cst[d]], [r_tmpC[d]])
                        for par in range(2):
                            rs = slice(par * 64, (par + 1) * 64)
                            V(lambda e, d=d, rs=rs, c=c: e.tensor_tensor(out=Cst[d][rs, :, :], in0=tmpC[d][rs, :, :],
                                                                       in1=decb[d][rs, :, c:c + 1].to_broadcast([64, 2, 65]), op=ALU.mult),
                              [r_tmpC[d], r_gT[d]], [r_Cst[d]])
                            G(lambda e, d=d, rs=rs, c=c: e.tensor_tensor(out=Cstb[d][rs, :, :], in0=tmpC[d][rs, :, :],
                                                                       in1=decb[d][rs, :, c:c + 1].to_broadcast([64, 2, 65]), op=ALU.mult),
                              [r_tmpC[d], r_gT[d]], [r_Cstb[d]])
                cx.barrier()
            with ExitStack() as ph2:
                def sb2(name, shape, dt=F32):
                    return ph2.enter_context(nc.sbuf_tensor(f"{name}_{l}", list(shape), dt))

                def ps2(name, shape, dt=F32):
                    return ph2.enter_context(nc.psum_tensor(f"{name}_{l}", list(shape), dt))
                gnw = sb2("gnw", [128, 256])
                r_gnw = R()
                cx.dma("sp", gnw[:], gn_w_in[l].partition_broadcast(128), writes=[r_gnw])
                uo_t = [sb2(f"uo_t{i}", [128, 256]) for i in range(2)]
                r_uot = [R(), R()]
                sq = sb2("sq", [128, 256]); hc = [sb2(f"hc{i}", [128, 256]) for i in range(2)]
                st4 = sb2("st4", [128, 4, 4])
                r_sq, r_st4 = R(), R()
                r_hc = [R(), R()]
                pyT = [ps2(f"pyT{i}", [128, 512]) for i in range(2)]
                r_pyT = [R(psum=True), R(psum=True)]
                ymT = [sb2(f"ymT{i}", [128, 2, 128]) for i in range(2)]
                r_ymT = [R(), R()]
                for t in range(NT):
                    b = t % 2
                    cx.dma("sp", uo_t[b][:], U_tok[t * 128:(t + 1) * 128, 512:768], reads=[r_Utok], writes=[r_uot[b]])
                    hv = hacc[:, t, :].rearrange("p (h k) -> p h k", k=64)
                    V(lambda e, hv=hv: e.tensor_reduce(out=st4[:, 0, :], in_=hv, axis=AX.X, op=ALU.add), [r_hacc[t]], [r_st4])
                    G(lambda e, t=t: e.tensor_tensor(out=sq[:], in0=hacc[:, t, :], in1=hacc[:, t, :], op=ALU.mult), [r_hacc[t]], [r_sq])
                    V(lambda e: e.tensor_reduce(out=st4[:, 1, :], in_=sq[:].rearrange("p (h k) -> p h k", k=64), axis=AX.X, op=ALU.add), [r_sq], [r_st4])
                    V(lambda e: e.tensor_scalar(out=st4[:, 2, :], in0=st4[:, 0, :], scalar1=1.0 / 64, scalar2=None, op0=ALU.mult), [r_st4], [r_st4])
                    V(lambda e: e.tensor_tensor(out=st4[:, 0, :], in0=st4[:, 2, :], in1=st4[:, 2, :], op=ALU.mult), [r_st4], [r_st4])
                    V(lambda e: e.scalar_tensor_tensor(out=st4[:, 3, :], in0=st4[:, 1, :], scalar=1.0 / 64, in1=st4[:, 0, :], op0=ALU.mult, op1=ALU.subtract), [r_st4], [r_st4])
                    A(lambda e: e.activation(out=st4[:, 3, :], in_=st4[:, 3, :], func=AF.Sqrt, bias=LN_EPS, scale=1.0), [r_st4], [r_st4])
                    V(lambda e: e.reciprocal(out=st4[:, 3, :], in_=st4[:, 3, :]), [r_st4], [r_st4])
                    hcv = hc[b][:].rearrange("p (h k) -> p h k", k=64)
                    V(lambda e, hv=hv, hcv=hcv: e.tensor_tensor(out=hcv, in0=hv, in1=st4[:, 2, :].unsqueeze(2).to_broadcast([128, 4, 64]), op=ALU.subtract),
                      [r_hacc[t], r_st4], [r_hc[b]])
                    V(lambda e, hcv=hcv: e.tensor_tensor(out=hcv, in0=hcv, in1=st4[:, 3, :].unsqueeze(2).to_broadcast([128, 4, 64]), op=ALU.mult),
                      [r_hc[b], r_st4], [r_hc[b]])
                    G(lambda e, b=b: e.tensor_tensor(out=hc[b][:], in0=hc[b][:], in1=gnw[:], op=ALU.mult), [r_hc[b], r_gnw], [r_hc[b]])
                    A(lambda e, b=b: e.activation(out=uo_t[b][:], in_=uo_t[b][:], func=AF.Sigmoid), [r_uot[b]], [r_uot[b]])
                    V(lambda e, b=b: e.tensor_tensor(out=hc[b][:], in0=hc[b][:], in1=uo_t[b][:], op=ALU.mult), [r_hc[b], r_uot[b]], [r_hc[b]])
                    for j in range(2):
                        P(lambda e, b=b, j=j: e.transpose(out=pyT[b][:, j * 128:(j + 1) * 128], in_=hc[b][:, j * 128:(j + 1) * 128], identity=ident[:]),
                          [r_hc[b], r_ident], [r_pyT[b]])
                    A(lambda e, b=b: e.copy(out=ymT[b][:].rearrange("p j t -> p (j t)"), in_=pyT[b][:, 0:256]), [r_pyT[b]], [r_ymT[b]])
                    cx.dma("sp", mixT[256:512, t * 128:(t + 1) * 128].rearrange("(j p) t -> p j t", p=128), ymT[b][:], reads=[r_ymT[b]], writes=[r_mixT])
                cx.barrier()
        if STOP_AFTER == "C":
            break
        with ExitStack() as ph:
            def sbp(name, shape, dt=F32):
                return ph.enter_context(nc.sbuf_tensor(f"{name}_{l}", list(shape), dt))

            def psp(name, shape, dt=F32):
                return ph.enter_context(nc.psum_tensor(f"{name}_{l}", list(shape), dt))
            SCALE = float(96 ** -0.5)
            cs2 = sbp("cs2", [128, 2, S], BF16)
            r_cs2 = R()
            for tb in range(2):
                cx.dma("pool", cs2[64:96, tb, :], ropeT[tb], reads=[r_ropeT], writes=[r_cs2])
            wuq = sbp("wuq", [128, 2, 768], BF16)
            wuqs = sbp("wuqs", [128, 2, 768], BF16)
            wuk = sbp("wuk", [128, 512], BF16)
            wuv = sbp("wuv", [128, 512], BF16)
            r_w = R()
            cx.dma("pool", wuq[:], w_uq_in[l].rearrange("(j p) n -> p j n", p=128), writes=[r_w])
            cx.dma("pool", wuqs[:], w_uq_sw_in[l].rearrange("(j p) n -> p j n", p=128), writes=[r_w])
            cx.dma("pool", wuk[:], w_uk_in[l], writes=[r_w])
            cx.dma("pool", wuv[:], w_uv_in[l], writes=[r_w])
            gqp = sbp("gqp", [128, 2]); gkvp = sbp("gkvp", [128, 1])
            r_g = R()
            cx.dma("sp", gqp[:], g_q_p[l], writes=[r_g])
            cx.dma("sp", gkvp[:], g_kv_p[l], writes=[r_g])
            sel64 = sbp("sel64", [65, 64])
            r_sel = R()
            cx.dma("sp", sel64[:], sel64_in[:, :], writes=[r_sel])
            onesb = sbp("onesb", [128, 128], BF16)
            r_ones = R()
            G(lambda e: e.memset(onesb[:], 1.0), [], [r_ones])
            qn = sbp("qn", [128, 2, S], BF16)
            ckv = sbp("ckv", [128, S], BF16)
            krope = sbp("krope", [128, S], BF16)
            vx2 = sbp("vx2", [128, NT, 8, 65], BF16)
            r_qn, r_ckv, r_krope, r_vx2 = R(), R(), R(), R()
            G(lambda e: e.memset(vx2[:, :, :, 64:65], 1.0), [], [r_vx2])
            pa = [psp(f"pa{i}", [128, 512]) for i in range(2)]
            pb_ = [psp(f"pb{i}", [128, 512]) for i in range(2)]
            pc_ = [psp(f"pc{i}", [128, 512]) for i in range(2)]
            pm = [psp(f"pm{i}", [128, 512]) for i in range(2)]
            r_pa = [R(psum=True), R(psum=True)]
            r_pb = [R(psum=True), R(psum=True)]
            r_pc = [R(psum=True), R(psum=True)]
            r_pm = [R(psum=True), R(psum=True)]
            with ExitStack() as ph2:
                def sb2(name, shape, dt=F32):
                    return ph2.enter_context(nc.sbuf_tensor(f"{name}_{l}", list(shape), dt))
                ub = [sb2(f"ub{i}", [128, 3, 512]) for i in range(2)]
                r_ub = [R(), R()]
                sqb = [sb2(f"sqb{i}", [128, 3, 512], BF16) for i in range(2)]
                r_sqb = [R(), R()]
                rs = [sb2(f"rs{i}", [128, 2, 512]) for i in range(2)]
                r_rs = [R(), R()]
                krr = sb2("krr", [128, 2, S], BF16)
                r_krr = R()
                cx.dma("pool", krr[64:96, 0, :], U_fm[912:944, :], reads=[r_Ufm], writes=[r_krr])
                cx.dma("pool", krr[64:96, 1, :], U_fm[944:976, :], reads=[r_Ufm], writes=[r_krr])
                tmpk = sb2("tmpk", [128, S])
                r_tmpk = R()
                V(lambda e: e.tensor_tensor(out=tmpk[64:96, :], in0=krr[64:96, 0, :], in1=cs2[64:96, 1, :], op=ALU.mult), [r_krr, r_cs2], [r_tmpk])
                G(lambda e: e.tensor_tensor(out=krr[64:96, 1, :], in0=krr[64:96, 1, :], in1=cs2[64:96, 0, :], op=ALU.mult), [r_krr, r_cs2], [r_krr])
                V(lambda e: e.tensor_tensor(out=krope[64:96, :], in0=tmpk[64:96, :], in1=krr[64:96, 1, :], op=ALU.add), [r_krr, r_tmpk], [r_krope])
                for blk in range(8):
                    b = blk % 2
                    bs = slice(blk * 512, (blk + 1) * 512)
                    cx.dma("sp", ub[b][:], U_fm[512:896, bs].rearrange("(j p) t -> p j t", p=128), reads=[r_Ufm], writes=[r_ub[b]])
                    A(lambda e, b=b: e.activation(out=sqb[b][:], in_=ub[b][:], func=AF.Square), [r_ub[b]], [r_sqb[b]])
                    for j in range(2):
                        P(lambda e, b=b, j=j: e.matmul(pm[0][:, :], lhsT=onesb[:], rhs=sqb[b][:, j, :], start=(j == 0), stop=(j == 1)),
                          [r_ones, r_sqb[b]], [r_pm[0]])
                    P(lambda e, b=b: e.matmul(pm[1][:, :], lhsT=onesb[:], rhs=sqb[b][:, 2, :], start=True, stop=True), [r_ones, r_sqb[b]], [r_pm[1]])
                    A(lambda e, b=b: e.activation(out=rs[b][:, 0, :], in_=pm[0][:, :], func=AF.Sqrt, bias=RMS_EPS, scale=1.0 / 256), [r_pm[0]], [r_rs[b]])
                    A(lambda e, b=b: e.activation(out=rs[b][:, 1, :], in_=pm[1][:, :], func=AF.Sqrt, bias=RMS_EPS, scale=1.0 / 128), [r_pm[1]], [r_rs[b]])
                    V(lambda e, b=b: e.reciprocal(out=rs[b][:], in_=rs[b][:]), [r_rs[b]], [r_rs[b]])
                    for j in range(2):
                        V(lambda e, b=b, j=j, bs=bs: e.scalar_tensor_tensor(out=qn[:, j, bs], in0=ub[b][:, j, :], scalar=gqp[:, j:j + 1], in1=rs[b][:, 0, :],
                                                                          op0=ALU.mult, op1=ALU.mult), [r_ub[b], r_g, r_rs[b]], [r_qn])
                    V(lambda e, b=b, bs=bs: e.scalar_tensor_tensor(out=ckv[:, bs], in0=ub[b][:, 2, :], scalar=gkvp[:, 0:1], in1=rs[b][:, 1, :],
                                                                 op0=ALU.mult, op1=ALU.mult), [r_ub[b], r_g, r_rs[b]], [r_ckv])
                for t in range(NT):
                    i = t % 2
                    P(lambda e, t=t, i=i: e.matmul(pc_[i][:, :], lhsT=ckv[:, t * 128:(t + 1) * 128], rhs=wuv[:], start=True, stop=True),
                      [r_ckv, r_w], [r_pc[i]])
                    A(lambda e, t=t, i=i: e.copy(out=vx2[:, t, :, 0:64], in_=pc_[i][:, :].rearrange("p (h c) -> p h c", c=64)), [r_pc[i]], [r_vx2])
                cx.barrier()
            kTh = [sbp(f"kTh{i}", [128, S], BF16) for i in range(2)]
            qTh = [sbp(f"qTh{i}", [128, S], BF16) for i in range(2)]
            r_kTh = [R(), R()]; r_qTh = [R(), R()]
            rt = [sbp(f"rt{i}", [128, 2, 512]) for i in range(2)]
            r_rt = [R(), R()]
            pT = [sbp(f"pTe{i}", [128, 512], BF16) for i in range(3)]
            r_pTs = [R(), R(), R()]
            osb = [sbp(f"osb{i}", [65, 512]) for i in range(2)]
            r_osb = [R(), R()]
            rec = [sbp(f"rec{i}", [64, 512]) for i in range(2)]
            r_rec = [R(), R()]
            npt = 0
            npc = 0
            pcnt = [0]

            def proj(h, blk):
                hb = h % 2
                kT, qT = kTh[hb], qTh[hb]
                if blk == 0:
                    G(lambda e: e.tensor_copy(out=kT[64:96, :], in_=krope[64:96, :]), [r_krope], [r_kTh[hb]])
                bs = slice(blk * 512, (blk + 1) * 512)
                i = pcnt[0] % 2
                pcnt[0] += 1
                P(lambda e: e.matmul(pc_[i][0:64, :], lhsT=wuk[:, h * 64:(h + 1) * 64], rhs=ckv[:, bs], start=True, stop=True),
                  [r_w, r_ckv], [r_pc[i]])
                V(lambda e: e.tensor_copy(out=kT[0:64, bs], in_=pc_[i][0:64, :]), [r_pc[i]], [r_kTh[hb]])
                i2 = pcnt[0] % 2
                pcnt[0] += 1
                for j in range(2):
                    P(lambda e, j=j: e.matmul(pc_[i2][0:96, :], lhsT=wuq[:, j, h * 96:(h + 1) * 96], rhs=qn[:, j, bs],
                                              start=(j == 0), stop=(j == 1)), [r_w, r_qn], [r_pc[i2]])
                for j in range(2):
                    P(lambda e, j=j: e.matmul(pm[i2][0:96, :], lhsT=wuqs[:, j, h * 96:(h + 1) * 96], rhs=qn[:, j, bs],
                                              start=(j == 0), stop=(j == 1)), [r_w, r_qn], [r_pm[i2]])
                A(lambda e: e.copy(out=qT[0:64, bs], in_=pc_[i2][0:64, :]), [r_pc[i2]], [r_qTh[hb]])
                V(lambda e: e.tensor_tensor(out=rt[i2][64:96, 0, :], in0=pc_[i2][64:96, :], in1=cs2[64:96, 1, bs], op=ALU.mult),
                  [r_pc[i2], r_cs2], [r_rt[i2]])
                V(lambda e: e.tensor_tensor(out=rt[i2][64:96, 1, :], in0=pm[i2][64:96, :], in1=cs2[64:96, 0, bs], op=ALU.mult),
                  [r_pm[i2], r_cs2], [r_rt[i2]])
                G(lambda e: e.tensor_tensor(out=qT[64:96, bs], in0=rt[i2][64:96, 0, :], in1=rt[i2][64:96, 1, :], op=ALU.add),
                  [r_rt[i2]], [r_qTh[hb]])

            for blk in range(8):
                proj(0, blk)
            for h in range(8):
                hb = h % 2
                kT, qT = kTh[hb], qTh[hb]
                items = [(qb, kt) for qb in range(8) for kt in range(NT)]

                def emitS(n, kT=kT, qT=qT, hb=hb):
                    qb, kt = items[n]
                    i = n % 2
                    qs = slice(qb * 512, (qb + 1) * 512)
                    P(lambda e: e.matmul(pa[i][:, :], lhsT=kT[0:96, kt * 128:(kt + 1) * 128], rhs=qT[0:96, qs], start=True, stop=True),
                      [r_kTh[hb], r_qTh[hb]], [r_pa[i]])

                def post_a(qb, h=h):
                    ob = qb % 2
                    V(lambda e: e.tensor_copy(out=osb[ob][:], in_=pb_[ob][0:65, :]), [r_pb[ob]], [r_osb[ob]])

                def post_b(qb, h=h):
                    ob = qb % 2
                    qs = slice(qb * 512, (qb + 1) * 512)
                    P(lambda e: e.matmul(pm[ob][0:64, :], lhsT=sel64[:], rhs=osb[ob][:], start=True, stop=True), [r_sel, r_osb[ob]], [r_pm[ob]])
                    V(lambda e: e.reciprocal(out=rec[ob][:], in_=pm[ob][0:64, :]), [r_pm[ob]], [r_rec[ob]])
                    G(lambda e: e.tensor_tensor(out=rec[ob][:], in0=rec[ob][:], in1=osb[ob][0:64, :], op=ALU.mult), [r_rec[ob], r_osb[ob]], [r_rec[ob]])
                    cx.dma("sp", mixT[512 + h * 64:512 + (h + 1) * 64, qs], rec[ob][:], reads=[r_rec[ob]], writes=[r_mixT])

                emitS(0)
                pending = None
                for n, (qb, kt) in enumerate(items):
                    if n + 1 < len(items):
                        emitS(n + 1)
                    i = n % 2
                    ip = n % 3
                    ob = qb % 2
                    A(lambda e, i=i, ip=ip: e.activation(out=pT[ip][:], in_=pa[i][:, :], func=AF.Exp, scale=SCALE), [r_pa[i]], [r_pTs[ip]])
                    P(lambda e, ip=ip, ob=ob, kt=kt, h=h: e.matmul(pb_[ob][0:65, :], lhsT=vx2[:, kt, h, :], rhs=pT[ip][:],
                                                                 start=(kt == 0), stop=(kt == NT - 1)), [r_vx2, r_pTs[ip]], [r_pb[ob]])
                    if h + 1 < 8 and n % 32 == 12:
                        proj(h + 1, n // 32)
                    if kt == NT - 1:
                        post_a(qb)
                        pending = (qb, n + 6)
                    if pending is not None and n >= pending[1]:
                        post_b(pending[0])
                        pending = None
                if pending is not None:
                    post_b(pending[0])
            cx.barrier()
        if STOP_AFTER == "D":
            break
        x_src = x_in if l == 0 else XL
        r_X1, r_H2, r_MS, r_WN, r_FFN = R(), R(), R(), R(), R()
        cnt = sb(f"cnt{l}", [128, 256])
        r_cnt = R()
        with ExitStack() as ph:
            def sbp(name, shape, dt=F32):
                return ph.enter_context(nc.sbuf_tensor(f"{name}_{l}", list(shape), dt))

            def psp(name, shape, dt=F32):
                return ph.enter_context(nc.psum_tensor(f"{name}_{l}", list(shape), dt))
            wout = sbp("wout", [128, 8, D], BF16)
            r_wout = R()
            for kc in range(8):
                cx.dma("pool", wout[:, kc, :], w_out_in[l, kc * 128:(kc + 1) * 128, :], writes=[r_wout])
            lnp = sbp("lnp", [128, 2, D])
            r_lnp = R()
            for j in range(2):
                cx.dma("sp", lnp[:, j, :], lnp_in[l, j].partition_broadcast(128), writes=[r_lnp])
            wr = sbp("wr", [128, 8, 256])
            r_wr = R()
            cx.dma("sp", wr[:], w_router_in[l].rearrange("(kc p) n -> p kc n", p=128), writes=[r_wr])
            ebias = sbp("ebias", [128, 256])
            r_eb = R()
            cx.dma("sp", ebias[:], e_bias_in[l].partition_broadcast(128), writes=[r_eb])
            ws13 = sbp("ws13", [128, 8, 512], BF16)
            ws2 = sbp("ws2", [128, 2, D], BF16)
            r_ws = R()
            for kc in range(8):
                cx.dma("pool", ws13[:, kc, :], ws13_in[l, kc * 128:(kc + 1) * 128, :], writes=[r_ws])
            for j in range(2):
                cx.dma("pool", ws2[:, j, :], ws2_in[l, j * 128:(j + 1) * 128, :], writes=[r_ws])
            onesb = sbp("onesbE", [128, 128], BF16)
            r_ones = R()
            G(lambda e: e.memset(onesb[:], 1.0), [], [r_ones])
            G(lambda e: e.memset(cnt[:], 0.0), [], [r_cnt])
            mxt = [sbp(f"mxt{i}", [128, 8, 512], BF16) for i in range(2)]
            r_mxt = [R(), R()]
            xt = [sbp(f"xtE{i}", [128, D]) for i in range(2)]
            r_xt = [R(), R()]
            z = [sbp(f"zE{i}", [128, D]) for i in range(2)]
            r_z = [R(), R()]
            x1 = [sbp(f"x1E{i}", [128, D]) for i in range(2)]
            r_x1 = [R(), R()]
            h2f = [sbp(f"h2f{i}", [128, D]) for i in range(2)]
            r_h2f = [R(), R()]
            h2b = [sbp(f"h2b{i}", [128, D], BF16) for i in range(2)]
            r_h2b = [R(), R()]
            h2T = sbp("h2T", [128, 8, 128]); h2Tb = sbp("h2Tb", [128, 8, 128], BF16)
            r_h2T, r_h2Tb = R(), R()
            st = sbp("stE", [128, 2, 6]); mv = sbp("mvE", [128, 2]); rstd = sbp("rstdE", [128, 1])
            r_st, r_mv, r_rstd = R(), R(), R()
            sc = sbp("scE", [128, 256]); sel = sbp("selE", [128, 256]); selm = sbp("selmE", [128, 256])
            m8g = sbp("m8g", [128, 8, 8]); gs = sbp("gsE", [128, 8]); m8 = sbp("m8E", [128, 8]); gm = sbp("gmE", [128, 2, 8])
            Mf = sbp("MfE", [128, 256]); Mb = [sbp(f"MbE{i}", [128, 256], BF16) for i in range(2)]
            wn = [sbp(f"wnE{i}", [128, 256]) for i in range(2)]
            ws_ = sbp("wsE", [128, 2])
            r_rt = R()
            r_Mb = [R(), R()]; r_wn = [R(), R()]
            s1 = sbp("s1E", [128, 256]); gsh = sbp("gshE", [128, 256], BF16); gT = sbp("gTE", [128, 2, 128], BF16)
            r_s1, r_gsh, r_gT = R(), R(), R()
            fo = [sbp(f"foE{i}", [128, D]) for i in range(2)]
            r_fo = [R(), R()]
            pX = [psp(f"pX{i}", [128, 512]) for i in range(2)]
            pY = [psp(f"pY{i}", [128, 512]) for i in range(2)]
            pZ = [psp(f"pZ{i}", [128, 512]) for i in range(2)]
            pW0 = psp("pW0", [128, 1024], BF16)
            pW1 = psp("pW1", [128, 512])
            r_pX = [R(psum=True), R(psum=True)]; r_pY = [R(psum=True), R(psum=True)]; r_pZ = [R(psum=True), R(psum=True)]
            r_pW0, r_pW1 = R(psum=True), R(psum=True)
            BIG = 1.0e4

            def layer_norm_stats(src, r_src):
                for hf in range(2):
                    V(lambda e, hf=hf: e.bn_stats(out=st[:, hf, :], in_=src[:, hf * 512:(hf + 1) * 512]), [r_src], [r_st])
                V(lambda e: e.bn_aggr(out=mv[:], in_=st[:].rearrange("p a b -> p (a b)")), [r_st], [r_mv])
                A(lambda e: e.activation(out=rstd[:], in_=mv[:, 1:2], func=AF.Sqrt, bias=LN_EPS, scale=1.0), [r_mv], [r_rstd])
                V(lambda e: e.reciprocal(out=rstd[:], in_=rstd[:]), [r_rstd], [r_rstd])

            def e_s1a(t):
                    b = t % 2
                    if t % 4 == 0:
                        g4 = (t // 4) % 2
                        cx.dma("pool", mxt[g4][:], mixT[:, t * 128:(t + 4) * 128].rearrange("(kc p) t -> p kc t", p=128),
                               reads=[r_mixT], writes=[r_mxt[g4]])
                    g4 = (t // 4) % 2
                    tt = t % 4
                    cx.dma("sp", xt[b][:], x_src[t * 128:(t + 1) * 128, :], writes=[r_xt[b]])
                    for hf in range(2):
                        for kc in range(8):
                            P(lambda e, hf=hf, kc=kc, g4=g4, tt=tt: e.matmul(pX[hf][:, :], lhsT=mxt[g4][:, kc, tt * 128:(tt + 1) * 128],
                                                                          rhs=wout[:, kc, hf * 512:(hf + 1) * 512], start=(kc == 0), stop=(kc == 7)),
                              [r_mxt[g4], r_wout], [r_pX[hf]])
                        V(lambda e, hf=hf, b=b: e.tensor_tensor(out=z[b][:, hf * 512:(hf + 1) * 512], in0=pX[hf][:, :], in1=gF[:, 0, hf * 512:(hf + 1) * 512], op=ALU.mult),
                          [r_pX[hf], r_gF], [r_z[b]])
                    V(lambda e, b=b: e.scalar_tensor_tensor(out=z[b][:], in0=xt[b][:], scalar=float(ALPHA), in1=z[b][:], op0=ALU.mult, op1=ALU.add),
                      [r_xt[b], r_z[b]], [r_z[b]])

            e_s1a(0)
            for t in range(NT):
                b = t % 2
                layer_norm_stats(z[b], r_z[b])
                V(lambda e, b=b: e.tensor_scalar(out=z[b][:], in0=z[b][:], scalar1=mv[:, 0:1], scalar2=rstd[:, 0:1], op0=ALU.subtract, op1=ALU.mult),
                  [r_z[b], r_mv, r_rstd], [r_z[b]])
                G(lambda e, b=b: e.tensor_tensor(out=z[b][:], in0=z[b][:], in1=lnp[:, 0, :], op=ALU.mult), [r_z[b], r_lnp], [r_z[b]])
                V(lambda e, b=b: e.tensor_tensor(out=x1[b][:], in0=z[b][:], in1=lnp[:, 1, :], op=ALU.add), [r_z[b], r_lnp], [r_x1[b]])
                cx.dma("sp", X1[t * 128:(t + 1) * 128, :], x1[b][:], reads=[r_x1[b]], writes=[r_X1])
                layer_norm_stats(x1[b], r_x1[b])
                V(lambda e, b=b: e.tensor_scalar(out=h2f[b][:], in0=x1[b][:], scalar1=mv[:, 0:1], scalar2=rstd[:, 0:1], op0=ALU.subtract, op1=ALU.mult),
                  [r_x1[b], r_mv, r_rstd], [r_h2f[b]])
                G(lambda e, b=b: e.tensor_tensor(out=h2f[b][:], in0=h2f[b][:], in1=gF[:, 3, :], op=ALU.mult), [r_h2f[b], r_gF], [r_h2f[b]])
                V(lambda e, b=b: e.tensor_tensor(out=h2f[b][:], in0=h2f[b][:], in1=gF[:, 2, :], op=ALU.add), [r_h2f[b], r_gF], [r_h2f[b]])
                A(lambda e, b=b: e.copy(out=h2b[b][:], in_=h2f[b][:]), [r_h2f[b]], [r_h2b[b]])
                cx.dma("sp", H2[t * 128:(t + 1) * 128, :], h2b[b][:], reads=[r_h2b[b]], writes=[r_H2])
                for q4 in range(2):
                    for k4 in range(4):
                        kc = q4 * 4 + k4
                        P(lambda e, q4=q4, k4=k4, kc=kc, b=b: e.transpose(out=pY[q4][:, k4 * 128:(k4 + 1) * 128], in_=h2f[b][:, kc * 128:(kc + 1) * 128], identity=ident[:]),
                          [r_h2f[b], r_ident], [r_pY[q4]])
                    V(lambda e, q4=q4: e.tensor_copy(out=h2T[:, q4 * 4:(q4 + 1) * 4, :].rearrange("p a t -> p (a t)"), in_=pY[q4][:, :]), [r_pY[q4]], [r_h2T])
                    A(lambda e, q4=q4: e.copy(out=h2Tb[:, q4 * 4:(q4 + 1) * 4, :].rearrange("p a t -> p (a t)"), in_=pY[q4][:, :]), [r_pY[q4]], [r_h2Tb])
                if t + 1 < NT:
                    e_s1a(t + 1)
                for kc in range(8):
                    P(lambda e, kc=kc: e.matmul(pZ[0][:, 0:256], lhsT=h2T[:, kc, :], rhs=wr[:, kc, :], start=(kc == 0), stop=(kc == 7)),
                      [r_h2T, r_wr], [r_pZ[0]])
                A(lambda e: e.activation(out=sc[:], in_=pZ[0][:, 0:256], func=AF.Sigmoid), [r_pZ[0]], [r_rt])
                V(lambda e: e.tensor_tensor(out=sel[:], in0=sc[:], in1=ebias[:], op=ALU.add), [r_rt, r_eb], [r_rt])
                for g in range(8):
                    V(lambda e, g=g: e.max(out=m8g[:, g, :], in_=sel[:, g * 32:(g + 1) * 32]), [r_rt], [r_rt])
                V(lambda e: e.tensor_tensor(out=gs[:], in0=m8g[:, :, 0], in1=m8g[:, :, 1], op=ALU.add), [r_rt], [r_rt])
                V(lambda e: e.max(out=m8[:], in_=gs[:]), [r_rt], [r_rt])
                V(lambda e: e.tensor_scalar(out=gm[:, 0, :], in0=gs[:], scalar1=m8[:, 3:4], scalar2=None, op0=ALU.is_ge), [r_rt], [r_rt])
                V(lambda e: e.tensor_scalar(out=gm[:, 1, :], in0=gm[:, 0, :], scalar1=BIG, scalar2=-BIG, op0=ALU.mult, op1=ALU.add), [r_rt], [r_rt])
                V(lambda e: e.tensor_tensor(out=selm[:].rearrange("p (g k) -> p g k", k=32), in0=sel[:].rearrange("p (g k) -> p g k", k=32),
                                            in1=gm[:, 0, :].unsqueeze(2).to_broadcast([128, 8, 32]), op=ALU.mult), [r_rt], [r_rt])
                V(lambda e: e.tensor_tensor(out=selm[:].rearrange("p (g k) -> p g k", k=32), in0=selm[:].rearrange("p (g k) -> p g k", k=32),
                                            in1=gm[:, 1, :].unsqueeze(2).to_broadcast([128, 8, 32]), op=ALU.add), [r_rt], [r_rt])
                V(lambda e: e.max(out=m8[:], in_=selm[:]), [r_rt], [r_rt])
                V(lambda e: e.tensor_scalar(out=Mf[:], in0=selm[:], scalar1=m8[:, 7:8], scalar2=None, op0=ALU.is_ge), [r_rt], [r_rt])
                G(lambda e, b=b: e.tensor_copy(out=Mb[b][:], in_=Mf[:]), [r_rt], [r_Mb[b]])
                V(lambda e: e.tensor_tensor(out=sel[:], in0=sc[:], in1=Mf[:], op=ALU.mult), [r_rt], [r_rt])
                V(lambda e: e.tensor_reduce(out=ws_[:, 0:1], in_=sel[:], axis=AX.X, op=ALU.add), [r_rt], [r_rt])
                V(lambda e: e.reciprocal(out=ws_[:, 1:2], in_=ws_[:, 0:1]), [r_rt], [r_rt])
                V(lambda e, b=b: e.tensor_scalar(out=wn[b][:], in0=sel[:], scalar1=ws_[:, 1:2], scalar2=2.5, op0=ALU.mult, op1=ALU.mult), [r_rt], [r_wn[b]])
                cx.dma("sp", MS[t * 128:(t + 1) * 128, :], Mb[b][:], reads=[r_Mb[b]], writes=[r_MS])
                cx.dma("sp", WN[t * 128:(t + 1) * 128, :], wn[b][:], reads=[r_wn[b]], writes=[r_WN])
                P(lambda e, b=b: e.matmul(pW1[:, 0:256], lhsT=onesb[:], rhs=Mb[b][:], start=True, stop=True), [r_ones, r_Mb[b]], [r_pW1])
                V(lambda e: e.tensor_tensor(out=cnt[:], in0=cnt[:], in1=pW1[:, 0:256], op=ALU.add), [r_pW1, r_cnt], [r_cnt])
                for kc in range(8):
                    P(lambda e, kc=kc: e.matmul(pZ[1][:, :], lhsT=h2Tb[:, kc, :], rhs=ws13[:, kc, :], start=(kc == 0), stop=(kc == 7)),
                      [r_h2Tb, r_ws], [r_pZ[1]])
                A(lambda e: e.activation(out=s1[:], in_=pZ[1][:, 0:256], func=AF.Silu), [r_pZ[1]], [r_s1])
                V(lambda e: e.tensor_tensor(out=gsh[:], in0=s1[:], in1=pZ[1][:, 256:512], op=ALU.mult), [r_s1, r_pZ[1]], [r_gsh])
                for j in range(2):
                    P(lambda e, j=j: e.transpose(out=pW0[:, j * 128:(j + 1) * 128], in_=gsh[:, j * 128:(j + 1) * 128], identity=identb[:]),
                      [r_gsh, r_identb], [r_pW0])
                A(lambda e: e.copy(out=gT[:].rearrange("p j t -> p (j t)"), in_=pW0[:, 0:256]), [r_pW0], [r_gT])
                for hf in range(2):
                    for j in range(2):
                        P(lambda e, hf=hf, j=j: e.matmul(pX[hf][:, :], lhsT=gT[:, j, :], rhs=ws2[:, j, hf * 512:(hf + 1) * 512], start=(j == 0), stop=(j == 1)),
                          [r_gT, r_ws], [r_pX[hf]])
                    if hf == 0:
                        A(lambda e, b=b: e.copy(out=fo[b][:, 0:512], in_=pX[0][:, :]), [r_pX[0]], [r_fo[b]])
                    else:
                        V(lambda e, b=b: e.tensor_copy(out=fo[b][:, 512:1024], in_=pX[1][:, :]), [r_pX[1]], [r_fo[b]])
                cx.dma("sp", FFN[t * 128:(t + 1) * 128, :], fo[b][:], reads=[r_fo[b]], writes=[r_FFN])
            cx.barrier()
        if STOP_AFTER == "E":
            break
        r_Xs, r_Ys = R(), R()
        idxs = sb(f"idxs{l}", [128, NT, 8], I32)
        wk = sb(f"wk{l}", [128, NT, 8])
        idxw = sb(f"idxw{l}", [128, 512], I32)
        r_idxs, r_wk, r_idxw = R(), R(), R()
        with ExitStack() as ph:
            def sbp(name, shape, dt=F32):
                return ph.enter_context(nc.sbuf_tensor(f"{name}_{l}", list(shape), dt))

            def psp(name, shape, dt=F32):
                return ph.enter_context(nc.psum_tensor(f"{name}_{l}", list(shape), dt))
            xq = sbp("xq", [128, 256]); qi = sbp("qi", [128, 256], I32); qf = sbp("qf", [128, 256]); gtm = sbp("gtm", [128, 256])
            pend = sbp("pend", [128, 256]); base = sbp("base", [128, 256])
            r_f1 = R(); r_base = R()
            V(lambda e: e.tensor_scalar(out=xq[:], in0=cnt[:], scalar1=127.0, scalar2=1.0 / 128, op0=ALU.add, op1=ALU.mult), [r_cnt], [r_f1])
            V(lambda e: e.tensor_copy(out=qi[:], in_=xq[:]), [r_f1], [r_f1])
            V(lambda e: e.tensor_copy(out=qf[:], in_=qi[:]), [r_f1], [r_f1])
            V(lambda e: e.tensor_tensor(out=gtm[:], in0=qf[:], in1=xq[:], op=ALU.is_gt), [r_f1], [r_f1])
            V(lambda e: e.tensor_tensor(out=qf[:], in0=qf[:], in1=gtm[:], op=ALU.subtract), [r_f1], [r_f1])
            V(lambda e: e.tensor_scalar(out=qf[:], in0=qf[:], scalar1=128.0, scalar2=None, op0=ALU.mult), [r_f1], [r_f1])
            V(lambda e: e.tensor_tensor_scan(out=pend[:], data0=qf[:], data1=qf[:], initial=0.0, op0=ALU.add, op1=ALU.max), [r_f1], [r_f1])
            V(lambda e: e.tensor_tensor(out=base[:], in0=pend[:], in1=qf[:], op=ALU.subtract), [r_f1], [r_base])
            V(lambda e: e.tensor_scalar(out=base[:], in0=base[:], scalar1=1.0, scalar2=None, op0=ALU.add), [r_base], [r_base])
            pF = [psp(f"pF{i}", [128, 512]) for i in range(2)]
            r_pF = [R(psum=True), R(psum=True)]
            pendT = sbp("pendT", [128, 2])
            for j in range(2):
                P(lambda e, j=j: e.transpose(out=pF[0][:, j:j + 1], in_=pend[0:1, j * 128:(j + 1) * 128], identity=ident[0:1, 0:1]), [r_f1, r_ident], [r_pF[0]])
            V(lambda e: e.tensor_copy(out=pendT[:], in_=pF[0][:, 0:2]), [r_pF[0]], [r_f1])
            blkpos = sbp("blkpos", [128, 512]); pidx = sbp("pidx", [128, 1])
            r_c2 = R()
            cx.dma("sp", blkpos[:], blkpos_in[:, :], writes=[r_c2])
            cx.dma("sp", pidx[:], pidx_in[:, :], writes=[r_c2])
            Gm = [sbp(f"Gm{j}", [128, 512], BF16) for j in range(2)]
            onesb = sbp("onesbF", [128, 128], BF16)
            ustr = sbp("ustr", [128, 128], BF16)
            r_ones = R()
            G(lambda e: e.memset(onesb[:], 1.0), [], [r_ones])
            cx.dma("pool", ustr[:], ustrict_in[:, :], writes=[r_ones])
            for j in range(2):
                V(lambda e, j=j: e.tensor_scalar(out=Gm[j][:], in0=blkpos[:], scalar1=pendT[:, j:j + 1], scalar2=None, op0=ALU.is_ge), [r_c2, r_f1], [r_f1])
            for j in range(2):
                P(lambda e, j=j: e.matmul(pF[1][:, :], lhsT=onesb[:], rhs=Gm[j][:], start=(j == 0), stop=(j == 1)), [r_ones, r_f1], [r_pF[1]])
            eall = sbp("eall", [128, 512])
            V(lambda e: e.tensor_scalar(out=eall[:], in0=pF[1][:, :], scalar1=255.0, scalar2=None, op0=ALU.min), [r_pF[1]], [r_f1])
            vld = sbp("vld", [128, 512]); vld2 = sbp("vld2", [128, 512])
            V(lambda e: e.tensor_scalar(out=vld[:], in0=blkpos[:], scalar1=pend[:, 255:256], scalar2=None, op0=ALU.is_lt), [r_f1, r_c2], [r_f1])
            V(lambda e: e.memset(vld2[:], 1.0), [r_f1], [r_f1])
            V(lambda e: e.tensor_tensor(out=vld2[:, 3:512], in0=eall[:, 3:512], in1=eall[:, 0:509], op=ALU.not_equal), [r_f1], [r_f1])
            V(lambda e: e.tensor_tensor(out=vld[:], in0=vld[:], in1=vld2[:], op=ALU.mult), [r_f1], [r_f1])
            V(lambda e: e.tensor_scalar(out=eall[:], in0=eall[:], scalar1=128.0, scalar2=float(l * 256 * 128) - OOB_IDX, op0=ALU.mult, op1=ALU.add), [r_f1], [r_f1])
            V(lambda e: e.tensor_scalar(out=eall[:], in0=eall[:], scalar1=pidx[:, 0:1], scalar2=None, op0=ALU.add), [r_f1, r_c2], [r_f1])
            V(lambda e: e.tensor_tensor(out=eall[:], in0=eall[:], in1=vld[:], op=ALU.mult), [r_f1], [r_f1])
            V(lambda e: e.tensor_scalar(out=idxw[:], in0=eall[:], scalar1=OOB_IDX, scalar2=None, op0=ALU.add), [r_f1], [r_idxw])
            Mt = [sbp(f"Mt{i}", [128, 256], BF16) for i in range(2)]
            wnt = [sbp(f"wnt{i}", [128, 256]) for i in range(2)]
            h2t = [sbp(f"h2t{i}", [128, D], BF16) for i in range(2)]
            r_Mt = [R(), R()]; r_wnt = [R(), R()]; r_h2t = [R(), R()]
            t1 = sbp("t1", [128, 256]); Vt = sbp("Vt", [128, 256]); junk = sbp("junk", [128, 256]); p8 = sbp("p8", [128, 8])
            r_t1, r_Vt, r_junk, r_p8 = R(), R(), R(), R()
            for t in range(NT):
                b = t % 2
                cx.dma("sp", Mt[b][:], MS[t * 128:(t + 1) * 128, :], reads=[r_MS], writes=[r_Mt[b]])
                cx.dma("sp", wnt[b][:], WN[t * 128:(t + 1) * 128, :], reads=[r_WN], writes=[r_wnt[b]])
                cx.dma("sp", h2t[b][:], H2[t * 128:(t + 1) * 128, :], reads=[r_H2], writes=[r_h2t[b]])
                P(lambda e, b=b: e.matmul(pF[0][:, 0:256], lhsT=ustr[:], rhs=Mt[b][:], start=True, stop=True), [r_ones, r_Mt[b]], [r_pF[0]])
                V(lambda e: e.tensor_tensor(out=t1[:], in0=pF[0][:, 0:256], in1=base[:], op=ALU.add), [r_pF[0], r_base], [r_t1])
                G(lambda e, b=b: e.tensor_tensor(out=Vt[:], in0=t1[:], in1=Mt[b][:], op=ALU.mult), [r_t1, r_Mt[b]], [r_Vt])
                V(lambda e: e.max(out=p8[:], in_=Vt[:]), [r_Vt], [r_p8])
                V(lambda e, t=t: e.tensor_scalar(out=idxs[:, t, :], in0=p8[:], scalar1=-1.0, scalar2=None, op0=ALU.add), [r_p8], [r_idxs])
                for k in range(8):
                    V(lambda e, t=t, k=k, b=b: e.scalar_tensor_tensor(out=junk[:], in0=Vt[:], scalar=p8[:, k:k + 1], in1=wnt[b][:], op0=ALU.is_equal, op1=ALU.mult,
                                                                    accum_out=wk[:, t, k:k + 1]), [r_Vt, r_p8, r_wnt[b]], [r_junk, r_wk])
                P(lambda e, b=b: e.matmul(pF[1][:, 0:256], lhsT=onesb[:], rhs=Mt[b][:], start=True, stop=True), [r_ones, r_Mt[b]], [r_pF[1]])
                V(lambda e: e.tensor_tensor(out=base[:], in0=base[:], in1=pF[1][:, 0:256], op=ALU.add), [r_pF[1], r_base, r_t1], [r_base])
                for k in range(8):
                    cx.dma("pool", None, None, reads=[r_h2t[b], r_idxs], writes=[r_Xs],
                           fn=lambda e, t=t, k=k, b=b: e.indirect_dma_start(
                               out=Xs[:, :], out_offset=bass.IndirectOffsetOnAxis(ap=idxs[:, t, k:k + 1].bitcast(U32), axis=0),
                               in_=h2t[b][:], in_offset=None))
            cx.barrier()
        with ExitStack() as ph:
            def sbp(name, shape, dt=F32):
                return ph.enter_context(nc.sbuf_tensor(f"{name}_{l}", list(shape), dt))

            def psp(name, shape, dt=F32):
                return ph.enter_context(nc.psum_tensor(f"{name}_{l}", list(shape), dt))
            NW = 3
            Xb = [sbp(f"Xb{i}", [128, D], BF16) for i in range(2)]
            w1b = [sbp(f"w1b{i}", [128, 2048], BF16) for i in range(NW)]
            w3b = [sbp(f"w3b{i}", [128, 2048], BF16) for i in range(NW)]
            w2b = [sbp(f"w2b{i}", [128, 2048], BF16) for i in range(NW)]
            xT = [sbp(f"xTb{i}", [128, 8, 128], BF16) for i in range(2)]
            s1 = [sbp(f"s1b{i}", [128, 256]) for i in range(2)]
            gb = [sbp(f"gbb{i}", [128, 256], BF16) for i in range(2)]
            gT = [sbp(f"gTb{i}", [128, 2, 128], BF16) for i in range(2)]
            Yb = [sbp(f"Yb{i}", [128, D], BF16) for i in range(2)]
            r_Xb = [R(), R()]; r_xT = [R(), R()]
            r_w1b = [R() for _ in range(NW)]; r_w3b = [R() for _ in range(NW)]; r_w2b = [R() for _ in range(NW)]
            r_s1 = [R(), R()]; r_gb = [R(), R()]; r_gT = [R(), R()]; r_Yb = [R(), R()]
            pxT = [psp(f"pxT{i}", [128, 1024], BF16) for i in range(2)]
            phh = [psp(f"phh{i}", [128, 512]) for i in range(2)]
            pgT = psp("pgT", [128, 1024], BF16)
            pyy = [psp(f"pyy{i}", [128, 512]) for i in range(2)]
            r_pxT = [R(psum=True), R(psum=True)]; r_phh = [R(psum=True), R(psum=True)]; r_pgT = R(psum=True); r_pyy = [R(psum=True), R(psum=True)]

            def st_load(b):
                i = b % 2
                iw = b % NW
                cx.dma("sp", Xb[i][:], Xs[b * 128:(b + 1) * 128, :], reads=[r_Xs], writes=[r_Xb[i]])
                for wsrc, wdst, rw in ((w1_in, w1b, r_w1b), (w3_in, w3b, r_w3b), (w2_in, w2b, r_w2b)):
                    cx.dma("pool", None, None, reads=[r_idxw], writes=[rw[iw]],
                           fn=lambda e, wsrc=wsrc, wdst=wdst, iw=iw, b=b: e.indirect_dma_start(
                               out=wdst[iw][:], out_offset=None, in_=wsrc[:, :],
                               in_offset=bass.IndirectOffsetOnAxis(ap=idxw[:, b:b + 1].bitcast(U32), axis=0),
                               bounds_check=bc_reg, oob_is_err=False))

            def st_T(b):
                i = b % 2
                for j in range(8):
                    P(lambda e, i=i, j=j: e.transpose(out=pxT[i][:, j * 128:(j + 1) * 128], in_=Xb[i][:, j::8], identity=identb[:]),
                      [r_Xb[i], r_identb], [r_pxT[i]])
                A(lambda e, i=i: e.copy(out=xT[i][:, 0:4, :].rearrange("p a t -> p (a t)"), in_=pxT[i][:, 0:512]), [r_pxT[i]], [r_xT[i]])
                V(lambda e, i=i: e.tensor_copy(out=xT[i][:, 4:8, :].rearrange("p a t -> p (a t)"), in_=pxT[i][:, 512:1024]), [r_pxT[i]], [r_xT[i]])

            def st_H(b):
                i = b % 2
                iw = b % NW
                for j in range(8):
                    P(lambda e, i=i, iw=iw, j=j: e.matmul(phh[i][:, 0:256], lhsT=xT[i][:, j, :], rhs=w1b[iw][:, j * 256:(j + 1) * 256], start=(j == 0), stop=(j == 7)),
                      [r_xT[i], r_w1b[iw]], [r_phh[i]])
                for j in range(8):
                    P(lambda e, i=i, iw=iw, j=j: e.matmul(phh[i][:, 256:512], lhsT=xT[i][:, j, :], rhs=w3b[iw][:, j * 256:(j + 1) * 256], start=(j == 0), stop=(j == 7)),
                      [r_xT[i], r_w3b[iw]], [r_phh[i]])
                A(lambda e, i=i: e.activation(out=s1[i][:], in_=phh[i][:, 0:256], func=AF.Silu), [r_phh[i]], [r_s1[i]])
                V(lambda e, i=i: e.tensor_tensor(out=gb[i][:], in0=s1[i][:], in1=phh[i][:, 256:512], op=ALU.mult), [r_s1[i], r_phh[i]], [r_gb[i]])

            def st_GT(b):
                i = b % 2
                for j in range(2):
                    P(lambda e, i=i, j=j: e.transpose(out=pgT[:, j * 128:(j + 1) * 128], in_=gb[i][:, j::2], identity=identb[:]), [r_gb[i], r_identb], [r_pgT])
                A(lambda e, i=i: e.copy(out=gT[i][:].rearrange("p j t -> p (j t)"), in_=pgT[:, 0:256]), [r_pgT], [r_gT[i]])

            def st_Y(b):
                i = b % 2
                iw = b % NW
                for hf in range(2):
                    for j in range(2):
                        P(lambda e, i=i, iw=iw, j=j, hf=hf: e.matmul(pyy[hf][:, :], lhsT=gT[i][:, j, :], rhs=w2b[iw][:, j * 1024 + hf * 512:j * 1024 + (hf + 1) * 512],
                                                                   start=(j == 0), stop=(j == 1)), [r_gT[i], r_w2b[iw]], [r_pyy[hf]])
                A(lambda e, i=i: e.copy(out=Yb[i][:, 0:512], in_=pyy[0][:, :]), [r_pyy[0]], [r_Yb[i]])
                V(lambda e, i=i: e.tensor_copy(out=Yb[i][:, 512:1024], in_=pyy[1][:, :]), [r_pyy[1]], [r_Yb[i]])
                cx.dma("sp", Ys[b * 128:(b + 1) * 128, :], Yb[i][:], reads=[r_Yb[i]], writes=[r_Ys])

            st_load(0)
            st_load(1)
            st_T(0)
            for n in range(NBLK + 1):
                if n < NBLK:
                    st_H(n)
                if 1 <= n:
                    st_GT(n - 1)
                if n + 1 < NBLK:
                    st_T(n + 1)
                if 1 <= n:
                    st_Y(n - 1)
                if n + 2 < NBLK:
                    st_load(n + 2)
            cx.barrier()
        with ExitStack() as ph:
            def sbp(name, shape, dt=F32):
                return ph.enter_context(nc.sbuf_tensor(f"{name}_{l}", list(shape), dt))
            lnp2 = sbp("lnp2", [128, 2, D])
            r_lnp2 = R()
            for j in range(2):
                cx.dma("sp", lnp2[:, j, :], lnp_in[l, 2 + j].partition_broadcast(128), writes=[r_lnp2])
            yg = [sbp(f"yg{i}", [128, 8, D], BF16) for i in range(2)]
            r_yg = [R(), R()]
            acc = [sbp(f"accF{i}", [128, D]) for i in range(2)]
            r_acc = [R(), R()]
            x1t = [sbp(f"x1t{i}", [128, D]) for i in range(2)]
            r_x1t = [R(), R()]
            st = sbp("stF", [128, 2, 6]); mv = sbp("mvF", [128, 2]); rstd = sbp("rstdF", [128, 1])
            r_st, r_mv, r_rstd = R(), R(), R()
            dst = XL if l < L - 1 else y_out
            r_dst = R()
            def f4_loads(t):
                b = t % 2
                for k in range(8):
                    cx.dma("pool", None, None, reads=[r_Ys, r_idxs], writes=[r_yg[b]],
                           fn=lambda e, t=t, k=k, b=b: e.indirect_dma_start(
                               out=yg[b][:, k, :], out_offset=None, in_=Ys[:, :],
                               in_offset=bass.IndirectOffsetOnAxis(ap=idxs[:, t, k:k + 1].bitcast(U32), axis=0)))
                cx.dma("sp", acc[b][:], FFN[t * 128:(t + 1) * 128, :], reads=[r_FFN], writes=[r_acc[b]])
                cx.dma("sp", x1t[b][:], X1[t * 128:(t + 1) * 128, :], reads=[r_X1], writes=[r_x1t[b]])

            f4_loads(0)
            for t in range(NT):
                b = t % 2
                if t + 1 < NT:
                    f4_loads(t + 1)
                for k in range(8):
                    V(lambda e, t=t, k=k, b=b: e.scalar_tensor_tensor(out=acc[b][:], in0=yg[b][:, k, :], scalar=wk[:, t, k:k + 1], in1=acc[b][:],
                                                                    op0=ALU.mult, op1=ALU.add), [r_yg[b], r_wk, r_acc[b]], [r_acc[b]])
                G(lambda e, b=b: e.tensor_tensor(out=acc[b][:], in0=acc[b][:], in1=gF[:, 1, :], op=ALU.mult), [r_acc[b], r_gF], [r_acc[b]])
                V(lambda e, b=b: e.scalar_tensor_tensor(out=acc[b][:], in0=x1t[b][:], scalar=float(ALPHA), in1=acc[b][:], op0=ALU.mult, op1=ALU.add),
                  [r_x1t[b], r_acc[b]], [r_acc[b]])
                for hf in range(2):
                    V(lambda e, hf=hf, b=b: e.bn_stats(out=st[:, hf, :], in_=acc[b][:, hf * 512:(hf + 1) * 512]), [r_acc[b]], [r_st])
                V(lambda e: e.bn_aggr(out=mv[:], in_=st[:].rearrange("p a b -> p (a b)")), [r_st], [r_mv])
                A(lambda e: e.activation(out=rstd[:], in_=mv[:, 1:2], func=AF.Sqrt, bias=LN_EPS, scale=1.0), [r_mv], [r_rstd])
                V(lambda e: e.reciprocal(out=rstd[:], in_=rstd[:]), [r_rstd], [r_rstd])
                V(lambda e, b=b: e.tensor_scalar(out=acc[b][:], in0=acc[b][:], scalar1=mv[:, 0:1], scalar2=rstd[:, 0:1], op0=ALU.subtract, op1=ALU.mult),
                  [r_acc[b], r_mv, r_rstd], [r_acc[b]])
                G(lambda e, b=b: e.tensor_tensor(out=acc[b][:], in0=acc[b][:], in1=lnp2[:, 0, :], op=ALU.mult), [r_acc[b], r_lnp2], [r_acc[b]])
                V(lambda e, b=b: e.tensor_tensor(out=x1t[b][:], in0=acc[b][:], in1=lnp2[:, 1, :], op=ALU.add), [r_acc[b], r_lnp2, r_x1t[b]], [r_x1t[b]])
                cx.dma("sp", dst[t * 128:(t + 1) * 128, :], x1t[b][:], reads=[r_x1t[b]], writes=[r_dst])
            cx.barrier()
        if STOP_AFTER == "F":
            break

    cx.finish()
    es.close()
    return nc


def prep_shared(inp):
    w_in = np.asarray(inp["w_in"], np.float32)
    o = IN_OFF
    w_tok = np.concatenate([w_in[:, :, o["pool"]:o["pool"] + 256], w_in[:, :, o["v"]:o["v"] + 256],
                            w_in[:, :, o["o"]:o["o"] + 256]], axis=2)
    kr = w_in[:, :, o["kr"]:o["kr"] + 32]
    kr_sw = np.concatenate([kr[:, :, 16:32], kr[:, :, 0:16]], axis=2)
    misc = np.concatenate([w_in[:, :, o["gate"]:o["gate"] + 16], kr, kr_sw, np.zeros((L, D, 48), np.float32)], axis=2)
    w_fm = np.concatenate([w_in[:, :, o["q"]:o["q"] + 256], w_in[:, :, o["k"]:o["k"] + 256],
                           w_in[:, :, o["dq"]:o["dq"] + 256], w_in[:, :, o["dkv"]:o["dkv"] + 128], misc], axis=2)
    b_ada = np.asarray(inp["b_ada"], np.float32)
    bp = np.stack([b_ada[:, v * D:(v + 1) * D].reshape(L, 8, 128).transpose(0, 2, 1) for v in (0, 1, 3, 4)], axis=2)
    band = np.zeros((4, 5, 128, 128), np.float32)
    for g, w in enumerate((2, 4, 8, 16)):
        A_ = np.zeros((S, S), np.float32) if False else None
        def arow(t):
            lo = max(t - w // 2, 0); hi = min(t + w // 2, S)
            return lo, hi, 1.0 / (hi - lo)
        def fill(mat, ti, tj):
            for tl in range(128):
                t = ti * 128 + tl
                lo, hi, inv = arow(t)
                for tp in range(max(lo, tj * 128), min(hi, tj * 128 + 128)):
                    mat[tp - tj * 128, tl] += inv
                if tj * 128 <= t < tj * 128 + 128:
                    mat[t - tj * 128, tl] -= 1.0
        fill(band[g, 0], 5, 4)
        fill(band[g, 1], 5, 5)
        fill(band[g, 2], 5, 6)
        fill(band[g, 3], 0, 0)
        fill(band[g, 4], NT - 1, NT - 1)
    conv_w = np.asarray(inp["conv_w"], np.float32)
    conv_b = np.asarray(inp["conv_b"], np.float32)
    conv_p = np.concatenate([conv_w.transpose(0, 2, 1), conv_b[:, :, None]], axis=2)
    conv_p = conv_p.reshape(L, 4, 128, 6).transpose(0, 2, 1, 3)
    gate_b4 = np.asarray(inp["gate_b"], np.float32).reshape(L, 4, 4).transpose(0, 2, 1)
    sel2 = np.zeros((4, 2, 128), np.float32)
    for j in range(2):
        sel2[2 * j, j, 0:64] = 1.0
        sel2[2 * j + 1, j, 64:128] = 1.0
    masks = np.zeros((2, 128, 128), np.float32)
    masks[0] = np.triu(np.ones((128, 128), np.float32))
    masks[1] = np.tril(np.ones((128, 128), np.float32))
    gb = np.asarray(inp["gate_b"], np.float32)
    gbp = np.zeros((L, 128, 4), np.float32)
    for h in range(4):
        for k in range(4):
            gbp[:, h * 32:(h + 1) * 32, k] = gb[:, k * 4 + h][:, None]
    trih = np.zeros((2, 128, 128), np.float32)
    cmask = np.zeros((128, 64), np.float32)
    psel = np.zeros((128, 128), np.float32)
    for h in range(4):
        for c in range(32):
            p = h * 32 + c
            trih[0, h * 32:h * 32 + c, p] = 1.0
            trih[1, h * 32 + c + 1:(h + 1) * 32, p] = 1.0
            cmask[p, (h // 2) * 32 + c] = 1.0
            psel[p, (h % 2) * 64:(h % 2) * 64 + 64] = 1.0
    inv_freq = (np.float32(10000.0) ** (-np.arange(0, 32, 2, dtype=np.float32) / np.float32(32))).astype(np.float32)
    ropec = np.zeros((32, 2), np.float32)
    ropec[:, 0] = np.concatenate([inv_freq, inv_freq])
    ropec[:16, 1] = -1.0
    ropec[16:, 1] = 1.0
    w_uq = np.asarray(inp["w_uq"], np.float32)
    w_uq_sw = w_uq.copy().reshape(L, 256, 8, 96)
    w_uq_sw[:, :, :, 64:80] = w_uq.reshape(L, 256, 8, 96)[:, :, :, 80:96]
    w_uq_sw[:, :, :, 80:96] = w_uq.reshape(L, 256, 8, 96)[:, :, :, 64:80]
    w_uq_sw = w_uq_sw.reshape(L, 256, 768)
    sel64 = np.zeros((65, 64), np.float32)
    sel64[64, :] = 1.0
    lnp = np.stack([np.asarray(inp[k], np.float32) for k in ("ln1_g", "ln1_b", "ln2_g", "ln2_b")], axis=1)
    ws13 = np.concatenate([np.asarray(inp["ws1"], np.float32), np.asarray(inp["ws3"], np.float32)], axis=2)
    blkpos = np.tile((np.arange(512, dtype=np.float32) * 128.0)[None, :], (128, 1))
    sh = {
        "w_out": np.ascontiguousarray(inp["w_out"], np.float32),
        "lnp": np.ascontiguousarray(lnp),
        "w_router": np.ascontiguousarray(inp["w_router"], np.float32),
        "e_bias": np.ascontiguousarray(inp["e_bias"], np.float32),
        "ws13": np.ascontiguousarray(ws13),
        "ws2": np.ascontiguousarray(inp["ws2"], np.float32),
        "w1": np.asarray(inp["w1"], np.float32).reshape(L * 256 * 128, 2048),
        "w3": np.asarray(inp["w3"], np.float32).reshape(L * 256 * 128, 2048),
        "w2": np.asarray(inp["w2"], np.float32).reshape(L * 256 * 128, 2048),
        "ustrict": np.triu(np.ones((128, 128), np.float32), 1),
        "blkpos": blkpos,
        "pidx": np.arange(128, dtype=np.float32).reshape(128, 1),
        "ropec": ropec,
        "g_q_p": np.ascontiguousarray(np.asarray(inp["g_q"], np.float32).reshape(L, 2, 128).transpose(0, 2, 1)),
        "g_kv_p": np.ascontiguousarray(np.asarray(inp["g_kv"], np.float32).reshape(L, 128, 1)),
        "w_uq": np.ascontiguousarray(w_uq), "w_uq_sw": np.ascontiguousarray(w_uq_sw),
        "w_uk": np.ascontiguousarray(inp["w_uk"], np.float32), "w_uv": np.ascontiguousarray(inp["w_uv"], np.float32),
        "sel64": sel64,
        "gbp": gbp, "trih": trih, "cmask": cmask, "psel": psel,
        "band": band,
        "w_pool": np.ascontiguousarray(inp["w_pool"], np.float32),
        "s_pool_p": np.ascontiguousarray(np.asarray(inp["s_pool"], np.float32).reshape(L, 4, 64).transpose(0, 2, 1)),
        "conv_p": np.ascontiguousarray(conv_p),
        "gate_b4": np.ascontiguousarray(gate_b4),
        "gn_w": np.ascontiguousarray(inp["gn_w"], np.float32),
        "sel2": sel2,
        "masks": masks,
        "ident": np.eye(128, dtype=np.float32),
        "w_ada": np.ascontiguousarray(inp["w_ada"], np.float32),
        "b_ada_p": np.ascontiguousarray(bp),
        "b_ada": np.ascontiguousarray(b_ada),
        "w_in_tok": np.ascontiguousarray(w_tok),
        "w_in_fm": np.ascontiguousarray(w_fm),
    }
    return sh


def prep_core(inp, b):
    x = np.asarray(inp["x"][b], np.float32)
    c = np.asarray(inp["c"][b], np.float32)
    return {"x": np.ascontiguousarray(x), "c_p": np.ascontiguousarray(c.reshape(8, 128).T),
            "pos": np.ascontiguousarray(np.asarray(inp["positions"][b], np.int32))}


def kernel(**inp):
    nc = build_program()
    sh = prep_shared(inp)
    in_maps = []
    for b in range(8):
        m = dict(sh)
        m.update(prep_core(inp, b))
        in_maps.append(m)
    res = run_bass_kernel_spmd(nc, in_maps, core_ids=list(range(8)))
    kernel.last = res
    return np.stack([r["y"] for r in res.results], axis=0)
```
